# Optimizing a Trainium2 kernel written in Bass

```python
import math
import jax, jax.numpy as jnp
from jax import lax
import numpy as np

D_MODEL = 1024
BATCH = 4
SEQ = 4096
DEPTH = 4

GRID_W = 64
CTX_LEN = 256
N_MIXERS = 3
LAYER_KINDS = tuple(i % N_MIXERS for i in range(DEPTH))
KIND_SLOT = tuple(LAYER_KINDS[:i].count(LAYER_KINDS[i]) for i in range(DEPTH))
N_HYENA = LAYER_KINDS.count(0)
N_ATTN = LAYER_KINDS.count(1)
N_CHUNK = LAYER_KINDS.count(2)
LAST_CTX_LAYER = max([i for i, k in enumerate(LAYER_KINDS) if k == 1], default=-1)

HY_ORDER = 2
HY_SHORT = 3
HY_BANDS = 16
HY_EMB_DIM = 1 + 2 * HY_BANDS
HY_FILTER_HIDDEN = 64
HY_DECAY_TARGET = 1e-2
HY_FAST_DECAY_PCT = 0.3
HY_SLOW_DECAY_PCT = 1.5
HY_MIN_DECAY = math.log(HY_DECAY_TARGET) / HY_SLOW_DECAY_PCT
HY_MAX_DECAY = math.log(HY_DECAY_TARGET) / HY_FAST_DECAY_PCT
HY_PROJ = (HY_ORDER + 1) * D_MODEL

HEAD_DIM = 64
N_Q_HEADS = D_MODEL // HEAD_DIM
N_KV_HEADS = 4
GQA_GROUP = N_Q_HEADS // N_KV_HEADS
Q_BLOCK = 128
ROPE_THETA = 10000.0
ATTN_SCALE = HEAD_DIM ** -0.5
Q_COLS = N_Q_HEADS * HEAD_DIM
QKV_COLS = (N_Q_HEADS + 2 * N_KV_HEADS) * HEAD_DIM

CHUNK = 128
GMLP_HALF = 2 * D_MODEL
GMLP_GROUPS = 16
GMLP_GROUP_W = GMLP_HALF // GMLP_GROUPS

N_EXPERTS = 16
EXPERT_FF = 2 * D_MODEL
CAPACITY_FACTOR = 2

RMS_EPS = 1e-6
LN_EPS = 1e-5
DEEPNORM_ALPHA = (2 * DEPTH) ** 0.25
DEEPNORM_BETA = (8 * DEPTH) ** -0.25

kernel_name = 'hybrid_hyena_gqa_gmlp_ecmoe_diffusion'

F32 = jnp.float32


def layer_norm(x, g, b):
    xf = x.astype(F32)
    mu = jnp.mean(xf, axis=-1, keepdims=True)
    var = jnp.mean(jnp.square(xf - mu), axis=-1, keepdims=True)
    return ((xf - mu) * lax.rsqrt(var + LN_EPS) * g.astype(F32) + b.astype(F32)).astype(x.dtype)


def rms_norm(x, g):
    xf = x.astype(F32)
    return (xf * lax.rsqrt(jnp.mean(jnp.square(xf), axis=-1, keepdims=True) + RMS_EPS) * g.astype(F32)).astype(x.dtype)


def adaln(cond, w_mod, b_mod):
    m = jax.nn.silu(cond) @ w_mod + b_mod
    return jnp.split(m, 6, axis=-1)


def modulate(x, shift, scale):
    return x * (1.0 + scale) + shift


def post_norm(x, y, g, b):
    return layer_norm(DEEPNORM_ALPHA * x + y, g, b)


def centred_depthwise_conv(x, w, b):
    k = w.shape[0]
    y = lax.conv_general_dilated(x, w[:, None, :].astype(x.dtype), window_strides=(1,),
                                 padding=[(k // 2, k // 2)], dimension_numbers=('NWC', 'WIO', 'NWC'),
                                 feature_group_count=x.shape[-1])
    return y + b


def hyena_filters(L, f_w1, f_b1, f_w2, f_b2, f_w3, f_freq):
    t = jnp.linspace(0.0, 1.0, L, dtype=F32)[:, None]
    w = (2.0 * math.pi / L) * jnp.arange(L, dtype=F32)[:, None]
    f = jnp.linspace(1e-4, HY_BANDS - 1, HY_BANDS, dtype=F32)[None, :]
    z = jnp.concatenate([t, jnp.cos(f * w), -jnp.sin(f * w)], axis=-1)
    freq = f_freq.astype(F32)
    h = jnp.sin(freq[0] * (z @ f_w1.astype(F32) + f_b1.astype(F32)))
    h = jnp.sin(freq[1] * (h @ f_w2.astype(F32) + f_b2.astype(F32)))
    h = (h @ f_w3.astype(F32)).reshape(L, HY_ORDER, 2, D_MODEL)
    deltas = jnp.abs(jnp.linspace(HY_MIN_DECAY, HY_MAX_DECAY, D_MODEL, dtype=F32))
    h = h * jnp.exp(-t * deltas)[:, None, None, :]
    fwd, bwd = h[:, :, 0], h[:, :, 1]
    k2 = jnp.concatenate([fwd, jnp.zeros((1, HY_ORDER, D_MODEL), F32), bwd[:0:-1]], axis=0)
    k2 = k2 / (jnp.sum(jnp.abs(k2), axis=0, keepdims=True) + RMS_EPS)
    return jnp.fft.rfft(k2, axis=0)


def long_conv(v, k_f, bias):
    L = v.shape[1]
    vf32 = v.astype(F32)
    y = jnp.fft.irfft(jnp.fft.rfft(vf32, n=2 * L, axis=1) * k_f[None], n=2 * L, axis=1)[:, :L]
    return (y + vf32 * bias.astype(F32)).astype(v.dtype)


def hyena_mixer(h, w_in, conv_w, conv_b, f_w1, f_b1, f_w2, f_b2, f_w3, f_freq, f_bias, w_out):
    L = h.shape[1]
    p = centred_depthwise_conv(h @ w_in, conv_w, conv_b)
    v, x1, x2 = jnp.split(p, 3, axis=-1)
    k_f = hyena_filters(L, f_w1, f_b1, f_w2, f_b2, f_w3, f_freq)
    z = v
    for n, gate in enumerate((x1, x2)):
        z = gate * long_conv(z, k_f[:, n], f_bias[n])
    return z @ w_out


def axial_rope_tables(L):
    rows = L // GRID_W
    row = jnp.broadcast_to(jnp.arange(rows, dtype=F32)[:, None], (rows, GRID_W)).reshape(L)
    col = jnp.broadcast_to(jnp.arange(GRID_W, dtype=F32)[None, :], (rows, GRID_W)).reshape(L)
    n_freq = HEAD_DIM // 4
    inv_freq = ROPE_THETA ** (-jnp.arange(n_freq, dtype=F32) / n_freq)
    ang = jnp.concatenate([row[:, None] * inv_freq, col[:, None] * inv_freq], axis=-1)
    return jnp.cos(ang), jnp.sin(ang)


def apply_rope(x, cos, sin):
    shape = (1, x.shape[1]) + (1,) * (x.ndim - 3) + (HEAD_DIM // 2,)
    cos, sin = cos.reshape(shape), sin.reshape(shape)
    xf = x.astype(F32)
    x1, x2 = xf[..., :HEAD_DIM // 2], xf[..., HEAD_DIM // 2:]
    return jnp.concatenate([x1 * cos - x2 * sin, x1 * sin + x2 * cos], axis=-1).astype(x.dtype)


def q_heads(h, w_qkv, q_gain):
    B, L, _ = h.shape
    q = (h @ w_qkv[:, :Q_COLS]).reshape(B, L, N_KV_HEADS, GQA_GROUP, HEAD_DIM)
    return rms_norm(q, q_gain)


def kv_heads(h, w_qkv, k_gain):
    B, L, _ = h.shape
    k, v = jnp.split(h @ w_qkv[:, Q_COLS:], 2, axis=-1)
    return rms_norm(k.reshape(B, L, N_KV_HEADS, HEAD_DIM), k_gain), v.reshape(B, L, N_KV_HEADS, HEAD_DIM)


def block_attention(q, keys, vals):
    B, Lq = q.shape[:2]
    qb = jnp.moveaxis(q.reshape((B, Lq // Q_BLOCK, Q_BLOCK) + q.shape[2:]), 1, 0)

    def one_block(qi):
        s = jnp.einsum('bqkgd,bskd->bkgqs', qi, keys, preferred_element_type=F32) * ATTN_SCALE
        p = jax.nn.softmax(s, axis=-1).astype(vals.dtype)
        return jnp.einsum('bkgqs,bskd->bqkgd', p, vals)

    o = lax.map(one_block, qb)
    return jnp.moveaxis(o, 0, 1).reshape(B, Lq, Q_COLS)


def chunk_gmlp_mixer(h, w_in, ln_g, ln_b, w_s, b_s, w_out):
    B, L, _ = h.shape
    z = jax.nn.gelu(h @ w_in, approximate=False)
    u, v = jnp.split(z, 2, axis=-1)
    v = layer_norm(v, ln_g, ln_b).reshape(B, L // CHUNK, CHUNK, GMLP_GROUPS, GMLP_GROUP_W)
    v = jnp.einsum('gts,bnsgc->bntgc', w_s, v) + b_s.T[None, None, :, :, None]
    return (u * v.reshape(B, L, GMLP_HALF)) @ w_out


def expert_choice_ffn(h, w_router, w_gate, w_up, w_down):
    B, n, _ = h.shape
    cap = CAPACITY_FACTOR * n // N_EXPERTS
    aff = jax.nn.softmax((h @ w_router).astype(F32), axis=-1)
    gate, idx = lax.top_k(jnp.swapaxes(aff, 1, 2), cap)
    bidx = jnp.arange(B)[:, None, None]
    xg = h[bidx, idx]
    a = jnp.einsum('becd,edf->becf', xg, w_gate)
    u = jnp.einsum('becd,edf->becf', xg, w_up)
    y = jnp.einsum('becf,efd->becd', jax.nn.silu(a) * u, w_down) * gate[..., None].astype(h.dtype)
    return jnp.zeros_like(h).at[bidx, idx].add(y)


def setup_inputs(seed: int = 0) -> dict:
    key = jax.random.key(seed)
    ks = jax.random.split(key, 33)
    D = D_MODEL

    def nrm(i, shape, scale):
        return scale * jax.random.normal(ks[i], shape, F32)

    return {
        'x': nrm(0, (BATCH, SEQ, D), 1.0),
        'c': nrm(1, (BATCH, D), 1.0),
        'ctx': nrm(2, (BATCH, CTX_LEN, D), 1.0),
        'c_ctx': nrm(3, (D,), 1.0),
        'mod_w': nrm(4, (DEPTH, D, 6 * D), D ** -0.5),
        'mod_b': nrm(5, (DEPTH, 6 * D), 0.02),
        'ln_g': 1.0 + nrm(6, (DEPTH, 2, D), 0.02),
        'ln_b': nrm(7, (DEPTH, 2, D), 0.02),
        'hy_w_in': nrm(8, (N_HYENA, D, HY_PROJ), D ** -0.5),
        'hy_conv_w': nrm(9, (N_HYENA, HY_SHORT, HY_PROJ), HY_SHORT ** -0.5),
        'hy_conv_b': nrm(10, (N_HYENA, HY_PROJ), 0.02),
        'hy_f_w1': nrm(11, (N_HYENA, HY_EMB_DIM, HY_FILTER_HIDDEN), HY_EMB_DIM ** -0.5),
        'hy_f_b1': nrm(12, (N_HYENA, HY_FILTER_HIDDEN), 0.02),
        'hy_f_w2': nrm(13, (N_HYENA, HY_FILTER_HIDDEN, HY_FILTER_HIDDEN), HY_FILTER_HIDDEN ** -0.5),
        'hy_f_b2': nrm(14, (N_HYENA, HY_FILTER_HIDDEN), 0.02),
        'hy_f_w3': nrm(15, (N_HYENA, HY_FILTER_HIDDEN, HY_ORDER * 2 * D), HY_FILTER_HIDDEN ** -0.5),
        'hy_f_freq': 1.0 + nrm(16, (N_HYENA, 2, HY_FILTER_HIDDEN), 0.02),
        'hy_f_bias': nrm(17, (N_HYENA, HY_ORDER, D), 0.02),
        'hy_w_out': nrm(18, (N_HYENA, D, D), DEEPNORM_BETA * D ** -0.5),
        'at_w_qkv': nrm(19, (N_ATTN, D, QKV_COLS), D ** -0.5),
        'at_q_gain': 1.0 + nrm(20, (N_ATTN, HEAD_DIM), 0.02),
        'at_k_gain': 1.0 + nrm(21, (N_ATTN, HEAD_DIM), 0.02),
        'at_w_out': nrm(22, (N_ATTN, Q_COLS, D), DEEPNORM_BETA * Q_COLS ** -0.5),
        'cm_w_in': nrm(23, (N_CHUNK, D, 2 * GMLP_HALF), D ** -0.5),
        'cm_ln_g': 1.0 + nrm(24, (N_CHUNK, GMLP_HALF), 0.02),
        'cm_ln_b': nrm(25, (N_CHUNK, GMLP_HALF), 0.02),
        'cm_w_s': nrm(26, (N_CHUNK, GMLP_GROUPS, CHUNK, CHUNK), CHUNK ** -0.5),
        'cm_b_s': 1.0 + nrm(27, (N_CHUNK, GMLP_GROUPS, CHUNK), 0.02),
        'cm_w_out': nrm(28, (N_CHUNK, GMLP_HALF, D), DEEPNORM_BETA * GMLP_HALF ** -0.5),
        'moe_router': nrm(29, (DEPTH, D, N_EXPERTS), D ** -0.5),
        'moe_w_gate': nrm(30, (DEPTH, N_EXPERTS, D, EXPERT_FF), D ** -0.5),
        'moe_w_up': nrm(31, (DEPTH, N_EXPERTS, D, EXPERT_FF), D ** -0.5),
        'moe_w_down': nrm(32, (DEPTH, N_EXPERTS, EXPERT_FF, D), DEEPNORM_BETA * EXPERT_FF ** -0.5),
    }


def reference(x, c, ctx, c_ctx, mod_w, mod_b, ln_g, ln_b,
              hy_w_in, hy_conv_w, hy_conv_b, hy_f_w1, hy_f_b1, hy_f_w2, hy_f_b2, hy_f_w3, hy_f_freq, hy_f_bias, hy_w_out,
              at_w_qkv, at_q_gain, at_k_gain, at_w_out,
              cm_w_in, cm_ln_g, cm_ln_b, cm_w_s, cm_b_s, cm_w_out,
              moe_router, moe_w_gate, moe_w_up, moe_w_down):
    x_lat, x_ctx = x, ctx
    for i in range(DEPTH):
        kind, slot = LAYER_KINDS[i], KIND_SLOT[i]
        ctx_read = i <= LAST_CTX_LAYER
        ctx_update = i < LAST_CTX_LAYER
        sh1, sc1, g1, sh2, sc2, g2 = adaln(c[:, None, :], mod_w[i], mod_b[i])
        h = modulate(x_lat, sh1, sc1)
        if ctx_read:
            cmod = adaln(c_ctx, mod_w[i], mod_b[i])
            hc = modulate(x_ctx, cmod[0], cmod[1])

        if kind == 0:
            hy = (hy_w_in[slot], hy_conv_w[slot], hy_conv_b[slot], hy_f_w1[slot], hy_f_b1[slot], hy_f_w2[slot],
                  hy_f_b2[slot], hy_f_w3[slot], hy_f_freq[slot], hy_f_bias[slot], hy_w_out[slot])
            y = hyena_mixer(h, *hy)
            if ctx_update:
                yc = hyena_mixer(hc, *hy)
        elif kind == 1:
            w_qkv = at_w_qkv[slot]
            ck, cv = kv_heads(hc, w_qkv, at_k_gain[slot])
            cos, sin = axial_rope_tables(h.shape[1])
            q = apply_rope(q_heads(h, w_qkv, at_q_gain[slot]), cos, sin)
            k, v = kv_heads(h, w_qkv, at_k_gain[slot])
            k = apply_rope(k, cos, sin)
            o = block_attention(q, jnp.concatenate([ck, k], axis=1), jnp.concatenate([cv, v], axis=1))
            y = o @ at_w_out[slot]
            if ctx_update:
                yc = block_attention(q_heads(hc, w_qkv, at_q_gain[slot]), ck, cv) @ at_w_out[slot]
        else:
            cm = (cm_w_in[slot], cm_ln_g[slot], cm_ln_b[slot], cm_w_s[slot], cm_b_s[slot], cm_w_out[slot])
            y = chunk_gmlp_mixer(h, *cm)
            if ctx_update:
                yc = chunk_gmlp_mixer(hc, *cm)

        moe = (moe_router[i], moe_w_gate[i], moe_w_up[i], moe_w_down[i])
        x_lat = post_norm(x_lat, g1 * y, ln_g[i, 0], ln_b[i, 0])
        x_lat = post_norm(x_lat, g2 * expert_choice_ffn(modulate(x_lat, sh2, sc2), *moe), ln_g[i, 1], ln_b[i, 1])
        if ctx_update:
            x_ctx = post_norm(x_ctx, cmod[2] * yc, ln_g[i, 0], ln_b[i, 0])
            x_ctx = post_norm(x_ctx, cmod[5] * expert_choice_ffn(modulate(x_ctx, cmod[3], cmod[4]), *moe),
                              ln_g[i, 1], ln_b[i, 1])
    return x_lat
```

```python
import math
from contextlib import ExitStack

import numpy as np
import ml_dtypes
import concourse.bass as bass
import concourse.mybir as mybir
from concourse.bass_utils import run_bass_kernel_spmd

F32 = mybir.dt.float32
BF16 = mybir.dt.bfloat16
I32 = mybir.dt.int32
U32 = mybir.dt.uint32
ALU = mybir.AluOpType
AF = mybir.ActivationFunctionType
AX = mybir.AxisListType
NPBF = ml_dtypes.bfloat16

D = 1024
NB = 4
SEQ = 4096
CTX = 256
DEPTH = 4
NE = 16
FF = 2048
LN_EPS = 1e-5
RMS_EPS = 1e-6
ALPHA = (2 * DEPTH) ** 0.25
NCORES = 8


class Buf:
    __slots__ = ("name", "w", "r")

    def __init__(self, name):
        self.name = name
        self.w = None
        self.r = {}


class FW:
    def __init__(self, nc, stack):
        self.nc = nc
        self.stack = stack
        self.engs = {"pe": nc.tensor, "dve": nc.vector, "act": nc.scalar, "pool": nc.gpsimd, "sp": nc.sync}
        self.sems = {}
        self.cnt = {}
        self.seen = {k: {} for k in self.engs}
        for k in self.engs:
            self.sems[k] = stack.enter_context(nc.semaphore("s_" + k))
            self.cnt[k] = 0
        self.same_engine_sync = True
        self.semstack = stack

    def sb(self, name, shape, dt):
        return self.stack.enter_context(self.nc.sbuf_tensor(name, list(shape), dt))

    def ps(self, name, shape, dt=F32):
        return self.stack.enter_context(self.nc.psum_tensor(name, list(shape), dt))

    def dma_sem(self, name):
        key = "d_" + name
        if key not in self.sems:
            self.sems[key] = self.semstack.enter_context(self.nc.semaphore(key))
            self.cnt[key] = 0
        return key

    def _wait(self, e, key, val):
        if key == e and not self.same_engine_sync:
            return
        if self.seen[e].get(key, 0) >= val:
            return
        self.engs[e].wait_ge(self.sems[key], val)
        self.seen[e][key] = val

    def _deps(self, e, reads, writes):
        for b in reads:
            if b.w is not None:
                self._wait(e, *b.w)
        for b in writes:
            if b.w is not None:
                self._wait(e, *b.w)
            for k, v in b.r.items():
                self._wait(e, k, v)

    def op(self, e, fn, reads=(), writes=()):
        self._deps(e, reads, writes)
        ins = fn()
        self.cnt[e] += 1
        ins.then_inc(self.sems[e], 1)
        for b in reads:
            b.r[e] = self.cnt[e]
        for b in writes:
            b.w = (e, self.cnt[e])
            b.r = {}
        return ins

    def dma(self, q, out, in_, reads=(), writes=(), semname=None, indirect=None, **kw):
        self._deps(q, reads, writes)
        name = semname or (writes[0].name if writes else reads[0].name + "_st")
        key = self.dma_sem(name)
        if indirect is None:
            ins = self.engs[q].dma_start(out=out, in_=in_, **kw)
        else:
            ins = self.nc.gpsimd.indirect_dma_start(out=out, in_=in_, **indirect)
        self.cnt[key] += 16
        ins.then_inc(self.sems[key], 16)
        for b in reads:
            b.r[key] = self.cnt[key]
        for b in writes:
            b.w = (key, self.cnt[key])
            b.r = {}
        return ins

    def barrier(self):
        for e in self.engs:
            for key, c in self.cnt.items():
                if key != e and c > 0:
                    self._wait(e, key, c)

    def scoped(self):
        fw = self

        class _Scope:
            def __enter__(self_):
                self_.prev = fw.stack
                self_.st = ExitStack()
                self_.st.__enter__()
                fw.stack = self_.st
                return fw

            def __exit__(self_, *a):
                fw.barrier()
                fw.stack = self_.prev
                return self_.st.__exit__(*a)
        return _Scope()

    def seal(self, bufs):
        key = bufs[0].w[0]
        for b in bufs:
            b.w = (key, self.cnt[key])

    def finish(self, bufs, e="sp"):
        for b in bufs:
            if b.w is not None:
                self._wait(e, *b.w)


def new_nc():
    return bass.Bass("TRN2", target_bir_lowering=False)


def dram_in(nc, name, shape, dt=F32):
    return nc.dram_tensor(name, list(shape), dt, kind="ExternalInput").ap()


def dram_out(nc, name, shape, dt=F32):
    return nc.dram_tensor(name, list(shape), dt, kind="ExternalOutput").ap()


def run(nc, in_maps):
    res = run_bass_kernel_spmd(nc, in_maps, core_ids=list(range(NCORES)))
    return res.results


def build_A():
    nc = new_nc()
    cT = dram_in(nc, "cT", [128, 8, 5])
    w = dram_in(nc, "w", [128, 8, 3072])
    b = dram_in(nc, "b", [1, 3072])
    m = dram_out(nc, "m", [5, 3072])
    with ExitStack() as st:
        fw = FW(nc, st)
        ct = fw.sb("ct", [128, 8, 5], F32)
        bt = fw.sb("bt", [5, 3072], F32)
        mt = fw.sb("mt", [5, 3072], F32)
        wt = [fw.sb(f"wt{i}", [128, 8, 512], F32) for i in range(2)]
        pt = [fw.ps(f"pt{i}", [5, 512]) for i in range(2)]
        Bc, Bb, Bm, Bo = Buf("ct"), Buf("bt"), Buf("mt"), Buf("mo")
        Bw = [Buf("wt0"), Buf("wt1")]
        Bp = [Buf("pt0"), Buf("pt1")]
        fw.dma("sp", ct[:], cT, writes=[Bc])
        fw.dma("sp", bt[:], b.partition_broadcast(5), writes=[Bb])
        fw.op("act", lambda: nc.scalar.activation(out=ct[:], in_=ct[:], func=AF.Silu), reads=[Bc], writes=[Bc])
        for j in range(6):
            s = j % 2
            fw.dma("sp" if s == 0 else "pool", wt[s][:], w[:, :, j * 512:(j + 1) * 512], writes=[Bw[s]])
            for k in range(8):
                fw.op("pe", lambda k=k: nc.tensor.matmul(pt[s][:], lhsT=ct[:, k, :], rhs=wt[s][:, k, :],
                                                           start=(k == 0), stop=(k == 7)),
                      reads=[Bc, Bw[s]], writes=[Bp[s]])
            fw.op("dve", lambda: nc.vector.tensor_tensor(out=mt[:, j * 512:(j + 1) * 512], in0=pt[s][:],
                                                         in1=bt[:, j * 512:(j + 1) * 512], op=ALU.add),
                  reads=[Bp[s], Bb], writes=[Bm])
        fw.dma("sp", m, mt[:], reads=[Bm], writes=[Bo])
        fw.finish([Bo])
    return nc


def run_A(c, c_ctx, mod_w, mod_b):
    nc = build_A()
    cc = np.concatenate([c, c_ctx[None, :]], axis=0)
    cT = np.ascontiguousarray(cc.T.reshape(8, 128, 5).transpose(1, 0, 2))
    maps = []
    for core in range(NCORES):
        i, hf = core // 2, core % 2
        wv = mod_w[i][:, hf * 3072:(hf + 1) * 3072].reshape(8, 128, 3072).transpose(1, 0, 2)
        maps.append({"cT": cT, "w": np.ascontiguousarray(wv),
                     "b": np.ascontiguousarray(mod_b[i][None, hf * 3072:(hf + 1) * 3072])})
    res = run(nc, maps)
    out = np.zeros((DEPTH, 5, 6 * D), np.float32)
    for core in range(NCORES):
        i, hf = core // 2, core % 2
        out[i][:, hf * 3072:(hf + 1) * 3072] = res[core]["m"]
    return out


def layer_norm_tile(fw, nc, u, Bu, stats, mv, rstd, Bs, nparts=128):
    for j in range(2):
        fw.op("dve", lambda j=j: nc.vector.bn_stats(out=stats[:, j, :], in_=u[:, j * 512:(j + 1) * 512]),
              reads=[Bu], writes=[Bs])
    fw.op("dve", lambda: nc.vector.bn_aggr(out=mv[:], in_=stats[:].rearrange("p a b -> p (a b)")), reads=[Bs], writes=[Bs])
    fw.op("act", lambda: nc.scalar.activation(out=rstd[:], in_=mv[:, 1:2], func=AF.Sqrt, bias=fw.eps_ln[:, 0:1], scale=1.0),
          reads=[Bs, fw.Bconst], writes=[Bs])
    fw.op("dve", lambda: nc.vector.reciprocal(out=rstd[:], in_=rstd[:]), reads=[Bs], writes=[Bs])
    fw.op("dve", lambda: nc.vector.tensor_scalar(out=u[:], in0=u[:], scalar1=mv[:, 0:1], scalar2=rstd[:, 0:1],
                                                 op0=ALU.subtract, op1=ALU.mult), reads=[Bs, Bu], writes=[Bu])


def make_consts(fw, nc):
    fw.eps_ln = fw.sb("eps_ln", [128, 1], F32)
    fw.Bconst = Buf("consts")
    fw.op("pool", lambda: nc.gpsimd.memset(fw.eps_ln[:], LN_EPS), writes=[fw.Bconst])


def build_N(KC, NT):
    T = NT * 128
    nc = new_nc()
    aT = dram_in(nc, "aT", [128, KC, T], BF16)
    wo = dram_in(nc, "wo", [128, KC, 1024])
    x = dram_in(nc, "x", [T, 1024])
    rows = dram_in(nc, "rows", [1, 5 * 1024])
    wr = dram_in(nc, "wr", [128, 8, 16])
    ident = dram_in(nc, "ident", [128, 128])
    x1o = dram_out(nc, "x1", [T, 1024])
    h2o = dram_out(nc, "h2", [T, 1024], BF16)
    affo = dram_out(nc, "aff", [T, 16])
    with ExitStack() as st:
        fw = FW(nc, st)
        make_consts(fw, nc)
        wob = fw.sb("wob", [128, KC, 1024], BF16)
        wst = [fw.sb(f"wst{i}", [128, 1024], F32) for i in range(2)]
        rw = fw.sb("rw", [128, 5, 1024], F32)
        wrt = fw.sb("wrt", [128, 8, 16], F32)
        idt = fw.sb("idt", [128, 128], F32)
        at = [fw.sb(f"at{i}", [128, KC, 128], BF16) for i in range(2)]
        xt = [fw.sb(f"xt{i}", [128, 1024], F32) for i in range(2)]
        u = [fw.sb(f"u{i}", [128, 1024], F32) for i in range(2)]
        h2 = [fw.sb(f"h2{i}", [128, 1024], F32) for i in range(2)]
        h2b = [fw.sb(f"h2b{i}", [128, 1024], BF16) for i in range(2)]
        h2T = fw.sb("h2T", [128, 8, 128], F32)
        stats = fw.sb("stats", [128, 2, 6], F32)
        mv = fw.sb("mv", [128, 2], F32)
        rstd = fw.sb("rstd", [128, 1], F32)
        sm = fw.sb("sm", [128, 4], F32)
        aft = [fw.sb(f"aft{i}", [128, 16], F32) for i in range(2)]
        py = [fw.ps(f"py{i}", [128, 512]) for i in range(2)]
        ptr = [fw.ps(f"ptr{i}", [128, 4, 128]) for i in range(2)]
        pl = fw.ps("pl", [128, 16])
        Bwo, Brw, Bwr, Bid = Buf("wob"), Buf("rw"), Buf("wrt"), Buf("idt")
        Bwst = [Buf("wst0"), Buf("wst1")]
        Bat = [Buf("at0"), Buf("at1")]
        Bxt = [Buf("xt0"), Buf("xt1")]
        Bu = [Buf("u0"), Buf("u1")]
        Bh2 = [Buf("h20"), Buf("h21")]
        Bh2b = [Buf("h2b0"), Buf("h2b1")]
        Bh2T, Bs, Bsm = Buf("h2T"), Buf("stats"), Buf("sm")
        Baf = [Buf("aft0"), Buf("aft1")]
        Bpy = [Buf("py0"), Buf("py1")]
        Bptr = [Buf("ptr0"), Buf("ptr1")]
        Bpl = Buf("pl")
        Bout = [Buf("o_x1"), Buf("o_h2"), Buf("o_aff")]
        fw.dma("sp", rw[:].rearrange("p a b -> p (a b)"), rows.partition_broadcast(128), writes=[Brw])
        fw.dma("sp", wrt[:], wr, writes=[Bwr])
        fw.dma("sp", idt[:], ident, writes=[Bid])
        for kc in range(KC):
            s = kc % 2
            fw.dma("sp" if s == 0 else "pool", wst[s][:], wo[:, kc, :], writes=[Bwst[s]])
            fw.op("act" if s == 0 else "pool",
                  (lambda: nc.scalar.copy(out=wob[:, kc, :], in_=wst[s][:])) if s == 0 else
                  (lambda: nc.gpsimd.tensor_copy(out=wob[:, kc, :], in_=wst[s][:])),
                  reads=[Bwst[s]], writes=[Bwo])
        fw.op("dve", lambda: nc.vector.tensor_scalar(out=rw[:, 3, :], in0=rw[:, 3, :], scalar1=1.0, scalar2=None, op0=ALU.add),
              reads=[Brw], writes=[Brw])
        for t in range(NT):
            s = t % 2
            fw.dma("sp", at[s][:], aT[:, :, t * 128:(t + 1) * 128], writes=[Bat[s]])
            fw.dma("pool", xt[s][:], x[t * 128:(t + 1) * 128, :], writes=[Bxt[s]])
            for hf in range(2):
                for kc in range(KC):
                    fw.op("pe", lambda kc=kc, hf=hf: nc.tensor.matmul(py[hf][:], lhsT=at[s][:, kc, :],
                                                                       rhs=wob[:, kc, hf * 512:(hf + 1) * 512],
                                                                       start=(kc == 0), stop=(kc == KC - 1)),
                          reads=[Bat[s], Bwo], writes=[Bpy[hf]])
            for hf in range(2):
                sl = slice(hf * 512, (hf + 1) * 512)
                fw.op("dve", lambda: nc.vector.tensor_tensor(out=u[s][:, sl], in0=py[hf][:], in1=rw[:, 0, sl], op=ALU.mult),
                      reads=[Bpy[hf], Brw], writes=[Bu[s]])
            fw.op("dve", lambda: nc.vector.scalar_tensor_tensor(out=u[s][:], in0=xt[s][:], scalar=ALPHA, in1=u[s][:],
                                                                op0=ALU.mult, op1=ALU.add),
                  reads=[Bxt[s], Bu[s]], writes=[Bu[s]])
            layer_norm_tile(fw, nc, u[s], Bu[s], stats, mv, rstd, Bs)
            fw.op("pool", lambda: nc.gpsimd.tensor_tensor(out=u[s][:], in0=u[s][:], in1=rw[:, 1, :], op=ALU.mult),
                  reads=[Bu[s], Brw], writes=[Bu[s]])
            fw.op("pool", lambda: nc.gpsimd.tensor_tensor(out=u[s][:], in0=u[s][:], in1=rw[:, 2, :], op=ALU.add),
                  reads=[Bu[s], Brw], writes=[Bu[s]])
            fw.dma("sp", x1o[t * 128:(t + 1) * 128, :], u[s][:], reads=[Bu[s]], writes=[Bout[0]])
            fw.op("dve", lambda: nc.vector.tensor_tensor(out=h2[s][:], in0=u[s][:], in1=rw[:, 3, :], op=ALU.mult),
                  reads=[Bu[s], Brw], writes=[Bh2[s]])
            fw.op("dve", lambda: nc.vector.tensor_tensor(out=h2[s][:], in0=h2[s][:], in1=rw[:, 4, :], op=ALU.add),
                  reads=[Bh2[s], Brw], writes=[Bh2[s]])
            fw.op("act", lambda: nc.scalar.copy(out=h2b[s][:], in_=h2[s][:]), reads=[Bh2[s]], writes=[Bh2b[s]])
            fw.dma("sp", h2o[t * 128:(t + 1) * 128, :], h2b[s][:], reads=[Bh2b[s]], writes=[Bout[1]])
            for g in range(2):
                for k4 in range(4):
                    k = g * 4 + k4
                    fw.op("pe", lambda k=k, k4=k4: nc.tensor.transpose(out=ptr[g][:, k4, :], in_=h2[s][:, k * 128:(k + 1) * 128],
                                                                        identity=idt[:]),
                          reads=[Bh2[s], Bid], writes=[Bptr[g]])
                fw.op("act", lambda: nc.scalar.copy(out=h2T[:, g * 4:(g + 1) * 4, :], in_=ptr[g][:]), reads=[Bptr[g]], writes=[Bh2T])
            for k in range(8):
                fw.op("pe", lambda k=k: nc.tensor.matmul(pl[:], lhsT=h2T[:, k, :], rhs=wrt[:, k, :], start=(k == 0), stop=(k == 7)),
                      reads=[Bh2T, Bwr], writes=[Bpl])
            fw.op("dve", lambda: nc.vector.reduce_max(out=sm[:, 0:1], in_=pl[:], axis=AX.X), reads=[Bpl], writes=[Bsm])
            fw.op("dve", lambda: nc.vector.tensor_scalar(out=sm[:, 1:2], in0=sm[:, 0:1], scalar1=-1.0, scalar2=None, op0=ALU.mult),
                  reads=[Bsm], writes=[Bsm])
            fw.op("act", lambda: nc.scalar.activation(out=aft[s][:], in_=pl[:], func=AF.Exp, bias=sm[:, 1:2], scale=1.0,
                                                      accum_out=sm[:, 2:3]), reads=[Bpl, Bsm], writes=[Baf[s], Bsm])
            fw.op("dve", lambda: nc.vector.reciprocal(out=sm[:, 3:4], in_=sm[:, 2:3]), reads=[Bsm], writes=[Bsm])
            fw.op("dve", lambda: nc.vector.tensor_scalar(out=aft[s][:], in0=aft[s][:], scalar1=sm[:, 3:4], scalar2=None, op0=ALU.mult),
                  reads=[Bsm, Baf[s]], writes=[Baf[s]])
            fw.dma("sp", affo[t * 128:(t + 1) * 128, :], aft[s][:], reads=[Baf[s]], writes=[Bout[2]])
        fw.finish(Bout)
    return nc


def fm_layout(a2d):
    K, T = a2d.shape
    return np.ascontiguousarray(a2d.reshape(K // 128, 128, T).transpose(1, 0, 2))


_NC_CACHE = {}


def cached(key, builder):
    if key not in _NC_CACHE:
        _NC_CACHE[key] = builder()
    return _NC_CACHE[key]


def build_E(NTT, CAP, GB, NITER=30):
    NG = NB // GB
    NS = GB * CAP
    NCH = NS // 128
    NC8 = 8 * NTT
    nc = new_nc()
    aff = dram_in(nc, "aff", [128, 8, NTT])
    h2 = dram_in(nc, "h2", [NB * NTT * 128, 1024], BF16)
    tok = dram_in(nc, "tok", [128, 2, NB, NTT])
    slotoff = dram_in(nc, "slotoff", [128, 8])
    tri = dram_in(nc, "tri", [128, 128], BF16)
    iota = dram_in(nc, "iota", [128, NS])
    identb = dram_in(nc, "identb", [128, 128], BF16)
    wg = dram_in(nc, "wg", [2, 128, 8, 2048])
    wu = dram_in(nc, "wu", [2, 128, 8, 2048])
    wd = dram_in(nc, "wd", [2, 128, 16, 1024])
    yc = dram_out(nc, "yc", [2, NG, NS + 128, 1024])
    BIGPOS = float(NS)
    postab = dram_out(nc, "postab", [128, 8, NTT], I32)
    with ExitStack() as st:
        fw = FW(nc, st)
        A = fw.sb("A", [128, 8, NTT], F32)
        tokt = fw.sb("tokt", [128, 2, NB, NTT], F32)
        sofft = fw.sb("sofft", [128, 8], F32)
        trit = fw.sb("trit", [128, 128], BF16)
        onesb = fw.sb("onesb", [128, 128], BF16)
        iot = fw.sb("iot", [128, NS], F32)
        idb = fw.sb("idb", [128, 128], BF16)
        lo = fw.sb("lo", [128, 8], F32)
        hi = fw.sb("hi", [128, 8], F32)
        mid = fw.sb("mid", [128, 8], F32)
        cnt = fw.sb("cnt", [128, 8], F32)
        ge = fw.sb("ge", [128, 8], F32)
        tmp8 = fw.sb("tmp8", [128, 8], F32)
        cmpb = fw.sb("cmpb", [128, 8, NTT], BF16)
        maskf = fw.sb("maskf", [128, 8, NTT], F32)
        pos = fw.sb("pos", [128, 8, NTT], F32)
        off = fw.sb("off", [128, 8, NTT], F32)
        tot = fw.sb("tot", [128, 8, NTT], F32)
        posi = fw.sb("posi", [128, 8, NTT], I32)
        vals = fw.sb("vals", [128, 8, NTT, 5], BF16)
        gres = fw.sb("gres", [128, 8, NTT], F32)
        gpc = fw.sb("gpc", [128, 8, NTT], F32)
        NSTEP = GB * NTT
        ohall = fw.sb("ohall", [128, NSTEP, NS], BF16)
        idxf = fw.sb("idxf", [128, NCH, 5], F32)
        gate = fw.sb("gate", [128, NCH], F32)
        idf = fw.sb("idf", [128, NCH], F32)
        idxu = fw.sb("idxu", [128, NCH], I32)
        xg = [fw.sb(f"xg{i}", [128, 1024], BF16) for i in range(2)]
        xgT = fw.sb("xgT", [128, 8, NS], BF16)
        wgb = fw.sb("wgb", [128, 8, 2048], BF16)
        wub = fw.sb("wub", [128, 8, 2048], BF16)
        wdb = fw.sb("wdb", [128, 16, 1024], BF16)
        wst = [fw.sb(f"wst{i}", [128, 2048], F32) for i in range(2)]
        sg = [fw.sb(f"sg{i}", [128, NS], F32) for i in range(2)]
        hT = fw.sb("hT", [128, 16, NS], BF16)
        ysb = [fw.sb(f"ysb{i}", [128, 1024], F32) for i in range(2)]
        pbank = [fw.ps(f"pb{i}", [128, 512]) for i in range(7)]
        ptrb = fw.ps("ptrb", [128, 4, 128], BF16)
        pcnt, pidx, pg, pu, py0, py1, ppos = pbank
        B = {n: Buf(n) for n in ["A", "tokt", "sofft", "trit", "onesb", "iot", "idb", "lo", "hi", "mid", "cnt", "ge", "tmp8",
                                 "cmpb", "maskf", "pos", "off", "tot", "posi", "vals", "gres", "ohall", "idxf", "idxu", "xg0", "xg1",
                                 "xgT", "wgb", "wub", "wdb", "wst0", "wst1", "sg0", "sg1", "hT", "ysb0", "ysb1",
                                 "pcnt", "pidx", "pg", "pu", "py0", "py1", "ppos", "ptrb", "o_yc", "o_pos"]}
        V = nc.vector
        fw.dma("sp", A[:], aff, writes=[B["A"]])
        fw.dma("sp", tokt[:], tok, writes=[B["tokt"]])
        fw.dma("sp", sofft[:], slotoff, writes=[B["sofft"]])
        fw.dma("sp", trit[:], tri, writes=[B["trit"]])
        fw.dma("sp", iot[:], iota, writes=[B["iot"]])
        fw.dma("sp", idb[:], identb, writes=[B["idb"]])
        fw.op("pool", lambda: nc.gpsimd.memset(onesb[:], 1.0), writes=[B["onesb"]])
        zt = fw.sb("zt", [128, 1024], F32)
        B["zt"] = Buf("zt")
        fw.op("pool", lambda: nc.gpsimd.memset(zt[:], 0.0), writes=[B["zt"]])
        for el in range(2):
            for g in range(NG):
                fw.dma("sp", yc[el, g, NS:NS + 128, :], zt[:], reads=[B["zt"]], writes=[B["o_yc"]])
        fw.op("pool", lambda: nc.gpsimd.memset(lo[:], 0.0), writes=[B["lo"]])
        fw.op("pool", lambda: nc.gpsimd.memset(hi[:], 1.0), writes=[B["hi"]])

        def bc(t8):
            return t8[:, :].unsqueeze(2).to_broadcast([128, 8, NTT])

        def count_ge(thr, Bthr, want_mask_f32=False):
            fw.op("dve", lambda: V.tensor_tensor(out=cmpb[:], in0=A[:], in1=bc(thr), op=ALU.is_ge),
                  reads=[B["A"], Bthr], writes=[B["cmpb"]])
            fw.op("pe", lambda: nc.tensor.matmul(pcnt[:, :NC8], lhsT=onesb[:], rhs=cmpb[:].rearrange("p a b -> p (a b)"),
                                                 start=True, stop=True), reads=[B["onesb"], B["cmpb"]], writes=[B["pcnt"]])

        for it in range(NITER):
            fw.op("dve", lambda: V.tensor_tensor(out=mid[:], in0=lo[:], in1=hi[:], op=ALU.add), reads=[B["lo"], B["hi"]], writes=[B["mid"]])
            fw.op("dve", lambda: V.tensor_scalar(out=mid[:], in0=mid[:], scalar1=0.5, scalar2=None, op0=ALU.mult),
                  reads=[B["mid"]], writes=[B["mid"]])
            count_ge(mid, B["mid"])
            fw.op("dve", lambda: V.tensor_reduce(out=cnt[:], in_=pcnt[:, :NC8].rearrange("p (a b) -> p a b", b=NTT), axis=AX.X, op=ALU.add),
                  reads=[B["pcnt"]], writes=[B["cnt"]])
            fw.op("dve", lambda: V.tensor_scalar(out=ge[:], in0=cnt[:], scalar1=float(CAP) - 0.5, scalar2=None, op0=ALU.is_ge),
                  reads=[B["cnt"]], writes=[B["ge"]])
            fw.op("dve", lambda: V.tensor_tensor(out=tmp8[:], in0=ge[:], in1=mid[:], op=ALU.mult), reads=[B["ge"], B["mid"]], writes=[B["tmp8"]])
            fw.op("dve", lambda: V.tensor_tensor(out=lo[:], in0=lo[:], in1=tmp8[:], op=ALU.max), reads=[B["tmp8"], B["lo"]], writes=[B["lo"]])
            fw.op("dve", lambda: V.scalar_tensor_tensor(out=tmp8[:], in0=ge[:], scalar=4.0, in1=mid[:], op0=ALU.mult, op1=ALU.add),
                  reads=[B["ge"], B["mid"]], writes=[B["tmp8"]])
            fw.op("dve", lambda: V.tensor_tensor(out=hi[:], in0=hi[:], in1=tmp8[:], op=ALU.min), reads=[B["tmp8"], B["hi"]], writes=[B["hi"]])
        count_ge(lo, B["lo"])
        fw.op("act", lambda: nc.scalar.copy(out=tot[:].rearrange("p a b -> p (a b)"), in_=pcnt[:, :NC8]), reads=[B["pcnt"]], writes=[B["tot"]])
        fw.op("dve", lambda: V.tensor_copy(out=maskf[:], in_=cmpb[:]), reads=[B["cmpb"]], writes=[B["maskf"]])
        fw.op("pe", lambda: nc.tensor.matmul(ppos[:, :NC8], lhsT=trit[:], rhs=cmpb[:].rearrange("p a b -> p (a b)"), start=True, stop=True),
              reads=[B["trit"], B["cmpb"]], writes=[B["ppos"]])
        fw.op("dve", lambda: V.tensor_copy(out=off[:, :, 0], in_=sofft[:]), reads=[B["sofft"]], writes=[B["off"]])
        for j in range(1, NTT):
            fw.op("dve", lambda j=j: V.tensor_tensor(out=off[:, :, j], in0=off[:, :, j - 1], in1=tot[:, :, j - 1], op=ALU.add),
                  reads=[B["off"], B["tot"]], writes=[B["off"]])
        fw.op("dve", lambda: V.tensor_tensor(out=pos[:].rearrange("p a b -> p (a b)"), in0=ppos[:, :NC8],
                                             in1=off[:].rearrange("p a b -> p (a b)"), op=ALU.add),
              reads=[B["ppos"], B["off"]], writes=[B["pos"]])
        fw.op("dve", lambda: V.scalar_tensor_tensor(out=pos[:], in0=pos[:], scalar=-BIGPOS, in1=maskf[:], op0=ALU.add, op1=ALU.mult),
              reads=[B["pos"], B["maskf"]], writes=[B["pos"]])
        fw.op("dve", lambda: V.tensor_scalar(out=pos[:], in0=pos[:], scalar1=BIGPOS, scalar2=None, op0=ALU.add),
              reads=[B["pos"]], writes=[B["pos"]])
        fw.op("dve", lambda: V.tensor_copy(out=posi[:], in_=pos[:]), reads=[B["pos"]], writes=[B["posi"]])
        fw.dma("sp", postab, posi[:], reads=[B["posi"]], writes=[B["o_pos"]])
        for b in range(NB):
            for el in range(2):
                for h in range(2):
                    fw.op("pool", lambda b=b, el=el, h=h: nc.gpsimd.tensor_copy(out=vals[:, b * 2 + el, :, h], in_=tokt[:, h, b, :]),
                          reads=[B["tokt"]], writes=[B["vals"]])
        fw.op("dve", lambda: V.tensor_copy(out=gres[:], in_=A[:]), reads=[B["A"]], writes=[B["gres"]])
        for q in range(3):
            fw.op("dve", lambda q=q: V.tensor_copy(out=vals[:, :, :, 2 + q], in_=gres[:]), reads=[B["gres"]], writes=[B["vals"]])
            if q < 2:
                fw.op("dve", lambda q=q: V.tensor_copy(out=gpc[:], in_=vals[:, :, :, 2 + q]), reads=[B["vals"]], writes=[B["gres"]])
                fw.op("dve", lambda: V.tensor_tensor(out=gres[:], in0=gres[:], in1=gpc[:], op=ALU.subtract), reads=[B["gres"]], writes=[B["gres"]])

        ld = [0]

        def load_w(dst, Bdst, src_fn, nk, width):
            for k in range(nk):
                s = ld[0] % 2
                ld[0] += 1
                fw.dma("sp" if s == 0 else "act", wst[s][:, :width], src_fn(k), writes=[B[f"wst{s}"]])
                if s == 0:
                    fw.op("act", lambda k=k: nc.scalar.copy(out=dst[:, k, :], in_=wst[s][:, :width]), reads=[B[f"wst{s}"]], writes=[Bdst])
                else:
                    fw.op("pool", lambda k=k: nc.gpsimd.tensor_copy(out=dst[:, k, :], in_=wst[s][:, :width]), reads=[B[f"wst{s}"]], writes=[Bdst])

        ycnt = [0]
        for el in range(2):
            load_w(wgb, B["wgb"], lambda k: wg[el, :, k, :], 8, 2048)
            load_w(wub, B["wub"], lambda k: wu[el, :, k, :], 8, 2048)
            load_w(wdb, B["wdb"], lambda k: wd[el, :, k, :], 16, 1024)
            for g in range(NG):
                steps = [(b, j) for b in range(g * GB, (g + 1) * GB) for j in range(NTT)]
                for si, (b, j) in enumerate(steps):
                    col = b * 2 + el
                    fw.op("dve", lambda: V.tensor_scalar(out=ohall[:, si, :], in0=iot[:], scalar1=pos[:, col, j:j + 1], scalar2=None, op0=ALU.is_equal),
                          reads=[B["iot"], B["pos"]], writes=[B["ohall"]])
                for c in range(NCH):
                    for si, (b, j) in enumerate(steps):
                        col = b * 2 + el
                        fw.op("pe", lambda: nc.tensor.matmul(pidx[:, c * 8:c * 8 + 5], lhsT=ohall[:, si, c * 128:(c + 1) * 128],
                                                             rhs=vals[:, col, j, :], start=(si == 0), stop=(si == len(steps) - 1)),
                              reads=[B["ohall"], B["vals"]], writes=[B["pidx"]])
                fw.op("dve", lambda: V.tensor_copy(out=idxf[:], in_=pidx[:, :NCH * 8].rearrange("p (a b) -> p a b", b=8)[:, :, 0:5]),
                      reads=[B["pidx"]], writes=[B["idxf"]])
                fw.op("dve", lambda: V.scalar_tensor_tensor(out=idf[:], in0=idxf[:, :, 0], scalar=128.0, in1=idxf[:, :, 1], op0=ALU.mult, op1=ALU.add),
                      reads=[B["idxf"]], writes=[B["idxf"]])
                fw.op("dve", lambda: V.tensor_copy(out=idxu[:], in_=idf[:]), reads=[B["idxf"]], writes=[B["idxu"]])
                fw.op("dve", lambda: V.tensor_tensor(out=gate[:], in0=idxf[:, :, 2], in1=idxf[:, :, 3], op=ALU.add), reads=[B["idxf"]], writes=[B["idxf"]])
                fw.op("dve", lambda: V.tensor_tensor(out=gate[:], in0=gate[:], in1=idxf[:, :, 4], op=ALU.add), reads=[B["idxf"]], writes=[B["idxf"]])
                for c in range(NCH):
                    s = c % 2
                    fw.dma("pool", xg[s][:], h2, reads=[B["idxu"]], writes=[B[f"xg{s}"]],
                           indirect=dict(out_offset=None, in_offset=bass.IndirectOffsetOnAxis(ap=idxu[:, c:c + 1], axis=0)))
                    for k4 in range(2):
                        for kk in range(4):
                            k = k4 * 4 + kk
                            fw.op("pe", lambda k=k, kk=kk: nc.tensor.transpose(out=ptrb[:, kk, :], in_=xg[s][:, k * 128:(k + 1) * 128], identity=idb[:]),
                                  reads=[B[f"xg{s}"], B["idb"]], writes=[B["ptrb"]])
                        fw.op("act", lambda k4=k4: nc.scalar.copy(out=xgT[:, k4 * 4:(k4 + 1) * 4, c * 128:(c + 1) * 128], in_=ptrb[:]),
                              reads=[B["ptrb"]], writes=[B["xgT"]])
                for ft in range(16):
                    s = ft % 2
                    for k in range(8):
                        fw.op("pe", lambda k=k: nc.tensor.matmul(pg[:, :NS], lhsT=wgb[:, k, ft * 128:(ft + 1) * 128], rhs=xgT[:, k, :],
                                                                  start=(k == 0), stop=(k == 7)), reads=[B["wgb"], B["xgT"]], writes=[B["pg"]])
                    for k in range(8):
                        fw.op("pe", lambda k=k: nc.tensor.matmul(pu[:, :NS], lhsT=wub[:, k, ft * 128:(ft + 1) * 128], rhs=xgT[:, k, :],
                                                                  start=(k == 0), stop=(k == 7)), reads=[B["wub"], B["xgT"]], writes=[B["pu"]])
                    fw.op("act", lambda: nc.scalar.activation(out=sg[s][:], in_=pg[:, :NS], func=AF.Silu), reads=[B["pg"]], writes=[B[f"sg{s}"]])
                    fw.op("dve", lambda: V.tensor_tensor(out=hT[:, ft, :], in0=sg[s][:], in1=pu[:, :NS], op=ALU.mult),
                          reads=[B[f"sg{s}"], B["pu"]], writes=[B["hT"]])
                for c in range(NCH):
                    s = ycnt[0] % 2
                    ycnt[0] += 1
                    for hf, (py, nm) in enumerate(((py0, "py0"), (py1, "py1"))):
                        for ft in range(16):
                            fw.op("pe", lambda ft=ft: nc.tensor.matmul(py[:], lhsT=hT[:, ft, c * 128:(c + 1) * 128],
                                                                        rhs=wdb[:, ft, hf * 512:(hf + 1) * 512], start=(ft == 0), stop=(ft == 15)),
                                  reads=[B["hT"], B["wdb"]], writes=[B[nm]])
                        if hf == 0:
                            fw.op("dve", lambda: V.tensor_scalar(out=ysb[s][:, 0:512], in0=py[:], scalar1=gate[:, c:c + 1], scalar2=None, op0=ALU.mult),
                                  reads=[B[nm], B["idxf"]], writes=[B[f"ysb{s}"]])
                        else:
                            fw.op("act", lambda: nc.scalar.activation(out=ysb[s][:, 512:1024], in_=py[:], func=AF.Copy, scale=gate[:, c:c + 1]),
                                  reads=[B[nm], B["idxf"]], writes=[B[f"ysb{s}"]])
                    fw.dma("sp", yc[el, g, c * 128:(c + 1) * 128, :], ysb[s][:], reads=[B[f"ysb{s}"]], writes=[B["o_yc"]])
        fw.finish([B["o_yc"], B["o_pos"]])
    return nc


def build_P(NT, NS):
    T = NT * 128
    RS = NS + 128
    nc = new_nc()
    x1 = dram_in(nc, "x1", [T, 1024])
    ycb = dram_in(nc, "ycb", [16 * RS, 1024])
    postab = dram_in(nc, "postab", [128, 16, NT], I32)
    rows = dram_in(nc, "rows", [1, 3 * 1024])
    x2 = dram_out(nc, "x2", [T, 1024])
    with ExitStack() as st:
        fw = FW(nc, st)
        make_consts(fw, nc)
        pt = fw.sb("pt", [128, 16, NT], I32)
        rw = fw.sb("rw", [128, 3, 1024], F32)
        xt = [fw.sb(f"xt{i}", [128, 1024], F32) for i in range(2)]
        acc = [fw.sb(f"acc{i}", [128, 1024], F32) for i in range(2)]
        gb = [fw.sb(f"gb{i}", [128, 1024], F32) for i in range(4)]
        stats = fw.sb("stats", [128, 2, 6], F32)
        mv = fw.sb("mv", [128, 2], F32)
        rstd = fw.sb("rstd", [128, 1], F32)
        Bpt, Brw, Bs, Bo = Buf("pt"), Buf("rw"), Buf("stats"), Buf("o_x2")
        Bxt = [Buf("xt0"), Buf("xt1")]
        Bacc = [Buf("acc0"), Buf("acc1")]
        Bgb = [Buf(f"gb{i}") for i in range(4)]
        fw.dma("sp", pt[:], postab, writes=[Bpt])
        fw.dma("sp", rw[:].rearrange("p a b -> p (a b)"), rows.partition_broadcast(128), writes=[Brw])
        gi = 0
        for t in range(NT):
            s = t % 2
            fw.dma("sp", xt[s][:], x1[t * 128:(t + 1) * 128, :], writes=[Bxt[s]])
            for e in range(16):
                q = gi % 4
                gi += 1
                dst = acc[s] if e == 0 else gb[q]
                Bd = Bacc[s] if e == 0 else Bgb[q]
                fw.dma("pool", dst[:], ycb, reads=[Bpt], writes=[Bd],
                       indirect=dict(out_offset=None, in_offset=bass.IndirectOffsetOnAxis(ap=pt[:, e, t:t + 1], axis=0),
                                     element_offset=e * RS * 1024))
                if e > 0:
                    fw.op("dve", lambda: nc.vector.tensor_tensor(out=acc[s][:], in0=acc[s][:], in1=gb[q][:], op=ALU.add),
                          reads=[Bacc[s], Bgb[q]], writes=[Bacc[s]])
            fw.op("dve", lambda: nc.vector.tensor_tensor(out=acc[s][:], in0=acc[s][:], in1=rw[:, 0, :], op=ALU.mult),
                  reads=[Bacc[s], Brw], writes=[Bacc[s]])
            fw.op("dve", lambda: nc.vector.scalar_tensor_tensor(out=acc[s][:], in0=xt[s][:], scalar=ALPHA, in1=acc[s][:], op0=ALU.mult, op1=ALU.add),
                  reads=[Bxt[s], Bacc[s]], writes=[Bacc[s]])
            layer_norm_tile(fw, nc, acc[s], Bacc[s], stats, mv, rstd, Bs)
            fw.op("pool", lambda: nc.gpsimd.tensor_tensor(out=acc[s][:], in0=acc[s][:], in1=rw[:, 1, :], op=ALU.mult),
                  reads=[Bacc[s], Brw], writes=[Bacc[s]])
            fw.op("pool", lambda: nc.gpsimd.tensor_tensor(out=acc[s][:], in0=acc[s][:], in1=rw[:, 2, :], op=ALU.add),
                  reads=[Bacc[s], Brw], writes=[Bacc[s]])
            fw.dma("sp", x2[t * 128:(t + 1) * 128, :], acc[s][:], reads=[Bacc[s]], writes=[Bo])
        fw.finish([Bo])
    return nc


def load_cast(fw, nc, dst, Bdst, src_fn, nk, width, wst, Bwst, ctr):
    for k in range(nk):
        s = ctr[0] % 2
        ctr[0] += 1
        fw.dma("sp" if s == 0 else "act", wst[s][:, :width], src_fn(k), writes=[Bwst[s]])
        if s == 0:
            fw.op("act", lambda k=k: nc.scalar.copy(out=dst[:, k, :], in_=wst[s][:, :width]), reads=[Bwst[s]], writes=[Bdst])
        else:
            fw.op("pool", lambda k=k: nc.gpsimd.tensor_copy(out=dst[:, k, :], in_=wst[s][:, :width]), reads=[Bwst[s]], writes=[Bdst])


def build_MG(NT, stage=9):
    T = NT * 128
    NGRP = T // 512
    nc = new_nc()
    xT = dram_in(nc, "xT", [128, 8, T])
    mcol = dram_in(nc, "mcol", [128, 8, 2])
    win = dram_in(nc, "win", [128, 8, 4096])
    lnr = dram_in(nc, "lnr", [1, 2 * 2048])
    wsT = dram_in(nc, "wsT", [128, 16, 128])
    bsr = dram_in(nc, "bsr", [1, 16 * 128])
    gTo = dram_out(nc, "gT", [128, 16, T], BF16)
    with ExitStack() as st:
        fw = FW(nc, st)
        make_consts(fw, nc)
        V = nc.vector
        mc = fw.sb("mc", [128, 8, 2], F32)
        xs = [fw.sb(f"xs{i}", [128, T], F32) for i in range(2)]
        hT = fw.sb("hT", [128, 8, T], BF16)
        wb = fw.sb("wb", [128, 8, 4096], BF16)
        wst = [fw.sb(f"wst{i}", [128, 2048], F32) for i in range(2)]
        lnt = fw.sb("lnt", [128, 2, 2048], F32)
        wsf = fw.sb("wsf", [128, 16, 128], F32)
        wsb = fw.sb("wsb", [128, 16, 128], BF16)
        bst = fw.sb("bst", [128, 16, 128], F32)
        uT = fw.sb("uT", [128, 16, 512], BF16)
        v = fw.sb("v", [128, 2048], F32)
        vln = fw.sb("vln", [128, 2048], BF16)
        tmp = fw.sb("tmp", [128, 4, 128], F32)
        go = [fw.sb(f"go{i}", [128, 16, 128], BF16) for i in range(2)]
        stats = fw.sb("stats", [128, 4, 6], F32)
        mv = fw.sb("mv", [128, 2], F32)
        rstd = fw.sb("rstd", [128, 1], F32)
        pu = [fw.ps(f"pu{i}", [128, 512]) for i in range(2)]
        pv = [fw.ps(f"pv{i}", [128, 512]) for i in range(2)]
        psp = [fw.ps(f"psp{i}", [128, 4, 128]) for i in range(2)]
        B = {n: Buf(n) for n in ["mc", "xs0", "xs1", "hT", "wb", "wst0", "wst1", "lnt", "wsf", "wsb", "bst", "uT", "v", "vln", "tmp",
                                 "go0", "go1", "stats", "pu0", "pu1", "pv0", "pv1", "psp0", "psp1", "o"]}
        fw.dma("sp", mc[:], mcol, writes=[B["mc"]])
        fw.dma("sp", lnt[:].rearrange("p a b -> p (a b)"), lnr.partition_broadcast(128), writes=[B["lnt"]])
        fw.dma("sp", wsf[:], wsT, writes=[B["wsf"]])
        fw.dma("sp", bst[:].rearrange("p a b -> p (a b)"), bsr.partition_broadcast(128), writes=[B["bst"]])
        fw.op("dve", lambda: V.tensor_copy(out=wsb[:], in_=wsf[:]), reads=[B["wsf"]], writes=[B["wsb"]])
        fw.op("dve", lambda: V.tensor_scalar(out=mc[:, :, 0], in0=mc[:, :, 0], scalar1=1.0, scalar2=None, op0=ALU.add), reads=[B["mc"]], writes=[B["mc"]])
        for k in range(8):
            s = k % 2
            fw.dma("sp", xs[s][:], xT[:, k, :], writes=[B[f"xs{s}"]])
            fw.op("act", lambda k=k: nc.scalar.activation(out=hT[:, k, :], in_=xs[s][:], func=AF.Identity, scale=mc[:, k, 0:1], bias=mc[:, k, 1:2]),
                  reads=[B[f"xs{s}"], B["mc"]], writes=[B["hT"]])
        ctr = [0]
        wbv = wb[:].rearrange("p k (h w) -> p (k h) w", h=2)
        winv = win.rearrange("p k (h w) -> p (k h) w", h=2)
        load_cast(fw, nc, wbv, B["wb"], lambda k: winv[:, k, :], 16, 2048, wst, [B["wst0"], B["wst1"]], ctr)
        ev = 0
        for grp in range(NGRP if stage > 0 else 0):
            tsl = slice(grp * 512, (grp + 1) * 512)
            for uf in range(16):
                s = uf % 2
                for k in range(8):
                    fw.op("pe", lambda k=k: nc.tensor.matmul(pu[s][:], lhsT=wb[:, k, uf * 128:(uf + 1) * 128], rhs=hT[:, k, tsl], start=(k == 0), stop=(k == 7)),
                          reads=[B["wb"], B["hT"]], writes=[B[f"pu{s}"]])
                fw.op("act", lambda: nc.scalar.activation(out=uT[:, uf, :], in_=pu[s][:], func=AF.Gelu), reads=[B[f"pu{s}"]], writes=[B["uT"]])
            for cc in range(4 if stage > 1 else 0):
                ch = grp * 4 + cc
                csl = slice(ch * 128, (ch + 1) * 128)
                for n in range(4):
                    s = n % 2
                    for k in range(8):
                        fw.op("pe", lambda k=k: nc.tensor.matmul(pv[s][:], lhsT=hT[:, k, csl], rhs=wb[:, k, 2048 + n * 512:2048 + (n + 1) * 512],
                                                                  start=(k == 0), stop=(k == 7)), reads=[B["wb"], B["hT"]], writes=[B[f"pv{s}"]])
                    fw.op("act", lambda: nc.scalar.activation(out=v[:, n * 512:(n + 1) * 512], in_=pv[s][:], func=AF.Gelu), reads=[B[f"pv{s}"]], writes=[B["v"]])
                for j in range(4):
                    fw.op("dve", lambda j=j: V.bn_stats(out=stats[:, j, :], in_=v[:, j * 512:(j + 1) * 512]), reads=[B["v"]], writes=[B["stats"]])
                fw.op("dve", lambda: V.bn_aggr(out=mv[:], in_=stats[:].rearrange("p a b -> p (a b)")), reads=[B["stats"]], writes=[B["stats"]])
                fw.op("act", lambda: nc.scalar.activation(out=rstd[:], in_=mv[:, 1:2], func=AF.Sqrt, bias=fw.eps_ln[:, 0:1], scale=1.0),
                      reads=[B["stats"], fw.Bconst], writes=[B["stats"]])
                fw.op("dve", lambda: V.reciprocal(out=rstd[:], in_=rstd[:]), reads=[B["stats"]], writes=[B["stats"]])
                fw.op("dve", lambda: V.tensor_scalar(out=v[:], in0=v[:], scalar1=mv[:, 0:1], scalar2=rstd[:, 0:1], op0=ALU.subtract, op1=ALU.mult),
                      reads=[B["stats"], B["v"]], writes=[B["v"]])
                fw.op("pool", lambda: nc.gpsimd.tensor_tensor(out=v[:], in0=v[:], in1=lnt[:, 0, :], op=ALU.mult), reads=[B["v"], B["lnt"]], writes=[B["v"]])
                fw.op("pool", lambda: nc.gpsimd.tensor_tensor(out=vln[:], in0=v[:], in1=lnt[:, 1, :], op=ALU.add), reads=[B["v"], B["lnt"]], writes=[B["vln"]])
                os_ = ch % 2
                for g4 in range(4 if stage > 2 else 0):
                    s = g4 % 2
                    for gg in range(4):
                        g = g4 * 4 + gg
                        fw.op("pe", lambda g=g, gg=gg: nc.tensor.matmul(psp[s][:, gg, :], lhsT=vln[:, g * 128:(g + 1) * 128], rhs=wsb[:, g, :], start=True, stop=True),
                              reads=[B["vln"], B["wsb"]], writes=[B[f"psp{s}"]])
                    fw.op("dve", lambda: V.tensor_tensor(out=tmp[:], in0=psp[s][:], in1=bst[:, g4 * 4:(g4 + 1) * 4, :], op=ALU.add),
                          reads=[B[f"psp{s}"], B["bst"]], writes=[B["tmp"]])
                    fw.op("dve", lambda: V.tensor_tensor(out=go[os_][:, g4 * 4:(g4 + 1) * 4, :], in0=tmp[:], in1=uT[:, g4 * 4:(g4 + 1) * 4, cc * 128:(cc + 1) * 128], op=ALU.mult),
                          reads=[B["tmp"], B["uT"]], writes=[B[f"go{os_}"]])
                if stage != 3:
                    for q4 in range(4):
                        fw.dma("sp", gTo[:, q4 * 4:(q4 + 1) * 4, csl], go[os_][:, q4 * 4:(q4 + 1) * 4, :], reads=[B[f"go{os_}"]], writes=[B["o"]])
        fw.finish([B["o"]])
    return nc


def build_MA():
    NQT = SEQ // 128
    NCT = CTX // 128
    NKT = NQT + NCT
    nc = new_nc()
    xT = dram_in(nc, "xT", [128, 8, SEQ])
    cxT = dram_in(nc, "cxT", [128, 8, CTX])
    mcol = dram_in(nc, "mcol", [128, 8, 4])
    w = dram_in(nc, "w", [128, 8, 768])
    gains = dram_in(nc, "gains", [1, 640])
    cs = dram_in(nc, "cs", [128, 2, NQT, 32])
    identb = dram_in(nc, "identb", [128, 128], BF16)
    oTo = dram_out(nc, "oT", [128, 4, SEQ], BF16)
    with ExitStack() as st:
        fw = FW(nc, st)
        V = nc.vector
        G = nc.gpsimd
        mc = fw.sb("mc", [128, 8, 4], F32)
        xs = [fw.sb(f"xs{i}", [128, 2048], F32) for i in range(2)]
        hT = fw.sb("hT", [128, 8, CTX + 2048], BF16)
        wst = [fw.sb(f"wst{i}", [128, 768], F32) for i in range(2)]
        wb = fw.sb("wb", [128, 8, 768], BF16)
        g10 = fw.sb("g10", [128, 10, 64], F32)
        cst = fw.sb("cst", [128, 2, NQT, 32], F32)
        idb = fw.sb("idb", [128, 128], BF16)
        qk = fw.sb("qk", [128, 10, 64], F32)
        sq = fw.sb("sq", [128, 10, 64], F32)
        ss = fw.sb("ss", [128, 10], F32)
        t1 = fw.sb("t1", [128, 10, 32], F32)
        t2 = fw.sb("t2", [128, 10, 32], F32)
        qr = fw.sb("qr", [128, 10, 64], BF16)
        qT = fw.sb("qT", [64, 8, SEQ], BF16)
        kT = fw.sb("kT", [64, 2, NKT * 128], BF16)
        vall = fw.sb("vall", [128, NKT, 2, 65], BF16)
        E = [fw.sb(f"E{i}", [128, 512], BF16) for i in range(2)]
        rden = fw.sb("rden", [128, 4], F32)
        otok = fw.sb("otok", [128, 8, 64], BF16)
        oTt = [fw.sb(f"oTt{i}", [128, 4, 128], BF16) for i in range(2)]
        epsr = fw.sb("epsr", [128, 1], F32)
        pA = [fw.ps(f"pA{i}", [128, 512]) for i in range(2)]
        pT = fw.ps("pT", [128, 4, 128], BF16)
        pO = [fw.ps(f"pO{i}", [128, 512]) for i in range(4)]
        B = {n: Buf(n) for n in ["mc", "xs0", "xs1", "hT", "wst0", "wst1", "wb", "g10", "cst", "idb", "qk", "sq", "ss", "t1", "t2", "qr", "qT", "kT",
                                 "vall", "E0", "E1", "rden", "otok", "oTt0", "oTt1", "epsr", "pA0", "pA1", "pT", "pO0", "pO1", "pO2", "pO3", "o"]}
        fw.dma("sp", mc[:], mcol, writes=[B["mc"]])
        fw.dma("sp", g10[:].rearrange("p a b -> p (a b)"), gains.partition_broadcast(128), writes=[B["g10"]])
        fw.dma("sp", cst[:], cs, writes=[B["cst"]])
        fw.dma("sp", idb[:], identb, writes=[B["idb"]])
        fw.op("pool", lambda: G.memset(epsr[:], RMS_EPS), writes=[B["epsr"]])
        fw.op("pool", lambda: G.memset(vall[:, :, :, 64:65], 1.0), writes=[B["vall"]])
        fw.op("dve", lambda: V.tensor_scalar(out=g10[:, 0:8, :], in0=g10[:, 0:8, :], scalar1=0.125, scalar2=None, op0=ALU.mult), reads=[B["g10"]], writes=[B["g10"]])
        for c in (0, 2):
            fw.op("dve", lambda c=c: V.tensor_scalar(out=mc[:, :, c], in0=mc[:, :, c], scalar1=1.0, scalar2=None, op0=ALU.add), reads=[B["mc"]], writes=[B["mc"]])
        li = [0]

        def load_half(hf):
            for k in range(8):
                if hf == 0:
                    s = li[0] % 2
                    li[0] += 1
                    fw.dma("sp", xs[s][:, :CTX], cxT[:, k, :], writes=[B[f"xs{s}"]])
                    fw.op("act", lambda k=k: nc.scalar.activation(out=hT[:, k, 0:CTX], in_=xs[s][:, :CTX], func=AF.Identity, scale=mc[:, k, 2:3], bias=mc[:, k, 3:4]),
                          reads=[B[f"xs{s}"], B["mc"]], writes=[B["hT"]])
                s = li[0] % 2
                li[0] += 1
                fw.dma("sp", xs[s][:], xT[:, k, hf * 2048:(hf + 1) * 2048], writes=[B[f"xs{s}"]])
                fw.op("act", lambda k=k: nc.scalar.activation(out=hT[:, k, CTX:CTX + 2048], in_=xs[s][:], func=AF.Identity,
                                                              scale=mc[:, k, 0:1], bias=mc[:, k, 1:2]),
                      reads=[B[f"xs{s}"], B["mc"]], writes=[B["hT"]])

        ctr = [0]
        load_cast(fw, nc, wb, B["wb"], lambda k: w[:, k, :], 8, 768, wst, [B["wst0"], B["wst1"]], ctr)

        def bc10(ap2, n):
            return ap2.unsqueeze(2).to_broadcast([128, 10, n])

        for tt in range(NKT):
            is_ctx = tt < NCT
            tsl = slice(tt * 128, (tt + 1) * 128)
            if tt == 0:
                load_half(0)
            if tt == NCT + 16:
                load_half(1)
            hc = tt * 128 if tt < NCT + 16 else (tt - 16) * 128
            hsl = slice(hc, hc + 128)
            for k in range(8):
                fw.op("pe", lambda k=k: nc.tensor.matmul(pA[0][:], lhsT=hT[:, k, hsl], rhs=wb[:, k, 0:512], start=(k == 0), stop=(k == 7)),
                      reads=[B["hT"], B["wb"]], writes=[B["pA0"]])
            for k in range(8):
                fw.op("pe", lambda k=k: nc.tensor.matmul(pA[1][:, 0:256], lhsT=hT[:, k, hsl], rhs=wb[:, k, 512:768], start=(k == 0), stop=(k == 7)),
                      reads=[B["hT"], B["wb"]], writes=[B["pA1"]])
            fw.op("act", lambda: nc.scalar.copy(out=qk[:, 0:8, :], in_=pA[0][:].rearrange("p (a b) -> p a b", b=64)), reads=[B["pA0"]], writes=[B["qk"]])
            fw.op("act", lambda: nc.scalar.copy(out=qk[:, 8:10, :], in_=pA[1][:, 0:128].rearrange("p (a b) -> p a b", b=64)), reads=[B["pA1"]], writes=[B["qk"]])
            fw.op("act", lambda: nc.scalar.copy(out=vall[:, tt, :, 0:64], in_=pA[1][:, 128:256].rearrange("p (a b) -> p a b", b=64)), reads=[B["pA1"]], writes=[B["vall"]])
            fw.op("pool", lambda: G.tensor_tensor(out=sq[:], in0=qk[:], in1=qk[:], op=ALU.mult), reads=[B["qk"]], writes=[B["sq"]])
            fw.op("dve", lambda: V.tensor_reduce(out=ss[:], in_=sq[:], axis=AX.X, op=ALU.add), reads=[B["sq"]], writes=[B["ss"]])
            fw.op("act", lambda: nc.scalar.activation(out=ss[:], in_=ss[:], func=AF.Sqrt, scale=1.0 / 64.0, bias=epsr[:, 0:1]), reads=[B["ss"], B["epsr"]], writes=[B["ss"]])
            fw.op("dve", lambda: V.reciprocal(out=ss[:], in_=ss[:]), reads=[B["ss"]], writes=[B["ss"]])
            fw.op("dve", lambda: V.tensor_tensor(out=qk[:], in0=qk[:], in1=bc10(ss[:, :], 64), op=ALU.mult), reads=[B["qk"], B["ss"]], writes=[B["qk"]])
            if is_ctx:
                fw.op("dve", lambda: V.tensor_tensor(out=qr[:], in0=qk[:], in1=g10[:], op=ALU.mult), reads=[B["qk"], B["g10"]], writes=[B["qr"]])
            else:
                lt = tt - NCT
                fw.op("pool", lambda: G.tensor_tensor(out=qk[:], in0=qk[:], in1=g10[:], op=ALU.mult), reads=[B["qk"], B["g10"]], writes=[B["qk"]])
                cosb = cst[:, 0, lt, :].unsqueeze(1).to_broadcast([128, 10, 32])
                sinb = cst[:, 1, lt, :].unsqueeze(1).to_broadcast([128, 10, 32])
                fw.op("dve", lambda: V.tensor_tensor(out=t1[:], in0=qk[:, :, 0:32], in1=cosb, op=ALU.mult), reads=[B["qk"], B["cst"]], writes=[B["t1"]])
                fw.op("pool", lambda: G.tensor_tensor(out=t2[:], in0=qk[:, :, 32:64], in1=sinb, op=ALU.mult), reads=[B["qk"], B["cst"]], writes=[B["t2"]])
                fw.op("dve", lambda: V.tensor_tensor(out=qr[:, :, 0:32], in0=t1[:], in1=t2[:], op=ALU.subtract), reads=[B["t1"], B["t2"]], writes=[B["qr"]])
                fw.op("dve", lambda: V.tensor_tensor(out=t1[:], in0=qk[:, :, 0:32], in1=sinb, op=ALU.mult), reads=[B["qk"], B["cst"], B["qr"]], writes=[B["t1"]])
                fw.op("pool", lambda: G.tensor_tensor(out=t2[:], in0=qk[:, :, 32:64], in1=cosb, op=ALU.mult), reads=[B["qk"], B["cst"], B["qr"]], writes=[B["t2"]])
                fw.op("dve", lambda: V.tensor_tensor(out=qr[:, :, 32:64], in0=t1[:], in1=t2[:], op=ALU.add), reads=[B["t1"], B["t2"]], writes=[B["qr"]])
            heads = [8, 9] if is_ctx else list(range(10))
            for i0 in range(0, len(heads), 4):
                hs = heads[i0:i0 + 4]
                for j, hh in enumerate(hs):
                    fw.op("pe", lambda j=j, hh=hh: nc.tensor.transpose(out=pT[0:64, j, :], in_=qr[:, hh, :], identity=idb[:]),
                          reads=[B["qr"], B["idb"]], writes=[B["pT"]])
                if hs[0] < 8:
                    lt = tt - NCT
                    fw.op("act", lambda: nc.scalar.copy(out=qT[:, hs[0]:hs[0] + 4, lt * 128:(lt + 1) * 128], in_=pT[0:64, 0:4, :]), reads=[B["pT"]], writes=[B["qT"]])
                else:
                    fw.op("act", lambda: nc.scalar.copy(out=kT[:, :, tsl], in_=pT[0:64, 0:2, :]), reads=[B["pT"]], writes=[B["kT"]])
        for qt in range(NQT):
            qsl = slice(qt * 128, (qt + 1) * 128)
            for kh in range(2):
                for kt in range(NKT):
                    s = kt % 2
                    fw.op("pe", lambda: nc.tensor.matmul(pA[s][:].rearrange("p (a b) -> p a b", b=128), lhsT=kT[:, kh, kt * 128:(kt + 1) * 128],
                                                         rhs=qT[:, kh * 4:(kh + 1) * 4, qsl], start=True, stop=True),
                          reads=[B["kT"], B["qT"]], writes=[B[f"pA{s}"]])
                    fw.op("act", lambda: nc.scalar.activation(out=E[s][:], in_=pA[s][:], func=AF.Exp), reads=[B[f"pA{s}"]], writes=[B[f"E{s}"]])
                    for g in range(4):
                        fw.op("pe", lambda g=g: nc.tensor.matmul(pO[g][:, 0:65], lhsT=E[s][:, g * 128:(g + 1) * 128], rhs=vall[:, kt, kh, :],
                                                                  start=(kt == 0), stop=(kt == NKT - 1)),
                              reads=[B[f"E{s}"], B["vall"]], writes=[B[f"pO{g}"]])
                for g in range(4):
                    fw.op("dve", lambda g=g: V.reciprocal(out=rden[:, g:g + 1], in_=pO[g][:, 64:65]), reads=[B[f"pO{g}"]], writes=[B["rden"]])
                    fw.op("dve", lambda g=g: V.tensor_scalar(out=otok[:, kh * 4 + g, :], in0=pO[g][:, 0:64], scalar1=rden[:, g:g + 1], scalar2=None, op0=ALU.mult),
                          reads=[B[f"pO{g}"], B["rden"]], writes=[B["otok"]])
            os_ = qt % 2
            for j in range(4):
                fw.op("pe", lambda j=j: nc.tensor.transpose(out=pT[:, j, :], in_=otok[:, 2 * j:2 * j + 2, :].rearrange("p a b -> p (a b)"), identity=idb[:]),
                      reads=[B["otok"], B["idb"]], writes=[B["pT"]])
            fw.op("act", lambda: nc.scalar.copy(out=oTt[os_][:], in_=pT[:]), reads=[B["pT"]], writes=[B[f"oTt{os_}"]])
            fw.dma("sp", oTo[:, :, qsl], oTt[os_][:], reads=[B[f"oTt{os_}"]], writes=[B["o"]])
        fw.finish([B["o"]])
    return nc


def rope_tables():
    L = SEQ
    rows = L // 64
    row = np.broadcast_to(np.arange(rows, dtype=np.float32)[:, None], (rows, 64)).reshape(L)
    col = np.broadcast_to(np.arange(64, dtype=np.float32)[None, :], (rows, 64)).reshape(L)
    inv = (10000.0 ** (-np.arange(16, dtype=np.float32) / 16)).astype(np.float32)
    ang = np.concatenate([row[:, None] * inv, col[:, None] * inv], axis=-1).astype(np.float32)
    cs = np.stack([np.cos(ang), np.sin(ang)], axis=0).astype(np.float32)
    return np.ascontiguousarray(cs.reshape(2, L // 128, 128, 32).transpose(2, 0, 1, 3))


_TAB = {}


def dft_tables(L):
    if L in _TAB:
        return _TAB[L]
    N = 2 * L
    TW = min(512, L)
    t = np.arange(L, dtype=np.int64)
    ph = (np.outer(t, t) % N).astype(np.float64) * (2.0 * np.pi / N)
    C = np.cos(ph)
    S = -np.sin(ph)
    S[:, 0] = np.where(t % 2 == 0, 1.0, -1.0)

    def slab(M):
        return np.ascontiguousarray(M.reshape(L // 128, 128, L // TW, TW).transpose(2, 1, 0, 3).astype(np.float32).astype(NPBF))
    _TAB[L] = (slab(C), slab(S), slab(np.ascontiguousarray(S.T)))
    return _TAB[L]


def hyena_consts(L):
    t = np.linspace(0.0, 1.0, L, dtype=np.float32)[:, None]
    w = ((2.0 * math.pi / L) * np.arange(L, dtype=np.float32))[:, None].astype(np.float32)
    f = np.linspace(1e-4, 15, 16, dtype=np.float32)[None, :]
    z = np.concatenate([t, np.cos(f * w), -np.sin(f * w)], axis=-1).astype(np.float32)
    min_decay = math.log(1e-2) / 1.5
    max_decay = math.log(1e-2) / 0.3
    deltas = np.abs(np.linspace(min_decay, max_decay, D, dtype=np.float32))
    decay = np.exp(-t * deltas).astype(np.float32)
    return np.ascontiguousarray(z.T), np.ascontiguousarray(decay.T)


def emit_fwd_dft(fw, nc, L, x_tm, Bx, tabC, tabS, slabs, Bslab, pacc, Bpacc, consume):
    TW = min(512, L)
    NK = L // 128
    sc = [0]
    for nt in range(L // TW):
        for which, tab in ((0, tabC), (1, tabS)):
            s = sc[0] % 2
            sc[0] += 1
            fw.dma("sp" if s == 0 else "act", slabs[s][:, :NK, :TW], tab[nt], writes=[Bslab[s]])
            for st_ in range(4):
                for k in range(NK):
                    fw.op("pe", lambda k=k: nc.tensor.matmul(pacc[st_][:, :TW], lhsT=x_tm[:, st_, k, :], rhs=slabs[s][:, k, :TW], start=(k == 0), stop=(k == NK - 1)),
                          reads=[Bx, Bslab[s]], writes=[Bpacc[st_]])
                consume(nt, which, st_, pacc[st_][:, :TW])


def wrap_pi(fw, nc, a, Ba, m, Bm, P):
    V = nc.vector
    PI = math.pi
    for _ in range(2):
        fw.op("dve", lambda: V.tensor_scalar(out=m[:P], in0=a[:P], scalar1=-PI, scalar2=2 * PI, op0=ALU.is_lt, op1=ALU.mult), reads=[Ba], writes=[Bm])
        fw.op("dve", lambda: V.tensor_tensor(out=a[:P], in0=a[:P], in1=m[:P], op=ALU.add), reads=[Ba, Bm], writes=[Ba])
        fw.op("dve", lambda: V.tensor_scalar(out=m[:P], in0=a[:P], scalar1=PI, scalar2=2 * PI, op0=ALU.is_gt, op1=ALU.mult), reads=[Ba], writes=[Bm])
        fw.op("dve", lambda: V.tensor_tensor(out=a[:P], in0=a[:P], in1=m[:P], op=ALU.subtract), reads=[Ba, Bm], writes=[Ba])
    fw.op("dve", lambda: V.tensor_scalar(out=a[:P], in0=a[:P], scalar1=-PI, scalar2=PI, op0=ALU.max, op1=ALU.min), reads=[Ba], writes=[Ba])


def build_F(L):
    TW = min(512, L)
    NK = L // 128
    NTL = L // TW
    N = 2 * L
    nc = new_nc()
    zT = dram_in(nc, "zT", [33, L])
    decay = dram_in(nc, "decay", [128, L])
    w1 = dram_in(nc, "w1", [33, 64])
    w2 = dram_in(nc, "w2", [64, 64])
    w3 = dram_in(nc, "w3", [64, 4, 128])
    vecs = dram_in(nc, "vecs", [64, 4])
    fbias = dram_in(nc, "fbias", [128, 2])
    identf = dram_in(nc, "identf", [128, 128])
    tabC = dram_in(nc, "tabC", [NTL, 128, NK, TW], BF16)
    tabS = dram_in(nc, "tabS", [NTL, 128, NK, TW], BF16)
    Kfo = dram_out(nc, "Kf", [128, 2, 2, L])
    with ExitStack() as st:
        fw = FW(nc, st)
        V = nc.vector
        G = nc.gpsimd
        w1t = fw.sb("w1t", [33, 64], F32)
        w2t = fw.sb("w2t", [64, 64], F32)
        w3t = fw.sb("w3t", [64, 4, 128], F32)
        vt = fw.sb("vt", [64, 6], F32)
        fbt = fw.sb("fbt", [128, 2], F32)
        idf = fw.sb("idf", [128, 128], F32)
        nrm = fw.sb("nrm", [128, 8], F32)
        x_tm = fw.sb("x_tm", [128, 4, NK, 128], BF16)
        pacc = [fw.ps(f"pacc{i}", [128, 512]) for i in range(4)]
        ptr = [fw.ps(f"ptr{i}", [128, 4, 128]) for i in range(2)]
        scope1 = fw.scoped()
        scope1.__enter__()
        zt = fw.sb("zt", [33, L], F32)
        dct = fw.sb("dct", [128, L], F32)
        h1 = fw.sb("h1", [64, L], F32)
        h2 = fw.sb("h2", [64, L], F32)
        mk = fw.sb("mk", [64, L], F32)
        kk = [fw.sb(f"kk{i}", [128, L], F32) for i in range(4)]
        B = {n: Buf(n) for n in ["zt", "dct", "w1t", "w2t", "w3t", "vt", "fbt", "idf", "h1", "h2", "mk", "kk0", "kk1", "kk2", "kk3", "nrm", "x_tm",
                                 "slab0", "slab1", "Kf", "pacc0", "pacc1", "pacc2", "pacc3", "ptr0", "ptr1", "o"]}
        for t_, src, nm in ((zt, zT, "zt"), (dct, decay, "dct"), (w1t, w1, "w1t"), (w2t, w2, "w2t"), (w3t, w3, "w3t"), (fbt, fbias, "fbt"), (idf, identf, "idf")):
            fw.dma("sp", t_[:], src, writes=[B[nm]])
        fw.dma("sp", vt[:, 0:4], vecs, writes=[B["vt"]])
        fw.op("dve", lambda: V.tensor_tensor(out=vt[:, 4:6], in0=vt[:, 0:2], in1=vt[:, 2:4], op=ALU.mult), reads=[B["vt"]], writes=[B["vt"]])
        for layer, (wt, Bw, src, Bsrc, dst, Bdst, KP) in enumerate(((w1t, B["w1t"], zt, B["zt"], h1, B["h1"], 33), (w2t, B["w2t"], h1, B["h1"], h2, B["h2"], 64))):
            for j in range(L // TW):
                s = j % 2
                sl = slice(j * TW, (j + 1) * TW)
                fw.op("pe", lambda: nc.tensor.matmul(pacc[s][0:64, :TW], lhsT=wt[:KP, :], rhs=src[:KP, sl], start=True, stop=True), reads=[Bw, Bsrc], writes=[B[f"pacc{s}"]])
                fw.op("act", lambda: nc.scalar.activation(out=dst[:, sl], in_=pacc[s][0:64, :TW], func=AF.Identity, scale=vt[:, 2 + layer:3 + layer], bias=vt[:, 4 + layer:5 + layer]),
                      reads=[B[f"pacc{s}"], B["vt"]], writes=[Bdst])
            wrap_pi(fw, nc, dst, Bdst, mk, B["mk"], 64)
            fw.op("act", lambda: nc.scalar.activation(out=dst[:, :], in_=dst[:, :], func=AF.Sin), reads=[Bdst], writes=[Bdst])
        for st_ in range(4):
            for j in range(L // TW):
                s = j % 2
                sl = slice(j * TW, (j + 1) * TW)
                fw.op("pe", lambda: nc.tensor.matmul(pacc[s][:, :TW], lhsT=w3t[:, st_, :], rhs=h2[:, sl], start=True, stop=True), reads=[B["w3t"], B["h2"]], writes=[B[f"pacc{s}"]])
                fw.op("dve", lambda: V.tensor_tensor(out=kk[st_][:, sl], in0=pacc[s][:, :TW], in1=dct[:, sl], op=ALU.mult), reads=[B[f"pacc{s}"], B["dct"]], writes=[B[f"kk{st_}"]])
            if st_ % 2 == 1:
                fw.op("pool", lambda: G.memset(kk[st_][:, 0:1], 0.0), reads=[], writes=[B[f"kk{st_}"]])
            fw.op("dve", lambda: V.tensor_reduce(out=nrm[:, st_:st_ + 1], in_=kk[st_][:], axis=AX.X, op=ALU.add, apply_absolute_value=True),
                  reads=[B[f"kk{st_}"]], writes=[B["nrm"]])
        for o in range(2):
            fw.op("dve", lambda: V.tensor_tensor(out=nrm[:, 4 + o:5 + o], in0=nrm[:, 2 * o:2 * o + 1], in1=nrm[:, 2 * o + 1:2 * o + 2], op=ALU.add), reads=[B["nrm"]], writes=[B["nrm"]])
            fw.op("dve", lambda: V.tensor_scalar(out=nrm[:, 4 + o:5 + o], in0=nrm[:, 4 + o:5 + o], scalar1=RMS_EPS, scalar2=None, op0=ALU.add), reads=[B["nrm"]], writes=[B["nrm"]])
            fw.op("dve", lambda: V.reciprocal(out=nrm[:, 6 + o:7 + o], in_=nrm[:, 4 + o:5 + o]), reads=[B["nrm"]], writes=[B["nrm"]])
        for st_ in range(4):
            o = st_ // 2
            fw.op("pool", lambda: G.tensor_scalar(out=kk[st_][:], in0=kk[st_][:], scalar1=nrm[:, 6 + o:7 + o], scalar2=None, op0=ALU.mult), reads=[B[f"kk{st_}"], B["nrm"]], writes=[B[f"kk{st_}"]])
            for k4 in range(NK // 4 if NK >= 4 else 1):
                s = k4 % 2
                nn = min(4, NK)
                for kq in range(nn):
                    k = k4 * 4 + kq
                    fw.op("pe", lambda k=k, kq=kq: nc.tensor.transpose(out=ptr[s][:, kq, :], in_=kk[st_][:, k * 128:(k + 1) * 128], identity=idf[:]),
                          reads=[B[f"kk{st_}"], B["idf"]], writes=[B[f"ptr{s}"]])
                fw.op("act", lambda: nc.scalar.copy(out=x_tm[:, st_, k4 * 4:k4 * 4 + nn, :], in_=ptr[s][:, 0:nn, :]), reads=[B[f"ptr{s}"]], writes=[B["x_tm"]])

        scope1.__exit__(None, None, None)
        slabs = [fw.sb(f"slab{i}", [128, NK, TW], BF16) for i in range(2)]
        Kf = fw.sb("Kfs", [128, 2, 2, L], F32)

        def consume(nt, which, st_, ps):
            o, d = st_ // 2, st_ % 2
            dst = Kf[:, o, which, nt * TW:(nt + 1) * TW]
            if d == 0:
                fw.op("act", lambda: nc.scalar.copy(out=dst, in_=ps), reads=[B[f"pacc{st_}"]], writes=[B["Kf"]])
            else:
                op = ALU.add if which == 0 else ALU.subtract
                fw.op("dve", lambda: V.tensor_tensor(out=dst, in0=dst, in1=ps, op=op), reads=[B[f"pacc{st_}"], B["Kf"]], writes=[B["Kf"]])
                if which == 1 and nt == 0:
                    fw.op("dve", lambda: V.scalar_tensor_tensor(out=Kf[:, o, 1, 0:1], in0=ps[:, 0:1], scalar=2.0, in1=Kf[:, o, 1, 0:1], op0=ALU.mult, op1=ALU.add),
                          reads=[B[f"pacc{st_}"], B["Kf"]], writes=[B["Kf"]])
        emit_fwd_dft(fw, nc, L, x_tm, B["x_tm"], tabC, tabS, slabs, [B["slab0"], B["slab1"]], pacc, [B[f"pacc{i}"] for i in range(4)], consume)
        for o in range(2):
            fw.op("dve", lambda: V.tensor_scalar(out=Kf[:, o, 0, :], in0=Kf[:, o, 0, :], scalar1=fbt[:, o:o + 1], scalar2=2.0 / N, op0=ALU.add, op1=ALU.mult),
                  reads=[B["Kf"], B["fbt"]], writes=[B["Kf"]])
            fw.op("dve", lambda: V.tensor_scalar(out=Kf[:, o, 1, 0:1], in0=Kf[:, o, 1, 0:1], scalar1=fbt[:, o:o + 1], scalar2=None, op0=ALU.add), reads=[B["Kf"], B["fbt"]], writes=[B["Kf"]])
            fw.op("dve", lambda: V.tensor_scalar(out=Kf[:, o, 1, :], in0=Kf[:, o, 1, :], scalar1=2.0 / N, scalar2=None, op0=ALU.mult), reads=[B["Kf"]], writes=[B["Kf"]])
            fw.op("dve", lambda: V.tensor_scalar(out=Kf[:, o, :, 0:1], in0=Kf[:, o, :, 0:1], scalar1=0.5, scalar2=None, op0=ALU.mult), reads=[B["Kf"]], writes=[B["Kf"]])
            for ri in range(2):
                fw.dma("sp", Kfo[:, o, ri, :], Kf[:, o, ri, :], reads=[B["Kf"]], writes=[B["o"]])
        fw.finish([B["o"]])
    return nc


def build_MH(L):
    TW = min(512, L)
    NK = L // 128
    NTL = L // TW
    TJ = TW // 128
    nc = new_nc()
    xT = dram_in(nc, "xT", [128, 8, L])
    mcol = dram_in(nc, "mcol", [128, 8, 2])
    win = dram_in(nc, "win", [128, 8, 1536])
    cw = dram_in(nc, "cw", [128, 12, 4])
    Kfi = dram_in(nc, "Kf", [4, 128, 2, 2, L])
    tabC = dram_in(nc, "tabC", [NTL, 128, NK, TW], BF16)
    tabS = dram_in(nc, "tabS", [NTL, 128, NK, TW], BF16)
    tabST = dram_in(nc, "tabST", [NTL, 128, NK, TW], BF16)
    identf = dram_in(nc, "identf", [128, 128])
    zTo = dram_out(nc, "zT", [128, 4, L], BF16)
    x12 = nc.dram_tensor("x12", [4, 2, 128, L], F32).ap()
    with ExitStack() as st:
        fw = FW(nc, st)
        V = nc.vector
        G = nc.gpsimd
        mc = fw.sb("mc", [128, 8, 2], F32)
        cwt = fw.sb("cwt", [128, 12, 4], F32)
        idf = fw.sb("idf", [128, 128], F32)
        x_tm = fw.sb("x_tm", [128, 4, NK, 128], BF16)
        pacc = [fw.ps(f"pacc{i}", [128, 512]) for i in range(4)]
        ptr = [fw.ps(f"ptr{i}", [128, 4, 128]) for i in range(2)]
        B = {n: Buf(n) for n in ["mc", "cwt", "idf", "x_tm", "pacc0", "pacc1", "pacc2", "pacc3", "ptr0", "ptr1", "x12", "o",
                                 "xs0", "xs1", "hT", "wb", "wst0", "wst1", "pb0", "pb1", "ob0", "ob1",
                                 "slab0", "slab1", "y_fm", "Xr", "kt0", "kt1", "Y", "ta", "tb", "xq0", "xq1", "zb0", "zb1", "zo0", "zo1"]}
        Bpacc = [B[f"pacc{i}"] for i in range(4)]
        fw.dma("sp", mc[:], mcol, writes=[B["mc"]])
        fw.dma("sp", cwt[:], cw, writes=[B["cwt"]])
        fw.dma("sp", idf[:], identf, writes=[B["idf"]])
        fw.op("dve", lambda: V.tensor_scalar(out=mc[:, :, 0], in0=mc[:, :, 0], scalar1=1.0, scalar2=None, op0=ALU.add), reads=[B["mc"]], writes=[B["mc"]])
        scA = fw.scoped()
        scA.__enter__()
        XW = min(1024, L)
        xs = [fw.sb(f"xs{i}", [128, XW], F32) for i in range(2)]
        hT = fw.sb("hT", [128, 8, L], BF16)
        wb = fw.sb("wb", [128, 8, 1536], BF16)
        wst = [fw.sb(f"wst{i}", [128, 768], F32) for i in range(2)]
        pb = [fw.sb(f"pb{i}", [128, L + 2], F32) for i in range(2)]
        ob = [fw.sb(f"ob{i}", [128, L], F32) for i in range(2)]
        li = 0
        for k in range(8):
            for hf in range(L // XW):
                s = li % 2
                li += 1
                fw.dma("sp", xs[s][:], xT[:, k, hf * XW:(hf + 1) * XW], writes=[B[f"xs{s}"]])
                fw.op("act", lambda k=k, hf=hf: nc.scalar.activation(out=hT[:, k, hf * XW:(hf + 1) * XW], in_=xs[s][:], func=AF.Identity, scale=mc[:, k, 0:1], bias=mc[:, k, 1:2]),
                      reads=[B[f"xs{s}"], B["mc"]], writes=[B["hT"]])
        ctr = [0]
        wbv = wb[:].rearrange("p k (h w) -> p (k h) w", h=2)
        winv = win.rearrange("p k (h w) -> p (k h) w", h=2)
        load_cast(fw, nc, wbv, B["wb"], lambda k: winv[:, k, :], 16, 768, wst, [B["wst0"], B["wst1"]], ctr)
        for s in range(2):
            fw.op("pool", lambda s=s: G.memset(pb[s][:, 0:1], 0.0), writes=[B[f"pb{s}"]])
            fw.op("pool", lambda s=s: G.memset(pb[s][:, L + 1:L + 2], 0.0), writes=[B[f"pb{s}"]])
        it = 0
        for st_ in range(4):
            for q in range(3):
                s = it % 2
                it += 1
                col0 = (st_ * 3 + q) * 128
                for tg in range(NTL):
                    pa = tg % 4
                    for k in range(8):
                        fw.op("pe", lambda k=k: nc.tensor.matmul(pacc[pa][:, :TW], lhsT=wb[:, k, col0:col0 + 128], rhs=hT[:, k, tg * TW:(tg + 1) * TW], start=(k == 0), stop=(k == 7)),
                              reads=[B["wb"], B["hT"]], writes=[Bpacc[pa]])
                    fw.op("act", lambda: nc.scalar.copy(out=pb[s][:, 1 + tg * TW:1 + (tg + 1) * TW], in_=pacc[pa][:, :TW]), reads=[Bpacc[pa]], writes=[B[f"pb{s}"]])
                ci = st_ * 3 + q
                fw.op("act", lambda: nc.scalar.activation(out=ob[s][:], in_=pb[s][:, 1:L + 1], func=AF.Identity, scale=cwt[:, ci, 1:2], bias=cwt[:, ci, 3:4]),
                      reads=[B[f"pb{s}"], B["cwt"]], writes=[B[f"ob{s}"]])
                fw.op("dve", lambda: V.scalar_tensor_tensor(out=ob[s][:], in0=pb[s][:, 0:L], scalar=cwt[:, ci, 0:1], in1=ob[s][:], op0=ALU.mult, op1=ALU.add),
                      reads=[B[f"pb{s}"], B["cwt"], B[f"ob{s}"]], writes=[B[f"ob{s}"]])
                fw.op("dve", lambda: V.scalar_tensor_tensor(out=ob[s][:], in0=pb[s][:, 2:L + 2], scalar=cwt[:, ci, 2:3], in1=ob[s][:], op0=ALU.mult, op1=ALU.add),
                      reads=[B[f"pb{s}"], B["cwt"], B[f"ob{s}"]], writes=[B[f"ob{s}"]])
                if q == 0:
                    for k4 in range(max(1, NK // 4)):
                        ps_ = k4 % 2
                        nn = min(4, NK)
                        for kq in range(nn):
                            k = k4 * 4 + kq
                            fw.op("pe", lambda k=k, kq=kq: nc.tensor.transpose(out=ptr[ps_][:, kq, :], in_=ob[s][:, k * 128:(k + 1) * 128], identity=idf[:]),
                                  reads=[B[f"ob{s}"], B["idf"]], writes=[B[f"ptr{ps_}"]])
                        fw.op("act", lambda: nc.scalar.copy(out=x_tm[:, st_, k4 * 4:k4 * 4 + nn, :], in_=ptr[ps_][:, 0:nn, :]), reads=[B[f"ptr{ps_}"]], writes=[B["x_tm"]])
                else:
                    fw.dma("sp", x12[st_, q - 1], ob[s][:], reads=[B[f"ob{s}"]], writes=[B["x12"]])
        scA.__exit__(None, None, None)
        slabs = [fw.sb(f"slab{i}", [128, NK, TW], BF16) for i in range(2)]
        Bslab = [B["slab0"], B["slab1"]]
        y_fm = fw.sb("y_fm", [128, 4, 2, NK, 128], BF16)
        Xr = fw.sb("Xr", [128, 4, TW], F32)
        kt = [fw.sb(f"kt{i}", [128, 2, TW], F32) for i in range(2)]
        Y = fw.sb("Y", [128, 2, TW], F32)
        ta = fw.sb("ta", [128, TW], F32)
        tb = fw.sb("tb", [128, TW], F32)
        xq = [fw.sb(f"xq{i}", [128, TW], F32) for i in range(2)]
        zb = [fw.sb(f"zb{i}", [128, TW], F32) for i in range(2)]
        zo = [fw.sb(f"zo{i}", [128, TW], BF16) for i in range(2)]
        cnt = {"kt": 0, "tr": 0, "xq": 0, "z": 0}
        for o in range(2):
            def consume(nt, which, st_, ps):
                fsl = slice(nt * TW, (nt + 1) * TW)
                if which == 0:
                    fw.op("act", lambda: nc.scalar.copy(out=Xr[:, st_, :], in_=ps), reads=[Bpacc[st_]], writes=[B["Xr"]])
                    return
                ks = cnt["kt"] % 2
                cnt["kt"] += 1
                fw.dma("pool", kt[ks][:], Kfi[st_, :, o, :, fsl], writes=[B[f"kt{ks}"]])
                Kr, Ki = kt[ks][:, 0, :], kt[ks][:, 1, :]
                rd = [B["Xr"], B[f"kt{ks}"]]
                fw.op("pool", lambda: G.tensor_tensor(out=ta[:], in0=Xr[:, st_, :], in1=Kr, op=ALU.mult), reads=rd, writes=[B["ta"]])
                fw.op("dve", lambda: V.tensor_tensor(out=tb[:], in0=ps, in1=Ki, op=ALU.mult), reads=[Bpacc[st_], B[f"kt{ks}"]], writes=[B["tb"]])
                fw.op("pool", lambda: G.tensor_tensor(out=Y[:, 0, :], in0=ta[:], in1=tb[:], op=ALU.subtract), reads=[B["ta"], B["tb"]], writes=[B["Y"]])
                fw.op("pool", lambda: G.tensor_tensor(out=ta[:], in0=Xr[:, st_, :], in1=Ki, op=ALU.mult), reads=rd, writes=[B["ta"]])
                fw.op("dve", lambda: V.tensor_tensor(out=tb[:], in0=ps, in1=Kr, op=ALU.mult), reads=[Bpacc[st_], B[f"kt{ks}"]], writes=[B["tb"]])
                fw.op("pool", lambda: G.tensor_tensor(out=Y[:, 1, :], in0=ta[:], in1=tb[:], op=ALU.add), reads=[B["ta"], B["tb"]], writes=[B["Y"]])
                if nt == 0:
                    fw.op("dve", lambda: V.tensor_tensor(out=Y[:, 0, 0:1], in0=Xr[:, st_, 0:1], in1=kt[ks][:, 0, 0:1], op=ALU.mult), reads=rd, writes=[B["Y"]])
                    fw.op("dve", lambda: V.tensor_tensor(out=Y[:, 1, 0:1], in0=ps[:, 0:1], in1=kt[ks][:, 1, 0:1], op=ALU.mult), reads=[Bpacc[st_], B[f"kt{ks}"]], writes=[B["Y"]])
                for ri in range(2):
                    ps_ = cnt["tr"] % 2
                    cnt["tr"] += 1
                    for j in range(TJ):
                        fw.op("pe", lambda j=j: nc.tensor.transpose(out=ptr[ps_][:, j, :], in_=Y[:, ri, j * 128:(j + 1) * 128], identity=idf[:]),
                              reads=[B["Y"], B["idf"]], writes=[B[f"ptr{ps_}"]])
                    fw.op("act", lambda: nc.scalar.copy(out=y_fm[:, st_, ri, nt * TJ:(nt + 1) * TJ, :], in_=ptr[ps_][:, 0:TJ, :]), reads=[B[f"ptr{ps_}"]], writes=[B["y_fm"]])
            emit_fwd_dft(fw, nc, L, x_tm, B["x_tm"], tabC, tabS, slabs, Bslab, pacc, Bpacc, consume)
            for tt in range(NTL):
                tsl = slice(tt * TW, (tt + 1) * TW)
                for ri, tab in ((0, tabC), (1, tabST)):
                    fw.dma("sp" if ri == 0 else "act", slabs[ri][:, :, :], tab[tt], writes=[Bslab[ri]])
                    for st_ in range(4):
                        for k in range(NK):
                            fw.op("pe", lambda k=k: nc.tensor.matmul(pacc[st_][:, :TW], lhsT=y_fm[:, st_, ri, k, :], rhs=slabs[ri][:, k, :],
                                                                      start=(ri == 0 and k == 0), stop=(ri == 1 and k == NK - 1)),
                                  reads=[B["y_fm"], Bslab[ri]], writes=[Bpacc[st_]])
                for st_ in range(4):
                    xs_ = cnt["xq"] % 2
                    cnt["xq"] += 1
                    fw.dma("pool", xq[xs_][:], x12[st_, o, :, tsl], reads=[B["x12"]], writes=[B[f"xq{xs_}"]])
                    zs = cnt["z"] % 2
                    cnt["z"] += 1
                    if o == 0:
                        fw.op("dve", lambda: V.tensor_tensor(out=zb[zs][:], in0=pacc[st_][:, :TW], in1=xq[xs_][:], op=ALU.mult), reads=[Bpacc[st_], B[f"xq{xs_}"]], writes=[B[f"zb{zs}"]])
                        ps_ = cnt["tr"] % 2
                        cnt["tr"] += 1
                        for j in range(TJ):
                            fw.op("pe", lambda j=j: nc.tensor.transpose(out=ptr[ps_][:, j, :], in_=zb[zs][:, j * 128:(j + 1) * 128], identity=idf[:]),
                                  reads=[B[f"zb{zs}"], B["idf"]], writes=[B[f"ptr{ps_}"]])
                        fw.op("act", lambda: nc.scalar.copy(out=x_tm[:, st_, tt * TJ:(tt + 1) * TJ, :], in_=ptr[ps_][:, 0:TJ, :]), reads=[B[f"ptr{ps_}"]], writes=[B["x_tm"]])
                    else:
                        fw.op("dve", lambda: V.tensor_tensor(out=zo[zs][:], in0=pacc[st_][:, :TW], in1=xq[xs_][:], op=ALU.mult), reads=[Bpacc[st_], B[f"xq{xs_}"]], writes=[B[f"zo{zs}"]])
                        fw.dma("sp", zTo[:, st_, tsl], zo[zs][:], reads=[B[f"zo{zs}"]], writes=[B["o"]])
        fw.finish([B["o"]])
    return nc


def _ident_f():
    return np.eye(128, dtype=np.float32)


def _ident_b():
    return np.eye(128, dtype=np.float32).astype(NPBF)


def stage_filters(L, slot, p):
    nc = cached(("F", L), lambda: build_F(L))
    zT, decay = hyena_consts(L)
    tC, tS, _ = dft_tables(L)
    vecs = np.ascontiguousarray(np.stack([p["hy_f_b1"][slot], p["hy_f_b2"][slot], p["hy_f_freq"][slot, 0], p["hy_f_freq"][slot, 1]], axis=-1))
    maps = []
    for core in range(NCORES):
        ch = slice(core * 128, (core + 1) * 128)
        w3 = p["hy_f_w3"][slot].reshape(64, 2, 2, D)[:, :, :, ch].reshape(64, 4, 128)
        maps.append({"zT": zT, "decay": np.ascontiguousarray(decay[ch]), "w1": np.ascontiguousarray(p["hy_f_w1"][slot]),
                     "w2": np.ascontiguousarray(p["hy_f_w2"][slot]), "w3": np.ascontiguousarray(w3), "vecs": vecs,
                     "fbias": np.ascontiguousarray(p["hy_f_bias"][slot][:, ch].T), "identf": _ident_f(), "tabC": tC, "tabS": tS})
    res = run(nc, maps)
    return np.stack([res[c]["Kf"] for c in range(NCORES)])


def stage_hyena(L, slot, xs, mrows, mv, KfAll, p):
    nc = cached(("MH", L), lambda: build_MH(L))
    tC, tS, tST = dft_tables(L)
    sh1, sc1 = mv[:, 0:D], mv[:, D:2 * D]
    maps = []
    for core in range(NCORES):
        b, h = core // 2, core % 2
        r = mrows[b]
        mc = np.stack([sc1[r], sh1[r]], axis=-1).reshape(8, 128, 2).transpose(1, 0, 2)
        cols = np.concatenate([np.arange(q * D + 512 * h + 128 * s, q * D + 512 * h + 128 * s + 128) for s in range(4) for q in range(3)])
        cwm = np.concatenate([p["hy_conv_w"][slot][:, cols], p["hy_conv_b"][slot][None, cols]], axis=0).reshape(4, 12, 128).transpose(2, 1, 0)
        maps.append({"xT": fm_layout(np.ascontiguousarray(xs[b].T)), "mcol": np.ascontiguousarray(mc), "win": fm_layout(p["hy_w_in"][slot][:, cols]),
                     "cw": np.ascontiguousarray(cwm), "Kf": np.ascontiguousarray(KfAll[4 * h:4 * h + 4]), "tabC": tC, "tabS": tS, "tabST": tST,
                     "identf": _ident_f()})
    res = run(nc, maps)
    return [np.concatenate([res[2 * b]["zT"], res[2 * b + 1]["zT"]], axis=1) for b in range(NB)]


def stage_attn(x_lat, x_ctx, mv, p):
    nc = cached(("MA",), build_MA)
    sh1, sc1 = mv[:, 0:D], mv[:, D:2 * D]
    wqkv = p["at_w_qkv"][0]
    gains = np.ascontiguousarray(np.concatenate([np.tile(p["at_q_gain"][0], 8), np.tile(p["at_k_gain"][0], 2)])[None, :])
    cs = rope_tables()
    maps = []
    for core in range(NCORES):
        b, h = core // 2, core % 2
        mc = np.stack([sc1[b], sh1[b], sc1[4], sh1[4]], axis=-1).reshape(8, 128, 4).transpose(1, 0, 2)
        wcat = np.concatenate([wqkv[:, 512 * h:512 * h + 512], wqkv[:, 1024 + 128 * h:1024 + 128 * h + 128],
                               wqkv[:, 1280 + 128 * h:1280 + 128 * h + 128]], axis=1)
        maps.append({"xT": fm_layout(np.ascontiguousarray(x_lat[b].T)), "cxT": fm_layout(np.ascontiguousarray(x_ctx[b].T)),
                     "mcol": np.ascontiguousarray(mc), "w": fm_layout(wcat), "gains": gains, "cs": cs, "identb": _ident_b()})
    res = run(nc, maps)
    return [np.concatenate([res[2 * b]["oT"], res[2 * b + 1]["oT"]], axis=1) for b in range(NB)]


def stage_gmlp(x_lat, mv, p):
    nc = cached(("MG",), lambda: build_MG(16))
    sh1, sc1 = mv[:, 0:D], mv[:, D:2 * D]
    lnr = np.ascontiguousarray(np.concatenate([p["cm_ln_g"][0], p["cm_ln_b"][0]])[None, :])
    wsT = np.ascontiguousarray(p["cm_w_s"][0].transpose(2, 0, 1))
    bsr = np.ascontiguousarray(p["cm_b_s"][0].reshape(1, -1))
    win = fm_layout(p["cm_w_in"][0])
    maps = []
    for core in range(NCORES):
        b, hf = core // 2, core % 2
        mc = np.stack([sc1[b], sh1[b]], axis=-1).reshape(8, 128, 2).transpose(1, 0, 2)
        maps.append({"xT": fm_layout(np.ascontiguousarray(x_lat[b, hf * 2048:(hf + 1) * 2048].T)), "mcol": np.ascontiguousarray(mc), "win": win,
                     "lnr": lnr, "wsT": wsT, "bsr": bsr})
    res = run(nc, maps)
    return [np.concatenate([res[2 * b]["gT"], res[2 * b + 1]["gT"]], axis=2) for b in range(NB)]


def stage_norm_router(aT, w_out, xs, mv, mrows, i, p, T):
    KC = aT[0].shape[1]
    NT = T // 128
    Lseq = xs.shape[1]
    per_b = Lseq // T
    nc = cached(("N", KC, NT), lambda: build_N(KC, NT))
    wo = fm_layout(w_out)
    wr = fm_layout(p["moe_router"][i])
    maps = []
    for core in range(NCORES):
        b, hf = core // per_b, core % per_b
        r = mrows[b]
        rows = np.concatenate([mv[r, 2 * D:3 * D], p["ln_g"][i, 0], p["ln_b"][i, 0], mv[r, 4 * D:5 * D], mv[r, 3 * D:4 * D]])[None, :]
        maps.append({"aT": np.ascontiguousarray(aT[b][:, :, hf * T:(hf + 1) * T]), "wo": wo, "x": np.ascontiguousarray(xs[b, hf * T:(hf + 1) * T]),
                     "rows": np.ascontiguousarray(rows), "wr": wr, "ident": _ident_f()})
    res = run(nc, maps)
    x1 = np.stack([np.concatenate([res[b * per_b + hf]["x1"] for hf in range(per_b)], axis=0) for b in range(NB)])
    h2 = np.concatenate([res[c]["h2"] for c in range(NCORES)], axis=0)
    aff = np.stack([np.concatenate([res[b * per_b + hf]["aff"] for hf in range(per_b)], axis=0) for b in range(NB)])
    return x1, h2, aff


def stage_experts(aff, h2, i, p, CAP, GB):
    Lseq = aff.shape[1]
    NTT = Lseq // 128
    NS = GB * CAP
    nc = cached(("E", NTT, CAP, GB), lambda: build_E(NTT, CAP, GB))
    tokid = (np.arange(NB)[None, :, None] * Lseq + np.arange(NTT)[None, None, :] * 128 + np.arange(128)[:, None, None])
    tok = np.ascontiguousarray(np.stack([tokid // 128, tokid % 128], axis=1).astype(np.float32))
    so = np.zeros((128, NB, 2), np.float32)
    so += ((np.arange(NB) % GB) * CAP).astype(np.float32)[None, :, None]
    so = np.ascontiguousarray(so.reshape(128, 8))
    tri = np.triu(np.ones((128, 128), np.float32), 1).astype(NPBF)
    iota = np.ascontiguousarray(np.broadcast_to(np.arange(NS, dtype=np.float32), (128, NS)))
    maps = []
    for core in range(NCORES):
        e0 = 2 * core
        a = aff[:, :, e0:e0 + 2].reshape(NB, NTT, 128, 2).transpose(2, 0, 3, 1).reshape(128, 8, NTT)
        maps.append({"aff": np.ascontiguousarray(a), "h2": h2, "tok": tok, "slotoff": so, "tri": tri, "iota": iota, "identb": _ident_b(),
                     "wg": np.ascontiguousarray(p["moe_w_gate"][i, e0:e0 + 2].reshape(2, 8, 128, FF).transpose(0, 2, 1, 3)),
                     "wu": np.ascontiguousarray(p["moe_w_up"][i, e0:e0 + 2].reshape(2, 8, 128, FF).transpose(0, 2, 1, 3)),
                     "wd": np.ascontiguousarray(p["moe_w_down"][i, e0:e0 + 2].reshape(2, 16, 128, D).transpose(0, 2, 1, 3))})
    res = run(nc, maps)
    yc = np.concatenate([res[c]["yc"] for c in range(NCORES)], axis=0)
    pt = np.stack([res[c]["postab"] for c in range(NCORES)], axis=0)
    return yc, pt


def stage_combine(x1, yc, pt, mv, mrows, i, p, T, GB):
    Lseq = x1.shape[1]
    NT = T // 128
    per_b = Lseq // T
    NS = yc.shape[2] - 128
    nc = cached(("P", NT, NS), lambda: build_P(NT, NS))
    maps = []
    for core in range(NCORES):
        b, hf = core // per_b, core % per_b
        r = mrows[b]
        rows = np.concatenate([mv[r, 5 * D:6 * D], p["ln_g"][i, 1], p["ln_b"][i, 1]])[None, :]
        ycb = yc[:, b // GB].reshape(16 * (NS + 128), D)
        ptb = pt[:, :, 2 * b:2 * b + 2, hf * NT:(hf + 1) * NT]
        ptb = ptb.transpose(1, 0, 2, 3).reshape(128, 16, NT)
        maps.append({"x1": np.ascontiguousarray(x1[b, hf * T:(hf + 1) * T]), "ycb": np.ascontiguousarray(ycb), "postab": np.ascontiguousarray(ptb),
                     "rows": np.ascontiguousarray(rows)})
    res = run(nc, maps)
    return np.stack([np.concatenate([res[b * per_b + hf]["x2"] for hf in range(per_b)], axis=0) for b in range(NB)])


def moe_block(aT, w_out, xs, mv, mrows, i, p, T, CAP, GB):
    x1, h2, aff = stage_norm_router(aT, w_out, xs, mv, mrows, i, p, T)
    yc, pt = stage_experts(aff, h2, i, p, CAP, GB)
    return stage_combine(x1, yc, pt, mv, mrows, i, p, T, GB)


def kernel(**inputs):
    p = {k: np.asarray(v) for k, v in inputs.items()}
    x_lat = np.ascontiguousarray(p["x"], dtype=np.float32)
    x_ctx = np.ascontiguousarray(p["ctx"], dtype=np.float32)
    modvec = run_A(p["c"], p["c_ctx"], p["mod_w"], p["mod_b"])
    lat_rows = [0, 1, 2, 3]
    ctx_rows = [4, 4, 4, 4]
    mv = modvec[0]
    Kf = stage_filters(SEQ, 0, p)
    aT = stage_hyena(SEQ, 0, x_lat, lat_rows, mv, Kf, p)
    Kfc = stage_filters(CTX, 0, p)
    aTc = stage_hyena(CTX, 0, x_ctx, ctx_rows, mv, Kfc, p)
    x_lat = moe_block(aT, p["hy_w_out"][0], x_lat, mv, lat_rows, 0, p, 2048, 512, 1)
    x_ctx = moe_block(aTc, p["hy_w_out"][0], x_ctx, mv, ctx_rows, 0, p, 128, 32, 4)
    mv = modvec[1]
    aT = stage_attn(x_lat, x_ctx, mv, p)
    x_lat = moe_block(aT, p["at_w_out"][0], x_lat, mv, lat_rows, 1, p, 2048, 512, 1)
    mv = modvec[2]
    aT = stage_gmlp(x_lat, mv, p)
    x_lat = moe_block(aT, p["cm_w_out"][0], x_lat, mv, lat_rows, 2, p, 2048, 512, 1)
    mv = modvec[3]
    Kf = stage_filters(SEQ, 1, p)
    aT = stage_hyena(SEQ, 1, x_lat, lat_rows, mv, Kf, p)
    x_lat = moe_block(aT, p["hy_w_out"][1], x_lat, mv, lat_rows, 3, p, 2048, 512, 1)
    return x_lat.astype(np.float32)
```

```python
import math
from contextlib import ExitStack

import numpy as np
import ml_dtypes
import concourse.bass as bass
import concourse.mybir as mybir
from concourse.bass_utils import run_bass_kernel_spmd

F32 = mybir.dt.float32
BF16 = mybir.dt.bfloat16
I32 = mybir.dt.int32
U32 = mybir.dt.uint32
ALU = mybir.AluOpType
AF = mybir.ActivationFunctionType
AX = mybir.AxisListType
NPBF = ml_dtypes.bfloat16

D = 1024
NB = 4
SEQ = 4096
CTX = 256
DEPTH = 4
NE = 16
FF = 2048
LN_EPS = 1e-5
RMS_EPS = 1e-6
ALPHA = (2 * DEPTH) ** 0.25
NCORES = 8


class Buf:
    __slots__ = ("name", "w", "r")

    def __init__(self, name):
        self.name = name
        self.w = None
        self.r = {}


class FW:
    def __init__(self, nc, stack):
        self.nc = nc
        self.stack = stack
        self.engs = {"pe": nc.tensor, "dve": nc.vector, "act": nc.scalar, "pool": nc.gpsimd, "sp": nc.sync}
        self.sems = {}
        self.cnt = {}
        self.seen = {k: {} for k in self.engs}
        for k in self.engs:
            self.sems[k] = stack.enter_context(nc.semaphore("s_" + k))
            self.cnt[k] = 0
        self.same_engine_sync = True
        self.semstack = stack

    def sb(self, name, shape, dt):
        return self.stack.enter_context(self.nc.sbuf_tensor(name, list(shape), dt))

    def ps(self, name, shape, dt=F32):
        return self.stack.enter_context(self.nc.psum_tensor(name, list(shape), dt))

    def dma_sem(self, name):
        key = "d_" + name
        if key not in self.sems:
            self.sems[key] = self.semstack.enter_context(self.nc.semaphore(key))
            self.cnt[key] = 0
        return key

    def _wait(self, e, key, val):
        if key == e and (e == "pe" or not self.same_engine_sync):
            return
        if self.seen[e].get(key, 0) >= val:
            return
        self.engs[e].wait_ge(self.sems[key], val)
        self.seen[e][key] = val

    def _deps(self, e, reads, writes):
        for b in reads:
            if b.w is not None:
                self._wait(e, *b.w)
        for b in writes:
            if b.w is not None:
                self._wait(e, *b.w)
            for k, v in b.r.items():
                self._wait(e, k, v)

    def op(self, e, fn, reads=(), writes=()):
        self._deps(e, reads, writes)
        ins = fn()
        self.cnt[e] += 1
        ins.then_inc(self.sems[e], 1)
        for b in reads:
            b.r[e] = self.cnt[e]
        for b in writes:
            b.w = (e, self.cnt[e])
            b.r = {}
        return ins

    def dma(self, q, out, in_, reads=(), writes=(), semname=None, indirect=None, **kw):
        self._deps(q, reads, writes)
        name = semname or (writes[0].name if writes else reads[0].name + "_st")
        key = self.dma_sem(name)
        if indirect is None:
            ins = self.engs[q].dma_start(out=out, in_=in_, **kw)
        else:
            ins = self.nc.gpsimd.indirect_dma_start(out=out, in_=in_, **indirect)
        self.cnt[key] += 16
        ins.then_inc(self.sems[key], 16)
        for b in reads:
            b.r[key] = self.cnt[key]
        for b in writes:
            b.w = (key, self.cnt[key])
            b.r = {}
        return ins

    def barrier(self):
        for e in self.engs:
            for key, c in self.cnt.items():
                if key != e and c > 0:
                    self._wait(e, key, c)

    def scoped(self):
        fw = self

        class _Scope:
            def __enter__(self_):
                self_.prev = fw.stack
                self_.st = ExitStack()
                self_.st.__enter__()
                fw.stack = self_.st
                return fw

            def __exit__(self_, *a):
                fw.barrier()
                fw.stack = self_.prev
                return self_.st.__exit__(*a)
        return _Scope()

    def seal(self, bufs):
        key = bufs[0].w[0]
        for b in bufs:
            b.w = (key, self.cnt[key])

    def finish(self, bufs, e="sp"):
        for b in bufs:
            if b.w is not None:
                self._wait(e, *b.w)


def new_nc():
    return bass.Bass("TRN2", target_bir_lowering=False)


def dram_in(nc, name, shape, dt=F32):
    return nc.dram_tensor(name, list(shape), dt, kind="ExternalInput").ap()


def dram_out(nc, name, shape, dt=F32):
    return nc.dram_tensor(name, list(shape), dt, kind="ExternalOutput").ap()


_PROFILE = []


def run(nc, in_maps, tag=""):
    res = run_bass_kernel_spmd(nc, in_maps, core_ids=list(range(NCORES)))
    if getattr(res, "exec_time_ns", None):
        _PROFILE.append((tag, res.exec_time_ns))
    return res.results


def build_A():
    nc = new_nc()
    cT = dram_in(nc, "cT", [128, 8, 5])
    w = dram_in(nc, "w", [128, 8, 3072])
    b = dram_in(nc, "b", [1, 3072])
    m = dram_out(nc, "m", [5, 3072])
    with ExitStack() as st:
        fw = FW(nc, st)
        ct = fw.sb("ct", [128, 8, 5], F32)
        bt = fw.sb("bt", [5, 3072], F32)
        mt = fw.sb("mt", [5, 3072], F32)
        wt = [fw.sb(f"wt{i}", [128, 8, 512], F32) for i in range(2)]
        pt = [fw.ps(f"pt{i}", [5, 512]) for i in range(2)]
        Bc, Bb, Bm, Bo = Buf("ct"), Buf("bt"), Buf("mt"), Buf("mo")
        Bw = [Buf("wt0"), Buf("wt1")]
        Bp = [Buf("pt0"), Buf("pt1")]
        fw.dma("sp", ct[:], cT, writes=[Bc])
        fw.dma("sp", bt[:], b.partition_broadcast(5), writes=[Bb])
        fw.op("act", lambda: nc.scalar.activation(out=ct[:], in_=ct[:], func=AF.Silu), reads=[Bc], writes=[Bc])
        for j in range(6):
            s = j % 2
            fw.dma("sp" if s == 0 else "pool", wt[s][:], w[:, :, j * 512:(j + 1) * 512], writes=[Bw[s]])
            for k in range(8):
                fw.op("pe", lambda k=k: nc.tensor.matmul(pt[s][:], lhsT=ct[:, k, :], rhs=wt[s][:, k, :],
                                                           start=(k == 0), stop=(k == 7)),
                      reads=[Bc, Bw[s]], writes=[Bp[s]])
            fw.op("dve", lambda: nc.vector.tensor_tensor(out=mt[:, j * 512:(j + 1) * 512], in0=pt[s][:],
                                                         in1=bt[:, j * 512:(j + 1) * 512], op=ALU.add),
                  reads=[Bp[s], Bb], writes=[Bm])
        fw.dma("sp", m, mt[:], reads=[Bm], writes=[Bo])
        fw.finish([Bo])
    return nc


def run_A(c, c_ctx, mod_w, mod_b):
    nc = build_A()
    cc = np.concatenate([c, c_ctx[None, :]], axis=0)
    cT = np.ascontiguousarray(cc.T.reshape(8, 128, 5).transpose(1, 0, 2))
    maps = []
    for core in range(NCORES):
        i, hf = core // 2, core % 2
        wv = mod_w[i][:, hf * 3072:(hf + 1) * 3072].reshape(8, 128, 3072).transpose(1, 0, 2)
        maps.append({"cT": cT, "w": np.ascontiguousarray(wv),
                     "b": np.ascontiguousarray(mod_b[i][None, hf * 3072:(hf + 1) * 3072])})
    res = run(nc, maps)
    out = np.zeros((DEPTH, 5, 6 * D), np.float32)
    for core in range(NCORES):
        i, hf = core // 2, core % 2
        out[i][:, hf * 3072:(hf + 1) * 3072] = res[core]["m"]
    return out


def layer_norm_tile(fw, nc, u, Bu, stats, mv, rstd, Bs, nparts=128):
    for j in range(2):
        fw.op("dve", lambda j=j: nc.vector.bn_stats(out=stats[:, j, :], in_=u[:, j * 512:(j + 1) * 512]),
              reads=[Bu], writes=[Bs])
    fw.op("dve", lambda: nc.vector.bn_aggr(out=mv[:], in_=stats[:].rearrange("p a b -> p (a b)")), reads=[Bs], writes=[Bs])
    fw.op("act", lambda: nc.scalar.activation(out=rstd[:], in_=mv[:, 1:2], func=AF.Sqrt, bias=fw.eps_ln[:, 0:1], scale=1.0),
          reads=[Bs, fw.Bconst], writes=[Bs])
    fw.op("dve", lambda: nc.vector.reciprocal(out=rstd[:], in_=rstd[:]), reads=[Bs], writes=[Bs])
    fw.op("dve", lambda: nc.vector.tensor_scalar(out=u[:], in0=u[:], scalar1=mv[:, 0:1], scalar2=rstd[:, 0:1],
                                                 op0=ALU.subtract, op1=ALU.mult), reads=[Bs, Bu], writes=[Bu])


def make_consts(fw, nc):
    fw.eps_ln = fw.sb("eps_ln", [128, 1], F32)
    fw.Bconst = Buf("consts")
    fw.op("pool", lambda: nc.gpsimd.memset(fw.eps_ln[:], LN_EPS), writes=[fw.Bconst])


def build_N(KC, NT):
    T = NT * 128
    nc = new_nc()
    aT = dram_in(nc, "aT", [128, KC, T], BF16)
    wo = dram_in(nc, "wo", [128, KC, 1024])
    x = dram_in(nc, "x", [T, 1024])
    rows = dram_in(nc, "rows", [1, 5 * 1024])
    wr = dram_in(nc, "wr", [128, 8, 16])
    ident = dram_in(nc, "ident", [128, 128])
    x1o = dram_out(nc, "x1", [T, 1024])
    h2o = dram_out(nc, "h2", [T, 1024], BF16)
    affo = dram_out(nc, "aff", [T, 16])
    with ExitStack() as st:
        fw = FW(nc, st)
        make_consts(fw, nc)
        wob = fw.sb("wob", [128, KC, 1024], BF16)
        wst = [fw.sb(f"wst{i}", [128, 1024], F32) for i in range(2)]
        rw = fw.sb("rw", [128, 5, 1024], F32)
        wrt = fw.sb("wrt", [128, 8, 16], F32)
        idt = fw.sb("idt", [128, 128], F32)
        at = [fw.sb(f"at{i}", [128, KC, 128], BF16) for i in range(2)]
        xt = [fw.sb(f"xt{i}", [128, 1024], F32) for i in range(2)]
        u = [fw.sb(f"u{i}", [128, 1024], F32) for i in range(2)]
        h2 = [fw.sb(f"h2{i}", [128, 1024], F32) for i in range(2)]
        h2b = [fw.sb(f"h2b{i}", [128, 1024], BF16) for i in range(2)]
        h2T = fw.sb("h2T", [128, 8, 128], F32)
        stats = fw.sb("stats", [128, 2, 6], F32)
        mv = fw.sb("mv", [128, 2], F32)
        rstd = fw.sb("rstd", [128, 1], F32)
        sm = fw.sb("sm", [128, 4], F32)
        aft = [fw.sb(f"aft{i}", [128, 16], F32) for i in range(2)]
        py = [fw.ps(f"py{i}", [128, 512]) for i in range(2)]
        ptr = [fw.ps(f"ptr{i}", [128, 4, 128]) for i in range(2)]
        pl = fw.ps("pl", [128, 16])
        Bwo, Brw, Bwr, Bid = Buf("wob"), Buf("rw"), Buf("wrt"), Buf("idt")
        Bwst = [Buf("wst0"), Buf("wst1")]
        Bat = [Buf("at0"), Buf("at1")]
        Bxt = [Buf("xt0"), Buf("xt1")]
        Bu = [Buf("u0"), Buf("u1")]
        Bh2 = [Buf("h20"), Buf("h21")]
        Bh2b = [Buf("h2b0"), Buf("h2b1")]
        Bh2T, Bs, Bsm = Buf("h2T"), Buf("stats"), Buf("sm")
        Baf = [Buf("aft0"), Buf("aft1")]
        Bpy = [Buf("py0"), Buf("py1")]
        Bptr = [Buf("ptr0"), Buf("ptr1")]
        Bpl = Buf("pl")
        Bout = [Buf("o_x1"), Buf("o_h2"), Buf("o_aff")]
        fw.dma("sp", rw[:].rearrange("p a b -> p (a b)"), rows.partition_broadcast(128), writes=[Brw])
        fw.dma("sp", wrt[:], wr, writes=[Bwr])
        fw.dma("sp", idt[:], ident, writes=[Bid])
        for kc in range(KC):
            s = kc % 2
            fw.dma("sp" if s == 0 else "pool", wst[s][:], wo[:, kc, :], writes=[Bwst[s]])
            fw.op("act" if s == 0 else "pool",
                  (lambda: nc.scalar.copy(out=wob[:, kc, :], in_=wst[s][:])) if s == 0 else
                  (lambda: nc.gpsimd.tensor_copy(out=wob[:, kc, :], in_=wst[s][:])),
                  reads=[Bwst[s]], writes=[Bwo])
        fw.op("dve", lambda: nc.vector.tensor_scalar(out=rw[:, 3, :], in0=rw[:, 3, :], scalar1=1.0, scalar2=None, op0=ALU.add),
              reads=[Brw], writes=[Brw])
        for t in range(NT):
            s = t % 2
            fw.dma("sp", at[s][:], aT[:, :, t * 128:(t + 1) * 128], writes=[Bat[s]])
            fw.dma("pool", xt[s][:], x[t * 128:(t + 1) * 128, :], writes=[Bxt[s]])
            for hf in range(2):
                for kc in range(KC):
                    fw.op("pe", lambda kc=kc, hf=hf: nc.tensor.matmul(py[hf][:], lhsT=at[s][:, kc, :],
                                                                       rhs=wob[:, kc, hf * 512:(hf + 1) * 512],
                                                                       start=(kc == 0), stop=(kc == KC - 1)),
                          reads=[Bat[s], Bwo], writes=[Bpy[hf]])
            for hf in range(2):
                sl = slice(hf * 512, (hf + 1) * 512)
                fw.op("dve", lambda: nc.vector.tensor_tensor(out=u[s][:, sl], in0=py[hf][:], in1=rw[:, 0, sl], op=ALU.mult),
                      reads=[Bpy[hf], Brw], writes=[Bu[s]])
            fw.op("dve", lambda: nc.vector.scalar_tensor_tensor(out=u[s][:], in0=xt[s][:], scalar=ALPHA, in1=u[s][:],
                                                                op0=ALU.mult, op1=ALU.add),
                  reads=[Bxt[s], Bu[s]], writes=[Bu[s]])
            layer_norm_tile(fw, nc, u[s], Bu[s], stats, mv, rstd, Bs)
            fw.op("pool", lambda: nc.gpsimd.tensor_tensor(out=u[s][:], in0=u[s][:], in1=rw[:, 1, :], op=ALU.mult),
                  reads=[Bu[s], Brw], writes=[Bu[s]])
            fw.op("pool", lambda: nc.gpsimd.tensor_tensor(out=u[s][:], in0=u[s][:], in1=rw[:, 2, :], op=ALU.add),
                  reads=[Bu[s], Brw], writes=[Bu[s]])
            fw.dma("sp", x1o[t * 128:(t + 1) * 128, :], u[s][:], reads=[Bu[s]], writes=[Bout[0]])
            fw.op("dve", lambda: nc.vector.tensor_tensor(out=h2[s][:], in0=u[s][:], in1=rw[:, 3, :], op=ALU.mult),
                  reads=[Bu[s], Brw], writes=[Bh2[s]])
            fw.op("dve", lambda: nc.vector.tensor_tensor(out=h2[s][:], in0=h2[s][:], in1=rw[:, 4, :], op=ALU.add),
                  reads=[Bh2[s], Brw], writes=[Bh2[s]])
            fw.op("act", lambda: nc.scalar.copy(out=h2b[s][:], in_=h2[s][:]), reads=[Bh2[s]], writes=[Bh2b[s]])
            fw.dma("sp", h2o[t * 128:(t + 1) * 128, :], h2b[s][:], reads=[Bh2b[s]], writes=[Bout[1]])
            for g in range(2):
                for k4 in range(4):
                    k = g * 4 + k4
                    fw.op("pe", lambda k=k, k4=k4: nc.tensor.transpose(out=ptr[g][:, k4, :], in_=h2[s][:, k * 128:(k + 1) * 128],
                                                                        identity=idt[:]),
                          reads=[Bh2[s], Bid], writes=[Bptr[g]])
                fw.op("act", lambda: nc.scalar.copy(out=h2T[:, g * 4:(g + 1) * 4, :], in_=ptr[g][:]), reads=[Bptr[g]], writes=[Bh2T])
            for k in range(8):
                fw.op("pe", lambda k=k: nc.tensor.matmul(pl[:], lhsT=h2T[:, k, :], rhs=wrt[:, k, :], start=(k == 0), stop=(k == 7)),
                      reads=[Bh2T, Bwr], writes=[Bpl])
            fw.op("dve", lambda: nc.vector.reduce_max(out=sm[:, 0:1], in_=pl[:], axis=AX.X), reads=[Bpl], writes=[Bsm])
            fw.op("dve", lambda: nc.vector.tensor_scalar(out=sm[:, 1:2], in0=sm[:, 0:1], scalar1=-1.0, scalar2=None, op0=ALU.mult),
                  reads=[Bsm], writes=[Bsm])
            fw.op("act", lambda: nc.scalar.activation(out=aft[s][:], in_=pl[:], func=AF.Exp, bias=sm[:, 1:2], scale=1.0,
                                                      accum_out=sm[:, 2:3]), reads=[Bpl, Bsm], writes=[Baf[s], Bsm])
            fw.op("dve", lambda: nc.vector.reciprocal(out=sm[:, 3:4], in_=sm[:, 2:3]), reads=[Bsm], writes=[Bsm])
            fw.op("dve", lambda: nc.vector.tensor_scalar(out=aft[s][:], in0=aft[s][:], scalar1=sm[:, 3:4], scalar2=None, op0=ALU.mult),
                  reads=[Bsm, Baf[s]], writes=[Baf[s]])
            fw.dma("sp", affo[t * 128:(t + 1) * 128, :], aft[s][:], reads=[Baf[s]], writes=[Bout[2]])
        fw.finish(Bout)
    return nc


def fm_layout(a2d):
    K, T = a2d.shape
    return np.ascontiguousarray(a2d.reshape(K // 128, 128, T).transpose(1, 0, 2))


_NC_CACHE = {}


def cached(key, builder):
    if key not in _NC_CACHE:
        _NC_CACHE[key] = builder()
    return _NC_CACHE[key]


def build_E(NTT, CAP, GB, NITER=30):
    NG = NB // GB
    NS = GB * CAP
    NCH = NS // 128
    NC8 = 8 * NTT
    nc = new_nc()
    aff = dram_in(nc, "aff", [128, 8, NTT])
    h2 = dram_in(nc, "h2", [NB * NTT * 128, 1024], BF16)
    tok = dram_in(nc, "tok", [128, 2, NB, NTT])
    slotoff = dram_in(nc, "slotoff", [128, 8])
    tri = dram_in(nc, "tri", [128, 128], BF16)
    iota = dram_in(nc, "iota", [128, NS])
    identb = dram_in(nc, "identb", [128, 128], BF16)
    wg = dram_in(nc, "wg", [2, 128, 8, 2048])
    wu = dram_in(nc, "wu", [2, 128, 8, 2048])
    wd = dram_in(nc, "wd", [2, 128, 16, 1024])
    yc = dram_out(nc, "yc", [2, NG, NS + 128, 1024])
    BIGPOS = float(NS)
    postab = dram_out(nc, "postab", [128, 8, NTT], I32)
    with ExitStack() as st:
        fw = FW(nc, st)
        A = fw.sb("A", [128, 8, NTT], F32)
        tokt = fw.sb("tokt", [128, 2, NB, NTT], F32)
        sofft = fw.sb("sofft", [128, 8], F32)
        trit = fw.sb("trit", [128, 128], BF16)
        onesb = fw.sb("onesb", [128, 128], BF16)
        iot = fw.sb("iot", [128, NS], F32)
        idb = fw.sb("idb", [128, 128], BF16)
        lo = fw.sb("lo", [128, 8], F32)
        hi = fw.sb("hi", [128, 8], F32)
        mid = fw.sb("mid", [128, 8], F32)
        cnt = fw.sb("cnt", [128, 8], F32)
        ge = fw.sb("ge", [128, 8], F32)
        tmp8 = fw.sb("tmp8", [128, 8], F32)
        cmpb = fw.sb("cmpb", [128, 8, NTT], BF16)
        maskf = fw.sb("maskf", [128, 8, NTT], F32)
        pos = fw.sb("pos", [128, 8, NTT], F32)
        off = fw.sb("off", [128, 8, NTT], F32)
        tot = fw.sb("tot", [128, 8, NTT], F32)
        posi = fw.sb("posi", [128, 8, NTT], I32)
        vals = fw.sb("vals", [128, 8, NTT, 5], BF16)
        gres = fw.sb("gres", [128, 8, NTT], F32)
        gpc = fw.sb("gpc", [128, 8, NTT], F32)
        NSTEP = GB * NTT
        ohall = fw.sb("ohall", [128, NSTEP, NS], BF16)
        idxf = fw.sb("idxf", [128, NCH, 5], F32)
        gate = fw.sb("gate", [128, NCH], F32)
        idf = fw.sb("idf", [128, NCH], F32)
        idxu = fw.sb("idxu", [128, NCH], I32)
        xg = [fw.sb(f"xg{i}", [128, 1024], BF16) for i in range(2)]
        xgT = fw.sb("xgT", [128, 8, NS], BF16)
        wgb = fw.sb("wgb", [128, 8, 2048], BF16)
        wub = fw.sb("wub", [128, 8, 2048], BF16)
        wdb = fw.sb("wdb", [128, 16, 1024], BF16)
        wst = [fw.sb(f"wst{i}", [128, 2048], F32) for i in range(2)]
        sg = [fw.sb(f"sg{i}", [128, NS], F32) for i in range(2)]
        hT = fw.sb("hT", [128, 16, NS], BF16)
        ysb = [fw.sb(f"ysb{i}", [128, 1024], F32) for i in range(2)]
        pbank = [fw.ps(f"pb{i}", [128, 512]) for i in range(7)]
        ptrb = fw.ps("ptrb", [128, 4, 128], BF16)
        pcnt, pidx, pg, pu, py0, py1, ppos = pbank
        B = {n: Buf(n) for n in ["A", "tokt", "sofft", "trit", "onesb", "iot", "idb", "lo", "hi", "mid", "cnt", "ge", "tmp8",
                                 "cmpb", "maskf", "pos", "off", "tot", "posi", "vals", "gres", "ohall", "idxf", "idxu", "xg0", "xg1",
                                 "xgT", "wgb", "wub", "wdb", "wst0", "wst1", "sg0", "sg1", "hT", "ysb0", "ysb1",
                                 "pcnt", "pidx", "pg", "pu", "py0", "py1", "ppos", "ptrb", "o_yc", "o_pos"]}
        V = nc.vector
        fw.dma("sp", A[:], aff, writes=[B["A"]])
        fw.dma("sp", tokt[:], tok, writes=[B["tokt"]])
        fw.dma("sp", sofft[:], slotoff, writes=[B["sofft"]])
        fw.dma("sp", trit[:], tri, writes=[B["trit"]])
        fw.dma("sp", iot[:], iota, writes=[B["iot"]])
        fw.dma("sp", idb[:], identb, writes=[B["idb"]])
        fw.op("pool", lambda: nc.gpsimd.memset(onesb[:], 1.0), writes=[B["onesb"]])
        zt = fw.sb("zt", [128, 1024], F32)
        B["zt"] = Buf("zt")
        fw.op("pool", lambda: nc.gpsimd.memset(zt[:], 0.0), writes=[B["zt"]])
        for el in range(2):
            for g in range(NG):
                fw.dma("sp", yc[el, g, NS:NS + 128, :], zt[:], reads=[B["zt"]], writes=[B["o_yc"]])
        fw.op("pool", lambda: nc.gpsimd.memset(lo[:], 0.0), writes=[B["lo"]])
        fw.op("pool", lambda: nc.gpsimd.memset(hi[:], 1.0), writes=[B["hi"]])

        ld = [0]

        def load_w(dst, Bdst, src_fn, nk, width):
            for k in range(nk):
                s = ld[0] % 2
                ld[0] += 1
                fw.dma("sp" if s == 0 else "act", wst[s][:, :width], src_fn(k), writes=[B[f"wst{s}"]])
                if s == 0:
                    fw.op("act", lambda k=k: nc.scalar.copy(out=dst[:, k, :], in_=wst[s][:, :width]), reads=[B[f"wst{s}"]], writes=[Bdst])
                else:
                    fw.op("pool", lambda k=k: nc.gpsimd.tensor_copy(out=dst[:, k, :], in_=wst[s][:, :width]), reads=[B[f"wst{s}"]], writes=[Bdst])

        def load_all_w(el):
            load_w(wgb, B["wgb"], lambda k: wg[el, :, k, :], 8, 2048)
            load_w(wub, B["wub"], lambda k: wu[el, :, k, :], 8, 2048)
            load_w(wdb, B["wdb"], lambda k: wd[el, :, k, :], 16, 1024)

        load_all_w(0)

        def bc(t8):
            return t8[:, :].unsqueeze(2).to_broadcast([128, 8, NTT])

        def count_ge(thr, Bthr, want_mask_f32=False):
            fw.op("dve", lambda: V.tensor_tensor(out=cmpb[:], in0=A[:], in1=bc(thr), op=ALU.is_ge),
                  reads=[B["A"], Bthr], writes=[B["cmpb"]])
            fw.op("pe", lambda: nc.tensor.matmul(pcnt[:, :NC8], lhsT=onesb[:], rhs=cmpb[:].rearrange("p a b -> p (a b)"),
                                                 start=True, stop=True), reads=[B["onesb"], B["cmpb"]], writes=[B["pcnt"]])

        for it in range(NITER):
            fw.op("dve", lambda: V.tensor_tensor(out=mid[:], in0=lo[:], in1=hi[:], op=ALU.add), reads=[B["lo"], B["hi"]], writes=[B["mid"]])
            fw.op("dve", lambda: V.tensor_scalar(out=mid[:], in0=mid[:], scalar1=0.5, scalar2=None, op0=ALU.mult),
                  reads=[B["mid"]], writes=[B["mid"]])
            count_ge(mid, B["mid"])
            fw.op("dve", lambda: V.tensor_reduce(out=cnt[:], in_=pcnt[:, :NC8].rearrange("p (a b) -> p a b", b=NTT), axis=AX.X, op=ALU.add),
                  reads=[B["pcnt"]], writes=[B["cnt"]])
            fw.op("dve", lambda: V.tensor_scalar(out=ge[:], in0=cnt[:], scalar1=float(CAP) - 0.5, scalar2=None, op0=ALU.is_ge),
                  reads=[B["cnt"]], writes=[B["ge"]])
            fw.op("dve", lambda: V.tensor_tensor(out=tmp8[:], in0=ge[:], in1=mid[:], op=ALU.mult), reads=[B["ge"], B["mid"]], writes=[B["tmp8"]])
            fw.op("dve", lambda: V.tensor_tensor(out=lo[:], in0=lo[:], in1=tmp8[:], op=ALU.max), reads=[B["tmp8"], B["lo"]], writes=[B["lo"]])
            fw.op("dve", lambda: V.scalar_tensor_tensor(out=tmp8[:], in0=ge[:], scalar=4.0, in1=mid[:], op0=ALU.mult, op1=ALU.add),
                  reads=[B["ge"], B["mid"]], writes=[B["tmp8"]])
            fw.op("dve", lambda: V.tensor_tensor(out=hi[:], in0=hi[:], in1=tmp8[:], op=ALU.min), reads=[B["tmp8"], B["hi"]], writes=[B["hi"]])
        count_ge(lo, B["lo"])
        fw.op("act", lambda: nc.scalar.copy(out=tot[:].rearrange("p a b -> p (a b)"), in_=pcnt[:, :NC8]), reads=[B["pcnt"]], writes=[B["tot"]])
        fw.op("dve", lambda: V.tensor_copy(out=maskf[:], in_=cmpb[:]), reads=[B["cmpb"]], writes=[B["maskf"]])
        fw.op("pe", lambda: nc.tensor.matmul(ppos[:, :NC8], lhsT=trit[:], rhs=cmpb[:].rearrange("p a b -> p (a b)"), start=True, stop=True),
              reads=[B["trit"], B["cmpb"]], writes=[B["ppos"]])
        fw.op("dve", lambda: V.tensor_copy(out=off[:, :, 0], in_=sofft[:]), reads=[B["sofft"]], writes=[B["off"]])
        for j in range(1, NTT):
            fw.op("dve", lambda j=j: V.tensor_tensor(out=off[:, :, j], in0=off[:, :, j - 1], in1=tot[:, :, j - 1], op=ALU.add),
                  reads=[B["off"], B["tot"]], writes=[B["off"]])
        fw.op("dve", lambda: V.tensor_tensor(out=pos[:].rearrange("p a b -> p (a b)"), in0=ppos[:, :NC8],
                                             in1=off[:].rearrange("p a b -> p (a b)"), op=ALU.add),
              reads=[B["ppos"], B["off"]], writes=[B["pos"]])
        fw.op("dve", lambda: V.scalar_tensor_tensor(out=pos[:], in0=pos[:], scalar=-BIGPOS, in1=maskf[:], op0=ALU.add, op1=ALU.mult),
              reads=[B["pos"], B["maskf"]], writes=[B["pos"]])
        fw.op("dve", lambda: V.tensor_scalar(out=pos[:], in0=pos[:], scalar1=BIGPOS, scalar2=None, op0=ALU.add),
              reads=[B["pos"]], writes=[B["pos"]])
        fw.op("dve", lambda: V.tensor_copy(out=posi[:], in_=pos[:]), reads=[B["pos"]], writes=[B["posi"]])
        fw.dma("sp", postab, posi[:], reads=[B["posi"]], writes=[B["o_pos"]])
        for b in range(NB):
            for el in range(2):
                for h in range(2):
                    fw.op("pool", lambda b=b, el=el, h=h: nc.gpsimd.tensor_copy(out=vals[:, b * 2 + el, :, h], in_=tokt[:, h, b, :]),
                          reads=[B["tokt"]], writes=[B["vals"]])
        fw.op("dve", lambda: V.tensor_copy(out=gres[:], in_=A[:]), reads=[B["A"]], writes=[B["gres"]])
        for q in range(3):
            fw.op("dve", lambda q=q: V.tensor_copy(out=vals[:, :, :, 2 + q], in_=gres[:]), reads=[B["gres"]], writes=[B["vals"]])
            if q < 2:
                fw.op("dve", lambda q=q: V.tensor_copy(out=gpc[:], in_=vals[:, :, :, 2 + q]), reads=[B["vals"]], writes=[B["gres"]])
                fw.op("dve", lambda: V.tensor_tensor(out=gres[:], in0=gres[:], in1=gpc[:], op=ALU.subtract), reads=[B["gres"]], writes=[B["gres"]])

        ycnt = [0]
        for el in range(2):
            if el > 0:
                load_all_w(el)
            for g in range(NG):
                steps = [(b, j) for b in range(g * GB, (g + 1) * GB) for j in range(NTT)]
                for si, (b, j) in enumerate(steps):
                    col = b * 2 + el
                    fw.op("dve", lambda: V.tensor_scalar(out=ohall[:, si, :], in0=iot[:], scalar1=pos[:, col, j:j + 1], scalar2=None, op0=ALU.is_equal),
                          reads=[B["iot"], B["pos"]], writes=[B["ohall"]])
                for c in range(NCH):
                    for si, (b, j) in enumerate(steps):
                        col = b * 2 + el
                        fw.op("pe", lambda: nc.tensor.matmul(pidx[:, c * 8:c * 8 + 5], lhsT=ohall[:, si, c * 128:(c + 1) * 128],
                                                             rhs=vals[:, col, j, :], start=(si == 0), stop=(si == len(steps) - 1)),
                              reads=[B["ohall"], B["vals"]], writes=[B["pidx"]])
                fw.op("dve", lambda: V.tensor_copy(out=idxf[:], in_=pidx[:, :NCH * 8].rearrange("p (a b) -> p a b", b=8)[:, :, 0:5]),
                      reads=[B["pidx"]], writes=[B["idxf"]])
                fw.op("dve", lambda: V.scalar_tensor_tensor(out=idf[:], in0=idxf[:, :, 0], scalar=128.0, in1=idxf[:, :, 1], op0=ALU.mult, op1=ALU.add),
                      reads=[B["idxf"]], writes=[B["idxf"]])
                fw.op("dve", lambda: V.tensor_copy(out=idxu[:], in_=idf[:]), reads=[B["idxf"]], writes=[B["idxu"]])
                fw.op("dve", lambda: V.tensor_tensor(out=gate[:], in0=idxf[:, :, 2], in1=idxf[:, :, 3], op=ALU.add), reads=[B["idxf"]], writes=[B["idxf"]])
                fw.op("dve", lambda: V.tensor_tensor(out=gate[:], in0=gate[:], in1=idxf[:, :, 4], op=ALU.add), reads=[B["idxf"]], writes=[B["idxf"]])
                for c in range(NCH):
                    s = c % 2
                    fw.dma("pool", xg[s][:], h2, reads=[B["idxu"]], writes=[B[f"xg{s}"]],
                           indirect=dict(out_offset=None, in_offset=bass.IndirectOffsetOnAxis(ap=idxu[:, c:c + 1], axis=0)))
                    for k4 in range(2):
                        for kk in range(4):
                            k = k4 * 4 + kk
                            fw.op("pe", lambda k=k, kk=kk: nc.tensor.transpose(out=ptrb[:, kk, :], in_=xg[s][:, k * 128:(k + 1) * 128], identity=idb[:]),
                                  reads=[B[f"xg{s}"], B["idb"]], writes=[B["ptrb"]])
                        fw.op("act", lambda k4=k4: nc.scalar.copy(out=xgT[:, k4 * 4:(k4 + 1) * 4, c * 128:(c + 1) * 128], in_=ptrb[:]),
                              reads=[B["ptrb"]], writes=[B["xgT"]])
                for ft in range(16):
                    s = ft % 2
                    pgx, pgn = (pg, "pg") if s == 0 else (pcnt, "pcnt")
                    pux, pun = (pu, "pu") if s == 0 else (ppos, "ppos")
                    for k in range(8):
                        fw.op("pe", lambda k=k: nc.tensor.matmul(pgx[:, :NS], lhsT=wgb[:, k, ft * 128:(ft + 1) * 128], rhs=xgT[:, k, :],
                                                                  start=(k == 0), stop=(k == 7)), reads=[B["wgb"], B["xgT"]], writes=[B[pgn]])
                    for k in range(8):
                        fw.op("pe", lambda k=k: nc.tensor.matmul(pux[:, :NS], lhsT=wub[:, k, ft * 128:(ft + 1) * 128], rhs=xgT[:, k, :],
                                                                  start=(k == 0), stop=(k == 7)), reads=[B["wub"], B["xgT"]], writes=[B[pun]])
                    fw.op("act", lambda: nc.scalar.activation(out=sg[s][:], in_=pgx[:, :NS], func=AF.Silu), reads=[B[pgn]], writes=[B[f"sg{s}"]])
                    fw.op("dve", lambda: V.tensor_tensor(out=hT[:, ft, :], in0=sg[s][:], in1=pux[:, :NS], op=ALU.mult),
                          reads=[B[f"sg{s}"], B[pun]], writes=[B["hT"]])
                for c in range(NCH):
                    s = ycnt[0] % 2
                    ycnt[0] += 1
                    for hf, (py, nm) in enumerate(((py0, "py0"), (py1, "py1"))):
                        for ft in range(16):
                            fw.op("pe", lambda ft=ft: nc.tensor.matmul(py[:], lhsT=hT[:, ft, c * 128:(c + 1) * 128],
                                                                        rhs=wdb[:, ft, hf * 512:(hf + 1) * 512], start=(ft == 0), stop=(ft == 15)),
                                  reads=[B["hT"], B["wdb"]], writes=[B[nm]])
                        if hf == 0:
                            fw.op("dve", lambda: V.tensor_scalar(out=ysb[s][:, 0:512], in0=py[:], scalar1=gate[:, c:c + 1], scalar2=None, op0=ALU.mult),
                                  reads=[B[nm], B["idxf"]], writes=[B[f"ysb{s}"]])
                        else:
                            fw.op("act", lambda: nc.scalar.activation(out=ysb[s][:, 512:1024], in_=py[:], func=AF.Copy, scale=gate[:, c:c + 1]),
                                  reads=[B[nm], B["idxf"]], writes=[B[f"ysb{s}"]])
                    fw.dma("sp", yc[el, g, c * 128:(c + 1) * 128, :], ysb[s][:], reads=[B[f"ysb{s}"]], writes=[B["o_yc"]])
        fw.finish([B["o_yc"], B["o_pos"]])
    return nc


def build_P(NT, NS):
    T = NT * 128
    RS = NS + 128
    nc = new_nc()
    x1 = dram_in(nc, "x1", [T, 1024])
    ycb = dram_in(nc, "ycb", [16 * RS, 1024])
    postab = dram_in(nc, "postab", [128, 16, NT], I32)
    rows = dram_in(nc, "rows", [1, 3 * 1024])
    x2 = dram_out(nc, "x2", [T, 1024])
    with ExitStack() as st:
        fw = FW(nc, st)
        make_consts(fw, nc)
        pt = fw.sb("pt", [128, 16, NT], I32)
        rw = fw.sb("rw", [128, 3, 1024], F32)
        xt = [fw.sb(f"xt{i}", [128, 1024], F32) for i in range(2)]
        acc = [fw.sb(f"acc{i}", [128, 1024], F32) for i in range(2)]
        gb = [fw.sb(f"gb{i}", [128, 1024], F32) for i in range(4)]
        stats = fw.sb("stats", [128, 2, 6], F32)
        mv = fw.sb("mv", [128, 2], F32)
        rstd = fw.sb("rstd", [128, 1], F32)
        Bpt, Brw, Bs, Bo = Buf("pt"), Buf("rw"), Buf("stats"), Buf("o_x2")
        Bxt = [Buf("xt0"), Buf("xt1")]
        Bacc = [Buf("acc0"), Buf("acc1")]
        Bgb = [Buf(f"gb{i}") for i in range(4)]
        fw.dma("sp", pt[:], postab, writes=[Bpt])
        fw.dma("sp", rw[:].rearrange("p a b -> p (a b)"), rows.partition_broadcast(128), writes=[Brw])
        gi = 0
        for t in range(NT):
            s = t % 2
            fw.dma("sp", xt[s][:], x1[t * 128:(t + 1) * 128, :], writes=[Bxt[s]])
            for e in range(16):
                q = gi % 4
                gi += 1
                dst = acc[s] if e == 0 else gb[q]
                Bd = Bacc[s] if e == 0 else Bgb[q]
                fw.dma("pool", dst[:], ycb, reads=[Bpt], writes=[Bd],
                       indirect=dict(out_offset=None, in_offset=bass.IndirectOffsetOnAxis(ap=pt[:, e, t:t + 1], axis=0),
                                     element_offset=e * RS * 1024))
                if e > 0:
                    fw.op("dve", lambda: nc.vector.tensor_tensor(out=acc[s][:], in0=acc[s][:], in1=gb[q][:], op=ALU.add),
                          reads=[Bacc[s], Bgb[q]], writes=[Bacc[s]])
            fw.op("dve", lambda: nc.vector.tensor_tensor(out=acc[s][:], in0=acc[s][:], in1=rw[:, 0, :], op=ALU.mult),
                  reads=[Bacc[s], Brw], writes=[Bacc[s]])
            fw.op("dve", lambda: nc.vector.scalar_tensor_tensor(out=acc[s][:], in0=xt[s][:], scalar=ALPHA, in1=acc[s][:], op0=ALU.mult, op1=ALU.add),
                  reads=[Bxt[s], Bacc[s]], writes=[Bacc[s]])
            layer_norm_tile(fw, nc, acc[s], Bacc[s], stats, mv, rstd, Bs)
            fw.op("pool", lambda: nc.gpsimd.tensor_tensor(out=acc[s][:], in0=acc[s][:], in1=rw[:, 1, :], op=ALU.mult),
                  reads=[Bacc[s], Brw], writes=[Bacc[s]])
            fw.op("pool", lambda: nc.gpsimd.tensor_tensor(out=acc[s][:], in0=acc[s][:], in1=rw[:, 2, :], op=ALU.add),
                  reads=[Bacc[s], Brw], writes=[Bacc[s]])
            fw.dma("sp", x2[t * 128:(t + 1) * 128, :], acc[s][:], reads=[Bacc[s]], writes=[Bo])
        fw.finish([Bo])
    return nc


def load_cast(fw, nc, dst, Bdst, src_fn, nk, width, wst, Bwst, ctr):
    for k in range(nk):
        s = ctr[0] % 2
        ctr[0] += 1
        fw.dma("sp" if s == 0 else "act", wst[s][:, :width], src_fn(k), writes=[Bwst[s]])
        if s == 0:
            fw.op("act", lambda k=k: nc.scalar.copy(out=dst[:, k, :], in_=wst[s][:, :width]), reads=[Bwst[s]], writes=[Bdst])
        else:
            fw.op("pool", lambda k=k: nc.gpsimd.tensor_copy(out=dst[:, k, :], in_=wst[s][:, :width]), reads=[Bwst[s]], writes=[Bdst])


def build_MG(NT, stage=9):
    T = NT * 128
    NGRP = T // 512
    nc = new_nc()
    xT = dram_in(nc, "xT", [128, 8, T])
    mcol = dram_in(nc, "mcol", [128, 8, 2])
    win = dram_in(nc, "win", [128, 8, 4096])
    lnr = dram_in(nc, "lnr", [1, 2 * 2048])
    wsT = dram_in(nc, "wsT", [128, 16, 128])
    bsr = dram_in(nc, "bsr", [1, 16 * 128])
    gTo = dram_out(nc, "gT", [128, 16, T], BF16)
    with ExitStack() as st:
        fw = FW(nc, st)
        make_consts(fw, nc)
        V = nc.vector
        mc = fw.sb("mc", [128, 8, 2], F32)
        xs = [fw.sb(f"xs{i}", [128, T], F32) for i in range(2)]
        hT = fw.sb("hT", [128, 8, T], BF16)
        wb = fw.sb("wb", [128, 8, 4096], BF16)
        wst = [fw.sb(f"wst{i}", [128, 2048], F32) for i in range(2)]
        lnt = fw.sb("lnt", [128, 2, 2048], F32)
        wsf = fw.sb("wsf", [128, 16, 128], F32)
        wsb = fw.sb("wsb", [128, 16, 128], BF16)
        bst = fw.sb("bst", [128, 16, 128], F32)
        uT = fw.sb("uT", [128, 16, 512], BF16)
        v = fw.sb("v", [128, 2048], F32)
        vln = fw.sb("vln", [128, 2048], BF16)
        tmp = fw.sb("tmp", [128, 4, 128], F32)
        go = [fw.sb(f"go{i}", [128, 16, 128], BF16) for i in range(2)]
        stats = fw.sb("stats", [128, 4, 6], F32)
        mv = fw.sb("mv", [128, 2], F32)
        rstd = fw.sb("rstd", [128, 1], F32)
        pu = [fw.ps(f"pu{i}", [128, 512]) for i in range(2)]
        pv = [fw.ps(f"pv{i}", [128, 512]) for i in range(2)]
        psp = [fw.ps(f"psp{i}", [128, 4, 128]) for i in range(2)]
        B = {n: Buf(n) for n in ["mc", "xs0", "xs1", "hT", "wb", "wst0", "wst1", "lnt", "wsf", "wsb", "bst", "uT", "v", "vln", "tmp",
                                 "go0", "go1", "stats", "pu0", "pu1", "pv0", "pv1", "psp0", "psp1", "o"]}
        fw.dma("sp", mc[:], mcol, writes=[B["mc"]])
        fw.dma("sp", lnt[:].rearrange("p a b -> p (a b)"), lnr.partition_broadcast(128), writes=[B["lnt"]])
        fw.dma("sp", wsf[:], wsT, writes=[B["wsf"]])
        fw.dma("sp", bst[:].rearrange("p a b -> p (a b)"), bsr.partition_broadcast(128), writes=[B["bst"]])
        fw.op("dve", lambda: V.tensor_copy(out=wsb[:], in_=wsf[:]), reads=[B["wsf"]], writes=[B["wsb"]])
        fw.op("dve", lambda: V.tensor_scalar(out=mc[:, :, 0], in0=mc[:, :, 0], scalar1=1.0, scalar2=None, op0=ALU.add), reads=[B["mc"]], writes=[B["mc"]])
        for k in range(8):
            s = k % 2
            fw.dma("sp", xs[s][:], xT[:, k, :], writes=[B[f"xs{s}"]])
            fw.op("act", lambda k=k: nc.scalar.activation(out=hT[:, k, :], in_=xs[s][:], func=AF.Identity, scale=mc[:, k, 0:1], bias=mc[:, k, 1:2]),
                  reads=[B[f"xs{s}"], B["mc"]], writes=[B["hT"]])
        ctr = [0]
        wbv = wb[:].rearrange("p k (h w) -> p (k h) w", h=2)
        winv = win.rearrange("p k (h w) -> p (k h) w", h=2)
        load_cast(fw, nc, wbv, B["wb"], lambda k: winv[:, k, :], 16, 2048, wst, [B["wst0"], B["wst1"]], ctr)
        ev = 0
        for grp in range(NGRP if stage > 0 else 0):
            tsl = slice(grp * 512, (grp + 1) * 512)
            for uf in range(16):
                s = uf % 2
                for k in range(8):
                    fw.op("pe", lambda k=k: nc.tensor.matmul(pu[s][:], lhsT=wb[:, k, uf * 128:(uf + 1) * 128], rhs=hT[:, k, tsl], start=(k == 0), stop=(k == 7)),
                          reads=[B["wb"], B["hT"]], writes=[B[f"pu{s}"]])
                fw.op("act", lambda: nc.scalar.activation(out=uT[:, uf, :], in_=pu[s][:], func=AF.Gelu), reads=[B[f"pu{s}"]], writes=[B["uT"]])
            for cc in range(4 if stage > 1 else 0):
                ch = grp * 4 + cc
                csl = slice(ch * 128, (ch + 1) * 128)
                for n in range(4):
                    s = n % 2
                    for k in range(8):
                        fw.op("pe", lambda k=k: nc.tensor.matmul(pv[s][:], lhsT=hT[:, k, csl], rhs=wb[:, k, 2048 + n * 512:2048 + (n + 1) * 512],
                                                                  start=(k == 0), stop=(k == 7)), reads=[B["wb"], B["hT"]], writes=[B[f"pv{s}"]])
                    fw.op("act", lambda: nc.scalar.activation(out=v[:, n * 512:(n + 1) * 512], in_=pv[s][:], func=AF.Gelu), reads=[B[f"pv{s}"]], writes=[B["v"]])
                for j in range(4):
                    fw.op("dve", lambda j=j: V.bn_stats(out=stats[:, j, :], in_=v[:, j * 512:(j + 1) * 512]), reads=[B["v"]], writes=[B["stats"]])
                fw.op("dve", lambda: V.bn_aggr(out=mv[:], in_=stats[:].rearrange("p a b -> p (a b)")), reads=[B["stats"]], writes=[B["stats"]])
                fw.op("act", lambda: nc.scalar.activation(out=rstd[:], in_=mv[:, 1:2], func=AF.Sqrt, bias=fw.eps_ln[:, 0:1], scale=1.0),
                      reads=[B["stats"], fw.Bconst], writes=[B["stats"]])
                fw.op("dve", lambda: V.reciprocal(out=rstd[:], in_=rstd[:]), reads=[B["stats"]], writes=[B["stats"]])
                fw.op("dve", lambda: V.tensor_scalar(out=v[:], in0=v[:], scalar1=mv[:, 0:1], scalar2=rstd[:, 0:1], op0=ALU.subtract, op1=ALU.mult),
                      reads=[B["stats"], B["v"]], writes=[B["v"]])
                fw.op("pool", lambda: nc.gpsimd.tensor_tensor(out=v[:], in0=v[:], in1=lnt[:, 0, :], op=ALU.mult), reads=[B["v"], B["lnt"]], writes=[B["v"]])
                fw.op("pool", lambda: nc.gpsimd.tensor_tensor(out=vln[:], in0=v[:], in1=lnt[:, 1, :], op=ALU.add), reads=[B["v"], B["lnt"]], writes=[B["vln"]])
                os_ = ch % 2
                for g4 in range(4 if stage > 2 else 0):
                    s = g4 % 2
                    for gg in range(4):
                        g = g4 * 4 + gg
                        fw.op("pe", lambda g=g, gg=gg: nc.tensor.matmul(psp[s][:, gg, :], lhsT=vln[:, g * 128:(g + 1) * 128], rhs=wsb[:, g, :], start=True, stop=True),
                              reads=[B["vln"], B["wsb"]], writes=[B[f"psp{s}"]])
                    fw.op("dve", lambda: V.tensor_tensor(out=tmp[:], in0=psp[s][:], in1=bst[:, g4 * 4:(g4 + 1) * 4, :], op=ALU.add),
                          reads=[B[f"psp{s}"], B["bst"]], writes=[B["tmp"]])
                    fw.op("dve", lambda: V.tensor_tensor(out=go[os_][:, g4 * 4:(g4 + 1) * 4, :], in0=tmp[:], in1=uT[:, g4 * 4:(g4 + 1) * 4, cc * 128:(cc + 1) * 128], op=ALU.mult),
                          reads=[B["tmp"], B["uT"]], writes=[B[f"go{os_}"]])
                if stage != 3:
                    for q4 in range(4):
                        fw.dma("sp", gTo[:, q4 * 4:(q4 + 1) * 4, csl], go[os_][:, q4 * 4:(q4 + 1) * 4, :], reads=[B[f"go{os_}"]], writes=[B["o"]])
        fw.finish([B["o"]])
    return nc


def build_MA():
    NQT = SEQ // 128
    NCT = CTX // 128
    NKT = NQT + NCT
    nc = new_nc()
    xT = dram_in(nc, "xT", [128, 8, SEQ])
    cxT = dram_in(nc, "cxT", [128, 8, CTX])
    mcol = dram_in(nc, "mcol", [128, 8, 4])
    w = dram_in(nc, "w", [128, 8, 768])
    gains = dram_in(nc, "gains", [1, 640])
    cs = dram_in(nc, "cs", [128, 2, NQT, 32])
    identb = dram_in(nc, "identb", [128, 128], BF16)
    oTo = dram_out(nc, "oT", [128, 4, SEQ], BF16)
    with ExitStack() as st:
        fw = FW(nc, st)
        V = nc.vector
        G = nc.gpsimd
        mc = fw.sb("mc", [128, 8, 4], F32)
        xs = [fw.sb(f"xs{i}", [128, 2048], F32) for i in range(2)]
        hT = fw.sb("hT", [128, 8, CTX + 2048], BF16)
        wst = [fw.sb(f"wst{i}", [128, 768], F32) for i in range(2)]
        wb = fw.sb("wb", [128, 8, 768], BF16)
        g10 = fw.sb("g10", [128, 10, 64], F32)
        cst = fw.sb("cst", [128, 2, NQT, 32], F32)
        idb = fw.sb("idb", [128, 128], BF16)
        qk = fw.sb("qk", [128, 10, 64], F32)
        sq = fw.sb("sq", [128, 10, 64], F32)
        ss = fw.sb("ss", [128, 10], F32)
        t1 = fw.sb("t1", [128, 10, 32], F32)
        t2 = fw.sb("t2", [128, 10, 32], F32)
        qr = fw.sb("qr", [128, 10, 64], BF16)
        qT = fw.sb("qT", [64, 8, SEQ], BF16)
        kT = fw.sb("kT", [64, 2, NKT * 128], BF16)
        vall = fw.sb("vall", [128, NKT, 2, 65], BF16)
        E = [fw.sb(f"E{i}", [128, 512], BF16) for i in range(2)]
        rden = fw.sb("rden", [128, 4], F32)
        otok = fw.sb("otok", [128, 8, 64], BF16)
        oTt = [fw.sb(f"oTt{i}", [128, 4, 128], BF16) for i in range(2)]
        epsr = fw.sb("epsr", [128, 1], F32)
        pA = [fw.ps(f"pA{i}", [128, 512]) for i in range(2)]
        pT = fw.ps("pT", [128, 4, 128], BF16)
        pO = [fw.ps(f"pO{i}", [128, 512]) for i in range(4)]
        B = {n: Buf(n) for n in ["mc", "xs0", "xs1", "hT", "wst0", "wst1", "wb", "g10", "cst", "idb", "qk", "sq", "ss", "t1", "t2", "qr", "qT", "kT",
                                 "vall", "E0", "E1", "rden", "otok", "oTt0", "oTt1", "epsr", "pA0", "pA1", "pT", "pO0", "pO1", "pO2", "pO3", "o"]}
        fw.dma("sp", mc[:], mcol, writes=[B["mc"]])
        fw.dma("sp", g10[:].rearrange("p a b -> p (a b)"), gains.partition_broadcast(128), writes=[B["g10"]])
        fw.dma("sp", cst[:], cs, writes=[B["cst"]])
        fw.dma("sp", idb[:], identb, writes=[B["idb"]])
        fw.op("pool", lambda: G.memset(epsr[:], RMS_EPS), writes=[B["epsr"]])
        fw.op("pool", lambda: G.memset(vall[:, :, :, 64:65], 1.0), writes=[B["vall"]])
        fw.op("dve", lambda: V.tensor_scalar(out=g10[:, 0:8, :], in0=g10[:, 0:8, :], scalar1=0.125, scalar2=None, op0=ALU.mult), reads=[B["g10"]], writes=[B["g10"]])
        for c in (0, 2):
            fw.op("dve", lambda c=c: V.tensor_scalar(out=mc[:, :, c], in0=mc[:, :, c], scalar1=1.0, scalar2=None, op0=ALU.add), reads=[B["mc"]], writes=[B["mc"]])
        li = [0]

        def load_half(hf):
            for k in range(8):
                if hf == 0:
                    s = li[0] % 2
                    li[0] += 1
                    fw.dma("sp", xs[s][:, :CTX], cxT[:, k, :], writes=[B[f"xs{s}"]])
                    fw.op("act", lambda k=k: nc.scalar.activation(out=hT[:, k, 0:CTX], in_=xs[s][:, :CTX], func=AF.Identity, scale=mc[:, k, 2:3], bias=mc[:, k, 3:4]),
                          reads=[B[f"xs{s}"], B["mc"]], writes=[B["hT"]])
                s = li[0] % 2
                li[0] += 1
                fw.dma("sp", xs[s][:], xT[:, k, hf * 2048:(hf + 1) * 2048], writes=[B[f"xs{s}"]])
                fw.op("act", lambda k=k: nc.scalar.activation(out=hT[:, k, CTX:CTX + 2048], in_=xs[s][:], func=AF.Identity,
                                                              scale=mc[:, k, 0:1], bias=mc[:, k, 1:2]),
                      reads=[B[f"xs{s}"], B["mc"]], writes=[B["hT"]])

        ctr = [0]
        load_cast(fw, nc, wb, B["wb"], lambda k: w[:, k, :], 8, 768, wst, [B["wst0"], B["wst1"]], ctr)

        def bc10(ap2, n):
            return ap2.unsqueeze(2).to_broadcast([128, 10, n])

        for tt in range(NKT):
            is_ctx = tt < NCT
            tsl = slice(tt * 128, (tt + 1) * 128)
            if tt == 0:
                load_half(0)
            if tt == NCT + 16:
                load_half(1)
            hc = tt * 128 if tt < NCT + 16 else (tt - 16) * 128
            hsl = slice(hc, hc + 128)
            for k in range(8):
                fw.op("pe", lambda k=k: nc.tensor.matmul(pA[0][:], lhsT=hT[:, k, hsl], rhs=wb[:, k, 0:512], start=(k == 0), stop=(k == 7)),
                      reads=[B["hT"], B["wb"]], writes=[B["pA0"]])
            for k in range(8):
                fw.op("pe", lambda k=k: nc.tensor.matmul(pA[1][:, 0:256], lhsT=hT[:, k, hsl], rhs=wb[:, k, 512:768], start=(k == 0), stop=(k == 7)),
                      reads=[B["hT"], B["wb"]], writes=[B["pA1"]])
            fw.op("act", lambda: nc.scalar.copy(out=qk[:, 0:8, :], in_=pA[0][:].rearrange("p (a b) -> p a b", b=64)), reads=[B["pA0"]], writes=[B["qk"]])
            fw.op("act", lambda: nc.scalar.copy(out=qk[:, 8:10, :], in_=pA[1][:, 0:128].rearrange("p (a b) -> p a b", b=64)), reads=[B["pA1"]], writes=[B["qk"]])
            fw.op("act", lambda: nc.scalar.copy(out=vall[:, tt, :, 0:64], in_=pA[1][:, 128:256].rearrange("p (a b) -> p a b", b=64)), reads=[B["pA1"]], writes=[B["vall"]])
            fw.op("pool", lambda: G.tensor_tensor(out=sq[:], in0=qk[:], in1=qk[:], op=ALU.mult), reads=[B["qk"]], writes=[B["sq"]])
            fw.op("dve", lambda: V.tensor_reduce(out=ss[:], in_=sq[:], axis=AX.X, op=ALU.add), reads=[B["sq"]], writes=[B["ss"]])
            fw.op("act", lambda: nc.scalar.activation(out=ss[:], in_=ss[:], func=AF.Sqrt, scale=1.0 / 64.0, bias=epsr[:, 0:1]), reads=[B["ss"], B["epsr"]], writes=[B["ss"]])
            fw.op("dve", lambda: V.reciprocal(out=ss[:], in_=ss[:]), reads=[B["ss"]], writes=[B["ss"]])
            fw.op("dve", lambda: V.tensor_tensor(out=qk[:], in0=qk[:], in1=bc10(ss[:, :], 64), op=ALU.mult), reads=[B["qk"], B["ss"]], writes=[B["qk"]])
            if is_ctx:
                fw.op("dve", lambda: V.tensor_tensor(out=qr[:], in0=qk[:], in1=g10[:], op=ALU.mult), reads=[B["qk"], B["g10"]], writes=[B["qr"]])
            else:
                lt = tt - NCT
                fw.op("pool", lambda: G.tensor_tensor(out=qk[:], in0=qk[:], in1=g10[:], op=ALU.mult), reads=[B["qk"], B["g10"]], writes=[B["qk"]])
                cosb = cst[:, 0, lt, :].unsqueeze(1).to_broadcast([128, 10, 32])
                sinb = cst[:, 1, lt, :].unsqueeze(1).to_broadcast([128, 10, 32])
                fw.op("dve", lambda: V.tensor_tensor(out=t1[:], in0=qk[:, :, 0:32], in1=cosb, op=ALU.mult), reads=[B["qk"], B["cst"]], writes=[B["t1"]])
                fw.op("pool", lambda: G.tensor_tensor(out=t2[:], in0=qk[:, :, 32:64], in1=sinb, op=ALU.mult), reads=[B["qk"], B["cst"]], writes=[B["t2"]])
                fw.op("dve", lambda: V.tensor_tensor(out=qr[:, :, 0:32], in0=t1[:], in1=t2[:], op=ALU.subtract), reads=[B["t1"], B["t2"]], writes=[B["qr"]])
                fw.op("dve", lambda: V.tensor_tensor(out=t1[:], in0=qk[:, :, 0:32], in1=sinb, op=ALU.mult), reads=[B["qk"], B["cst"], B["qr"]], writes=[B["t1"]])
                fw.op("pool", lambda: G.tensor_tensor(out=t2[:], in0=qk[:, :, 32:64], in1=cosb, op=ALU.mult), reads=[B["qk"], B["cst"], B["qr"]], writes=[B["t2"]])
                fw.op("dve", lambda: V.tensor_tensor(out=qr[:, :, 32:64], in0=t1[:], in1=t2[:], op=ALU.add), reads=[B["t1"], B["t2"]], writes=[B["qr"]])
            heads = [8, 9] if is_ctx else list(range(10))
            for i0 in range(0, len(heads), 4):
                hs = heads[i0:i0 + 4]
                for j, hh in enumerate(hs):
                    fw.op("pe", lambda j=j, hh=hh: nc.tensor.transpose(out=pT[0:64, j, :], in_=qr[:, hh, :], identity=idb[:]),
                          reads=[B["qr"], B["idb"]], writes=[B["pT"]])
                if hs[0] < 8:
                    lt = tt - NCT
                    fw.op("act", lambda: nc.scalar.copy(out=qT[:, hs[0]:hs[0] + 4, lt * 128:(lt + 1) * 128], in_=pT[0:64, 0:4, :]), reads=[B["pT"]], writes=[B["qT"]])
                else:
                    fw.op("act", lambda: nc.scalar.copy(out=kT[:, :, tsl], in_=pT[0:64, 0:2, :]), reads=[B["pT"]], writes=[B["kT"]])
        steps = [(qt, kh, kt) for qt in range(NQT) for kh in range(2) for kt in range(NKT)]

        def emit_S(i):
            qt, kh, kt = steps[i]
            s = i % 2
            qsl = slice(qt * 128, (qt + 1) * 128)
            fw.op("pe", lambda: nc.tensor.matmul(pA[s][:].rearrange("p (a b) -> p a b", b=128), lhsT=kT[:, kh, kt * 128:(kt + 1) * 128],
                                                 rhs=qT[:, kh * 4:(kh + 1) * 4, qsl], start=True, stop=True),
                  reads=[B["kT"], B["qT"]], writes=[B[f"pA{s}"]])
            fw.op("act", lambda: nc.scalar.activation(out=E[s][:], in_=pA[s][:], func=AF.Exp), reads=[B[f"pA{s}"]], writes=[B[f"E{s}"]])

        emit_S(0)
        for i, (qt, kh, kt) in enumerate(steps):
            s = i % 2
            if i + 1 < len(steps):
                emit_S(i + 1)
            for g in range(4):
                fw.op("pe", lambda g=g: nc.tensor.matmul(pO[g][:, 0:65], lhsT=E[s][:, g * 128:(g + 1) * 128], rhs=vall[:, kt, kh, :],
                                                          start=(kt == 0), stop=(kt == NKT - 1)),
                      reads=[B[f"E{s}"], B["vall"]], writes=[B[f"pO{g}"]])
            if kt == NKT - 1:
                for g in range(4):
                    fw.op("dve", lambda g=g: V.reciprocal(out=rden[:, g:g + 1], in_=pO[g][:, 64:65]), reads=[B[f"pO{g}"]], writes=[B["rden"]])
                    fw.op("dve", lambda g=g: V.tensor_scalar(out=otok[:, kh * 4 + g, :], in0=pO[g][:, 0:64], scalar1=rden[:, g:g + 1], scalar2=None, op0=ALU.mult),
                          reads=[B[f"pO{g}"], B["rden"]], writes=[B["otok"]])
                if kh == 1:
                    os_ = qt % 2
                    qsl = slice(qt * 128, (qt + 1) * 128)
                    for j in range(4):
                        fw.op("pe", lambda j=j: nc.tensor.transpose(out=pT[:, j, :], in_=otok[:, 2 * j:2 * j + 2, :].rearrange("p a b -> p (a b)"), identity=idb[:]),
                              reads=[B["otok"], B["idb"]], writes=[B["pT"]])
                    fw.op("act", lambda: nc.scalar.copy(out=oTt[os_][:], in_=pT[:]), reads=[B["pT"]], writes=[B[f"oTt{os_}"]])
                    fw.dma("sp", oTo[:, :, qsl], oTt[os_][:], reads=[B[f"oTt{os_}"]], writes=[B["o"]])
        fw.finish([B["o"]])
    return nc


def rope_tables():
    L = SEQ
    rows = L // 64
    row = np.broadcast_to(np.arange(rows, dtype=np.float32)[:, None], (rows, 64)).reshape(L)
    col = np.broadcast_to(np.arange(64, dtype=np.float32)[None, :], (rows, 64)).reshape(L)
    inv = (10000.0 ** (-np.arange(16, dtype=np.float32) / 16)).astype(np.float32)
    ang = np.concatenate([row[:, None] * inv, col[:, None] * inv], axis=-1).astype(np.float32)
    cs = np.stack([np.cos(ang), np.sin(ang)], axis=0).astype(np.float32)
    return np.ascontiguousarray(cs.reshape(2, L // 128, 128, 32).transpose(2, 0, 1, 3))


_TAB = {}


def dft_tables(L):
    if L in _TAB:
        return _TAB[L]
    N = 2 * L
    TW = min(512, L)
    t = np.arange(L, dtype=np.int64)
    ph = (np.outer(t, t) % N).astype(np.float64) * (2.0 * np.pi / N)
    C = np.cos(ph)
    S = -np.sin(ph)
    S[:, 0] = np.where(t % 2 == 0, 1.0, -1.0)

    def slab(M):
        return np.ascontiguousarray(M.reshape(L // 128, 128, L // TW, TW).transpose(2, 1, 0, 3).astype(np.float32).astype(NPBF))
    _TAB[L] = (slab(C), slab(S), slab(np.ascontiguousarray(S.T)))
    return _TAB[L]


def hyena_consts(L):
    t = np.linspace(0.0, 1.0, L, dtype=np.float32)[:, None]
    w = ((2.0 * math.pi / L) * np.arange(L, dtype=np.float32))[:, None].astype(np.float32)
    f = np.linspace(1e-4, 15, 16, dtype=np.float32)[None, :]
    z = np.concatenate([t, np.cos(f * w), -np.sin(f * w)], axis=-1).astype(np.float32)
    min_decay = math.log(1e-2) / 1.5
    max_decay = math.log(1e-2) / 0.3
    deltas = np.abs(np.linspace(min_decay, max_decay, D, dtype=np.float32))
    decay = np.exp(-t * deltas).astype(np.float32)
    return np.ascontiguousarray(z.T), np.ascontiguousarray(decay.T)


def emit_fwd_dft(fw, nc, L, x_tm, Bx, tabC, tabS, slabs, Bslab, pacc, Bpacc, consume):
    TW = min(512, L)
    NK = L // 128
    sc = [0]
    for nt in range(L // TW):
        for which, tab in ((0, tabC), (1, tabS)):
            s = sc[0] % 2
            sc[0] += 1
            fw.dma("sp" if s == 0 else "act", slabs[s][:, :NK, :TW], tab[nt], writes=[Bslab[s]])
            for st_ in range(4):
                for k in range(NK):
                    fw.op("pe", lambda k=k: nc.tensor.matmul(pacc[st_][:, :TW], lhsT=x_tm[:, st_, k, :], rhs=slabs[s][:, k, :TW], start=(k == 0), stop=(k == NK - 1)),
                          reads=[Bx, Bslab[s]], writes=[Bpacc[st_]])
                consume(nt, which, st_, pacc[st_][:, :TW])


def wrap_pi(fw, nc, a, Ba, m, Bm, P):
    V = nc.vector
    PI = math.pi
    for _ in range(2):
        fw.op("dve", lambda: V.tensor_scalar(out=m[:P], in0=a[:P], scalar1=-PI, scalar2=2 * PI, op0=ALU.is_lt, op1=ALU.mult), reads=[Ba], writes=[Bm])
        fw.op("dve", lambda: V.tensor_tensor(out=a[:P], in0=a[:P], in1=m[:P], op=ALU.add), reads=[Ba, Bm], writes=[Ba])
        fw.op("dve", lambda: V.tensor_scalar(out=m[:P], in0=a[:P], scalar1=PI, scalar2=2 * PI, op0=ALU.is_gt, op1=ALU.mult), reads=[Ba], writes=[Bm])
        fw.op("dve", lambda: V.tensor_tensor(out=a[:P], in0=a[:P], in1=m[:P], op=ALU.subtract), reads=[Ba, Bm], writes=[Ba])
    fw.op("dve", lambda: V.tensor_scalar(out=a[:P], in0=a[:P], scalar1=-PI, scalar2=PI, op0=ALU.max, op1=ALU.min), reads=[Ba], writes=[Ba])


def build_F(L):
    TW = min(512, L)
    NK = L // 128
    NTL = L // TW
    N = 2 * L
    nc = new_nc()
    zT = dram_in(nc, "zT", [33, L])
    decay = dram_in(nc, "decay", [128, L])
    w1 = dram_in(nc, "w1", [33, 64])
    w2 = dram_in(nc, "w2", [64, 64])
    w3 = dram_in(nc, "w3", [64, 4, 128])
    vecs = dram_in(nc, "vecs", [64, 4])
    fbias = dram_in(nc, "fbias", [128, 2])
    identf = dram_in(nc, "identf", [128, 128])
    tabC = dram_in(nc, "tabC", [NTL, 128, NK, TW], BF16)
    tabS = dram_in(nc, "tabS", [NTL, 128, NK, TW], BF16)
    Kfo = dram_out(nc, "Kf", [128, 2, 2, L])
    with ExitStack() as st:
        fw = FW(nc, st)
        V = nc.vector
        G = nc.gpsimd
        w1t = fw.sb("w1t", [33, 64], F32)
        w2t = fw.sb("w2t", [64, 64], F32)
        w3t = fw.sb("w3t", [64, 4, 128], F32)
        vt = fw.sb("vt", [64, 6], F32)
        fbt = fw.sb("fbt", [128, 2], F32)
        idf = fw.sb("idf", [128, 128], F32)
        nrm = fw.sb("nrm", [128, 8], F32)
        x_tm = fw.sb("x_tm", [128, 4, NK, 128], BF16)
        pacc = [fw.ps(f"pacc{i}", [128, 512]) for i in range(4)]
        ptr = [fw.ps(f"ptr{i}", [128, 4, 128]) for i in range(2)]
        scope1 = fw.scoped()
        scope1.__enter__()
        zt = fw.sb("zt", [33, L], F32)
        dct = fw.sb("dct", [128, L], F32)
        h1 = fw.sb("h1", [64, L], F32)
        h2 = fw.sb("h2", [64, L], F32)
        mk = fw.sb("mk", [64, L], F32)
        kk = [fw.sb(f"kk{i}", [128, L], F32) for i in range(4)]
        B = {n: Buf(n) for n in ["zt", "dct", "w1t", "w2t", "w3t", "vt", "fbt", "idf", "h1", "h2", "mk", "kk0", "kk1", "kk2", "kk3", "nrm", "x_tm",
                                 "slab0", "slab1", "Kf", "pacc0", "pacc1", "pacc2", "pacc3", "ptr0", "ptr1", "o"]}
        for t_, src, nm in ((zt, zT, "zt"), (dct, decay, "dct"), (w1t, w1, "w1t"), (w2t, w2, "w2t"), (w3t, w3, "w3t"), (fbt, fbias, "fbt"), (idf, identf, "idf")):
            fw.dma("sp", t_[:], src, writes=[B[nm]])
        fw.dma("sp", vt[:, 0:4], vecs, writes=[B["vt"]])
        fw.op("dve", lambda: V.tensor_tensor(out=vt[:, 4:6], in0=vt[:, 0:2], in1=vt[:, 2:4], op=ALU.mult), reads=[B["vt"]], writes=[B["vt"]])
        for layer, (wt, Bw, src, Bsrc, dst, Bdst, KP) in enumerate(((w1t, B["w1t"], zt, B["zt"], h1, B["h1"], 33), (w2t, B["w2t"], h1, B["h1"], h2, B["h2"], 64))):
            for j in range(L // TW):
                s = j % 2
                sl = slice(j * TW, (j + 1) * TW)
                fw.op("pe", lambda: nc.tensor.matmul(pacc[s][0:64, :TW], lhsT=wt[:KP, :], rhs=src[:KP, sl], start=True, stop=True), reads=[Bw, Bsrc], writes=[B[f"pacc{s}"]])
                fw.op("act", lambda: nc.scalar.activation(out=dst[:, sl], in_=pacc[s][0:64, :TW], func=AF.Identity, scale=vt[:, 2 + layer:3 + layer], bias=vt[:, 4 + layer:5 + layer]),
                      reads=[B[f"pacc{s}"], B["vt"]], writes=[Bdst])
            wrap_pi(fw, nc, dst, Bdst, mk, B["mk"], 64)
            fw.op("act", lambda: nc.scalar.activation(out=dst[:, :], in_=dst[:, :], func=AF.Sin), reads=[Bdst], writes=[Bdst])
        for st_ in range(4):
            for j in range(L // TW):
                s = j % 2
                sl = slice(j * TW, (j + 1) * TW)
                fw.op("pe", lambda: nc.tensor.matmul(pacc[s][:, :TW], lhsT=w3t[:, st_, :], rhs=h2[:, sl], start=True, stop=True), reads=[B["w3t"], B["h2"]], writes=[B[f"pacc{s}"]])
                fw.op("dve", lambda: V.tensor_tensor(out=kk[st_][:, sl], in0=pacc[s][:, :TW], in1=dct[:, sl], op=ALU.mult), reads=[B[f"pacc{s}"], B["dct"]], writes=[B[f"kk{st_}"]])
            if st_ % 2 == 1:
                fw.op("pool", lambda: G.memset(kk[st_][:, 0:1], 0.0), reads=[], writes=[B[f"kk{st_}"]])
            fw.op("dve", lambda: V.tensor_reduce(out=nrm[:, st_:st_ + 1], in_=kk[st_][:], axis=AX.X, op=ALU.add, apply_absolute_value=True),
                  reads=[B[f"kk{st_}"]], writes=[B["nrm"]])
        for o in range(2):
            fw.op("dve", lambda: V.tensor_tensor(out=nrm[:, 4 + o:5 + o], in0=nrm[:, 2 * o:2 * o + 1], in1=nrm[:, 2 * o + 1:2 * o + 2], op=ALU.add), reads=[B["nrm"]], writes=[B["nrm"]])
            fw.op("dve", lambda: V.tensor_scalar(out=nrm[:, 4 + o:5 + o], in0=nrm[:, 4 + o:5 + o], scalar1=RMS_EPS, scalar2=None, op0=ALU.add), reads=[B["nrm"]], writes=[B["nrm"]])
            fw.op("dve", lambda: V.reciprocal(out=nrm[:, 6 + o:7 + o], in_=nrm[:, 4 + o:5 + o]), reads=[B["nrm"]], writes=[B["nrm"]])
        for st_ in range(4):
            o = st_ // 2
            fw.op("pool", lambda: G.tensor_scalar(out=kk[st_][:], in0=kk[st_][:], scalar1=nrm[:, 6 + o:7 + o], scalar2=None, op0=ALU.mult), reads=[B[f"kk{st_}"], B["nrm"]], writes=[B[f"kk{st_}"]])
            for k4 in range(NK // 4 if NK >= 4 else 1):
                s = k4 % 2
                nn = min(4, NK)
                for kq in range(nn):
                    k = k4 * 4 + kq
                    fw.op("pe", lambda k=k, kq=kq: nc.tensor.transpose(out=ptr[s][:, kq, :], in_=kk[st_][:, k * 128:(k + 1) * 128], identity=idf[:]),
                          reads=[B[f"kk{st_}"], B["idf"]], writes=[B[f"ptr{s}"]])
                fw.op("act", lambda: nc.scalar.copy(out=x_tm[:, st_, k4 * 4:k4 * 4 + nn, :], in_=ptr[s][:, 0:nn, :]), reads=[B[f"ptr{s}"]], writes=[B["x_tm"]])

        scope1.__exit__(None, None, None)
        slabs = [fw.sb(f"slab{i}", [128, NK, TW], BF16) for i in range(2)]
        Kf = fw.sb("Kfs", [128, 2, 2, L], F32)

        def consume(nt, which, st_, ps):
            o, d = st_ // 2, st_ % 2
            dst = Kf[:, o, which, nt * TW:(nt + 1) * TW]
            if d == 0:
                fw.op("act", lambda: nc.scalar.copy(out=dst, in_=ps), reads=[B[f"pacc{st_}"]], writes=[B["Kf"]])
            else:
                op = ALU.add if which == 0 else ALU.subtract
                fw.op("dve", lambda: V.tensor_tensor(out=dst, in0=dst, in1=ps, op=op), reads=[B[f"pacc{st_}"], B["Kf"]], writes=[B["Kf"]])
                if which == 1 and nt == 0:
                    fw.op("dve", lambda: V.scalar_tensor_tensor(out=Kf[:, o, 1, 0:1], in0=ps[:, 0:1], scalar=2.0, in1=Kf[:, o, 1, 0:1], op0=ALU.mult, op1=ALU.add),
                          reads=[B[f"pacc{st_}"], B["Kf"]], writes=[B["Kf"]])
        emit_fwd_dft(fw, nc, L, x_tm, B["x_tm"], tabC, tabS, slabs, [B["slab0"], B["slab1"]], pacc, [B[f"pacc{i}"] for i in range(4)], consume)
        for o in range(2):
            fw.op("dve", lambda: V.tensor_scalar(out=Kf[:, o, 0, :], in0=Kf[:, o, 0, :], scalar1=fbt[:, o:o + 1], scalar2=2.0 / N, op0=ALU.add, op1=ALU.mult),
                  reads=[B["Kf"], B["fbt"]], writes=[B["Kf"]])
            fw.op("dve", lambda: V.tensor_scalar(out=Kf[:, o, 1, 0:1], in0=Kf[:, o, 1, 0:1], scalar1=fbt[:, o:o + 1], scalar2=None, op0=ALU.add), reads=[B["Kf"], B["fbt"]], writes=[B["Kf"]])
            fw.op("dve", lambda: V.tensor_scalar(out=Kf[:, o, 1, :], in0=Kf[:, o, 1, :], scalar1=2.0 / N, scalar2=None, op0=ALU.mult), reads=[B["Kf"]], writes=[B["Kf"]])
            fw.op("dve", lambda: V.tensor_scalar(out=Kf[:, o, :, 0:1], in0=Kf[:, o, :, 0:1], scalar1=0.5, scalar2=None, op0=ALU.mult), reads=[B["Kf"]], writes=[B["Kf"]])
            for ri in range(2):
                fw.dma("sp", Kfo[:, o, ri, :], Kf[:, o, ri, :], reads=[B["Kf"]], writes=[B["o"]])
        fw.finish([B["o"]])
    return nc


def build_MH(L):
    TW = min(512, L)
    NK = L // 128
    NTL = L // TW
    TJ = TW // 128
    nc = new_nc()
    xT = dram_in(nc, "xT", [128, 8, L])
    mcol = dram_in(nc, "mcol", [128, 8, 2])
    win = dram_in(nc, "win", [128, 8, 1536])
    cw = dram_in(nc, "cw", [128, 12, 4])
    Kfi = dram_in(nc, "Kf", [4, 128, 2, 2, L])
    tabC = dram_in(nc, "tabC", [NTL, 128, NK, TW], BF16)
    tabS = dram_in(nc, "tabS", [NTL, 128, NK, TW], BF16)
    tabST = dram_in(nc, "tabST", [NTL, 128, NK, TW], BF16)
    identf = dram_in(nc, "identf", [128, 128])
    zTo = dram_out(nc, "zT", [128, 4, L], BF16)
    x12 = nc.dram_tensor("x12", [4, 2, 128, L], F32).ap()
    with ExitStack() as st:
        fw = FW(nc, st)
        V = nc.vector
        G = nc.gpsimd
        mc = fw.sb("mc", [128, 8, 2], F32)
        cwt = fw.sb("cwt", [128, 12, 4], F32)
        idf = fw.sb("idf", [128, 128], F32)
        x_tm = fw.sb("x_tm", [128, 4, NK, 128], BF16)
        pacc = [fw.ps(f"pacc{i}", [128, 512]) for i in range(4)]
        ptr = [fw.ps(f"ptr{i}", [128, 4, 128]) for i in range(2)]
        B = {n: Buf(n) for n in ["mc", "cwt", "idf", "x_tm", "pacc0", "pacc1", "pacc2", "pacc3", "ptr0", "ptr1", "x12", "o",
                                 "xs0", "xs1", "hT", "wb", "wst0", "wst1", "pb0", "pb1", "ob0", "ob1",
                                 "slab0", "slab1", "y_fm", "Xr", "kt0", "kt1", "Y", "ta", "tb", "xq0", "xq1", "zb0", "zb1", "zo0", "zo1"]}
        Bpacc = [B[f"pacc{i}"] for i in range(4)]
        fw.dma("sp", mc[:], mcol, writes=[B["mc"]])
        fw.dma("sp", cwt[:], cw, writes=[B["cwt"]])
        fw.dma("sp", idf[:], identf, writes=[B["idf"]])
        fw.op("dve", lambda: V.tensor_scalar(out=mc[:, :, 0], in0=mc[:, :, 0], scalar1=1.0, scalar2=None, op0=ALU.add), reads=[B["mc"]], writes=[B["mc"]])
        scA = fw.scoped()
        scA.__enter__()
        XW = min(1024, L)
        xs = [fw.sb(f"xs{i}", [128, XW], F32) for i in range(2)]
        hT = fw.sb("hT", [128, 8, L], BF16)
        wb = fw.sb("wb", [128, 8, 1536], BF16)
        wst = [fw.sb(f"wst{i}", [128, 768], F32) for i in range(2)]
        pb = [fw.sb(f"pb{i}", [128, L + 2], F32) for i in range(2)]
        ob = [fw.sb(f"ob{i}", [128, L], F32) for i in range(2)]
        li = 0
        for k in range(8):
            for hf in range(L // XW):
                s = li % 2
                li += 1
                fw.dma("sp", xs[s][:], xT[:, k, hf * XW:(hf + 1) * XW], writes=[B[f"xs{s}"]])
                fw.op("act", lambda k=k, hf=hf: nc.scalar.activation(out=hT[:, k, hf * XW:(hf + 1) * XW], in_=xs[s][:], func=AF.Identity, scale=mc[:, k, 0:1], bias=mc[:, k, 1:2]),
                      reads=[B[f"xs{s}"], B["mc"]], writes=[B["hT"]])
        ctr = [0]
        wbv = wb[:].rearrange("p k (h w) -> p (k h) w", h=2)
        winv = win.rearrange("p k (h w) -> p (k h) w", h=2)
        load_cast(fw, nc, wbv, B["wb"], lambda k: winv[:, k, :], 16, 768, wst, [B["wst0"], B["wst1"]], ctr)
        for s in range(2):
            fw.op("pool", lambda s=s: G.memset(pb[s][:, 0:1], 0.0), writes=[B[f"pb{s}"]])
            fw.op("pool", lambda s=s: G.memset(pb[s][:, L + 1:L + 2], 0.0), writes=[B[f"pb{s}"]])
        it = 0
        for st_ in range(4):
            for q in range(3):
                s = it % 2
                it += 1
                col0 = (st_ * 3 + q) * 128
                for tg in range(NTL):
                    pa = tg % 4
                    for k in range(8):
                        fw.op("pe", lambda k=k: nc.tensor.matmul(pacc[pa][:, :TW], lhsT=wb[:, k, col0:col0 + 128], rhs=hT[:, k, tg * TW:(tg + 1) * TW], start=(k == 0), stop=(k == 7)),
                              reads=[B["wb"], B["hT"]], writes=[Bpacc[pa]])
                    fw.op("act", lambda: nc.scalar.copy(out=pb[s][:, 1 + tg * TW:1 + (tg + 1) * TW], in_=pacc[pa][:, :TW]), reads=[Bpacc[pa]], writes=[B[f"pb{s}"]])
                ci = st_ * 3 + q
                fw.op("act", lambda: nc.scalar.activation(out=ob[s][:], in_=pb[s][:, 1:L + 1], func=AF.Identity, scale=cwt[:, ci, 1:2], bias=cwt[:, ci, 3:4]),
                      reads=[B[f"pb{s}"], B["cwt"]], writes=[B[f"ob{s}"]])
                fw.op("dve", lambda: V.scalar_tensor_tensor(out=ob[s][:], in0=pb[s][:, 0:L], scalar=cwt[:, ci, 0:1], in1=ob[s][:], op0=ALU.mult, op1=ALU.add),
                      reads=[B[f"pb{s}"], B["cwt"], B[f"ob{s}"]], writes=[B[f"ob{s}"]])
                fw.op("dve", lambda: V.scalar_tensor_tensor(out=ob[s][:], in0=pb[s][:, 2:L + 2], scalar=cwt[:, ci, 2:3], in1=ob[s][:], op0=ALU.mult, op1=ALU.add),
                      reads=[B[f"pb{s}"], B["cwt"], B[f"ob{s}"]], writes=[B[f"ob{s}"]])
                if q == 0:
                    for k4 in range(max(1, NK // 4)):
                        ps_ = k4 % 2
                        nn = min(4, NK)
                        for kq in range(nn):
                            k = k4 * 4 + kq
                            fw.op("pe", lambda k=k, kq=kq: nc.tensor.transpose(out=ptr[ps_][:, kq, :], in_=ob[s][:, k * 128:(k + 1) * 128], identity=idf[:]),
                                  reads=[B[f"ob{s}"], B["idf"]], writes=[B[f"ptr{ps_}"]])
                        fw.op("act", lambda: nc.scalar.copy(out=x_tm[:, st_, k4 * 4:k4 * 4 + nn, :], in_=ptr[ps_][:, 0:nn, :]), reads=[B[f"ptr{ps_}"]], writes=[B["x_tm"]])
                else:
                    fw.dma("sp", x12[st_, q - 1], ob[s][:], reads=[B[f"ob{s}"]], writes=[B["x12"]])
        scA.__exit__(None, None, None)
        slabs = [fw.sb(f"slab{i}", [128, NK, TW], BF16) for i in range(2)]
        Bslab = [B["slab0"], B["slab1"]]
        y_fm = fw.sb("y_fm", [128, 4, 2, NK, 128], BF16)
        Xr = fw.sb("Xr", [128, 4, TW], F32)
        kt = [fw.sb(f"kt{i}", [128, 2, TW], F32) for i in range(2)]
        Y = fw.sb("Y", [128, 2, TW], F32)
        ta = fw.sb("ta", [128, TW], F32)
        tb = fw.sb("tb", [128, TW], F32)
        xq = [fw.sb(f"xq{i}", [128, TW], F32) for i in range(2)]
        zb = [fw.sb(f"zb{i}", [128, TW], F32) for i in range(2)]
        zo = [fw.sb(f"zo{i}", [128, TW], BF16) for i in range(2)]
        cnt = {"kt": 0, "tr": 0, "xq": 0, "z": 0}
        for o in range(2):
            def consume(nt, which, st_, ps):
                fsl = slice(nt * TW, (nt + 1) * TW)
                if which == 0:
                    fw.op("act", lambda: nc.scalar.copy(out=Xr[:, st_, :], in_=ps), reads=[Bpacc[st_]], writes=[B["Xr"]])
                    return
                ks = cnt["kt"] % 2
                cnt["kt"] += 1
                fw.dma("pool", kt[ks][:], Kfi[st_, :, o, :, fsl], writes=[B[f"kt{ks}"]])
                Kr, Ki = kt[ks][:, 0, :], kt[ks][:, 1, :]
                rd = [B["Xr"], B[f"kt{ks}"]]
                fw.op("pool", lambda: G.tensor_tensor(out=ta[:], in0=Xr[:, st_, :], in1=Kr, op=ALU.mult), reads=rd, writes=[B["ta"]])
                fw.op("dve", lambda: V.tensor_tensor(out=tb[:], in0=ps, in1=Ki, op=ALU.mult), reads=[Bpacc[st_], B[f"kt{ks}"]], writes=[B["tb"]])
                fw.op("pool", lambda: G.tensor_tensor(out=Y[:, 0, :], in0=ta[:], in1=tb[:], op=ALU.subtract), reads=[B["ta"], B["tb"]], writes=[B["Y"]])
                fw.op("pool", lambda: G.tensor_tensor(out=ta[:], in0=Xr[:, st_, :], in1=Ki, op=ALU.mult), reads=rd, writes=[B["ta"]])
                fw.op("dve", lambda: V.tensor_tensor(out=tb[:], in0=ps, in1=Kr, op=ALU.mult), reads=[Bpacc[st_], B[f"kt{ks}"]], writes=[B["tb"]])
                fw.op("pool", lambda: G.tensor_tensor(out=Y[:, 1, :], in0=ta[:], in1=tb[:], op=ALU.add), reads=[B["ta"], B["tb"]], writes=[B["Y"]])
                if nt == 0:
                    fw.op("dve", lambda: V.tensor_tensor(out=Y[:, 0, 0:1], in0=Xr[:, st_, 0:1], in1=kt[ks][:, 0, 0:1], op=ALU.mult), reads=rd, writes=[B["Y"]])
                    fw.op("dve", lambda: V.tensor_tensor(out=Y[:, 1, 0:1], in0=ps[:, 0:1], in1=kt[ks][:, 1, 0:1], op=ALU.mult), reads=[Bpacc[st_], B[f"kt{ks}"]], writes=[B["Y"]])
                for ri in range(2):
                    ps_ = cnt["tr"] % 2
                    cnt["tr"] += 1
                    for j in range(TJ):
                        fw.op("pe", lambda j=j: nc.tensor.transpose(out=ptr[ps_][:, j, :], in_=Y[:, ri, j * 128:(j + 1) * 128], identity=idf[:]),
                              reads=[B["Y"], B["idf"]], writes=[B[f"ptr{ps_}"]])
                    fw.op("act", lambda: nc.scalar.copy(out=y_fm[:, st_, ri, nt * TJ:(nt + 1) * TJ, :], in_=ptr[ps_][:, 0:TJ, :]), reads=[B[f"ptr{ps_}"]], writes=[B["y_fm"]])
            emit_fwd_dft(fw, nc, L, x_tm, B["x_tm"], tabC, tabS, slabs, Bslab, pacc, Bpacc, consume)
            for tt in range(NTL):
                tsl = slice(tt * TW, (tt + 1) * TW)
                for ri, tab in ((0, tabC), (1, tabST)):
                    fw.dma("sp" if ri == 0 else "act", slabs[ri][:, :, :], tab[tt], writes=[Bslab[ri]])
                    for st_ in range(4):
                        for k in range(NK):
                            fw.op("pe", lambda k=k: nc.tensor.matmul(pacc[st_][:, :TW], lhsT=y_fm[:, st_, ri, k, :], rhs=slabs[ri][:, k, :],
                                                                      start=(ri == 0 and k == 0), stop=(ri == 1 and k == NK - 1)),
                                  reads=[B["y_fm"], Bslab[ri]], writes=[Bpacc[st_]])
                for st_ in range(4):
                    xs_ = cnt["xq"] % 2
                    cnt["xq"] += 1
                    fw.dma("pool", xq[xs_][:], x12[st_, o, :, tsl], reads=[B["x12"]], writes=[B[f"xq{xs_}"]])
                    zs = cnt["z"] % 2
                    cnt["z"] += 1
                    if o == 0:
                        fw.op("dve", lambda: V.tensor_tensor(out=zb[zs][:], in0=pacc[st_][:, :TW], in1=xq[xs_][:], op=ALU.mult), reads=[Bpacc[st_], B[f"xq{xs_}"]], writes=[B[f"zb{zs}"]])
                        ps_ = cnt["tr"] % 2
                        cnt["tr"] += 1
                        for j in range(TJ):
                            fw.op("pe", lambda j=j: nc.tensor.transpose(out=ptr[ps_][:, j, :], in_=zb[zs][:, j * 128:(j + 1) * 128], identity=idf[:]),
                                  reads=[B[f"zb{zs}"], B["idf"]], writes=[B[f"ptr{ps_}"]])
                        fw.op("act", lambda: nc.scalar.copy(out=x_tm[:, st_, tt * TJ:(tt + 1) * TJ, :], in_=ptr[ps_][:, 0:TJ, :]), reads=[B[f"ptr{ps_}"]], writes=[B["x_tm"]])
                    else:
                        fw.op("dve", lambda: V.tensor_tensor(out=zo[zs][:], in0=pacc[st_][:, :TW], in1=xq[xs_][:], op=ALU.mult), reads=[Bpacc[st_], B[f"xq{xs_}"]], writes=[B[f"zo{zs}"]])
                        fw.dma("sp", zTo[:, st_, tsl], zo[zs][:], reads=[B[f"zo{zs}"]], writes=[B["o"]])
        fw.finish([B["o"]])
    return nc


def _ident_f():
    return np.eye(128, dtype=np.float32)


def _ident_b():
    return np.eye(128, dtype=np.float32).astype(NPBF)


def stage_filters(L, slot, p):
    nc = cached(("F", L), lambda: build_F(L))
    zT, decay = hyena_consts(L)
    tC, tS, _ = dft_tables(L)
    vecs = np.ascontiguousarray(np.stack([p["hy_f_b1"][slot], p["hy_f_b2"][slot], p["hy_f_freq"][slot, 0], p["hy_f_freq"][slot, 1]], axis=-1))
    maps = []
    for core in range(NCORES):
        ch = slice(core * 128, (core + 1) * 128)
        w3 = p["hy_f_w3"][slot].reshape(64, 2, 2, D)[:, :, :, ch].reshape(64, 4, 128)
        maps.append({"zT": zT, "decay": np.ascontiguousarray(decay[ch]), "w1": np.ascontiguousarray(p["hy_f_w1"][slot]),
                     "w2": np.ascontiguousarray(p["hy_f_w2"][slot]), "w3": np.ascontiguousarray(w3), "vecs": vecs,
                     "fbias": np.ascontiguousarray(p["hy_f_bias"][slot][:, ch].T), "identf": _ident_f(), "tabC": tC, "tabS": tS})
    res = run(nc, maps)
    return np.stack([res[c]["Kf"] for c in range(NCORES)])


def stage_hyena(L, slot, xs, mrows, mv, KfAll, p):
    nc = cached(("MH", L), lambda: build_MH(L))
    tC, tS, tST = dft_tables(L)
    sh1, sc1 = mv[:, 0:D], mv[:, D:2 * D]
    maps = []
    for core in range(NCORES):
        b, h = core // 2, core % 2
        r = mrows[b]
        mc = np.stack([sc1[r], sh1[r]], axis=-1).reshape(8, 128, 2).transpose(1, 0, 2)
        cols = np.concatenate([np.arange(q * D + 512 * h + 128 * s, q * D + 512 * h + 128 * s + 128) for s in range(4) for q in range(3)])
        cwm = np.concatenate([p["hy_conv_w"][slot][:, cols], p["hy_conv_b"][slot][None, cols]], axis=0).reshape(4, 12, 128).transpose(2, 1, 0)
        maps.append({"xT": fm_layout(np.ascontiguousarray(xs[b].T)), "mcol": np.ascontiguousarray(mc), "win": fm_layout(p["hy_w_in"][slot][:, cols]),
                     "cw": np.ascontiguousarray(cwm), "Kf": np.ascontiguousarray(KfAll[4 * h:4 * h + 4]), "tabC": tC, "tabS": tS, "tabST": tST,
                     "identf": _ident_f()})
    res = run(nc, maps)
    return [np.concatenate([res[2 * b]["zT"], res[2 * b + 1]["zT"]], axis=1) for b in range(NB)]


def stage_attn(x_lat, x_ctx, mv, p):
    nc = cached(("MA",), build_MA)
    sh1, sc1 = mv[:, 0:D], mv[:, D:2 * D]
    wqkv = p["at_w_qkv"][0]
    gains = np.ascontiguousarray(np.concatenate([np.tile(p["at_q_gain"][0], 8), np.tile(p["at_k_gain"][0], 2)])[None, :])
    cs = rope_tables()
    maps = []
    for core in range(NCORES):
        b, h = core // 2, core % 2
        mc = np.stack([sc1[b], sh1[b], sc1[4], sh1[4]], axis=-1).reshape(8, 128, 4).transpose(1, 0, 2)
        wcat = np.concatenate([wqkv[:, 512 * h:512 * h + 512], wqkv[:, 1024 + 128 * h:1024 + 128 * h + 128],
                               wqkv[:, 1280 + 128 * h:1280 + 128 * h + 128]], axis=1)
        maps.append({"xT": fm_layout(np.ascontiguousarray(x_lat[b].T)), "cxT": fm_layout(np.ascontiguousarray(x_ctx[b].T)),
                     "mcol": np.ascontiguousarray(mc), "w": fm_layout(wcat), "gains": gains, "cs": cs, "identb": _ident_b()})
    res = run(nc, maps)
    return [np.concatenate([res[2 * b]["oT"], res[2 * b + 1]["oT"]], axis=1) for b in range(NB)]


def stage_gmlp(x_lat, mv, p):
    nc = cached(("MG",), lambda: build_MG(16))
    sh1, sc1 = mv[:, 0:D], mv[:, D:2 * D]
    lnr = np.ascontiguousarray(np.concatenate([p["cm_ln_g"][0], p["cm_ln_b"][0]])[None, :])
    wsT = np.ascontiguousarray(p["cm_w_s"][0].transpose(2, 0, 1))
    bsr = np.ascontiguousarray(p["cm_b_s"][0].reshape(1, -1))
    win = fm_layout(p["cm_w_in"][0])
    maps = []
    for core in range(NCORES):
        b, hf = core // 2, core % 2
        mc = np.stack([sc1[b], sh1[b]], axis=-1).reshape(8, 128, 2).transpose(1, 0, 2)
        maps.append({"xT": fm_layout(np.ascontiguousarray(x_lat[b, hf * 2048:(hf + 1) * 2048].T)), "mcol": np.ascontiguousarray(mc), "win": win,
                     "lnr": lnr, "wsT": wsT, "bsr": bsr})
    res = run(nc, maps)
    return [np.concatenate([res[2 * b]["gT"], res[2 * b + 1]["gT"]], axis=2) for b in range(NB)]


def stage_norm_router(aT, w_out, xs, mv, mrows, i, p, T):
    KC = aT[0].shape[1]
    NT = T // 128
    Lseq = xs.shape[1]
    per_b = Lseq // T
    nc = cached(("N", KC, NT), lambda: build_N(KC, NT))
    wo = fm_layout(w_out)
    wr = fm_layout(p["moe_router"][i])
    maps = []
    for core in range(NCORES):
        b, hf = core // per_b, core % per_b
        r = mrows[b]
        rows = np.concatenate([mv[r, 2 * D:3 * D], p["ln_g"][i, 0], p["ln_b"][i, 0], mv[r, 4 * D:5 * D], mv[r, 3 * D:4 * D]])[None, :]
        maps.append({"aT": np.ascontiguousarray(aT[b][:, :, hf * T:(hf + 1) * T]), "wo": wo, "x": np.ascontiguousarray(xs[b, hf * T:(hf + 1) * T]),
                     "rows": np.ascontiguousarray(rows), "wr": wr, "ident": _ident_f()})
    res = run(nc, maps)
    x1 = np.stack([np.concatenate([res[b * per_b + hf]["x1"] for hf in range(per_b)], axis=0) for b in range(NB)])
    h2 = np.concatenate([res[c]["h2"] for c in range(NCORES)], axis=0)
    aff = np.stack([np.concatenate([res[b * per_b + hf]["aff"] for hf in range(per_b)], axis=0) for b in range(NB)])
    return x1, h2, aff


def stage_experts(aff, h2, i, p, CAP, GB):
    Lseq = aff.shape[1]
    NTT = Lseq // 128
    NS = GB * CAP
    nc = cached(("E", NTT, CAP, GB), lambda: build_E(NTT, CAP, GB))
    tokid = (np.arange(NB)[None, :, None] * Lseq + np.arange(NTT)[None, None, :] * 128 + np.arange(128)[:, None, None])
    tok = np.ascontiguousarray(np.stack([tokid // 128, tokid % 128], axis=1).astype(np.float32))
    so = np.zeros((128, NB, 2), np.float32)
    so += ((np.arange(NB) % GB) * CAP).astype(np.float32)[None, :, None]
    so = np.ascontiguousarray(so.reshape(128, 8))
    tri = np.triu(np.ones((128, 128), np.float32), 1).astype(NPBF)
    iota = np.ascontiguousarray(np.broadcast_to(np.arange(NS, dtype=np.float32), (128, NS)))
    maps = []
    for core in range(NCORES):
        e0 = 2 * core
        a = aff[:, :, e0:e0 + 2].reshape(NB, NTT, 128, 2).transpose(2, 0, 3, 1).reshape(128, 8, NTT)
        maps.append({"aff": np.ascontiguousarray(a), "h2": h2, "tok": tok, "slotoff": so, "tri": tri, "iota": iota, "identb": _ident_b(),
                     "wg": np.ascontiguousarray(p["moe_w_gate"][i, e0:e0 + 2].reshape(2, 8, 128, FF).transpose(0, 2, 1, 3)),
                     "wu": np.ascontiguousarray(p["moe_w_up"][i, e0:e0 + 2].reshape(2, 8, 128, FF).transpose(0, 2, 1, 3)),
                     "wd": np.ascontiguousarray(p["moe_w_down"][i, e0:e0 + 2].reshape(2, 16, 128, D).transpose(0, 2, 1, 3))})
    res = run(nc, maps)
    yc = np.concatenate([res[c]["yc"] for c in range(NCORES)], axis=0)
    pt = np.stack([res[c]["postab"] for c in range(NCORES)], axis=0)
    return yc, pt


def stage_combine(x1, yc, pt, mv, mrows, i, p, T, GB):
    Lseq = x1.shape[1]
    NT = T // 128
    per_b = Lseq // T
    NS = yc.shape[2] - 128
    nc = cached(("P", NT, NS), lambda: build_P(NT, NS))
    maps = []
    for core in range(NCORES):
        b, hf = core // per_b, core % per_b
        r = mrows[b]
        rows = np.concatenate([mv[r, 5 * D:6 * D], p["ln_g"][i, 1], p["ln_b"][i, 1]])[None, :]
        ycb = yc[:, b // GB].reshape(16 * (NS + 128), D)
        ptb = pt[:, :, 2 * b:2 * b + 2, hf * NT:(hf + 1) * NT]
        ptb = ptb.transpose(1, 0, 2, 3).reshape(128, 16, NT)
        maps.append({"x1": np.ascontiguousarray(x1[b, hf * T:(hf + 1) * T]), "ycb": np.ascontiguousarray(ycb), "postab": np.ascontiguousarray(ptb),
                     "rows": np.ascontiguousarray(rows)})
    res = run(nc, maps)
    return np.stack([np.concatenate([res[b * per_b + hf]["x2"] for hf in range(per_b)], axis=0) for b in range(NB)])


def moe_block(aT, w_out, xs, mv, mrows, i, p, T, CAP, GB):
    x1, h2, aff = stage_norm_router(aT, w_out, xs, mv, mrows, i, p, T)
    yc, pt = stage_experts(aff, h2, i, p, CAP, GB)
    return stage_combine(x1, yc, pt, mv, mrows, i, p, T, GB)


def kernel(**inputs):
    p = {k: np.asarray(v) for k, v in inputs.items()}
    x_lat = np.ascontiguousarray(p["x"], dtype=np.float32)
    x_ctx = np.ascontiguousarray(p["ctx"], dtype=np.float32)
    modvec = run_A(p["c"], p["c_ctx"], p["mod_w"], p["mod_b"])
    lat_rows = [0, 1, 2, 3]
    ctx_rows = [4, 4, 4, 4]
    mv = modvec[0]
    Kf = stage_filters(SEQ, 0, p)
    aT = stage_hyena(SEQ, 0, x_lat, lat_rows, mv, Kf, p)
    Kfc = stage_filters(CTX, 0, p)
    aTc = stage_hyena(CTX, 0, x_ctx, ctx_rows, mv, Kfc, p)
    x_lat = moe_block(aT, p["hy_w_out"][0], x_lat, mv, lat_rows, 0, p, 2048, 512, 1)
    x_ctx = moe_block(aTc, p["hy_w_out"][0], x_ctx, mv, ctx_rows, 0, p, 128, 32, 4)
    mv = modvec[1]
    aT = stage_attn(x_lat, x_ctx, mv, p)
    x_lat = moe_block(aT, p["at_w_out"][0], x_lat, mv, lat_rows, 1, p, 2048, 512, 1)
    mv = modvec[2]
    aT = stage_gmlp(x_lat, mv, p)
    x_lat = moe_block(aT, p["cm_w_out"][0], x_lat, mv, lat_rows, 2, p, 2048, 512, 1)
    mv = modvec[3]
    Kf = stage_filters(SEQ, 1, p)
    aT = stage_hyena(SEQ, 1, x_lat, lat_rows, mv, Kf, p)
    x_lat = moe_block(aT, p["hy_w_out"][1], x_lat, mv, lat_rows, 3, p, 2048, 512, 1)
    return x_lat.astype(np.float32)
```

```python
import math
from contextlib import ExitStack

import numpy as np
import ml_dtypes
import concourse.bass as bass
import concourse.mybir as mybir
from concourse.bass_utils import run_bass_kernel_spmd

F32 = mybir.dt.float32
BF16 = mybir.dt.bfloat16
I32 = mybir.dt.int32
U32 = mybir.dt.uint32
ALU = mybir.AluOpType
AF = mybir.ActivationFunctionType
AX = mybir.AxisListType
NPBF = ml_dtypes.bfloat16

D = 1024
NB = 4
SEQ = 4096
CTX = 256
DEPTH = 4
NE = 16
FF = 2048
LN_EPS = 1e-5
RMS_EPS = 1e-6
ALPHA = (2 * DEPTH) ** 0.25
NCORES = 8
SAME_ENGINE_SYNC = True


class Buf:
    __slots__ = ("name", "w", "r")

    def __init__(self, name):
        self.name = name
        self.w = None
        self.r = {}


class FW:
    def __init__(self, nc, stack):
        self.nc = nc
        self.stack = stack
        self.engs = {"pe": nc.tensor, "dve": nc.vector, "act": nc.scalar, "pool": nc.gpsimd, "sp": nc.sync}
        self.sems = {}
        self.cnt = {}
        self.seen = {k: {} for k in self.engs}
        for k in self.engs:
            self.sems[k] = stack.enter_context(nc.semaphore("s_" + k))
            self.cnt[k] = 0
        self.same_engine_sync = SAME_ENGINE_SYNC
        self.semstack = stack

    def sb(self, name, shape, dt):
        return self.stack.enter_context(self.nc.sbuf_tensor(name, list(shape), dt))

    def ps(self, name, shape, dt=F32):
        return self.stack.enter_context(self.nc.psum_tensor(name, list(shape), dt))

    def dma_sem(self, name):
        key = "d_" + name
        if key not in self.sems:
            self.sems[key] = self.semstack.enter_context(self.nc.semaphore(key))
            self.cnt[key] = 0
        return key

    def _wait(self, e, key, val):
        if key == e and (e == "pe" or not self.same_engine_sync):
            return
        if self.seen[e].get(key, 0) >= val:
            return
        self.engs[e].wait_ge(self.sems[key], val)
        self.seen[e][key] = val

    def _deps(self, e, reads, writes):
        for b in reads:
            if b.w is not None:
                self._wait(e, *b.w)
        for b in writes:
            if b.w is not None:
                self._wait(e, *b.w)
            for k, v in b.r.items():
                self._wait(e, k, v)

    def op(self, e, fn, reads=(), writes=()):
        self._deps(e, reads, writes)
        ins = fn()
        self.cnt[e] += 1
        ins.then_inc(self.sems[e], 1)
        for b in reads:
            b.r[e] = self.cnt[e]
        for b in writes:
            b.w = (e, self.cnt[e])
            b.r = {}
        return ins

    def dma(self, q, out, in_, reads=(), writes=(), semname=None, indirect=None, **kw):
        self._deps(q, reads, writes)
        name = semname or (writes[0].name if writes else reads[0].name + "_st")
        key = self.dma_sem(name)
        if indirect is None:
            ins = self.engs[q].dma_start(out=out, in_=in_, **kw)
        else:
            ins = self.nc.gpsimd.indirect_dma_start(out=out, in_=in_, **indirect)
        self.cnt[key] += 16
        ins.then_inc(self.sems[key], 16)
        for b in reads:
            b.r[key] = self.cnt[key]
        for b in writes:
            b.w = (key, self.cnt[key])
            b.r = {}
        return ins

    def barrier(self):
        for e in self.engs:
            for key, c in self.cnt.items():
                if key != e and c > 0:
                    self._wait(e, key, c)

    def scoped(self):
        fw = self

        class _Scope:
            def __enter__(self_):
                self_.prev = fw.stack
                self_.st = ExitStack()
                self_.st.__enter__()
                fw.stack = self_.st
                return fw

            def __exit__(self_, *a):
                fw.barrier()
                fw.stack = self_.prev
                return self_.st.__exit__(*a)
        return _Scope()

    def seal(self, bufs):
        key = bufs[0].w[0]
        for b in bufs:
            b.w = (key, self.cnt[key])

    def finish(self, bufs, e="sp"):
        for b in bufs:
            if b.w is not None:
                self._wait(e, *b.w)


def new_nc():
    return bass.Bass("TRN2", target_bir_lowering=False)


def dram_in(nc, name, shape, dt=F32):
    return nc.dram_tensor(name, list(shape), dt, kind="ExternalInput").ap()


def dram_out(nc, name, shape, dt=F32):
    return nc.dram_tensor(name, list(shape), dt, kind="ExternalOutput").ap()


_PROFILE = []


def run(nc, in_maps, tag=""):
    res = run_bass_kernel_spmd(nc, in_maps, core_ids=list(range(NCORES)))
    if getattr(res, "exec_time_ns", None):
        _PROFILE.append((tag, res.exec_time_ns))
    return res.results


def build_A():
    nc = new_nc()
    cT = dram_in(nc, "cT", [128, 8, 5])
    w = dram_in(nc, "w", [128, 8, 3072])
    b = dram_in(nc, "b", [1, 3072])
    m = dram_out(nc, "m", [5, 3072])
    with ExitStack() as st:
        fw = FW(nc, st)
        ct = fw.sb("ct", [128, 8, 5], F32)
        bt = fw.sb("bt", [5, 3072], F32)
        mt = fw.sb("mt", [5, 3072], F32)
        wt = [fw.sb(f"wt{i}", [128, 8, 512], F32) for i in range(2)]
        pt = [fw.ps(f"pt{i}", [5, 512]) for i in range(2)]
        Bc, Bb, Bm, Bo = Buf("ct"), Buf("bt"), Buf("mt"), Buf("mo")
        Bw = [Buf("wt0"), Buf("wt1")]
        Bp = [Buf("pt0"), Buf("pt1")]
        fw.dma("sp", ct[:], cT, writes=[Bc])
        fw.dma("sp", bt[:], b.partition_broadcast(5), writes=[Bb])
        fw.op("act", lambda: nc.scalar.activation(out=ct[:], in_=ct[:], func=AF.Silu), reads=[Bc], writes=[Bc])
        for j in range(6):
            s = j % 2
            fw.dma("sp" if s == 0 else "pool", wt[s][:], w[:, :, j * 512:(j + 1) * 512], writes=[Bw[s]])
            for k in range(8):
                fw.op("pe", lambda k=k: nc.tensor.matmul(pt[s][:], lhsT=ct[:, k, :], rhs=wt[s][:, k, :],
                                                           start=(k == 0), stop=(k == 7)),
                      reads=[Bc, Bw[s]], writes=[Bp[s]])
            fw.op("dve", lambda: nc.vector.tensor_tensor(out=mt[:, j * 512:(j + 1) * 512], in0=pt[s][:],
                                                         in1=bt[:, j * 512:(j + 1) * 512], op=ALU.add),
                  reads=[Bp[s], Bb], writes=[Bm])
        fw.dma("sp", m, mt[:], reads=[Bm], writes=[Bo])
        fw.finish([Bo])
    return nc


def run_A(c, c_ctx, mod_w, mod_b):
    nc = build_A()
    cc = np.concatenate([c, c_ctx[None, :]], axis=0)
    cT = np.ascontiguousarray(cc.T.reshape(8, 128, 5).transpose(1, 0, 2))
    maps = []
    for core in range(NCORES):
        i, hf = core // 2, core % 2
        wv = mod_w[i][:, hf * 3072:(hf + 1) * 3072].reshape(8, 128, 3072).transpose(1, 0, 2)
        maps.append({"cT": cT, "w": np.ascontiguousarray(wv),
                     "b": np.ascontiguousarray(mod_b[i][None, hf * 3072:(hf + 1) * 3072])})
    res = run(nc, maps)
    out = np.zeros((DEPTH, 5, 6 * D), np.float32)
    for core in range(NCORES):
        i, hf = core // 2, core % 2
        out[i][:, hf * 3072:(hf + 1) * 3072] = res[core]["m"]
    return out


def layer_norm_tile(fw, nc, u, Bu, stats, mv, rstd, Bs, nparts=128):
    for j in range(2):
        fw.op("dve", lambda j=j: nc.vector.bn_stats(out=stats[:, j, :], in_=u[:, j * 512:(j + 1) * 512]),
              reads=[Bu], writes=[Bs])
    fw.op("dve", lambda: nc.vector.bn_aggr(out=mv[:], in_=stats[:].rearrange("p a b -> p (a b)")), reads=[Bs], writes=[Bs])
    fw.op("act", lambda: nc.scalar.activation(out=rstd[:], in_=mv[:, 1:2], func=AF.Sqrt, bias=fw.eps_ln[:, 0:1], scale=1.0),
          reads=[Bs, fw.Bconst], writes=[Bs])
    fw.op("dve", lambda: nc.vector.reciprocal(out=rstd[:], in_=rstd[:]), reads=[Bs], writes=[Bs])
    fw.op("dve", lambda: nc.vector.tensor_scalar(out=u[:], in0=u[:], scalar1=mv[:, 0:1], scalar2=rstd[:, 0:1],
                                                 op0=ALU.subtract, op1=ALU.mult), reads=[Bs, Bu], writes=[Bu])


def make_consts(fw, nc):
    fw.eps_ln = fw.sb("eps_ln", [128, 1], F32)
    fw.Bconst = Buf("consts")
    fw.op("pool", lambda: nc.gpsimd.memset(fw.eps_ln[:], LN_EPS), writes=[fw.Bconst])


def build_N(KC, NT):
    T = NT * 128
    nc = new_nc()
    aT = dram_in(nc, "aT", [128, KC, T], BF16)
    wo = dram_in(nc, "wo", [128, KC, 1024])
    x = dram_in(nc, "x", [T, 1024])
    rows = dram_in(nc, "rows", [1, 5 * 1024])
    wr = dram_in(nc, "wr", [128, 8, 16])
    ident = dram_in(nc, "ident", [128, 128])
    x1o = dram_out(nc, "x1", [T, 1024])
    h2o = dram_out(nc, "h2", [T, 1024], BF16)
    affo = dram_out(nc, "aff", [T, 16])
    with ExitStack() as st:
        fw = FW(nc, st)
        make_consts(fw, nc)
        wob = fw.sb("wob", [128, KC, 1024], BF16)
        wst = [fw.sb(f"wst{i}", [128, 1024], F32) for i in range(2)]
        rw = fw.sb("rw", [128, 5, 1024], F32)
        wrt = fw.sb("wrt", [128, 8, 16], F32)
        idt = fw.sb("idt", [128, 128], F32)
        at = [fw.sb(f"at{i}", [128, KC, 128], BF16) for i in range(2)]
        xt = [fw.sb(f"xt{i}", [128, 1024], F32) for i in range(2)]
        u = [fw.sb(f"u{i}", [128, 1024], F32) for i in range(2)]
        h2 = [fw.sb(f"h2{i}", [128, 1024], F32) for i in range(2)]
        h2b = [fw.sb(f"h2b{i}", [128, 1024], BF16) for i in range(2)]
        h2T = fw.sb("h2T", [128, 8, 128], F32)
        stats = fw.sb("stats", [128, 2, 6], F32)
        mv = fw.sb("mv", [128, 2], F32)
        rstd = fw.sb("rstd", [128, 1], F32)
        sm = fw.sb("sm", [128, 4], F32)
        aft = [fw.sb(f"aft{i}", [128, 16], F32) for i in range(2)]
        py = [fw.ps(f"py{i}", [128, 512]) for i in range(2)]
        ptr = [fw.ps(f"ptr{i}", [128, 4, 128]) for i in range(2)]
        pl = fw.ps("pl", [128, 16])
        Bwo, Brw, Bwr, Bid = Buf("wob"), Buf("rw"), Buf("wrt"), Buf("idt")
        Bwst = [Buf("wst0"), Buf("wst1")]
        Bat = [Buf("at0"), Buf("at1")]
        Bxt = [Buf("xt0"), Buf("xt1")]
        Bu = [Buf("u0"), Buf("u1")]
        Bh2 = [Buf("h20"), Buf("h21")]
        Bh2b = [Buf("h2b0"), Buf("h2b1")]
        Bh2T, Bs, Bsm = Buf("h2T"), Buf("stats"), Buf("sm")
        Baf = [Buf("aft0"), Buf("aft1")]
        Bpy = [Buf("py0"), Buf("py1")]
        Bptr = [Buf("ptr0"), Buf("ptr1")]
        Bpl = Buf("pl")
        Bout = [Buf("o_x1"), Buf("o_h2"), Buf("o_aff")]
        fw.dma("sp", rw[:].rearrange("p a b -> p (a b)"), rows.partition_broadcast(128), writes=[Brw])
        fw.dma("sp", wrt[:], wr, writes=[Bwr])
        fw.dma("sp", idt[:], ident, writes=[Bid])
        for kc in range(KC):
            s = kc % 2
            fw.dma("sp" if s == 0 else "pool", wst[s][:], wo[:, kc, :], writes=[Bwst[s]])
            fw.op("act" if s == 0 else "pool",
                  (lambda: nc.scalar.copy(out=wob[:, kc, :], in_=wst[s][:])) if s == 0 else
                  (lambda: nc.gpsimd.tensor_copy(out=wob[:, kc, :], in_=wst[s][:])),
                  reads=[Bwst[s]], writes=[Bwo])
        fw.op("dve", lambda: nc.vector.tensor_scalar(out=rw[:, 3, :], in0=rw[:, 3, :], scalar1=1.0, scalar2=None, op0=ALU.add),
              reads=[Brw], writes=[Brw])
        for t in range(NT):
            s = t % 2
            fw.dma("sp", at[s][:], aT[:, :, t * 128:(t + 1) * 128], writes=[Bat[s]])
            fw.dma("pool", xt[s][:], x[t * 128:(t + 1) * 128, :], writes=[Bxt[s]])
            for hf in range(2):
                for kc in range(KC):
                    fw.op("pe", lambda kc=kc, hf=hf: nc.tensor.matmul(py[hf][:], lhsT=at[s][:, kc, :],
                                                                       rhs=wob[:, kc, hf * 512:(hf + 1) * 512],
                                                                       start=(kc == 0), stop=(kc == KC - 1)),
                          reads=[Bat[s], Bwo], writes=[Bpy[hf]])
            for hf in range(2):
                sl = slice(hf * 512, (hf + 1) * 512)
                fw.op("dve", lambda: nc.vector.tensor_tensor(out=u[s][:, sl], in0=py[hf][:], in1=rw[:, 0, sl], op=ALU.mult),
                      reads=[Bpy[hf], Brw], writes=[Bu[s]])
            fw.op("dve", lambda: nc.vector.scalar_tensor_tensor(out=u[s][:], in0=xt[s][:], scalar=ALPHA, in1=u[s][:],
                                                                op0=ALU.mult, op1=ALU.add),
                  reads=[Bxt[s], Bu[s]], writes=[Bu[s]])
            layer_norm_tile(fw, nc, u[s], Bu[s], stats, mv, rstd, Bs)
            fw.op("pool", lambda: nc.gpsimd.tensor_tensor(out=u[s][:], in0=u[s][:], in1=rw[:, 1, :], op=ALU.mult),
                  reads=[Bu[s], Brw], writes=[Bu[s]])
            fw.op("pool", lambda: nc.gpsimd.tensor_tensor(out=u[s][:], in0=u[s][:], in1=rw[:, 2, :], op=ALU.add),
                  reads=[Bu[s], Brw], writes=[Bu[s]])
            fw.dma("sp", x1o[t * 128:(t + 1) * 128, :], u[s][:], reads=[Bu[s]], writes=[Bout[0]])
            fw.op("dve", lambda: nc.vector.tensor_tensor(out=h2[s][:], in0=u[s][:], in1=rw[:, 3, :], op=ALU.mult),
                  reads=[Bu[s], Brw], writes=[Bh2[s]])
            fw.op("dve", lambda: nc.vector.tensor_tensor(out=h2[s][:], in0=h2[s][:], in1=rw[:, 4, :], op=ALU.add),
                  reads=[Bh2[s], Brw], writes=[Bh2[s]])
            fw.op("act", lambda: nc.scalar.copy(out=h2b[s][:], in_=h2[s][:]), reads=[Bh2[s]], writes=[Bh2b[s]])
            fw.dma("sp", h2o[t * 128:(t + 1) * 128, :], h2b[s][:], reads=[Bh2b[s]], writes=[Bout[1]])
            for g in range(2):
                for k4 in range(4):
                    k = g * 4 + k4
                    fw.op("pe", lambda k=k, k4=k4: nc.tensor.transpose(out=ptr[g][:, k4, :], in_=h2[s][:, k * 128:(k + 1) * 128],
                                                                        identity=idt[:]),
                          reads=[Bh2[s], Bid], writes=[Bptr[g]])
                fw.op("act", lambda: nc.scalar.copy(out=h2T[:, g * 4:(g + 1) * 4, :], in_=ptr[g][:]), reads=[Bptr[g]], writes=[Bh2T])
            for k in range(8):
                fw.op("pe", lambda k=k: nc.tensor.matmul(pl[:], lhsT=h2T[:, k, :], rhs=wrt[:, k, :], start=(k == 0), stop=(k == 7)),
                      reads=[Bh2T, Bwr], writes=[Bpl])
            fw.op("dve", lambda: nc.vector.reduce_max(out=sm[:, 0:1], in_=pl[:], axis=AX.X), reads=[Bpl], writes=[Bsm])
            fw.op("dve", lambda: nc.vector.tensor_scalar(out=sm[:, 1:2], in0=sm[:, 0:1], scalar1=-1.0, scalar2=None, op0=ALU.mult),
                  reads=[Bsm], writes=[Bsm])
            fw.op("act", lambda: nc.scalar.activation(out=aft[s][:], in_=pl[:], func=AF.Exp, bias=sm[:, 1:2], scale=1.0,
                                                      accum_out=sm[:, 2:3]), reads=[Bpl, Bsm], writes=[Baf[s], Bsm])
            fw.op("dve", lambda: nc.vector.reciprocal(out=sm[:, 3:4], in_=sm[:, 2:3]), reads=[Bsm], writes=[Bsm])
            fw.op("dve", lambda: nc.vector.tensor_scalar(out=aft[s][:], in0=aft[s][:], scalar1=sm[:, 3:4], scalar2=None, op0=ALU.mult),
                  reads=[Bsm, Baf[s]], writes=[Baf[s]])
            fw.dma("sp", affo[t * 128:(t + 1) * 128, :], aft[s][:], reads=[Baf[s]], writes=[Bout[2]])
        fw.finish(Bout)
    return nc


def fm_layout(a2d):
    K, T = a2d.shape
    return np.ascontiguousarray(a2d.reshape(K // 128, 128, T).transpose(1, 0, 2))


_NC_CACHE = {}


def cached(key, builder):
    if key not in _NC_CACHE:
        _NC_CACHE[key] = builder()
    return _NC_CACHE[key]


def build_E(NTT, CAP, GB, NITER=30):
    NG = NB // GB
    NS = GB * CAP
    NCH = NS // 128
    NC8 = 8 * NTT
    nc = new_nc()
    aff = dram_in(nc, "aff", [128, 8, NTT])
    h2 = dram_in(nc, "h2", [NB * NTT * 128, 1024], BF16)
    tok = dram_in(nc, "tok", [128, 2, NB, NTT])
    slotoff = dram_in(nc, "slotoff", [128, 8])
    tri = dram_in(nc, "tri", [128, 128], BF16)
    iota = dram_in(nc, "iota", [128, NS])
    identb = dram_in(nc, "identb", [128, 128], BF16)
    wg = dram_in(nc, "wg", [2, 128, 8, 2048])
    wu = dram_in(nc, "wu", [2, 128, 8, 2048])
    wd = dram_in(nc, "wd", [2, 128, 16, 1024])
    yc = dram_out(nc, "yc", [2, NG, NS + 128, 1024], BF16)
    BIGPOS = float(NS)
    postab = dram_out(nc, "postab", [128, 8, NTT], I32)
    with ExitStack() as st:
        fw = FW(nc, st)
        A = fw.sb("A", [128, 8, NTT], F32)
        tokt = fw.sb("tokt", [128, 2, NB, NTT], F32)
        sofft = fw.sb("sofft", [128, 8], F32)
        trit = fw.sb("trit", [128, 128], BF16)
        onesb = fw.sb("onesb", [128, 128], BF16)
        iot = fw.sb("iot", [128, NS], F32)
        idb = fw.sb("idb", [128, 128], BF16)
        lo = fw.sb("lo", [128, 8], F32)
        hi = fw.sb("hi", [128, 8], F32)
        mid = fw.sb("mid", [128, 8], F32)
        cnt = fw.sb("cnt", [128, 8], F32)
        ge = fw.sb("ge", [128, 8], F32)
        tmp8 = fw.sb("tmp8", [128, 8], F32)
        cmpb = fw.sb("cmpb", [128, 8, NTT], BF16)
        maskf = fw.sb("maskf", [128, 8, NTT], F32)
        pos = fw.sb("pos", [128, 8, NTT], F32)
        off = fw.sb("off", [128, 8, NTT], F32)
        tot = fw.sb("tot", [128, 8, NTT], F32)
        posi = fw.sb("posi", [128, 8, NTT], I32)
        vals = fw.sb("vals", [128, 8, NTT, 5], BF16)
        gres = fw.sb("gres", [128, 8, NTT], F32)
        gpc = fw.sb("gpc", [128, 8, NTT], F32)
        NSTEP = GB * NTT
        ohall = fw.sb("ohall", [128, NSTEP, NS], BF16)
        idxf = fw.sb("idxf", [128, NCH, 5], F32)
        gate = [fw.sb(f"gate{i}", [128, NCH], F32) for i in range(2)]
        idf = fw.sb("idf", [128, NCH], F32)
        idxu = fw.sb("idxu", [128, NCH], I32)
        xg = [fw.sb(f"xg{i}", [128, 1024], BF16) for i in range(2)]
        xgT = [fw.sb(f"xgT{i}", [128, 8, NS], BF16) for i in range(2)]
        wgb = fw.sb("wgb", [128, 8, 2048], BF16)
        wub = fw.sb("wub", [128, 8, 2048], BF16)
        wdb = fw.sb("wdb", [128, 16, 1024], BF16)
        wst = [fw.sb(f"wst{i}", [128, 2048], F32) for i in range(2)]
        sg = [fw.sb(f"sg{i}", [128, NS], F32) for i in range(2)]
        hT = fw.sb("hT", [128, 16, NS], BF16)
        ysb = [fw.sb(f"ysb{i}", [128, 1024], BF16) for i in range(2)]
        pbank = [fw.ps(f"pb{i}", [128, 512]) for i in range(7)]
        ptrb = fw.ps("ptrb", [128, 4, 128], BF16)
        pcnt, pidx, pg, pu, py0, py1, ppos = pbank
        B = {n: Buf(n) for n in ["A", "tokt", "sofft", "trit", "onesb", "iot", "idb", "lo", "hi", "mid", "cnt", "ge", "tmp8",
                                 "cmpb", "maskf", "pos", "off", "tot", "posi", "vals", "gres", "ohall", "idxf", "idxu", "xg0", "xg1",
                                 "xgT0", "xgT1", "gate0", "gate1", "wgb", "wub", "wdb", "wst0", "wst1", "sg0", "sg1", "hT", "ysb0", "ysb1",
                                 "pcnt", "pidx", "pg", "pu", "py0", "py1", "ppos", "ptrb", "o_yc", "o_pos"]}
        V = nc.vector
        fw.dma("sp", A[:], aff, writes=[B["A"]])
        fw.dma("sp", tokt[:], tok, writes=[B["tokt"]])
        fw.dma("sp", sofft[:], slotoff, writes=[B["sofft"]])
        fw.dma("sp", trit[:], tri, writes=[B["trit"]])
        fw.dma("sp", iot[:], iota, writes=[B["iot"]])
        fw.dma("sp", idb[:], identb, writes=[B["idb"]])
        fw.op("pool", lambda: nc.gpsimd.memset(onesb[:], 1.0), writes=[B["onesb"]])
        zt = fw.sb("zt", [128, 1024], BF16)
        B["zt"] = Buf("zt")
        fw.op("pool", lambda: nc.gpsimd.memset(zt[:], 0.0), writes=[B["zt"]])
        for el in range(2):
            for g in range(NG):
                fw.dma("sp", yc[el, g, NS:NS + 128, :], zt[:], reads=[B["zt"]], writes=[B["o_yc"]])
        fw.op("pool", lambda: nc.gpsimd.memset(lo[:], 0.0), writes=[B["lo"]])
        fw.op("pool", lambda: nc.gpsimd.memset(hi[:], 1.0), writes=[B["hi"]])

        ld = [0]

        def load_w(dst, Bdst, src_fn, nk, width):
            for k in range(nk):
                s = ld[0] % 2
                ld[0] += 1
                fw.dma("sp" if s == 0 else "act", wst[s][:, :width], src_fn(k), writes=[B[f"wst{s}"]])
                if s == 0:
                    fw.op("act", lambda k=k: nc.scalar.copy(out=dst[:, k, :], in_=wst[s][:, :width]), reads=[B[f"wst{s}"]], writes=[Bdst])
                else:
                    fw.op("pool", lambda k=k: nc.gpsimd.tensor_copy(out=dst[:, k, :], in_=wst[s][:, :width]), reads=[B[f"wst{s}"]], writes=[Bdst])

        def load_all_w(el):
            load_w(wgb, B["wgb"], lambda k: wg[el, :, k, :], 8, 2048)
            load_w(wub, B["wub"], lambda k: wu[el, :, k, :], 8, 2048)
            load_w(wdb, B["wdb"], lambda k: wd[el, :, k, :], 16, 1024)

        load_all_w(0)

        def bc(t8):
            return t8[:, :].unsqueeze(2).to_broadcast([128, 8, NTT])

        def count_ge(thr, Bthr, want_mask_f32=False):
            fw.op("dve", lambda: V.tensor_tensor(out=cmpb[:], in0=A[:], in1=bc(thr), op=ALU.is_ge),
                  reads=[B["A"], Bthr], writes=[B["cmpb"]])
            fw.op("pe", lambda: nc.tensor.matmul(pcnt[:, :NC8], lhsT=onesb[:], rhs=cmpb[:].rearrange("p a b -> p (a b)"),
                                                 start=True, stop=True), reads=[B["onesb"], B["cmpb"]], writes=[B["pcnt"]])

        for it in range(NITER):
            fw.op("dve", lambda: V.tensor_tensor(out=mid[:], in0=lo[:], in1=hi[:], op=ALU.add), reads=[B["lo"], B["hi"]], writes=[B["mid"]])
            fw.op("dve", lambda: V.tensor_scalar(out=mid[:], in0=mid[:], scalar1=0.5, scalar2=None, op0=ALU.mult),
                  reads=[B["mid"]], writes=[B["mid"]])
            count_ge(mid, B["mid"])
            fw.op("dve", lambda: V.tensor_reduce(out=cnt[:], in_=pcnt[:, :NC8].rearrange("p (a b) -> p a b", b=NTT), axis=AX.X, op=ALU.add),
                  reads=[B["pcnt"]], writes=[B["cnt"]])
            fw.op("dve", lambda: V.tensor_scalar(out=ge[:], in0=cnt[:], scalar1=float(CAP) - 0.5, scalar2=None, op0=ALU.is_ge),
                  reads=[B["cnt"]], writes=[B["ge"]])
            fw.op("dve", lambda: V.tensor_tensor(out=tmp8[:], in0=ge[:], in1=mid[:], op=ALU.mult), reads=[B["ge"], B["mid"]], writes=[B["tmp8"]])
            fw.op("dve", lambda: V.tensor_tensor(out=lo[:], in0=lo[:], in1=tmp8[:], op=ALU.max), reads=[B["tmp8"], B["lo"]], writes=[B["lo"]])
            fw.op("dve", lambda: V.scalar_tensor_tensor(out=tmp8[:], in0=ge[:], scalar=4.0, in1=mid[:], op0=ALU.mult, op1=ALU.add),
                  reads=[B["ge"], B["mid"]], writes=[B["tmp8"]])
            fw.op("dve", lambda: V.tensor_tensor(out=hi[:], in0=hi[:], in1=tmp8[:], op=ALU.min), reads=[B["tmp8"], B["hi"]], writes=[B["hi"]])
        count_ge(lo, B["lo"])
        fw.op("act", lambda: nc.scalar.copy(out=tot[:].rearrange("p a b -> p (a b)"), in_=pcnt[:, :NC8]), reads=[B["pcnt"]], writes=[B["tot"]])
        fw.op("dve", lambda: V.tensor_copy(out=maskf[:], in_=cmpb[:]), reads=[B["cmpb"]], writes=[B["maskf"]])
        fw.op("pe", lambda: nc.tensor.matmul(ppos[:, :NC8], lhsT=trit[:], rhs=cmpb[:].rearrange("p a b -> p (a b)"), start=True, stop=True),
              reads=[B["trit"], B["cmpb"]], writes=[B["ppos"]])
        fw.op("dve", lambda: V.tensor_copy(out=off[:, :, 0], in_=sofft[:]), reads=[B["sofft"]], writes=[B["off"]])
        for j in range(1, NTT):
            fw.op("dve", lambda j=j: V.tensor_tensor(out=off[:, :, j], in0=off[:, :, j - 1], in1=tot[:, :, j - 1], op=ALU.add),
                  reads=[B["off"], B["tot"]], writes=[B["off"]])
        fw.op("dve", lambda: V.tensor_tensor(out=pos[:].rearrange("p a b -> p (a b)"), in0=ppos[:, :NC8],
                                             in1=off[:].rearrange("p a b -> p (a b)"), op=ALU.add),
              reads=[B["ppos"], B["off"]], writes=[B["pos"]])
        fw.op("dve", lambda: V.scalar_tensor_tensor(out=pos[:], in0=pos[:], scalar=-BIGPOS, in1=maskf[:], op0=ALU.add, op1=ALU.mult),
              reads=[B["pos"], B["maskf"]], writes=[B["pos"]])
        fw.op("dve", lambda: V.tensor_scalar(out=pos[:], in0=pos[:], scalar1=BIGPOS, scalar2=None, op0=ALU.add),
              reads=[B["pos"]], writes=[B["pos"]])
        fw.op("dve", lambda: V.tensor_copy(out=posi[:], in_=pos[:]), reads=[B["pos"]], writes=[B["posi"]])
        fw.dma("sp", postab, posi[:], reads=[B["posi"]], writes=[B["o_pos"]])
        for b in range(NB):
            for el in range(2):
                for h in range(2):
                    fw.op("pool", lambda b=b, el=el, h=h: nc.gpsimd.tensor_copy(out=vals[:, b * 2 + el, :, h], in_=tokt[:, h, b, :]),
                          reads=[B["tokt"]], writes=[B["vals"]])
        fw.op("dve", lambda: V.tensor_copy(out=gres[:], in_=A[:]), reads=[B["A"]], writes=[B["gres"]])
        for q in range(3):
            fw.op("dve", lambda q=q: V.tensor_copy(out=vals[:, :, :, 2 + q], in_=gres[:]), reads=[B["gres"]], writes=[B["vals"]])
            if q < 2:
                fw.op("dve", lambda q=q: V.tensor_copy(out=gpc[:], in_=vals[:, :, :, 2 + q]), reads=[B["vals"]], writes=[B["gres"]])
                fw.op("dve", lambda: V.tensor_tensor(out=gres[:], in0=gres[:], in1=gpc[:], op=ALU.subtract), reads=[B["gres"]], writes=[B["gres"]])

        ycnt = [0]
        units = [(el, g) for el in range(2) for g in range(NG)]

        def prep(ui):
            el, g = units[ui]
            ub = ui % 2
            steps = [(b, j) for b in range(g * GB, (g + 1) * GB) for j in range(NTT)]
            for si, (b, j) in enumerate(steps):
                col = b * 2 + el
                fw.op("dve", lambda: V.tensor_scalar(out=ohall[:, si, :], in0=iot[:], scalar1=pos[:, col, j:j + 1], scalar2=None, op0=ALU.is_equal),
                      reads=[B["iot"], B["pos"]], writes=[B["ohall"]])
            for c in range(NCH):
                for si, (b, j) in enumerate(steps):
                    col = b * 2 + el
                    fw.op("pe", lambda: nc.tensor.matmul(pidx[:, c * 8:c * 8 + 5], lhsT=ohall[:, si, c * 128:(c + 1) * 128],
                                                         rhs=vals[:, col, j, :], start=(si == 0), stop=(si == len(steps) - 1)),
                          reads=[B["ohall"], B["vals"]], writes=[B["pidx"]])
            fw.op("dve", lambda: V.tensor_copy(out=idxf[:], in_=pidx[:, :NCH * 8].rearrange("p (a b) -> p a b", b=8)[:, :, 0:5]),
                  reads=[B["pidx"]], writes=[B["idxf"]])
            fw.op("dve", lambda: V.scalar_tensor_tensor(out=idf[:], in0=idxf[:, :, 0], scalar=128.0, in1=idxf[:, :, 1], op0=ALU.mult, op1=ALU.add),
                  reads=[B["idxf"]], writes=[B["idxf"]])
            fw.op("dve", lambda: V.tensor_copy(out=idxu[:], in_=idf[:]), reads=[B["idxf"]], writes=[B["idxu"]])
            fw.op("dve", lambda: V.tensor_tensor(out=gate[ub][:], in0=idxf[:, :, 2], in1=idxf[:, :, 3], op=ALU.add), reads=[B["idxf"]], writes=[B[f"gate{ub}"]])
            fw.op("dve", lambda: V.tensor_tensor(out=gate[ub][:], in0=gate[ub][:], in1=idxf[:, :, 4], op=ALU.add), reads=[B["idxf"], B[f"gate{ub}"]], writes=[B[f"gate{ub}"]])
            for c in range(NCH):
                s = c % 2
                fw.dma("pool", xg[s][:], h2, reads=[B["idxu"]], writes=[B[f"xg{s}"]],
                       indirect=dict(out_offset=None, in_offset=bass.IndirectOffsetOnAxis(ap=idxu[:, c:c + 1], axis=0)))
                for k4 in range(2):
                    for kk in range(4):
                        k = k4 * 4 + kk
                        fw.op("pe", lambda k=k, kk=kk: nc.tensor.transpose(out=ptrb[:, kk, :], in_=xg[s][:, k * 128:(k + 1) * 128], identity=idb[:]),
                              reads=[B[f"xg{s}"], B["idb"]], writes=[B["ptrb"]])
                    fw.op("act", lambda k4=k4: nc.scalar.copy(out=xgT[ub][:, k4 * 4:(k4 + 1) * 4, c * 128:(c + 1) * 128], in_=ptrb[:]),
                          reads=[B["ptrb"]], writes=[B[f"xgT{ub}"]])

        def ffn(ui):
            el, g = units[ui]
            ub = ui % 2
            for ft in range(16):
                s = ft % 2
                pgx, pgn = (pg, "pg") if s == 0 else (pcnt, "pcnt")
                pux, pun = (pu, "pu") if s == 0 else (ppos, "ppos")
                for k in range(8):
                    fw.op("pe", lambda k=k: nc.tensor.matmul(pgx[:, :NS], lhsT=wgb[:, k, ft * 128:(ft + 1) * 128], rhs=xgT[ub][:, k, :],
                                                              start=(k == 0), stop=(k == 7)), reads=[B["wgb"], B[f"xgT{ub}"]], writes=[B[pgn]])
                for k in range(8):
                    fw.op("pe", lambda k=k: nc.tensor.matmul(pux[:, :NS], lhsT=wub[:, k, ft * 128:(ft + 1) * 128], rhs=xgT[ub][:, k, :],
                                                              start=(k == 0), stop=(k == 7)), reads=[B["wub"], B[f"xgT{ub}"]], writes=[B[pun]])
                fw.op("act", lambda: nc.scalar.activation(out=sg[s][:], in_=pgx[:, :NS], func=AF.Silu), reads=[B[pgn]], writes=[B[f"sg{s}"]])
                fw.op("dve", lambda: V.tensor_tensor(out=hT[:, ft, :], in0=sg[s][:], in1=pux[:, :NS], op=ALU.mult),
                      reads=[B[f"sg{s}"], B[pun]], writes=[B["hT"]])
            for c in range(NCH):
                s = ycnt[0] % 2
                ycnt[0] += 1
                for hf, (py, nm) in enumerate(((py0, "py0"), (py1, "py1"))):
                    for ft in range(16):
                        fw.op("pe", lambda ft=ft: nc.tensor.matmul(py[:], lhsT=hT[:, ft, c * 128:(c + 1) * 128],
                                                                    rhs=wdb[:, ft, hf * 512:(hf + 1) * 512], start=(ft == 0), stop=(ft == 15)),
                              reads=[B["hT"], B["wdb"]], writes=[B[nm]])
                    if hf == 0:
                        fw.op("dve", lambda: V.tensor_scalar(out=ysb[s][:, 0:512], in0=py[:], scalar1=gate[ub][:, c:c + 1], scalar2=None, op0=ALU.mult),
                              reads=[B[nm], B[f"gate{ub}"]], writes=[B[f"ysb{s}"]])
                    else:
                        fw.op("act", lambda: nc.scalar.activation(out=ysb[s][:, 512:1024], in_=py[:], func=AF.Copy, scale=gate[ub][:, c:c + 1]),
                              reads=[B[nm], B[f"gate{ub}"]], writes=[B[f"ysb{s}"]])
                fw.dma("sp", yc[el, g, c * 128:(c + 1) * 128, :], ysb[s][:], reads=[B[f"ysb{s}"]], writes=[B["o_yc"]])

        prep(0)
        for ui in range(len(units)):
            if ui + 1 < len(units):
                prep(ui + 1)
            if ui > 0 and units[ui][0] != units[ui - 1][0]:
                load_all_w(units[ui][0])
            ffn(ui)
        fw.finish([B["o_yc"], B["o_pos"]])
    return nc


def build_P(NT, NS):
    T = NT * 128
    RS = NS + 128
    nc = new_nc()
    x1 = dram_in(nc, "x1", [T, 1024])
    ycb = dram_in(nc, "ycb", [16 * RS, 1024], BF16)
    postab = dram_in(nc, "postab", [128, 16, NT], I32)
    rows = dram_in(nc, "rows", [1, 3 * 1024])
    x2 = dram_out(nc, "x2", [T, 1024])
    with ExitStack() as st:
        fw = FW(nc, st)
        make_consts(fw, nc)
        pt = fw.sb("pt", [128, 16, NT], I32)
        rw = fw.sb("rw", [128, 3, 1024], F32)
        xt = [fw.sb(f"xt{i}", [128, 1024], F32) for i in range(2)]
        acc = [fw.sb(f"acc{i}", [128, 1024], F32) for i in range(2)]
        gb = [fw.sb(f"gb{i}", [128, 1024], BF16) for i in range(4)]
        stats = fw.sb("stats", [128, 2, 6], F32)
        mv = fw.sb("mv", [128, 2], F32)
        rstd = fw.sb("rstd", [128, 1], F32)
        Bpt, Brw, Bs, Bo = Buf("pt"), Buf("rw"), Buf("stats"), Buf("o_x2")
        Bxt = [Buf("xt0"), Buf("xt1")]
        Bacc = [Buf("acc0"), Buf("acc1")]
        Bgb = [Buf(f"gb{i}") for i in range(4)]
        fw.dma("sp", pt[:], postab, writes=[Bpt])
        fw.dma("sp", rw[:].rearrange("p a b -> p (a b)"), rows.partition_broadcast(128), writes=[Brw])
        gi = 0
        for t in range(NT):
            s = t % 2
            fw.dma("sp", xt[s][:], x1[t * 128:(t + 1) * 128, :], writes=[Bxt[s]])
            for e in range(16):
                q = gi % 4
                gi += 1
                fw.dma("pool", gb[q][:], ycb, reads=[Bpt], writes=[Bgb[q]],
                       indirect=dict(out_offset=None, in_offset=bass.IndirectOffsetOnAxis(ap=pt[:, e, t:t + 1], axis=0),
                                     element_offset=e * RS * 1024))
                if e == 0:
                    fw.op("dve", lambda: nc.vector.tensor_copy(out=acc[s][:], in_=gb[q][:]), reads=[Bgb[q]], writes=[Bacc[s]])
                else:
                    fw.op("dve", lambda: nc.vector.tensor_tensor(out=acc[s][:], in0=acc[s][:], in1=gb[q][:], op=ALU.add),
                          reads=[Bacc[s], Bgb[q]], writes=[Bacc[s]])
            fw.op("dve", lambda: nc.vector.tensor_tensor(out=acc[s][:], in0=acc[s][:], in1=rw[:, 0, :], op=ALU.mult),
                  reads=[Bacc[s], Brw], writes=[Bacc[s]])
            fw.op("dve", lambda: nc.vector.scalar_tensor_tensor(out=acc[s][:], in0=xt[s][:], scalar=ALPHA, in1=acc[s][:], op0=ALU.mult, op1=ALU.add),
                  reads=[Bxt[s], Bacc[s]], writes=[Bacc[s]])
            layer_norm_tile(fw, nc, acc[s], Bacc[s], stats, mv, rstd, Bs)
            fw.op("pool", lambda: nc.gpsimd.tensor_tensor(out=acc[s][:], in0=acc[s][:], in1=rw[:, 1, :], op=ALU.mult),
                  reads=[Bacc[s], Brw], writes=[Bacc[s]])
            fw.op("pool", lambda: nc.gpsimd.tensor_tensor(out=acc[s][:], in0=acc[s][:], in1=rw[:, 2, :], op=ALU.add),
                  reads=[Bacc[s], Brw], writes=[Bacc[s]])
            fw.dma("sp", x2[t * 128:(t + 1) * 128, :], acc[s][:], reads=[Bacc[s]], writes=[Bo])
        fw.finish([Bo])
    return nc


def load_cast(fw, nc, dst, Bdst, src_fn, nk, width, wst, Bwst, ctr):
    for k in range(nk):
        s = ctr[0] % 2
        ctr[0] += 1
        fw.dma("sp" if s == 0 else "act", wst[s][:, :width], src_fn(k), writes=[Bwst[s]])
        if s == 0:
            fw.op("act", lambda k=k: nc.scalar.copy(out=dst[:, k, :], in_=wst[s][:, :width]), reads=[Bwst[s]], writes=[Bdst])
        else:
            fw.op("pool", lambda k=k: nc.gpsimd.tensor_copy(out=dst[:, k, :], in_=wst[s][:, :width]), reads=[Bwst[s]], writes=[Bdst])


def build_MG(NT, stage=9):
    T = NT * 128
    NGRP = T // 512
    nc = new_nc()
    xT = dram_in(nc, "xT", [128, 8, T])
    mcol = dram_in(nc, "mcol", [128, 8, 2])
    win = dram_in(nc, "win", [128, 8, 4096])
    lnr = dram_in(nc, "lnr", [1, 2 * 2048])
    wsT = dram_in(nc, "wsT", [128, 16, 128])
    bsr = dram_in(nc, "bsr", [1, 16 * 128])
    gTo = dram_out(nc, "gT", [128, 16, T], BF16)
    with ExitStack() as st:
        fw = FW(nc, st)
        make_consts(fw, nc)
        V = nc.vector
        mc = fw.sb("mc", [128, 8, 2], F32)
        xs = [fw.sb(f"xs{i}", [128, T], F32) for i in range(2)]
        hT = fw.sb("hT", [128, 8, T], BF16)
        wb = fw.sb("wb", [128, 8, 4096], BF16)
        wst = [fw.sb(f"wst{i}", [128, 2048], F32) for i in range(2)]
        lnt = fw.sb("lnt", [128, 2, 2048], F32)
        wsf = fw.sb("wsf", [128, 16, 128], F32)
        wsb = fw.sb("wsb", [128, 16, 128], BF16)
        bst = fw.sb("bst", [128, 16, 128], F32)
        uT = fw.sb("uT", [128, 16, 512], BF16)
        v = fw.sb("v", [128, 2048], F32)
        vln = fw.sb("vln", [128, 2048], BF16)
        tmp = fw.sb("tmp", [128, 4, 128], F32)
        go = [fw.sb(f"go{i}", [128, 16, 128], BF16) for i in range(2)]
        stats = fw.sb("stats", [128, 4, 6], F32)
        mv = fw.sb("mv", [128, 2], F32)
        rstd = fw.sb("rstd", [128, 1], F32)
        pu = [fw.ps(f"pu{i}", [128, 512]) for i in range(2)]
        pv = [fw.ps(f"pv{i}", [128, 512]) for i in range(2)]
        psp = [fw.ps(f"psp{i}", [128, 4, 128]) for i in range(2)]
        B = {n: Buf(n) for n in ["mc", "xs0", "xs1", "hT", "wb", "wst0", "wst1", "lnt", "wsf", "wsb", "bst", "uT", "v", "vln", "tmp",
                                 "go0", "go1", "stats", "pu0", "pu1", "pv0", "pv1", "psp0", "psp1", "o"]}
        fw.dma("sp", mc[:], mcol, writes=[B["mc"]])
        fw.dma("sp", lnt[:].rearrange("p a b -> p (a b)"), lnr.partition_broadcast(128), writes=[B["lnt"]])
        fw.dma("sp", wsf[:], wsT, writes=[B["wsf"]])
        fw.dma("sp", bst[:].rearrange("p a b -> p (a b)"), bsr.partition_broadcast(128), writes=[B["bst"]])
        fw.op("dve", lambda: V.tensor_copy(out=wsb[:], in_=wsf[:]), reads=[B["wsf"]], writes=[B["wsb"]])
        fw.op("dve", lambda: V.tensor_scalar(out=mc[:, :, 0], in0=mc[:, :, 0], scalar1=1.0, scalar2=None, op0=ALU.add), reads=[B["mc"]], writes=[B["mc"]])
        for k in range(8):
            s = k % 2
            fw.dma("sp", xs[s][:], xT[:, k, :], writes=[B[f"xs{s}"]])
            fw.op("act", lambda k=k: nc.scalar.activation(out=hT[:, k, :], in_=xs[s][:], func=AF.Identity, scale=mc[:, k, 0:1], bias=mc[:, k, 1:2]),
                  reads=[B[f"xs{s}"], B["mc"]], writes=[B["hT"]])
        ctr = [0]
        wbv = wb[:].rearrange("p k (h w) -> p (k h) w", h=2)
        winv = win.rearrange("p k (h w) -> p (k h) w", h=2)
        load_cast(fw, nc, wbv, B["wb"], lambda k: winv[:, k, :], 16, 2048, wst, [B["wst0"], B["wst1"]], ctr)
        ev = 0
        for grp in range(NGRP if stage > 0 else 0):
            tsl = slice(grp * 512, (grp + 1) * 512)
            for uf in range(16):
                s = uf % 2
                for k in range(8):
                    fw.op("pe", lambda k=k: nc.tensor.matmul(pu[s][:], lhsT=wb[:, k, uf * 128:(uf + 1) * 128], rhs=hT[:, k, tsl], start=(k == 0), stop=(k == 7)),
                          reads=[B["wb"], B["hT"]], writes=[B[f"pu{s}"]])
                fw.op("act", lambda: nc.scalar.activation(out=uT[:, uf, :], in_=pu[s][:], func=AF.Gelu), reads=[B[f"pu{s}"]], writes=[B["uT"]])
            for cc in range(4 if stage > 1 else 0):
                ch = grp * 4 + cc
                csl = slice(ch * 128, (ch + 1) * 128)
                for n in range(4):
                    s = n % 2
                    for k in range(8):
                        fw.op("pe", lambda k=k: nc.tensor.matmul(pv[s][:], lhsT=hT[:, k, csl], rhs=wb[:, k, 2048 + n * 512:2048 + (n + 1) * 512],
                                                                  start=(k == 0), stop=(k == 7)), reads=[B["wb"], B["hT"]], writes=[B[f"pv{s}"]])
                    fw.op("act", lambda: nc.scalar.activation(out=v[:, n * 512:(n + 1) * 512], in_=pv[s][:], func=AF.Gelu), reads=[B[f"pv{s}"]], writes=[B["v"]])
                for j in range(4):
                    fw.op("dve", lambda j=j: V.bn_stats(out=stats[:, j, :], in_=v[:, j * 512:(j + 1) * 512]), reads=[B["v"]], writes=[B["stats"]])
                fw.op("dve", lambda: V.bn_aggr(out=mv[:], in_=stats[:].rearrange("p a b -> p (a b)")), reads=[B["stats"]], writes=[B["stats"]])
                fw.op("act", lambda: nc.scalar.activation(out=rstd[:], in_=mv[:, 1:2], func=AF.Sqrt, bias=fw.eps_ln[:, 0:1], scale=1.0),
                      reads=[B["stats"], fw.Bconst], writes=[B["stats"]])
                fw.op("dve", lambda: V.reciprocal(out=rstd[:], in_=rstd[:]), reads=[B["stats"]], writes=[B["stats"]])
                fw.op("dve", lambda: V.tensor_scalar(out=v[:], in0=v[:], scalar1=mv[:, 0:1], scalar2=rstd[:, 0:1], op0=ALU.subtract, op1=ALU.mult),
                      reads=[B["stats"], B["v"]], writes=[B["v"]])
                fw.op("pool", lambda: nc.gpsimd.tensor_tensor(out=v[:], in0=v[:], in1=lnt[:, 0, :], op=ALU.mult), reads=[B["v"], B["lnt"]], writes=[B["v"]])
                fw.op("pool", lambda: nc.gpsimd.tensor_tensor(out=vln[:], in0=v[:], in1=lnt[:, 1, :], op=ALU.add), reads=[B["v"], B["lnt"]], writes=[B["vln"]])
                os_ = ch % 2
                for g4 in range(4 if stage > 2 else 0):
                    s = g4 % 2
                    for gg in range(4):
                        g = g4 * 4 + gg
                        fw.op("pe", lambda g=g, gg=gg: nc.tensor.matmul(psp[s][:, gg, :], lhsT=vln[:, g * 128:(g + 1) * 128], rhs=wsb[:, g, :], start=True, stop=True),
                              reads=[B["vln"], B["wsb"]], writes=[B[f"psp{s}"]])
                    fw.op("dve", lambda: V.tensor_tensor(out=tmp[:], in0=psp[s][:], in1=bst[:, g4 * 4:(g4 + 1) * 4, :], op=ALU.add),
                          reads=[B[f"psp{s}"], B["bst"]], writes=[B["tmp"]])
                    fw.op("dve", lambda: V.tensor_tensor(out=go[os_][:, g4 * 4:(g4 + 1) * 4, :], in0=tmp[:], in1=uT[:, g4 * 4:(g4 + 1) * 4, cc * 128:(cc + 1) * 128], op=ALU.mult),
                          reads=[B["tmp"], B["uT"]], writes=[B[f"go{os_}"]])
                if stage != 3:
                    for q4 in range(4):
                        fw.dma("sp", gTo[:, q4 * 4:(q4 + 1) * 4, csl], go[os_][:, q4 * 4:(q4 + 1) * 4, :], reads=[B[f"go{os_}"]], writes=[B["o"]])
        fw.finish([B["o"]])
    return nc


def build_MA():
    NQT = SEQ // 128
    NCT = CTX // 128
    NKT = NQT + NCT
    nc = new_nc()
    xT = dram_in(nc, "xT", [128, 8, SEQ])
    cxT = dram_in(nc, "cxT", [128, 8, CTX])
    mcol = dram_in(nc, "mcol", [128, 8, 4])
    w = dram_in(nc, "w", [128, 8, 768])
    gains = dram_in(nc, "gains", [1, 640])
    cs = dram_in(nc, "cs", [128, 2, NQT, 32])
    identb = dram_in(nc, "identb", [128, 128], BF16)
    oTo = dram_out(nc, "oT", [128, 4, SEQ], BF16)
    with ExitStack() as st:
        fw = FW(nc, st)
        V = nc.vector
        G = nc.gpsimd
        mc = fw.sb("mc", [128, 8, 4], F32)
        xs = [fw.sb(f"xs{i}", [128, 2048], F32) for i in range(2)]
        hT = fw.sb("hT", [128, 8, CTX + 2048], BF16)
        wst = [fw.sb(f"wst{i}", [128, 768], F32) for i in range(2)]
        wb = fw.sb("wb", [128, 8, 768], BF16)
        g10 = fw.sb("g10", [128, 10, 64], F32)
        cst = fw.sb("cst", [128, 2, NQT, 32], F32)
        idb = fw.sb("idb", [128, 128], BF16)
        qk = fw.sb("qk", [128, 10, 64], F32)
        sq = fw.sb("sq", [128, 10, 64], F32)
        ss = fw.sb("ss", [128, 10], F32)
        t1 = fw.sb("t1", [128, 10, 32], F32)
        t2 = fw.sb("t2", [128, 10, 32], F32)
        qr = fw.sb("qr", [128, 10, 64], BF16)
        qT = fw.sb("qT", [64, 8, SEQ], BF16)
        kT = fw.sb("kT", [64, 2, NKT * 128], BF16)
        vall = fw.sb("vall", [128, NKT, 2, 65], BF16)
        E = [fw.sb(f"E{i}", [128, 512], BF16) for i in range(2)]
        rden = fw.sb("rden", [128, 4], F32)
        otok = fw.sb("otok", [128, 8, 64], BF16)
        oTt = [fw.sb(f"oTt{i}", [128, 4, 128], BF16) for i in range(2)]
        epsr = fw.sb("epsr", [128, 1], F32)
        pA = [fw.ps(f"pA{i}", [128, 512]) for i in range(2)]
        pT = fw.ps("pT", [128, 4, 128], BF16)
        pO = [fw.ps(f"pO{i}", [128, 512]) for i in range(4)]
        B = {n: Buf(n) for n in ["mc", "xs0", "xs1", "hT", "wst0", "wst1", "wb", "g10", "cst", "idb", "qk", "sq", "ss", "t1", "t2", "qr", "qT", "kT",
                                 "vall", "E0", "E1", "rden", "otok", "oTt0", "oTt1", "epsr", "pA0", "pA1", "pT", "pO0", "pO1", "pO2", "pO3", "o"]}
        fw.dma("sp", mc[:], mcol, writes=[B["mc"]])
        fw.dma("sp", g10[:].rearrange("p a b -> p (a b)"), gains.partition_broadcast(128), writes=[B["g10"]])
        fw.dma("sp", cst[:], cs, writes=[B["cst"]])
        fw.dma("sp", idb[:], identb, writes=[B["idb"]])
        fw.op("pool", lambda: G.memset(epsr[:], RMS_EPS), writes=[B["epsr"]])
        fw.op("pool", lambda: G.memset(vall[:, :, :, 64:65], 1.0), writes=[B["vall"]])
        fw.op("dve", lambda: V.tensor_scalar(out=g10[:, 0:8, :], in0=g10[:, 0:8, :], scalar1=0.125, scalar2=None, op0=ALU.mult), reads=[B["g10"]], writes=[B["g10"]])
        for c in (0, 2):
            fw.op("dve", lambda c=c: V.tensor_scalar(out=mc[:, :, c], in0=mc[:, :, c], scalar1=1.0, scalar2=None, op0=ALU.add), reads=[B["mc"]], writes=[B["mc"]])
        li = [0]

        def load_half(hf):
            for k in range(8):
                if hf == 0:
                    s = li[0] % 2
                    li[0] += 1
                    fw.dma("sp", xs[s][:, :CTX], cxT[:, k, :], writes=[B[f"xs{s}"]])
                    fw.op("act", lambda k=k: nc.scalar.activation(out=hT[:, k, 0:CTX], in_=xs[s][:, :CTX], func=AF.Identity, scale=mc[:, k, 2:3], bias=mc[:, k, 3:4]),
                          reads=[B[f"xs{s}"], B["mc"]], writes=[B["hT"]])
                s = li[0] % 2
                li[0] += 1
                fw.dma("sp", xs[s][:], xT[:, k, hf * 2048:(hf + 1) * 2048], writes=[B[f"xs{s}"]])
                fw.op("act", lambda k=k: nc.scalar.activation(out=hT[:, k, CTX:CTX + 2048], in_=xs[s][:], func=AF.Identity,
                                                              scale=mc[:, k, 0:1], bias=mc[:, k, 1:2]),
                      reads=[B[f"xs{s}"], B["mc"]], writes=[B["hT"]])

        ctr = [0]
        load_cast(fw, nc, wb, B["wb"], lambda k: w[:, k, :], 8, 768, wst, [B["wst0"], B["wst1"]], ctr)

        def bc10(ap2, n):
            return ap2.unsqueeze(2).to_broadcast([128, 10, n])

        for tt in range(NKT):
            is_ctx = tt < NCT
            tsl = slice(tt * 128, (tt + 1) * 128)
            if tt == 0:
                load_half(0)
            if tt == NCT + 16:
                load_half(1)
            hc = tt * 128 if tt < NCT + 16 else (tt - 16) * 128
            hsl = slice(hc, hc + 128)
            for k in range(8):
                fw.op("pe", lambda k=k: nc.tensor.matmul(pA[0][:], lhsT=hT[:, k, hsl], rhs=wb[:, k, 0:512], start=(k == 0), stop=(k == 7)),
                      reads=[B["hT"], B["wb"]], writes=[B["pA0"]])
            for k in range(8):
                fw.op("pe", lambda k=k: nc.tensor.matmul(pA[1][:, 0:256], lhsT=hT[:, k, hsl], rhs=wb[:, k, 512:768], start=(k == 0), stop=(k == 7)),
                      reads=[B["hT"], B["wb"]], writes=[B["pA1"]])
            fw.op("act", lambda: nc.scalar.copy(out=qk[:, 0:8, :], in_=pA[0][:].rearrange("p (a b) -> p a b", b=64)), reads=[B["pA0"]], writes=[B["qk"]])
            fw.op("act", lambda: nc.scalar.copy(out=qk[:, 8:10, :], in_=pA[1][:, 0:128].rearrange("p (a b) -> p a b", b=64)), reads=[B["pA1"]], writes=[B["qk"]])
            fw.op("act", lambda: nc.scalar.copy(out=vall[:, tt, :, 0:64], in_=pA[1][:, 128:256].rearrange("p (a b) -> p a b", b=64)), reads=[B["pA1"]], writes=[B["vall"]])
            fw.op("pool", lambda: G.tensor_tensor(out=sq[:], in0=qk[:], in1=qk[:], op=ALU.mult), reads=[B["qk"]], writes=[B["sq"]])
            fw.op("dve", lambda: V.tensor_reduce(out=ss[:], in_=sq[:], axis=AX.X, op=ALU.add), reads=[B["sq"]], writes=[B["ss"]])
            fw.op("act", lambda: nc.scalar.activation(out=ss[:], in_=ss[:], func=AF.Sqrt, scale=1.0 / 64.0, bias=epsr[:, 0:1]), reads=[B["ss"], B["epsr"]], writes=[B["ss"]])
            fw.op("dve", lambda: V.reciprocal(out=ss[:], in_=ss[:]), reads=[B["ss"]], writes=[B["ss"]])
            fw.op("dve", lambda: V.tensor_tensor(out=qk[:], in0=qk[:], in1=bc10(ss[:, :], 64), op=ALU.mult), reads=[B["qk"], B["ss"]], writes=[B["qk"]])
            if is_ctx:
                fw.op("dve", lambda: V.tensor_tensor(out=qr[:], in0=qk[:], in1=g10[:], op=ALU.mult), reads=[B["qk"], B["g10"]], writes=[B["qr"]])
            else:
                lt = tt - NCT
                fw.op("pool", lambda: G.tensor_tensor(out=qk[:], in0=qk[:], in1=g10[:], op=ALU.mult), reads=[B["qk"], B["g10"]], writes=[B["qk"]])
                cosb = cst[:, 0, lt, :].unsqueeze(1).to_broadcast([128, 10, 32])
                sinb = cst[:, 1, lt, :].unsqueeze(1).to_broadcast([128, 10, 32])
                fw.op("dve", lambda: V.tensor_tensor(out=t1[:], in0=qk[:, :, 0:32], in1=cosb, op=ALU.mult), reads=[B["qk"], B["cst"]], writes=[B["t1"]])
                fw.op("pool", lambda: G.tensor_tensor(out=t2[:], in0=qk[:, :, 32:64], in1=sinb, op=ALU.mult), reads=[B["qk"], B["cst"]], writes=[B["t2"]])
                fw.op("dve", lambda: V.tensor_tensor(out=qr[:, :, 0:32], in0=t1[:], in1=t2[:], op=ALU.subtract), reads=[B["t1"], B["t2"]], writes=[B["qr"]])
                fw.op("dve", lambda: V.tensor_tensor(out=t1[:], in0=qk[:, :, 0:32], in1=sinb, op=ALU.mult), reads=[B["qk"], B["cst"], B["qr"]], writes=[B["t1"]])
                fw.op("pool", lambda: G.tensor_tensor(out=t2[:], in0=qk[:, :, 32:64], in1=cosb, op=ALU.mult), reads=[B["qk"], B["cst"], B["qr"]], writes=[B["t2"]])
                fw.op("dve", lambda: V.tensor_tensor(out=qr[:, :, 32:64], in0=t1[:], in1=t2[:], op=ALU.add), reads=[B["t1"], B["t2"]], writes=[B["qr"]])
            heads = [8, 9] if is_ctx else list(range(10))
            for i0 in range(0, len(heads), 4):
                hs = heads[i0:i0 + 4]
                for j, hh in enumerate(hs):
                    fw.op("pe", lambda j=j, hh=hh: nc.tensor.transpose(out=pT[0:64, j, :], in_=qr[:, hh, :], identity=idb[:]),
                          reads=[B["qr"], B["idb"]], writes=[B["pT"]])
                if hs[0] < 8:
                    lt = tt - NCT
                    fw.op("act", lambda: nc.scalar.copy(out=qT[:, hs[0]:hs[0] + 4, lt * 128:(lt + 1) * 128], in_=pT[0:64, 0:4, :]), reads=[B["pT"]], writes=[B["qT"]])
                else:
                    fw.op("act", lambda: nc.scalar.copy(out=kT[:, :, tsl], in_=pT[0:64, 0:2, :]), reads=[B["pT"]], writes=[B["kT"]])
        steps = [(qt, kh, kt) for qt in range(NQT) for kh in range(2) for kt in range(NKT)]

        def emit_S(i):
            qt, kh, kt = steps[i]
            s = i % 2
            qsl = slice(qt * 128, (qt + 1) * 128)
            fw.op("pe", lambda: nc.tensor.matmul(pA[s][:].rearrange("p (a b) -> p a b", b=128), lhsT=kT[:, kh, kt * 128:(kt + 1) * 128],
                                                 rhs=qT[:, kh * 4:(kh + 1) * 4, qsl], start=True, stop=True),
                  reads=[B["kT"], B["qT"]], writes=[B[f"pA{s}"]])
            fw.op("act", lambda: nc.scalar.activation(out=E[s][:], in_=pA[s][:], func=AF.Exp), reads=[B[f"pA{s}"]], writes=[B[f"E{s}"]])

        emit_S(0)
        for i, (qt, kh, kt) in enumerate(steps):
            s = i % 2
            if i + 1 < len(steps):
                emit_S(i + 1)
            for g in range(4):
                fw.op("pe", lambda g=g: nc.tensor.matmul(pO[g][:, 0:65], lhsT=E[s][:, g * 128:(g + 1) * 128], rhs=vall[:, kt, kh, :],
                                                          start=(kt == 0), stop=(kt == NKT - 1)),
                      reads=[B[f"E{s}"], B["vall"]], writes=[B[f"pO{g}"]])
            if kt == NKT - 1:
                for g in range(4):
                    fw.op("dve", lambda g=g: V.reciprocal(out=rden[:, g:g + 1], in_=pO[g][:, 64:65]), reads=[B[f"pO{g}"]], writes=[B["rden"]])
                    fw.op("dve", lambda g=g: V.tensor_scalar(out=otok[:, kh * 4 + g, :], in0=pO[g][:, 0:64], scalar1=rden[:, g:g + 1], scalar2=None, op0=ALU.mult),
                          reads=[B[f"pO{g}"], B["rden"]], writes=[B["otok"]])
                if kh == 1:
                    os_ = qt % 2
                    qsl = slice(qt * 128, (qt + 1) * 128)
                    for j in range(4):
                        fw.op("pe", lambda j=j: nc.tensor.transpose(out=pT[:, j, :], in_=otok[:, 2 * j:2 * j + 2, :].rearrange("p a b -> p (a b)"), identity=idb[:]),
                              reads=[B["otok"], B["idb"]], writes=[B["pT"]])
                    fw.op("act", lambda: nc.scalar.copy(out=oTt[os_][:], in_=pT[:]), reads=[B["pT"]], writes=[B[f"oTt{os_}"]])
                    fw.dma("sp", oTo[:, :, qsl], oTt[os_][:], reads=[B[f"oTt{os_}"]], writes=[B["o"]])
        fw.finish([B["o"]])
    return nc


def rope_tables():
    L = SEQ
    rows = L // 64
    row = np.broadcast_to(np.arange(rows, dtype=np.float32)[:, None], (rows, 64)).reshape(L)
    col = np.broadcast_to(np.arange(64, dtype=np.float32)[None, :], (rows, 64)).reshape(L)
    inv = (10000.0 ** (-np.arange(16, dtype=np.float32) / 16)).astype(np.float32)
    ang = np.concatenate([row[:, None] * inv, col[:, None] * inv], axis=-1).astype(np.float32)
    cs = np.stack([np.cos(ang), np.sin(ang)], axis=0).astype(np.float32)
    return np.ascontiguousarray(cs.reshape(2, L // 128, 128, 32).transpose(2, 0, 1, 3))


_TAB = {}


def dft_tables(L):
    if L in _TAB:
        return _TAB[L]
    N = 2 * L
    TW = min(512, L)
    t = np.arange(L, dtype=np.int64)
    ph = (np.outer(t, t) % N).astype(np.float64) * (2.0 * np.pi / N)
    C = np.cos(ph)
    S = -np.sin(ph)
    S[:, 0] = np.where(t % 2 == 0, 1.0, -1.0)

    def slab(M):
        return np.ascontiguousarray(M.reshape(L // 128, 128, L // TW, TW).transpose(2, 1, 0, 3).astype(np.float32).astype(NPBF))
    _TAB[L] = (slab(C), slab(S), slab(np.ascontiguousarray(S.T)))
    return _TAB[L]


def hyena_consts(L):
    t = np.linspace(0.0, 1.0, L, dtype=np.float32)[:, None]
    w = ((2.0 * math.pi / L) * np.arange(L, dtype=np.float32))[:, None].astype(np.float32)
    f = np.linspace(1e-4, 15, 16, dtype=np.float32)[None, :]
    z = np.concatenate([t, np.cos(f * w), -np.sin(f * w)], axis=-1).astype(np.float32)
    min_decay = math.log(1e-2) / 1.5
    max_decay = math.log(1e-2) / 0.3
    deltas = np.abs(np.linspace(min_decay, max_decay, D, dtype=np.float32))
    decay = np.exp(-t * deltas).astype(np.float32)
    return np.ascontiguousarray(z.T), np.ascontiguousarray(decay.T)


def emit_fwd_dft(fw, nc, L, x_tm, Bx, tabC, tabS, slabs, Bslab, pacc, Bpacc, consume):
    TW = min(512, L)
    NK = L // 128
    sc = [0]
    for nt in range(L // TW):
        for which, tab in ((0, tabC), (1, tabS)):
            s = sc[0] % 2
            sc[0] += 1
            fw.dma("sp" if s == 0 else "act", slabs[s][:, :NK, :TW], tab[nt], writes=[Bslab[s]])
            for st_ in range(4):
                for k in range(NK):
                    fw.op("pe", lambda k=k: nc.tensor.matmul(pacc[st_][:, :TW], lhsT=x_tm[:, st_, k, :], rhs=slabs[s][:, k, :TW], start=(k == 0), stop=(k == NK - 1)),
                          reads=[Bx, Bslab[s]], writes=[Bpacc[st_]])
                consume(nt, which, st_, pacc[st_][:, :TW])


def wrap_pi(fw, nc, a, Ba, m, Bm, P):
    V = nc.vector
    PI = math.pi
    for _ in range(2):
        fw.op("dve", lambda: V.tensor_scalar(out=m[:P], in0=a[:P], scalar1=-PI, scalar2=2 * PI, op0=ALU.is_lt, op1=ALU.mult), reads=[Ba], writes=[Bm])
        fw.op("dve", lambda: V.tensor_tensor(out=a[:P], in0=a[:P], in1=m[:P], op=ALU.add), reads=[Ba, Bm], writes=[Ba])
        fw.op("dve", lambda: V.tensor_scalar(out=m[:P], in0=a[:P], scalar1=PI, scalar2=2 * PI, op0=ALU.is_gt, op1=ALU.mult), reads=[Ba], writes=[Bm])
        fw.op("dve", lambda: V.tensor_tensor(out=a[:P], in0=a[:P], in1=m[:P], op=ALU.subtract), reads=[Ba, Bm], writes=[Ba])
    fw.op("dve", lambda: V.tensor_scalar(out=a[:P], in0=a[:P], scalar1=-PI, scalar2=PI, op0=ALU.max, op1=ALU.min), reads=[Ba], writes=[Ba])


def build_F(L):
    TW = min(512, L)
    NK = L // 128
    NTL = L // TW
    N = 2 * L
    nc = new_nc()
    zT = dram_in(nc, "zT", [33, L])
    decay = dram_in(nc, "decay", [128, L])
    w1 = dram_in(nc, "w1", [33, 64])
    w2 = dram_in(nc, "w2", [64, 64])
    w3 = dram_in(nc, "w3", [64, 4, 128])
    vecs = dram_in(nc, "vecs", [64, 4])
    fbias = dram_in(nc, "fbias", [128, 2])
    identf = dram_in(nc, "identf", [128, 128])
    tabC = dram_in(nc, "tabC", [NTL, 128, NK, TW], BF16)
    tabS = dram_in(nc, "tabS", [NTL, 128, NK, TW], BF16)
    Kfo = dram_out(nc, "Kf", [128, 2, 2, L])
    with ExitStack() as st:
        fw = FW(nc, st)
        V = nc.vector
        G = nc.gpsimd
        w1t = fw.sb("w1t", [33, 64], F32)
        w2t = fw.sb("w2t", [64, 64], F32)
        w3t = fw.sb("w3t", [64, 4, 128], F32)
        vt = fw.sb("vt", [64, 6], F32)
        fbt = fw.sb("fbt", [128, 2], F32)
        idf = fw.sb("idf", [128, 128], F32)
        nrm = fw.sb("nrm", [128, 8], F32)
        x_tm = fw.sb("x_tm", [128, 4, NK, 128], BF16)
        pacc = [fw.ps(f"pacc{i}", [128, 512]) for i in range(4)]
        ptr = [fw.ps(f"ptr{i}", [128, 4, 128]) for i in range(2)]
        scope1 = fw.scoped()
        scope1.__enter__()
        zt = fw.sb("zt", [33, L], F32)
        dct = fw.sb("dct", [128, L], F32)
        h1 = fw.sb("h1", [64, L], F32)
        h2 = fw.sb("h2", [64, L], F32)
        mk = fw.sb("mk", [64, L], F32)
        kk = [fw.sb(f"kk{i}", [128, L], F32) for i in range(4)]
        B = {n: Buf(n) for n in ["zt", "dct", "w1t", "w2t", "w3t", "vt", "fbt", "idf", "h1", "h2", "mk", "kk0", "kk1", "kk2", "kk3", "nrm", "x_tm",
                                 "slab0", "slab1", "Kf", "pacc0", "pacc1", "pacc2", "pacc3", "ptr0", "ptr1", "o"]}
        for t_, src, nm in ((zt, zT, "zt"), (dct, decay, "dct"), (w1t, w1, "w1t"), (w2t, w2, "w2t"), (w3t, w3, "w3t"), (fbt, fbias, "fbt"), (idf, identf, "idf")):
            fw.dma("sp", t_[:], src, writes=[B[nm]])
        fw.dma("sp", vt[:, 0:4], vecs, writes=[B["vt"]])
        fw.op("dve", lambda: V.tensor_tensor(out=vt[:, 4:6], in0=vt[:, 0:2], in1=vt[:, 2:4], op=ALU.mult), reads=[B["vt"]], writes=[B["vt"]])
        for layer, (wt, Bw, src, Bsrc, dst, Bdst, KP) in enumerate(((w1t, B["w1t"], zt, B["zt"], h1, B["h1"], 33), (w2t, B["w2t"], h1, B["h1"], h2, B["h2"], 64))):
            for j in range(L // TW):
                s = j % 2
                sl = slice(j * TW, (j + 1) * TW)
                fw.op("pe", lambda: nc.tensor.matmul(pacc[s][0:64, :TW], lhsT=wt[:KP, :], rhs=src[:KP, sl], start=True, stop=True), reads=[Bw, Bsrc], writes=[B[f"pacc{s}"]])
                fw.op("act", lambda: nc.scalar.activation(out=dst[:, sl], in_=pacc[s][0:64, :TW], func=AF.Identity, scale=vt[:, 2 + layer:3 + layer], bias=vt[:, 4 + layer:5 + layer]),
                      reads=[B[f"pacc{s}"], B["vt"]], writes=[Bdst])
            wrap_pi(fw, nc, dst, Bdst, mk, B["mk"], 64)
            fw.op("act", lambda: nc.scalar.activation(out=dst[:, :], in_=dst[:, :], func=AF.Sin), reads=[Bdst], writes=[Bdst])
        for st_ in range(4):
            for j in range(L // TW):
                s = j % 2
                sl = slice(j * TW, (j + 1) * TW)
                fw.op("pe", lambda: nc.tensor.matmul(pacc[s][:, :TW], lhsT=w3t[:, st_, :], rhs=h2[:, sl], start=True, stop=True), reads=[B["w3t"], B["h2"]], writes=[B[f"pacc{s}"]])
                fw.op("dve", lambda: V.tensor_tensor(out=kk[st_][:, sl], in0=pacc[s][:, :TW], in1=dct[:, sl], op=ALU.mult), reads=[B[f"pacc{s}"], B["dct"]], writes=[B[f"kk{st_}"]])
            if st_ % 2 == 1:
                fw.op("pool", lambda: G.memset(kk[st_][:, 0:1], 0.0), reads=[], writes=[B[f"kk{st_}"]])
            fw.op("dve", lambda: V.tensor_reduce(out=nrm[:, st_:st_ + 1], in_=kk[st_][:], axis=AX.X, op=ALU.add, apply_absolute_value=True),
                  reads=[B[f"kk{st_}"]], writes=[B["nrm"]])
        for o in range(2):
            fw.op("dve", lambda: V.tensor_tensor(out=nrm[:, 4 + o:5 + o], in0=nrm[:, 2 * o:2 * o + 1], in1=nrm[:, 2 * o + 1:2 * o + 2], op=ALU.add), reads=[B["nrm"]], writes=[B["nrm"]])
            fw.op("dve", lambda: V.tensor_scalar(out=nrm[:, 4 + o:5 + o], in0=nrm[:, 4 + o:5 + o], scalar1=RMS_EPS, scalar2=None, op0=ALU.add), reads=[B["nrm"]], writes=[B["nrm"]])
            fw.op("dve", lambda: V.reciprocal(out=nrm[:, 6 + o:7 + o], in_=nrm[:, 4 + o:5 + o]), reads=[B["nrm"]], writes=[B["nrm"]])
        for st_ in range(4):
            o = st_ // 2
            fw.op("pool", lambda: G.tensor_scalar(out=kk[st_][:], in0=kk[st_][:], scalar1=nrm[:, 6 + o:7 + o], scalar2=None, op0=ALU.mult), reads=[B[f"kk{st_}"], B["nrm"]], writes=[B[f"kk{st_}"]])
            for k4 in range(NK // 4 if NK >= 4 else 1):
                s = k4 % 2
                nn = min(4, NK)
                for kq in range(nn):
                    k = k4 * 4 + kq
                    fw.op("pe", lambda k=k, kq=kq: nc.tensor.transpose(out=ptr[s][:, kq, :], in_=kk[st_][:, k * 128:(k + 1) * 128], identity=idf[:]),
                          reads=[B[f"kk{st_}"], B["idf"]], writes=[B[f"ptr{s}"]])
                fw.op("act", lambda: nc.scalar.copy(out=x_tm[:, st_, k4 * 4:k4 * 4 + nn, :], in_=ptr[s][:, 0:nn, :]), reads=[B[f"ptr{s}"]], writes=[B["x_tm"]])

        scope1.__exit__(None, None, None)
        slabs = [fw.sb(f"slab{i}", [128, NK, TW], BF16) for i in range(2)]
        Kf = fw.sb("Kfs", [128, 2, 2, L], F32)

        def consume(nt, which, st_, ps):
            o, d = st_ // 2, st_ % 2
            dst = Kf[:, o, which, nt * TW:(nt + 1) * TW]
            if d == 0:
                fw.op("act", lambda: nc.scalar.copy(out=dst, in_=ps), reads=[B[f"pacc{st_}"]], writes=[B["Kf"]])
            else:
                op = ALU.add if which == 0 else ALU.subtract
                fw.op("dve", lambda: V.tensor_tensor(out=dst, in0=dst, in1=ps, op=op), reads=[B[f"pacc{st_}"], B["Kf"]], writes=[B["Kf"]])
                if which == 1 and nt == 0:
                    fw.op("dve", lambda: V.scalar_tensor_tensor(out=Kf[:, o, 1, 0:1], in0=ps[:, 0:1], scalar=2.0, in1=Kf[:, o, 1, 0:1], op0=ALU.mult, op1=ALU.add),
                          reads=[B[f"pacc{st_}"], B["Kf"]], writes=[B["Kf"]])
        emit_fwd_dft(fw, nc, L, x_tm, B["x_tm"], tabC, tabS, slabs, [B["slab0"], B["slab1"]], pacc, [B[f"pacc{i}"] for i in range(4)], consume)
        for o in range(2):
            fw.op("dve", lambda: V.tensor_scalar(out=Kf[:, o, 0, :], in0=Kf[:, o, 0, :], scalar1=fbt[:, o:o + 1], scalar2=2.0 / N, op0=ALU.add, op1=ALU.mult),
                  reads=[B["Kf"], B["fbt"]], writes=[B["Kf"]])
            fw.op("dve", lambda: V.tensor_scalar(out=Kf[:, o, 1, 0:1], in0=Kf[:, o, 1, 0:1], scalar1=fbt[:, o:o + 1], scalar2=None, op0=ALU.add), reads=[B["Kf"], B["fbt"]], writes=[B["Kf"]])
            fw.op("dve", lambda: V.tensor_scalar(out=Kf[:, o, 1, :], in0=Kf[:, o, 1, :], scalar1=2.0 / N, scalar2=None, op0=ALU.mult), reads=[B["Kf"]], writes=[B["Kf"]])
            fw.op("dve", lambda: V.tensor_scalar(out=Kf[:, o, :, 0:1], in0=Kf[:, o, :, 0:1], scalar1=0.5, scalar2=None, op0=ALU.mult), reads=[B["Kf"]], writes=[B["Kf"]])
            for ri in range(2):
                fw.dma("sp", Kfo[:, o, ri, :], Kf[:, o, ri, :], reads=[B["Kf"]], writes=[B["o"]])
        fw.finish([B["o"]])
    return nc


def build_MH(L):
    TW = min(512, L)
    NK = L // 128
    NTL = L // TW
    TJ = TW // 128
    nc = new_nc()
    xT = dram_in(nc, "xT", [128, 8, L])
    mcol = dram_in(nc, "mcol", [128, 8, 2])
    win = dram_in(nc, "win", [128, 8, 1536])
    cw = dram_in(nc, "cw", [128, 12, 4])
    Kfi = dram_in(nc, "Kf", [4, 128, 2, 2, L])
    tabC = dram_in(nc, "tabC", [NTL, 128, NK, TW], BF16)
    tabS = dram_in(nc, "tabS", [NTL, 128, NK, TW], BF16)
    tabST = dram_in(nc, "tabST", [NTL, 128, NK, TW], BF16)
    identf = dram_in(nc, "identf", [128, 128])
    zTo = dram_out(nc, "zT", [128, 4, L], BF16)
    x12 = nc.dram_tensor("x12", [4, 2, 128, L], F32).ap()
    with ExitStack() as st:
        fw = FW(nc, st)
        V = nc.vector
        G = nc.gpsimd
        mc = fw.sb("mc", [128, 8, 2], F32)
        cwt = fw.sb("cwt", [128, 12, 4], F32)
        idf = fw.sb("idf", [128, 128], F32)
        x_tm = fw.sb("x_tm", [128, 4, NK, 128], BF16)
        pacc = [fw.ps(f"pacc{i}", [128, 512]) for i in range(4)]
        ptr = [fw.ps(f"ptr{i}", [128, 4, 128]) for i in range(2)]
        B = {n: Buf(n) for n in ["mc", "cwt", "idf", "x_tm", "pacc0", "pacc1", "pacc2", "pacc3", "ptr0", "ptr1", "x12", "o",
                                 "xs0", "xs1", "hT", "wb", "wst0", "wst1", "pb0", "pb1", "ob0", "ob1",
                                 "slab0", "slab1", "y_fm", "Xr", "kt0", "kt1", "Y", "ta", "tb", "xq0", "xq1", "zb0", "zb1", "zo0", "zo1"]}
        Bpacc = [B[f"pacc{i}"] for i in range(4)]
        fw.dma("sp", mc[:], mcol, writes=[B["mc"]])
        fw.dma("sp", cwt[:], cw, writes=[B["cwt"]])
        fw.dma("sp", idf[:], identf, writes=[B["idf"]])
        fw.op("dve", lambda: V.tensor_scalar(out=mc[:, :, 0], in0=mc[:, :, 0], scalar1=1.0, scalar2=None, op0=ALU.add), reads=[B["mc"]], writes=[B["mc"]])
        scA = fw.scoped()
        scA.__enter__()
        XW = min(1024, L)
        xs = [fw.sb(f"xs{i}", [128, XW], F32) for i in range(2)]
        hT = fw.sb("hT", [128, 8, L], BF16)
        wb = fw.sb("wb", [128, 8, 1536], BF16)
        wst = [fw.sb(f"wst{i}", [128, 768], F32) for i in range(2)]
        pb = [fw.sb(f"pb{i}", [128, L + 2], F32) for i in range(2)]
        ob = [fw.sb(f"ob{i}", [128, L], F32) for i in range(2)]
        li = 0
        for k in range(8):
            for hf in range(L // XW):
                s = li % 2
                li += 1
                fw.dma("sp", xs[s][:], xT[:, k, hf * XW:(hf + 1) * XW], writes=[B[f"xs{s}"]])
                fw.op("act", lambda k=k, hf=hf: nc.scalar.activation(out=hT[:, k, hf * XW:(hf + 1) * XW], in_=xs[s][:], func=AF.Identity, scale=mc[:, k, 0:1], bias=mc[:, k, 1:2]),
                      reads=[B[f"xs{s}"], B["mc"]], writes=[B["hT"]])
        ctr = [0]
        wbv = wb[:].rearrange("p k (h w) -> p (k h) w", h=2)
        winv = win.rearrange("p k (h w) -> p (k h) w", h=2)
        load_cast(fw, nc, wbv, B["wb"], lambda k: winv[:, k, :], 16, 768, wst, [B["wst0"], B["wst1"]], ctr)
        for s in range(2):
            fw.op("pool", lambda s=s: G.memset(pb[s][:, 0:1], 0.0), writes=[B[f"pb{s}"]])
            fw.op("pool", lambda s=s: G.memset(pb[s][:, L + 1:L + 2], 0.0), writes=[B[f"pb{s}"]])
        it = 0
        for st_ in range(4):
            for q in range(3):
                s = it % 2
                it += 1
                col0 = (st_ * 3 + q) * 128
                for tg in range(NTL):
                    pa = tg % 4
                    for k in range(8):
                        fw.op("pe", lambda k=k: nc.tensor.matmul(pacc[pa][:, :TW], lhsT=wb[:, k, col0:col0 + 128], rhs=hT[:, k, tg * TW:(tg + 1) * TW], start=(k == 0), stop=(k == 7)),
                              reads=[B["wb"], B["hT"]], writes=[Bpacc[pa]])
                    fw.op("act", lambda: nc.scalar.copy(out=pb[s][:, 1 + tg * TW:1 + (tg + 1) * TW], in_=pacc[pa][:, :TW]), reads=[Bpacc[pa]], writes=[B[f"pb{s}"]])
                ci = st_ * 3 + q
                fw.op("act", lambda: nc.scalar.activation(out=ob[s][:], in_=pb[s][:, 1:L + 1], func=AF.Identity, scale=cwt[:, ci, 1:2], bias=cwt[:, ci, 3:4]),
                      reads=[B[f"pb{s}"], B["cwt"]], writes=[B[f"ob{s}"]])
                fw.op("dve", lambda: V.scalar_tensor_tensor(out=ob[s][:], in0=pb[s][:, 0:L], scalar=cwt[:, ci, 0:1], in1=ob[s][:], op0=ALU.mult, op1=ALU.add),
                      reads=[B[f"pb{s}"], B["cwt"], B[f"ob{s}"]], writes=[B[f"ob{s}"]])
                fw.op("dve", lambda: V.scalar_tensor_tensor(out=ob[s][:], in0=pb[s][:, 2:L + 2], scalar=cwt[:, ci, 2:3], in1=ob[s][:], op0=ALU.mult, op1=ALU.add),
                      reads=[B[f"pb{s}"], B["cwt"], B[f"ob{s}"]], writes=[B[f"ob{s}"]])
                if q == 0:
                    for k4 in range(max(1, NK // 4)):
                        ps_ = k4 % 2
                        nn = min(4, NK)
                        for kq in range(nn):
                            k = k4 * 4 + kq
                            fw.op("pe", lambda k=k, kq=kq: nc.tensor.transpose(out=ptr[ps_][:, kq, :], in_=ob[s][:, k * 128:(k + 1) * 128], identity=idf[:]),
                                  reads=[B[f"ob{s}"], B["idf"]], writes=[B[f"ptr{ps_}"]])
                        fw.op("act", lambda: nc.scalar.copy(out=x_tm[:, st_, k4 * 4:k4 * 4 + nn, :], in_=ptr[ps_][:, 0:nn, :]), reads=[B[f"ptr{ps_}"]], writes=[B["x_tm"]])
                else:
                    fw.dma("sp", x12[st_, q - 1], ob[s][:], reads=[B[f"ob{s}"]], writes=[B["x12"]])
        scA.__exit__(None, None, None)
        slabs = [fw.sb(f"slab{i}", [128, NK, TW], BF16) for i in range(2)]
        Bslab = [B["slab0"], B["slab1"]]
        y_fm = fw.sb("y_fm", [128, 4, 2, NK, 128], BF16)
        Xr = fw.sb("Xr", [128, 4, TW], F32)
        kt = [fw.sb(f"kt{i}", [128, 2, TW], F32) for i in range(2)]
        Y = fw.sb("Y", [128, 2, TW], F32)
        ta = fw.sb("ta", [128, TW], F32)
        tb = fw.sb("tb", [128, TW], F32)
        xq = [fw.sb(f"xq{i}", [128, TW], F32) for i in range(2)]
        zb = [fw.sb(f"zb{i}", [128, TW], F32) for i in range(2)]
        zo = [fw.sb(f"zo{i}", [128, TW], BF16) for i in range(2)]
        cnt = {"kt": 0, "tr": 0, "xq": 0, "z": 0}
        for o in range(2):
            def consume(nt, which, st_, ps):
                fsl = slice(nt * TW, (nt + 1) * TW)
                if which == 0:
                    fw.op("act", lambda: nc.scalar.copy(out=Xr[:, st_, :], in_=ps), reads=[Bpacc[st_]], writes=[B["Xr"]])
                    return
                ks = cnt["kt"] % 2
                cnt["kt"] += 1
                fw.dma("pool", kt[ks][:], Kfi[st_, :, o, :, fsl], writes=[B[f"kt{ks}"]])
                Kr, Ki = kt[ks][:, 0, :], kt[ks][:, 1, :]
                rd = [B["Xr"], B[f"kt{ks}"]]
                fw.op("pool", lambda: G.tensor_tensor(out=ta[:], in0=Xr[:, st_, :], in1=Kr, op=ALU.mult), reads=rd, writes=[B["ta"]])
                fw.op("dve", lambda: V.tensor_tensor(out=tb[:], in0=ps, in1=Ki, op=ALU.mult), reads=[Bpacc[st_], B[f"kt{ks}"]], writes=[B["tb"]])
                fw.op("pool", lambda: G.tensor_tensor(out=Y[:, 0, :], in0=ta[:], in1=tb[:], op=ALU.subtract), reads=[B["ta"], B["tb"]], writes=[B["Y"]])
                fw.op("pool", lambda: G.tensor_tensor(out=ta[:], in0=Xr[:, st_, :], in1=Ki, op=ALU.mult), reads=rd, writes=[B["ta"]])
                fw.op("dve", lambda: V.tensor_tensor(out=tb[:], in0=ps, in1=Kr, op=ALU.mult), reads=[Bpacc[st_], B[f"kt{ks}"]], writes=[B["tb"]])
                fw.op("pool", lambda: G.tensor_tensor(out=Y[:, 1, :], in0=ta[:], in1=tb[:], op=ALU.add), reads=[B["ta"], B["tb"]], writes=[B["Y"]])
                if nt == 0:
                    fw.op("dve", lambda: V.tensor_tensor(out=Y[:, 0, 0:1], in0=Xr[:, st_, 0:1], in1=kt[ks][:, 0, 0:1], op=ALU.mult), reads=rd, writes=[B["Y"]])
                    fw.op("dve", lambda: V.tensor_tensor(out=Y[:, 1, 0:1], in0=ps[:, 0:1], in1=kt[ks][:, 1, 0:1], op=ALU.mult), reads=[Bpacc[st_], B[f"kt{ks}"]], writes=[B["Y"]])
                for ri in range(2):
                    ps_ = cnt["tr"] % 2
                    cnt["tr"] += 1
                    for j in range(TJ):
                        fw.op("pe", lambda j=j: nc.tensor.transpose(out=ptr[ps_][:, j, :], in_=Y[:, ri, j * 128:(j + 1) * 128], identity=idf[:]),
                              reads=[B["Y"], B["idf"]], writes=[B[f"ptr{ps_}"]])
                    fw.op("act", lambda: nc.scalar.copy(out=y_fm[:, st_, ri, nt * TJ:(nt + 1) * TJ, :], in_=ptr[ps_][:, 0:TJ, :]), reads=[B[f"ptr{ps_}"]], writes=[B["y_fm"]])
            emit_fwd_dft(fw, nc, L, x_tm, B["x_tm"], tabC, tabS, slabs, Bslab, pacc, Bpacc, consume)
            for tt in range(NTL):
                tsl = slice(tt * TW, (tt + 1) * TW)
                for ri, tab in ((0, tabC), (1, tabST)):
                    fw.dma("sp" if ri == 0 else "act", slabs[ri][:, :, :], tab[tt], writes=[Bslab[ri]])
                    for st_ in range(4):
                        for k in range(NK):
                            fw.op("pe", lambda k=k: nc.tensor.matmul(pacc[st_][:, :TW], lhsT=y_fm[:, st_, ri, k, :], rhs=slabs[ri][:, k, :],
                                                                      start=(ri == 0 and k == 0), stop=(ri == 1 and k == NK - 1)),
                                  reads=[B["y_fm"], Bslab[ri]], writes=[Bpacc[st_]])
                for st_ in range(4):
                    xs_ = cnt["xq"] % 2
                    cnt["xq"] += 1
                    fw.dma("pool", xq[xs_][:], x12[st_, o, :, tsl], reads=[B["x12"]], writes=[B[f"xq{xs_}"]])
                    zs = cnt["z"] % 2
                    cnt["z"] += 1
                    if o == 0:
                        fw.op("dve", lambda: V.tensor_tensor(out=zb[zs][:], in0=pacc[st_][:, :TW], in1=xq[xs_][:], op=ALU.mult), reads=[Bpacc[st_], B[f"xq{xs_}"]], writes=[B[f"zb{zs}"]])
                        ps_ = cnt["tr"] % 2
                        cnt["tr"] += 1
                        for j in range(TJ):
                            fw.op("pe", lambda j=j: nc.tensor.transpose(out=ptr[ps_][:, j, :], in_=zb[zs][:, j * 128:(j + 1) * 128], identity=idf[:]),
                                  reads=[B[f"zb{zs}"], B["idf"]], writes=[B[f"ptr{ps_}"]])
                        fw.op("act", lambda: nc.scalar.copy(out=x_tm[:, st_, tt * TJ:(tt + 1) * TJ, :], in_=ptr[ps_][:, 0:TJ, :]), reads=[B[f"ptr{ps_}"]], writes=[B["x_tm"]])
                    else:
                        fw.op("dve", lambda: V.tensor_tensor(out=zo[zs][:], in0=pacc[st_][:, :TW], in1=xq[xs_][:], op=ALU.mult), reads=[Bpacc[st_], B[f"xq{xs_}"]], writes=[B[f"zo{zs}"]])
                        fw.dma("sp", zTo[:, st_, tsl], zo[zs][:], reads=[B[f"zo{zs}"]], writes=[B["o"]])
        fw.finish([B["o"]])
    return nc


def _ident_f():
    return np.eye(128, dtype=np.float32)


def _ident_b():
    return np.eye(128, dtype=np.float32).astype(NPBF)


def stage_filters(L, slot, p):
    nc = cached(("F", L), lambda: build_F(L))
    zT, decay = hyena_consts(L)
    tC, tS, _ = dft_tables(L)
    vecs = np.ascontiguousarray(np.stack([p["hy_f_b1"][slot], p["hy_f_b2"][slot], p["hy_f_freq"][slot, 0], p["hy_f_freq"][slot, 1]], axis=-1))
    maps = []
    for core in range(NCORES):
        ch = slice(core * 128, (core + 1) * 128)
        w3 = p["hy_f_w3"][slot].reshape(64, 2, 2, D)[:, :, :, ch].reshape(64, 4, 128)
        maps.append({"zT": zT, "decay": np.ascontiguousarray(decay[ch]), "w1": np.ascontiguousarray(p["hy_f_w1"][slot]),
                     "w2": np.ascontiguousarray(p["hy_f_w2"][slot]), "w3": np.ascontiguousarray(w3), "vecs": vecs,
                     "fbias": np.ascontiguousarray(p["hy_f_bias"][slot][:, ch].T), "identf": _ident_f(), "tabC": tC, "tabS": tS})
    res = run(nc, maps)
    return np.stack([res[c]["Kf"] for c in range(NCORES)])


def stage_hyena(L, slot, xs, mrows, mv, KfAll, p):
    nc = cached(("MH", L), lambda: build_MH(L))
    tC, tS, tST = dft_tables(L)
    sh1, sc1 = mv[:, 0:D], mv[:, D:2 * D]
    maps = []
    for core in range(NCORES):
        b, h = core // 2, core % 2
        r = mrows[b]
        mc = np.stack([sc1[r], sh1[r]], axis=-1).reshape(8, 128, 2).transpose(1, 0, 2)
        cols = np.concatenate([np.arange(q * D + 512 * h + 128 * s, q * D + 512 * h + 128 * s + 128) for s in range(4) for q in range(3)])
        cwm = np.concatenate([p["hy_conv_w"][slot][:, cols], p["hy_conv_b"][slot][None, cols]], axis=0).reshape(4, 12, 128).transpose(2, 1, 0)
        maps.append({"xT": fm_layout(np.ascontiguousarray(xs[b].T)), "mcol": np.ascontiguousarray(mc), "win": fm_layout(p["hy_w_in"][slot][:, cols]),
                     "cw": np.ascontiguousarray(cwm), "Kf": np.ascontiguousarray(KfAll[4 * h:4 * h + 4]), "tabC": tC, "tabS": tS, "tabST": tST,
                     "identf": _ident_f()})
    res = run(nc, maps)
    return [np.concatenate([res[2 * b]["zT"], res[2 * b + 1]["zT"]], axis=1) for b in range(NB)]


def stage_attn(x_lat, x_ctx, mv, p):
    nc = cached(("MA",), build_MA)
    sh1, sc1 = mv[:, 0:D], mv[:, D:2 * D]
    wqkv = p["at_w_qkv"][0]
    gains = np.ascontiguousarray(np.concatenate([np.tile(p["at_q_gain"][0], 8), np.tile(p["at_k_gain"][0], 2)])[None, :])
    cs = rope_tables()
    maps = []
    for core in range(NCORES):
        b, h = core // 2, core % 2
        mc = np.stack([sc1[b], sh1[b], sc1[4], sh1[4]], axis=-1).reshape(8, 128, 4).transpose(1, 0, 2)
        wcat = np.concatenate([wqkv[:, 512 * h:512 * h + 512], wqkv[:, 1024 + 128 * h:1024 + 128 * h + 128],
                               wqkv[:, 1280 + 128 * h:1280 + 128 * h + 128]], axis=1)
        maps.append({"xT": fm_layout(np.ascontiguousarray(x_lat[b].T)), "cxT": fm_layout(np.ascontiguousarray(x_ctx[b].T)),
                     "mcol": np.ascontiguousarray(mc), "w": fm_layout(wcat), "gains": gains, "cs": cs, "identb": _ident_b()})
    res = run(nc, maps)
    return [np.concatenate([res[2 * b]["oT"], res[2 * b + 1]["oT"]], axis=1) for b in range(NB)]


def stage_gmlp(x_lat, mv, p):
    nc = cached(("MG",), lambda: build_MG(16))
    sh1, sc1 = mv[:, 0:D], mv[:, D:2 * D]
    lnr = np.ascontiguousarray(np.concatenate([p["cm_ln_g"][0], p["cm_ln_b"][0]])[None, :])
    wsT = np.ascontiguousarray(p["cm_w_s"][0].transpose(2, 0, 1))
    bsr = np.ascontiguousarray(p["cm_b_s"][0].reshape(1, -1))
    win = fm_layout(p["cm_w_in"][0])
    maps = []
    for core in range(NCORES):
        b, hf = core // 2, core % 2
        mc = np.stack([sc1[b], sh1[b]], axis=-1).reshape(8, 128, 2).transpose(1, 0, 2)
        maps.append({"xT": fm_layout(np.ascontiguousarray(x_lat[b, hf * 2048:(hf + 1) * 2048].T)), "mcol": np.ascontiguousarray(mc), "win": win,
                     "lnr": lnr, "wsT": wsT, "bsr": bsr})
    res = run(nc, maps)
    return [np.concatenate([res[2 * b]["gT"], res[2 * b + 1]["gT"]], axis=2) for b in range(NB)]


def stage_norm_router(aT, w_out, xs, mv, mrows, i, p, T):
    KC = aT[0].shape[1]
    NT = T // 128
    Lseq = xs.shape[1]
    per_b = Lseq // T
    nc = cached(("N", KC, NT), lambda: build_N(KC, NT))
    wo = fm_layout(w_out)
    wr = fm_layout(p["moe_router"][i])
    maps = []
    for core in range(NCORES):
        b, hf = core // per_b, core % per_b
        r = mrows[b]
        rows = np.concatenate([mv[r, 2 * D:3 * D], p["ln_g"][i, 0], p["ln_b"][i, 0], mv[r, 4 * D:5 * D], mv[r, 3 * D:4 * D]])[None, :]
        maps.append({"aT": np.ascontiguousarray(aT[b][:, :, hf * T:(hf + 1) * T]), "wo": wo, "x": np.ascontiguousarray(xs[b, hf * T:(hf + 1) * T]),
                     "rows": np.ascontiguousarray(rows), "wr": wr, "ident": _ident_f()})
    res = run(nc, maps)
    x1 = np.stack([np.concatenate([res[b * per_b + hf]["x1"] for hf in range(per_b)], axis=0) for b in range(NB)])
    h2 = np.concatenate([res[c]["h2"] for c in range(NCORES)], axis=0)
    aff = np.stack([np.concatenate([res[b * per_b + hf]["aff"] for hf in range(per_b)], axis=0) for b in range(NB)])
    return x1, h2, aff


def stage_experts(aff, h2, i, p, CAP, GB):
    Lseq = aff.shape[1]
    NTT = Lseq // 128
    NS = GB * CAP
    nc = cached(("E", NTT, CAP, GB), lambda: build_E(NTT, CAP, GB))
    tokid = (np.arange(NB)[None, :, None] * Lseq + np.arange(NTT)[None, None, :] * 128 + np.arange(128)[:, None, None])
    tok = np.ascontiguousarray(np.stack([tokid // 128, tokid % 128], axis=1).astype(np.float32))
    so = np.zeros((128, NB, 2), np.float32)
    so += ((np.arange(NB) % GB) * CAP).astype(np.float32)[None, :, None]
    so = np.ascontiguousarray(so.reshape(128, 8))
    tri = np.triu(np.ones((128, 128), np.float32), 1).astype(NPBF)
    iota = np.ascontiguousarray(np.broadcast_to(np.arange(NS, dtype=np.float32), (128, NS)))
    maps = []
    for core in range(NCORES):
        e0 = 2 * core
        a = aff[:, :, e0:e0 + 2].reshape(NB, NTT, 128, 2).transpose(2, 0, 3, 1).reshape(128, 8, NTT)
        maps.append({"aff": np.ascontiguousarray(a), "h2": h2, "tok": tok, "slotoff": so, "tri": tri, "iota": iota, "identb": _ident_b(),
                     "wg": np.ascontiguousarray(p["moe_w_gate"][i, e0:e0 + 2].reshape(2, 8, 128, FF).transpose(0, 2, 1, 3)),
                     "wu": np.ascontiguousarray(p["moe_w_up"][i, e0:e0 + 2].reshape(2, 8, 128, FF).transpose(0, 2, 1, 3)),
                     "wd": np.ascontiguousarray(p["moe_w_down"][i, e0:e0 + 2].reshape(2, 16, 128, D).transpose(0, 2, 1, 3))})
    res = run(nc, maps)
    yc = np.concatenate([res[c]["yc"] for c in range(NCORES)], axis=0)
    pt = np.stack([res[c]["postab"] for c in range(NCORES)], axis=0)
    return yc, pt


def stage_combine(x1, yc, pt, mv, mrows, i, p, T, GB):
    Lseq = x1.shape[1]
    NT = T // 128
    per_b = Lseq // T
    NS = yc.shape[2] - 128
    nc = cached(("P", NT, NS), lambda: build_P(NT, NS))
    maps = []
    for core in range(NCORES):
        b, hf = core // per_b, core % per_b
        r = mrows[b]
        rows = np.concatenate([mv[r, 5 * D:6 * D], p["ln_g"][i, 1], p["ln_b"][i, 1]])[None, :]
        ycb = yc[:, b // GB].reshape(16 * (NS + 128), D)
        ptb = pt[:, :, 2 * b:2 * b + 2, hf * NT:(hf + 1) * NT]
        ptb = ptb.transpose(1, 0, 2, 3).reshape(128, 16, NT)
        maps.append({"x1": np.ascontiguousarray(x1[b, hf * T:(hf + 1) * T]), "ycb": np.ascontiguousarray(ycb), "postab": np.ascontiguousarray(ptb),
                     "rows": np.ascontiguousarray(rows)})
    res = run(nc, maps)
    return np.stack([np.concatenate([res[b * per_b + hf]["x2"] for hf in range(per_b)], axis=0) for b in range(NB)])


def moe_block(aT, w_out, xs, mv, mrows, i, p, T, CAP, GB):
    x1, h2, aff = stage_norm_router(aT, w_out, xs, mv, mrows, i, p, T)
    yc, pt = stage_experts(aff, h2, i, p, CAP, GB)
    return stage_combine(x1, yc, pt, mv, mrows, i, p, T, GB)


def kernel(**inputs):
    p = {k: np.asarray(v) for k, v in inputs.items()}
    x_lat = np.ascontiguousarray(p["x"], dtype=np.float32)
    x_ctx = np.ascontiguousarray(p["ctx"], dtype=np.float32)
    modvec = run_A(p["c"], p["c_ctx"], p["mod_w"], p["mod_b"])
    lat_rows = [0, 1, 2, 3]
    ctx_rows = [4, 4, 4, 4]
    mv = modvec[0]
    Kf = stage_filters(SEQ, 0, p)
    aT = stage_hyena(SEQ, 0, x_lat, lat_rows, mv, Kf, p)
    Kfc = stage_filters(CTX, 0, p)
    aTc = stage_hyena(CTX, 0, x_ctx, ctx_rows, mv, Kfc, p)
    x_lat = moe_block(aT, p["hy_w_out"][0], x_lat, mv, lat_rows, 0, p, 2048, 512, 1)
    x_ctx = moe_block(aTc, p["hy_w_out"][0], x_ctx, mv, ctx_rows, 0, p, 128, 32, 4)
    mv = modvec[1]
    aT = stage_attn(x_lat, x_ctx, mv, p)
    x_lat = moe_block(aT, p["at_w_out"][0], x_lat, mv, lat_rows, 1, p, 2048, 512, 1)
    mv = modvec[2]
    aT = stage_gmlp(x_lat, mv, p)
    x_lat = moe_block(aT, p["cm_w_out"][0], x_lat, mv, lat_rows, 2, p, 2048, 512, 1)
    mv = modvec[3]
    Kf = stage_filters(SEQ, 1, p)
    aT = stage_hyena(SEQ, 1, x_lat, lat_rows, mv, Kf, p)
    x_lat = moe_block(aT, p["hy_w_out"][1], x_lat, mv, lat_rows, 3, p, 2048, 512, 1)
    return x_lat.astype(np.float32)
```

```python
import math
from contextlib import ExitStack

import numpy as np
import ml_dtypes
import concourse.bass as bass
import concourse.mybir as mybir
from concourse.bass_utils import run_bass_kernel_spmd

F32 = mybir.dt.float32
BF16 = mybir.dt.bfloat16
I32 = mybir.dt.int32
U32 = mybir.dt.uint32
ALU = mybir.AluOpType
AF = mybir.ActivationFunctionType
AX = mybir.AxisListType
NPBF = ml_dtypes.bfloat16

D = 1024
NB = 4
SEQ = 4096
CTX = 256
DEPTH = 4
NE = 16
FF = 2048
LN_EPS = 1e-5
RMS_EPS = 1e-6
ALPHA = (2 * DEPTH) ** 0.25
NCORES = 8
SAME_ENGINE_SYNC = True


class Buf:
    __slots__ = ("name", "w", "r")

    def __init__(self, name):
        self.name = name
        self.w = None
        self.r = {}


class FW:
    def __init__(self, nc, stack):
        self.nc = nc
        self.stack = stack
        self.engs = {"pe": nc.tensor, "dve": nc.vector, "act": nc.scalar, "pool": nc.gpsimd, "sp": nc.sync}
        self.sems = {}
        self.cnt = {}
        self.seen = {k: {} for k in self.engs}
        for k in self.engs:
            self.sems[k] = stack.enter_context(nc.semaphore("s_" + k))
            self.cnt[k] = 0
        self.same_engine_sync = SAME_ENGINE_SYNC
        self.semstack = stack

    def sb(self, name, shape, dt):
        return self.stack.enter_context(self.nc.sbuf_tensor(name, list(shape), dt))

    def ps(self, name, shape, dt=F32):
        return self.stack.enter_context(self.nc.psum_tensor(name, list(shape), dt))

    def dma_sem(self, name):
        key = "d_" + name
        if key not in self.sems:
            self.sems[key] = self.semstack.enter_context(self.nc.semaphore(key))
            self.cnt[key] = 0
        return key

    def _wait(self, e, key, val):
        if key == e and (e == "pe" or not self.same_engine_sync):
            return
        if self.seen[e].get(key, 0) >= val:
            return
        self.engs[e].wait_ge(self.sems[key], val)
        self.seen[e][key] = val

    def _deps(self, e, reads, writes):
        for b in reads:
            if b.w is not None:
                self._wait(e, *b.w)
        for b in writes:
            if b.w is not None:
                self._wait(e, *b.w)
            for k, v in b.r.items():
                self._wait(e, k, v)

    def op(self, e, fn, reads=(), writes=()):
        self._deps(e, reads, writes)
        ins = fn()
        self.cnt[e] += 1
        ins.then_inc(self.sems[e], 1)
        for b in reads:
            b.r[e] = self.cnt[e]
        for b in writes:
            b.w = (e, self.cnt[e])
            b.r = {}
        return ins

    def dma(self, q, out, in_, reads=(), writes=(), semname=None, indirect=None, **kw):
        self._deps(q, reads, writes)
        name = semname or (writes[0].name if writes else reads[0].name + "_st")
        key = self.dma_sem(name)
        if indirect is None:
            ins = self.engs[q].dma_start(out=out, in_=in_, **kw)
        else:
            ins = self.nc.gpsimd.indirect_dma_start(out=out, in_=in_, **indirect)
        self.cnt[key] += 16
        ins.then_inc(self.sems[key], 16)
        for b in reads:
            b.r[key] = self.cnt[key]
        for b in writes:
            b.w = (key, self.cnt[key])
            b.r = {}
        return ins

    def barrier(self):
        for e in self.engs:
            for key, c in self.cnt.items():
                if key != e and c > 0:
                    self._wait(e, key, c)

    def scoped(self):
        fw = self

        class _Scope:
            def __enter__(self_):
                self_.prev = fw.stack
                self_.st = ExitStack()
                self_.st.__enter__()
                fw.stack = self_.st
                return fw

            def __exit__(self_, *a):
                fw.barrier()
                fw.stack = self_.prev
                return self_.st.__exit__(*a)
        return _Scope()

    def seal(self, bufs):
        key = bufs[0].w[0]
        for b in bufs:
            b.w = (key, self.cnt[key])

    def finish(self, bufs, e="sp"):
        for b in bufs:
            if b.w is not None:
                self._wait(e, *b.w)


def new_nc():
    return bass.Bass("TRN2", target_bir_lowering=False)


def dram_in(nc, name, shape, dt=F32):
    return nc.dram_tensor(name, list(shape), dt, kind="ExternalInput").ap()


def dram_out(nc, name, shape, dt=F32):
    return nc.dram_tensor(name, list(shape), dt, kind="ExternalOutput").ap()


_PROFILE = []


def run(nc, in_maps, tag=""):
    res = run_bass_kernel_spmd(nc, in_maps, core_ids=list(range(NCORES)))
    if getattr(res, "exec_time_ns", None):
        _PROFILE.append((tag, res.exec_time_ns))
    return res.results


def build_A():
    nc = new_nc()
    cT = dram_in(nc, "cT", [128, 8, 5])
    w = dram_in(nc, "w", [128, 8, 3072])
    b = dram_in(nc, "b", [1, 3072])
    m = dram_out(nc, "m", [5, 3072])
    with ExitStack() as st:
        fw = FW(nc, st)
        ct = fw.sb("ct", [128, 8, 5], F32)
        bt = fw.sb("bt", [5, 3072], F32)
        mt = fw.sb("mt", [5, 3072], F32)
        wt = [fw.sb(f"wt{i}", [128, 8, 512], F32) for i in range(2)]
        pt = [fw.ps(f"pt{i}", [5, 512]) for i in range(2)]
        Bc, Bb, Bm, Bo = Buf("ct"), Buf("bt"), Buf("mt"), Buf("mo")
        Bw = [Buf("wt0"), Buf("wt1")]
        Bp = [Buf("pt0"), Buf("pt1")]
        fw.dma("sp", ct[:], cT, writes=[Bc])
        fw.dma("sp", bt[:], b.partition_broadcast(5), writes=[Bb])
        fw.op("act", lambda: nc.scalar.activation(out=ct[:], in_=ct[:], func=AF.Silu), reads=[Bc], writes=[Bc])
        for j in range(6):
            s = j % 2
            fw.dma("sp" if s == 0 else "pool", wt[s][:], w[:, :, j * 512:(j + 1) * 512], writes=[Bw[s]])
            for k in range(8):
                fw.op("pe", lambda k=k: nc.tensor.matmul(pt[s][:], lhsT=ct[:, k, :], rhs=wt[s][:, k, :],
                                                           start=(k == 0), stop=(k == 7)),
                      reads=[Bc, Bw[s]], writes=[Bp[s]])
            fw.op("dve", lambda: nc.vector.tensor_tensor(out=mt[:, j * 512:(j + 1) * 512], in0=pt[s][:],
                                                         in1=bt[:, j * 512:(j + 1) * 512], op=ALU.add),
                  reads=[Bp[s], Bb], writes=[Bm])
        fw.dma("sp", m, mt[:], reads=[Bm], writes=[Bo])
        fw.finish([Bo])
    return nc


def run_A(c, c_ctx, mod_w, mod_b):
    nc = build_A()
    cc = np.concatenate([c, c_ctx[None, :]], axis=0)
    cT = np.ascontiguousarray(cc.T.reshape(8, 128, 5).transpose(1, 0, 2))
    maps = []
    for core in range(NCORES):
        i, hf = core // 2, core % 2
        wv = mod_w[i][:, hf * 3072:(hf + 1) * 3072].reshape(8, 128, 3072).transpose(1, 0, 2)
        maps.append({"cT": cT, "w": np.ascontiguousarray(wv),
                     "b": np.ascontiguousarray(mod_b[i][None, hf * 3072:(hf + 1) * 3072])})
    res = run(nc, maps)
    out = np.zeros((DEPTH, 5, 6 * D), np.float32)
    for core in range(NCORES):
        i, hf = core // 2, core % 2
        out[i][:, hf * 3072:(hf + 1) * 3072] = res[core]["m"]
    return out


def layer_norm_tile(fw, nc, u, Bu, stats, mv, rstd, Bs, nparts=128):
    for j in range(2):
        fw.op("dve", lambda j=j: nc.vector.bn_stats(out=stats[:, j, :], in_=u[:, j * 512:(j + 1) * 512]),
              reads=[Bu], writes=[Bs])
    fw.op("dve", lambda: nc.vector.bn_aggr(out=mv[:], in_=stats[:].rearrange("p a b -> p (a b)")), reads=[Bs], writes=[Bs])
    fw.op("act", lambda: nc.scalar.activation(out=rstd[:], in_=mv[:, 1:2], func=AF.Sqrt, bias=fw.eps_ln[:, 0:1], scale=1.0),
          reads=[Bs, fw.Bconst], writes=[Bs])
    fw.op("dve", lambda: nc.vector.reciprocal(out=rstd[:], in_=rstd[:]), reads=[Bs], writes=[Bs])
    fw.op("dve", lambda: nc.vector.tensor_scalar(out=u[:], in0=u[:], scalar1=mv[:, 0:1], scalar2=rstd[:, 0:1],
                                                 op0=ALU.subtract, op1=ALU.mult), reads=[Bs, Bu], writes=[Bu])


def make_consts(fw, nc):
    fw.eps_ln = fw.sb("eps_ln", [128, 1], F32)
    fw.Bconst = Buf("consts")
    fw.op("pool", lambda: nc.gpsimd.memset(fw.eps_ln[:], LN_EPS), writes=[fw.Bconst])


def build_N(KC, NT):
    T = NT * 128
    nc = new_nc()
    aT = dram_in(nc, "aT", [128, KC, T], BF16)
    wo = dram_in(nc, "wo", [128, KC, 1024])
    x = dram_in(nc, "x", [T, 1024])
    rows = dram_in(nc, "rows", [1, 5 * 1024])
    wr = dram_in(nc, "wr", [128, 8, 16])
    ident = dram_in(nc, "ident", [128, 128])
    x1o = dram_out(nc, "x1", [T, 1024])
    h2o = dram_out(nc, "h2", [T, 1024], BF16)
    affo = dram_out(nc, "aff", [T, 16])
    with ExitStack() as st:
        fw = FW(nc, st)
        make_consts(fw, nc)
        wob = fw.sb("wob", [128, KC, 1024], BF16)
        wst = [fw.sb(f"wst{i}", [128, 1024], F32) for i in range(2)]
        rw = fw.sb("rw", [128, 5, 1024], F32)
        wrt = fw.sb("wrt", [128, 8, 16], F32)
        idt = fw.sb("idt", [128, 128], F32)
        at = [fw.sb(f"at{i}", [128, KC, 128], BF16) for i in range(2)]
        xt = [fw.sb(f"xt{i}", [128, 1024], F32) for i in range(2)]
        u = [fw.sb(f"u{i}", [128, 1024], F32) for i in range(2)]
        h2 = [fw.sb(f"h2{i}", [128, 1024], F32) for i in range(2)]
        h2b = [fw.sb(f"h2b{i}", [128, 1024], BF16) for i in range(2)]
        h2T = fw.sb("h2T", [128, 8, 128], F32)
        stats = fw.sb("stats", [128, 2, 6], F32)
        mv = fw.sb("mv", [128, 2], F32)
        rstd = fw.sb("rstd", [128, 1], F32)
        sm = fw.sb("sm", [128, 4], F32)
        aft = [fw.sb(f"aft{i}", [128, 16], F32) for i in range(2)]
        py = [fw.ps(f"py{i}", [128, 512]) for i in range(2)]
        ptr = [fw.ps(f"ptr{i}", [128, 4, 128]) for i in range(2)]
        pl = fw.ps("pl", [128, 16])
        Bwo, Brw, Bwr, Bid = Buf("wob"), Buf("rw"), Buf("wrt"), Buf("idt")
        Bwst = [Buf("wst0"), Buf("wst1")]
        Bat = [Buf("at0"), Buf("at1")]
        Bxt = [Buf("xt0"), Buf("xt1")]
        Bu = [Buf("u0"), Buf("u1")]
        Bh2 = [Buf("h20"), Buf("h21")]
        Bh2b = [Buf("h2b0"), Buf("h2b1")]
        Bh2T, Bs, Bsm = Buf("h2T"), Buf("stats"), Buf("sm")
        Baf = [Buf("aft0"), Buf("aft1")]
        Bpy = [Buf("py0"), Buf("py1")]
        Bptr = [Buf("ptr0"), Buf("ptr1")]
        Bpl = Buf("pl")
        Bout = [Buf("o_x1"), Buf("o_h2"), Buf("o_aff")]
        fw.dma("sp", rw[:].rearrange("p a b -> p (a b)"), rows.partition_broadcast(128), writes=[Brw])
        fw.dma("sp", wrt[:], wr, writes=[Bwr])
        fw.dma("sp", idt[:], ident, writes=[Bid])
        for kc in range(KC):
            s = kc % 2
            fw.dma("sp" if s == 0 else "pool", wst[s][:], wo[:, kc, :], writes=[Bwst[s]])
            fw.op("act" if s == 0 else "pool",
                  (lambda: nc.scalar.copy(out=wob[:, kc, :], in_=wst[s][:])) if s == 0 else
                  (lambda: nc.gpsimd.tensor_copy(out=wob[:, kc, :], in_=wst[s][:])),
                  reads=[Bwst[s]], writes=[Bwo])
        fw.op("dve", lambda: nc.vector.tensor_scalar(out=rw[:, 3, :], in0=rw[:, 3, :], scalar1=1.0, scalar2=None, op0=ALU.add),
              reads=[Brw], writes=[Brw])
        for t in range(NT):
            s = t % 2
            fw.dma("sp", at[s][:], aT[:, :, t * 128:(t + 1) * 128], writes=[Bat[s]])
            fw.dma("pool", xt[s][:], x[t * 128:(t + 1) * 128, :], writes=[Bxt[s]])
            for hf in range(2):
                for kc in range(KC):
                    fw.op("pe", lambda kc=kc, hf=hf: nc.tensor.matmul(py[hf][:], lhsT=at[s][:, kc, :],
                                                                       rhs=wob[:, kc, hf * 512:(hf + 1) * 512],
                                                                       start=(kc == 0), stop=(kc == KC - 1)),
                          reads=[Bat[s], Bwo], writes=[Bpy[hf]])
            for hf in range(2):
                sl = slice(hf * 512, (hf + 1) * 512)
                fw.op("dve", lambda: nc.vector.tensor_tensor(out=u[s][:, sl], in0=py[hf][:], in1=rw[:, 0, sl], op=ALU.mult),
                      reads=[Bpy[hf], Brw], writes=[Bu[s]])
            fw.op("dve", lambda: nc.vector.scalar_tensor_tensor(out=u[s][:], in0=xt[s][:], scalar=ALPHA, in1=u[s][:],
                                                                op0=ALU.mult, op1=ALU.add),
                  reads=[Bxt[s], Bu[s]], writes=[Bu[s]])
            layer_norm_tile(fw, nc, u[s], Bu[s], stats, mv, rstd, Bs)
            fw.op("pool", lambda: nc.gpsimd.tensor_tensor(out=u[s][:], in0=u[s][:], in1=rw[:, 1, :], op=ALU.mult),
                  reads=[Bu[s], Brw], writes=[Bu[s]])
            fw.op("pool", lambda: nc.gpsimd.tensor_tensor(out=u[s][:], in0=u[s][:], in1=rw[:, 2, :], op=ALU.add),
                  reads=[Bu[s], Brw], writes=[Bu[s]])
            fw.dma("sp", x1o[t * 128:(t + 1) * 128, :], u[s][:], reads=[Bu[s]], writes=[Bout[0]])
            fw.op("dve", lambda: nc.vector.tensor_tensor(out=h2[s][:], in0=u[s][:], in1=rw[:, 3, :], op=ALU.mult),
                  reads=[Bu[s], Brw], writes=[Bh2[s]])
            fw.op("dve", lambda: nc.vector.tensor_tensor(out=h2[s][:], in0=h2[s][:], in1=rw[:, 4, :], op=ALU.add),
                  reads=[Bh2[s], Brw], writes=[Bh2[s]])
            fw.op("act", lambda: nc.scalar.copy(out=h2b[s][:], in_=h2[s][:]), reads=[Bh2[s]], writes=[Bh2b[s]])
            fw.dma("sp", h2o[t * 128:(t + 1) * 128, :], h2b[s][:], reads=[Bh2b[s]], writes=[Bout[1]])
            for g in range(2):
                for k4 in range(4):
                    k = g * 4 + k4
                    fw.op("pe", lambda k=k, k4=k4: nc.tensor.transpose(out=ptr[g][:, k4, :], in_=h2[s][:, k * 128:(k + 1) * 128],
                                                                        identity=idt[:]),
                          reads=[Bh2[s], Bid], writes=[Bptr[g]])
                fw.op("act", lambda: nc.scalar.copy(out=h2T[:, g * 4:(g + 1) * 4, :], in_=ptr[g][:]), reads=[Bptr[g]], writes=[Bh2T])
            for k in range(8):
                fw.op("pe", lambda k=k: nc.tensor.matmul(pl[:], lhsT=h2T[:, k, :], rhs=wrt[:, k, :], start=(k == 0), stop=(k == 7)),
                      reads=[Bh2T, Bwr], writes=[Bpl])
            fw.op("dve", lambda: nc.vector.reduce_max(out=sm[:, 0:1], in_=pl[:], axis=AX.X), reads=[Bpl], writes=[Bsm])
            fw.op("dve", lambda: nc.vector.tensor_scalar(out=sm[:, 1:2], in0=sm[:, 0:1], scalar1=-1.0, scalar2=None, op0=ALU.mult),
                  reads=[Bsm], writes=[Bsm])
            fw.op("act", lambda: nc.scalar.activation(out=aft[s][:], in_=pl[:], func=AF.Exp, bias=sm[:, 1:2], scale=1.0,
                                                      accum_out=sm[:, 2:3]), reads=[Bpl, Bsm], writes=[Baf[s], Bsm])
            fw.op("dve", lambda: nc.vector.reciprocal(out=sm[:, 3:4], in_=sm[:, 2:3]), reads=[Bsm], writes=[Bsm])
            fw.op("dve", lambda: nc.vector.tensor_scalar(out=aft[s][:], in0=aft[s][:], scalar1=sm[:, 3:4], scalar2=None, op0=ALU.mult),
                  reads=[Bsm, Baf[s]], writes=[Baf[s]])
            fw.dma("sp", affo[t * 128:(t + 1) * 128, :], aft[s][:], reads=[Baf[s]], writes=[Bout[2]])
        fw.finish(Bout)
    return nc


def fm_layout(a2d):
    K, T = a2d.shape
    return np.ascontiguousarray(a2d.reshape(K // 128, 128, T).transpose(1, 0, 2))


_NC_CACHE = {}


def cached(key, builder):
    if key not in _NC_CACHE:
        _NC_CACHE[key] = builder()
    return _NC_CACHE[key]


def build_E(NTT, CAP, GB, NITER=30):
    NG = NB // GB
    NS = GB * CAP
    NCH = NS // 128
    NC8 = 8 * NTT
    nc = new_nc()
    aff = dram_in(nc, "aff", [128, 8, NTT])
    h2 = dram_in(nc, "h2", [NB * NTT * 128, 1024], BF16)
    tok = dram_in(nc, "tok", [128, 2, NB, NTT])
    slotoff = dram_in(nc, "slotoff", [128, 8])
    tri = dram_in(nc, "tri", [128, 128], BF16)
    iota = dram_in(nc, "iota", [128, NS])
    identb = dram_in(nc, "identb", [128, 128], BF16)
    wg = dram_in(nc, "wg", [2, 128, 8, 2048])
    wu = dram_in(nc, "wu", [2, 128, 8, 2048])
    wd = dram_in(nc, "wd", [2, 128, 16, 1024])
    yc = dram_out(nc, "yc", [2, NG, NS + 128, 1024], BF16)
    BIGPOS = float(NS)
    postab = dram_out(nc, "postab", [128, 8, NTT], I32)
    with ExitStack() as st:
        fw = FW(nc, st)
        A = fw.sb("A", [128, 8, NTT], F32)
        tokt = fw.sb("tokt", [128, 2, NB, NTT], F32)
        sofft = fw.sb("sofft", [128, 8], F32)
        trit = fw.sb("trit", [128, 128], BF16)
        onesb = fw.sb("onesb", [128, 128], BF16)
        iot = fw.sb("iot", [128, NS], F32)
        idb = fw.sb("idb", [128, 128], BF16)
        lo = fw.sb("lo", [128, 8], F32)
        hi = fw.sb("hi", [128, 8], F32)
        mid = fw.sb("mid", [128, 8], F32)
        cnt = fw.sb("cnt", [128, 8], F32)
        ge = fw.sb("ge", [128, 8], F32)
        tmp8 = fw.sb("tmp8", [128, 8], F32)
        cmpb = fw.sb("cmpb", [128, 8, NTT], BF16)
        maskf = fw.sb("maskf", [128, 8, NTT], F32)
        pos = fw.sb("pos", [128, 8, NTT], F32)
        off = fw.sb("off", [128, 8, NTT], F32)
        tot = fw.sb("tot", [128, 8, NTT], F32)
        posi = fw.sb("posi", [128, 8, NTT], I32)
        vals = fw.sb("vals", [128, 8, NTT, 5], BF16)
        gres = fw.sb("gres", [128, 8, NTT], F32)
        gpc = fw.sb("gpc", [128, 8, NTT], F32)
        NSTEP = GB * NTT
        ohall = fw.sb("ohall", [128, NSTEP, NS], BF16)
        idxf = fw.sb("idxf", [128, NCH, 5], F32)
        gate = [fw.sb(f"gate{i}", [128, NCH], F32) for i in range(2)]
        idf = fw.sb("idf", [128, NCH], F32)
        idxu = fw.sb("idxu", [128, NCH], I32)
        xg = [fw.sb(f"xg{i}", [128, 1024], BF16) for i in range(2)]
        xgT = [fw.sb(f"xgT{i}", [128, 8, NS], BF16) for i in range(2)]
        wgb = fw.sb("wgb", [128, 8, 2048], BF16)
        wub = fw.sb("wub", [128, 8, 2048], BF16)
        wdb = fw.sb("wdb", [128, 16, 1024], BF16)
        wst = [fw.sb(f"wst{i}", [128, 2048], F32) for i in range(2)]
        sg = [fw.sb(f"sg{i}", [128, NS], F32) for i in range(2)]
        hT = fw.sb("hT", [128, 16, NS], BF16)
        ysb = [fw.sb(f"ysb{i}", [128, 1024], BF16) for i in range(2)]
        pbank = [fw.ps(f"pb{i}", [128, 512]) for i in range(7)]
        ptrb = fw.ps("ptrb", [128, 4, 128], BF16)
        pcnt, pidx, pg, pu, py0, py1, ppos = pbank
        B = {n: Buf(n) for n in ["A", "tokt", "sofft", "trit", "onesb", "iot", "idb", "lo", "hi", "mid", "cnt", "ge", "tmp8",
                                 "cmpb", "maskf", "pos", "off", "tot", "posi", "vals", "gres", "ohall", "idxf", "idxu", "xg0", "xg1",
                                 "xgT0", "xgT1", "gate0", "gate1", "wgb", "wub", "wdb", "wst0", "wst1", "sg0", "sg1", "hT", "ysb0", "ysb1",
                                 "pcnt", "pidx", "pg", "pu", "py0", "py1", "ppos", "ptrb", "o_yc", "o_pos"]}
        V = nc.vector
        fw.dma("sp", A[:], aff, writes=[B["A"]])
        fw.dma("sp", tokt[:], tok, writes=[B["tokt"]])
        fw.dma("sp", sofft[:], slotoff, writes=[B["sofft"]])
        fw.dma("sp", trit[:], tri, writes=[B["trit"]])
        fw.dma("sp", iot[:], iota, writes=[B["iot"]])
        fw.dma("sp", idb[:], identb, writes=[B["idb"]])
        fw.op("pool", lambda: nc.gpsimd.memset(onesb[:], 1.0), writes=[B["onesb"]])
        zt = fw.sb("zt", [128, 1024], BF16)
        B["zt"] = Buf("zt")
        fw.op("pool", lambda: nc.gpsimd.memset(zt[:], 0.0), writes=[B["zt"]])
        for el in range(2):
            for g in range(NG):
                fw.dma("sp", yc[el, g, NS:NS + 128, :], zt[:], reads=[B["zt"]], writes=[B["o_yc"]])
        fw.op("pool", lambda: nc.gpsimd.memset(lo[:], 0.0), writes=[B["lo"]])
        fw.op("pool", lambda: nc.gpsimd.memset(hi[:], 1.0), writes=[B["hi"]])

        ld = [0]

        def load_w(dst, Bdst, src_fn, nk, width):
            for k in range(nk):
                s = ld[0] % 2
                ld[0] += 1
                fw.dma("sp" if s == 0 else "act", wst[s][:, :width], src_fn(k), writes=[B[f"wst{s}"]])
                if s == 0:
                    fw.op("act", lambda k=k: nc.scalar.copy(out=dst[:, k, :], in_=wst[s][:, :width]), reads=[B[f"wst{s}"]], writes=[Bdst])
                else:
                    fw.op("pool", lambda k=k: nc.gpsimd.tensor_copy(out=dst[:, k, :], in_=wst[s][:, :width]), reads=[B[f"wst{s}"]], writes=[Bdst])

        def load_all_w(el):
            load_w(wgb, B["wgb"], lambda k: wg[el, :, k, :], 8, 2048)
            load_w(wub, B["wub"], lambda k: wu[el, :, k, :], 8, 2048)
            load_w(wdb, B["wdb"], lambda k: wd[el, :, k, :], 16, 1024)

        load_all_w(0)

        def bc(t8):
            return t8[:, :].unsqueeze(2).to_broadcast([128, 8, NTT])

        def count_ge(thr, Bthr, want_mask_f32=False):
            fw.op("dve", lambda: V.tensor_tensor(out=cmpb[:], in0=A[:], in1=bc(thr), op=ALU.is_ge),
                  reads=[B["A"], Bthr], writes=[B["cmpb"]])
            fw.op("pe", lambda: nc.tensor.matmul(pcnt[:, :NC8], lhsT=onesb[:], rhs=cmpb[:].rearrange("p a b -> p (a b)"),
                                                 start=True, stop=True), reads=[B["onesb"], B["cmpb"]], writes=[B["pcnt"]])

        for it in range(NITER):
            fw.op("dve", lambda: V.tensor_tensor(out=mid[:], in0=lo[:], in1=hi[:], op=ALU.add), reads=[B["lo"], B["hi"]], writes=[B["mid"]])
            fw.op("dve", lambda: V.tensor_scalar(out=mid[:], in0=mid[:], scalar1=0.5, scalar2=None, op0=ALU.mult),
                  reads=[B["mid"]], writes=[B["mid"]])
            count_ge(mid, B["mid"])
            fw.op("dve", lambda: V.tensor_reduce(out=cnt[:], in_=pcnt[:, :NC8].rearrange("p (a b) -> p a b", b=NTT), axis=AX.X, op=ALU.add),
                  reads=[B["pcnt"]], writes=[B["cnt"]])
            fw.op("dve", lambda: V.tensor_scalar(out=ge[:], in0=cnt[:], scalar1=float(CAP) - 0.5, scalar2=None, op0=ALU.is_ge),
                  reads=[B["cnt"]], writes=[B["ge"]])
            fw.op("dve", lambda: V.tensor_tensor(out=tmp8[:], in0=ge[:], in1=mid[:], op=ALU.mult), reads=[B["ge"], B["mid"]], writes=[B["tmp8"]])
            fw.op("dve", lambda: V.tensor_tensor(out=lo[:], in0=lo[:], in1=tmp8[:], op=ALU.max), reads=[B["tmp8"], B["lo"]], writes=[B["lo"]])
            fw.op("dve", lambda: V.scalar_tensor_tensor(out=tmp8[:], in0=ge[:], scalar=4.0, in1=mid[:], op0=ALU.mult, op1=ALU.add),
                  reads=[B["ge"], B["mid"]], writes=[B["tmp8"]])
            fw.op("dve", lambda: V.tensor_tensor(out=hi[:], in0=hi[:], in1=tmp8[:], op=ALU.min), reads=[B["tmp8"], B["hi"]], writes=[B["hi"]])
        count_ge(lo, B["lo"])
        fw.op("act", lambda: nc.scalar.copy(out=tot[:].rearrange("p a b -> p (a b)"), in_=pcnt[:, :NC8]), reads=[B["pcnt"]], writes=[B["tot"]])
        fw.op("dve", lambda: V.tensor_copy(out=maskf[:], in_=cmpb[:]), reads=[B["cmpb"]], writes=[B["maskf"]])
        fw.op("pe", lambda: nc.tensor.matmul(ppos[:, :NC8], lhsT=trit[:], rhs=cmpb[:].rearrange("p a b -> p (a b)"), start=True, stop=True),
              reads=[B["trit"], B["cmpb"]], writes=[B["ppos"]])
        fw.op("dve", lambda: V.tensor_copy(out=off[:, :, 0], in_=sofft[:]), reads=[B["sofft"]], writes=[B["off"]])
        for j in range(1, NTT):
            fw.op("dve", lambda j=j: V.tensor_tensor(out=off[:, :, j], in0=off[:, :, j - 1], in1=tot[:, :, j - 1], op=ALU.add),
                  reads=[B["off"], B["tot"]], writes=[B["off"]])
        fw.op("dve", lambda: V.tensor_tensor(out=pos[:].rearrange("p a b -> p (a b)"), in0=ppos[:, :NC8],
                                             in1=off[:].rearrange("p a b -> p (a b)"), op=ALU.add),
              reads=[B["ppos"], B["off"]], writes=[B["pos"]])
        fw.op("dve", lambda: V.scalar_tensor_tensor(out=pos[:], in0=pos[:], scalar=-BIGPOS, in1=maskf[:], op0=ALU.add, op1=ALU.mult),
              reads=[B["pos"], B["maskf"]], writes=[B["pos"]])
        fw.op("dve", lambda: V.tensor_scalar(out=pos[:], in0=pos[:], scalar1=BIGPOS, scalar2=None, op0=ALU.add),
              reads=[B["pos"]], writes=[B["pos"]])
        fw.op("dve", lambda: V.tensor_copy(out=posi[:], in_=pos[:]), reads=[B["pos"]], writes=[B["posi"]])
        fw.dma("sp", postab, posi[:], reads=[B["posi"]], writes=[B["o_pos"]])
        for b in range(NB):
            for el in range(2):
                for h in range(2):
                    fw.op("pool", lambda b=b, el=el, h=h: nc.gpsimd.tensor_copy(out=vals[:, b * 2 + el, :, h], in_=tokt[:, h, b, :]),
                          reads=[B["tokt"]], writes=[B["vals"]])
        fw.op("dve", lambda: V.tensor_copy(out=gres[:], in_=A[:]), reads=[B["A"]], writes=[B["gres"]])
        for q in range(3):
            fw.op("dve", lambda q=q: V.tensor_copy(out=vals[:, :, :, 2 + q], in_=gres[:]), reads=[B["gres"]], writes=[B["vals"]])
            if q < 2:
                fw.op("dve", lambda q=q: V.tensor_copy(out=gpc[:], in_=vals[:, :, :, 2 + q]), reads=[B["vals"]], writes=[B["gres"]])
                fw.op("dve", lambda: V.tensor_tensor(out=gres[:], in0=gres[:], in1=gpc[:], op=ALU.subtract), reads=[B["gres"]], writes=[B["gres"]])

        ycnt = [0]
        units = [(el, g) for el in range(2) for g in range(NG)]

        def prep(ui):
            el, g = units[ui]
            ub = ui % 2
            steps = [(b, j) for b in range(g * GB, (g + 1) * GB) for j in range(NTT)]
            for si, (b, j) in enumerate(steps):
                col = b * 2 + el
                fw.op("dve", lambda: V.tensor_scalar(out=ohall[:, si, :], in0=iot[:], scalar1=pos[:, col, j:j + 1], scalar2=None, op0=ALU.is_equal),
                      reads=[B["iot"], B["pos"]], writes=[B["ohall"]])
            for c in range(NCH):
                for si, (b, j) in enumerate(steps):
                    col = b * 2 + el
                    fw.op("pe", lambda: nc.tensor.matmul(pidx[:, c * 8:c * 8 + 5], lhsT=ohall[:, si, c * 128:(c + 1) * 128],
                                                         rhs=vals[:, col, j, :], start=(si == 0), stop=(si == len(steps) - 1)),
                          reads=[B["ohall"], B["vals"]], writes=[B["pidx"]])
            fw.op("dve", lambda: V.tensor_copy(out=idxf[:], in_=pidx[:, :NCH * 8].rearrange("p (a b) -> p a b", b=8)[:, :, 0:5]),
                  reads=[B["pidx"]], writes=[B["idxf"]])
            fw.op("dve", lambda: V.scalar_tensor_tensor(out=idf[:], in0=idxf[:, :, 0], scalar=128.0, in1=idxf[:, :, 1], op0=ALU.mult, op1=ALU.add),
                  reads=[B["idxf"]], writes=[B["idxf"]])
            fw.op("dve", lambda: V.tensor_copy(out=idxu[:], in_=idf[:]), reads=[B["idxf"]], writes=[B["idxu"]])
            fw.op("dve", lambda: V.tensor_tensor(out=gate[ub][:], in0=idxf[:, :, 2], in1=idxf[:, :, 3], op=ALU.add), reads=[B["idxf"]], writes=[B[f"gate{ub}"]])
            fw.op("dve", lambda: V.tensor_tensor(out=gate[ub][:], in0=gate[ub][:], in1=idxf[:, :, 4], op=ALU.add), reads=[B["idxf"], B[f"gate{ub}"]], writes=[B[f"gate{ub}"]])
            for c in range(NCH):
                s = c % 2
                fw.dma("pool", xg[s][:], h2, reads=[B["idxu"]], writes=[B[f"xg{s}"]],
                       indirect=dict(out_offset=None, in_offset=bass.IndirectOffsetOnAxis(ap=idxu[:, c:c + 1], axis=0)))
                for k4 in range(2):
                    for kk in range(4):
                        k = k4 * 4 + kk
                        fw.op("pe", lambda k=k, kk=kk: nc.tensor.transpose(out=ptrb[:, kk, :], in_=xg[s][:, k * 128:(k + 1) * 128], identity=idb[:]),
                              reads=[B[f"xg{s}"], B["idb"]], writes=[B["ptrb"]])
                    fw.op("act", lambda k4=k4: nc.scalar.copy(out=xgT[ub][:, k4 * 4:(k4 + 1) * 4, c * 128:(c + 1) * 128], in_=ptrb[:]),
                          reads=[B["ptrb"]], writes=[B[f"xgT{ub}"]])

        def ffn(ui):
            el, g = units[ui]
            ub = ui % 2
            for ft in range(16):
                s = ft % 2
                pgx, pgn = (pg, "pg") if s == 0 else (pcnt, "pcnt")
                pux, pun = (pu, "pu") if s == 0 else (ppos, "ppos")
                for k in range(8):
                    fw.op("pe", lambda k=k: nc.tensor.matmul(pgx[:, :NS], lhsT=wgb[:, k, ft * 128:(ft + 1) * 128], rhs=xgT[ub][:, k, :],
                                                              start=(k == 0), stop=(k == 7)), reads=[B["wgb"], B[f"xgT{ub}"]], writes=[B[pgn]])
                for k in range(8):
                    fw.op("pe", lambda k=k: nc.tensor.matmul(pux[:, :NS], lhsT=wub[:, k, ft * 128:(ft + 1) * 128], rhs=xgT[ub][:, k, :],
                                                              start=(k == 0), stop=(k == 7)), reads=[B["wub"], B[f"xgT{ub}"]], writes=[B[pun]])
                fw.op("act", lambda: nc.scalar.activation(out=sg[s][:], in_=pgx[:, :NS], func=AF.Silu), reads=[B[pgn]], writes=[B[f"sg{s}"]])
                fw.op("dve", lambda: V.tensor_tensor(out=hT[:, ft, :], in0=sg[s][:], in1=pux[:, :NS], op=ALU.mult),
                      reads=[B[f"sg{s}"], B[pun]], writes=[B["hT"]])
            for c in range(NCH):
                s = ycnt[0] % 2
                ycnt[0] += 1
                for hf, (py, nm) in enumerate(((py0, "py0"), (py1, "py1"))):
                    for ft in range(16):
                        fw.op("pe", lambda ft=ft: nc.tensor.matmul(py[:], lhsT=hT[:, ft, c * 128:(c + 1) * 128],
                                                                    rhs=wdb[:, ft, hf * 512:(hf + 1) * 512], start=(ft == 0), stop=(ft == 15)),
                              reads=[B["hT"], B["wdb"]], writes=[B[nm]])
                    if hf == 0:
                        fw.op("dve", lambda: V.tensor_scalar(out=ysb[s][:, 0:512], in0=py[:], scalar1=gate[ub][:, c:c + 1], scalar2=None, op0=ALU.mult),
                              reads=[B[nm], B[f"gate{ub}"]], writes=[B[f"ysb{s}"]])
                    else:
                        fw.op("act", lambda: nc.scalar.activation(out=ysb[s][:, 512:1024], in_=py[:], func=AF.Copy, scale=gate[ub][:, c:c + 1]),
                              reads=[B[nm], B[f"gate{ub}"]], writes=[B[f"ysb{s}"]])
                fw.dma("sp", yc[el, g, c * 128:(c + 1) * 128, :], ysb[s][:], reads=[B[f"ysb{s}"]], writes=[B["o_yc"]])

        prep(0)
        for ui in range(len(units)):
            if ui + 1 < len(units):
                prep(ui + 1)
            if ui > 0 and units[ui][0] != units[ui - 1][0]:
                load_all_w(units[ui][0])
            ffn(ui)
        fw.finish([B["o_yc"], B["o_pos"]])
    return nc


def build_P(NT, NS):
    T = NT * 128
    RS = NS + 128
    nc = new_nc()
    x1 = dram_in(nc, "x1", [T, 1024])
    ycb = dram_in(nc, "ycb", [16 * RS, 1024], BF16)
    postab = dram_in(nc, "postab", [128, 16, NT], I32)
    rows = dram_in(nc, "rows", [1, 3 * 1024])
    x2 = dram_out(nc, "x2", [T, 1024])
    with ExitStack() as st:
        fw = FW(nc, st)
        make_consts(fw, nc)
        pt = fw.sb("pt", [128, 16, NT], I32)
        rw = fw.sb("rw", [128, 3, 1024], F32)
        xt = [fw.sb(f"xt{i}", [128, 1024], F32) for i in range(2)]
        acc = [fw.sb(f"acc{i}", [128, 1024], F32) for i in range(2)]
        gb = [fw.sb(f"gb{i}", [128, 1024], BF16) for i in range(4)]
        stats = fw.sb("stats", [128, 2, 6], F32)
        mv = fw.sb("mv", [128, 2], F32)
        rstd = fw.sb("rstd", [128, 1], F32)
        Bpt, Brw, Bs, Bo = Buf("pt"), Buf("rw"), Buf("stats"), Buf("o_x2")
        Bxt = [Buf("xt0"), Buf("xt1")]
        Bacc = [Buf("acc0"), Buf("acc1")]
        Bgb = [Buf(f"gb{i}") for i in range(4)]
        fw.dma("sp", pt[:], postab, writes=[Bpt])
        fw.dma("sp", rw[:].rearrange("p a b -> p (a b)"), rows.partition_broadcast(128), writes=[Brw])
        gi = 0
        for t in range(NT):
            s = t % 2
            fw.dma("sp", xt[s][:], x1[t * 128:(t + 1) * 128, :], writes=[Bxt[s]])
            for e in range(16):
                q = gi % 4
                gi += 1
                fw.dma("pool", gb[q][:], ycb, reads=[Bpt], writes=[Bgb[q]],
                       indirect=dict(out_offset=None, in_offset=bass.IndirectOffsetOnAxis(ap=pt[:, e, t:t + 1], axis=0),
                                     element_offset=e * RS * 1024))
                if e == 0:
                    fw.op("dve", lambda: nc.vector.tensor_copy(out=acc[s][:], in_=gb[q][:]), reads=[Bgb[q]], writes=[Bacc[s]])
                else:
                    fw.op("dve", lambda: nc.vector.tensor_tensor(out=acc[s][:], in0=acc[s][:], in1=gb[q][:], op=ALU.add),
                          reads=[Bacc[s], Bgb[q]], writes=[Bacc[s]])
            fw.op("dve", lambda: nc.vector.tensor_tensor(out=acc[s][:], in0=acc[s][:], in1=rw[:, 0, :], op=ALU.mult),
                  reads=[Bacc[s], Brw], writes=[Bacc[s]])
            fw.op("dve", lambda: nc.vector.scalar_tensor_tensor(out=acc[s][:], in0=xt[s][:], scalar=ALPHA, in1=acc[s][:], op0=ALU.mult, op1=ALU.add),
                  reads=[Bxt[s], Bacc[s]], writes=[Bacc[s]])
            layer_norm_tile(fw, nc, acc[s], Bacc[s], stats, mv, rstd, Bs)
            fw.op("dve", lambda: nc.vector.tensor_tensor(out=acc[s][:], in0=acc[s][:], in1=rw[:, 1, :], op=ALU.mult),
                  reads=[Bacc[s], Brw], writes=[Bacc[s]])
            fw.op("dve", lambda: nc.vector.tensor_tensor(out=acc[s][:], in0=acc[s][:], in1=rw[:, 2, :], op=ALU.add),
                  reads=[Bacc[s], Brw], writes=[Bacc[s]])
            fw.dma("sp", x2[t * 128:(t + 1) * 128, :], acc[s][:], reads=[Bacc[s]], writes=[Bo])
        fw.finish([Bo])
    return nc


def load_cast(fw, nc, dst, Bdst, src_fn, nk, width, wst, Bwst, ctr):
    for k in range(nk):
        s = ctr[0] % 2
        ctr[0] += 1
        fw.dma("sp" if s == 0 else "act", wst[s][:, :width], src_fn(k), writes=[Bwst[s]])
        if s == 0:
            fw.op("act", lambda k=k: nc.scalar.copy(out=dst[:, k, :], in_=wst[s][:, :width]), reads=[Bwst[s]], writes=[Bdst])
        else:
            fw.op("pool", lambda k=k: nc.gpsimd.tensor_copy(out=dst[:, k, :], in_=wst[s][:, :width]), reads=[Bwst[s]], writes=[Bdst])


def build_MG(NT, stage=9):
    T = NT * 128
    NGRP = T // 512
    nc = new_nc()
    xT = dram_in(nc, "xT", [128, 8, T])
    mcol = dram_in(nc, "mcol", [128, 8, 2])
    win = dram_in(nc, "win", [128, 8, 4096])
    lnr = dram_in(nc, "lnr", [1, 2 * 2048])
    wsT = dram_in(nc, "wsT", [128, 16, 128])
    bsr = dram_in(nc, "bsr", [1, 16 * 128])
    gTo = dram_out(nc, "gT", [128, 16, T], BF16)
    with ExitStack() as st:
        fw = FW(nc, st)
        make_consts(fw, nc)
        V = nc.vector
        mc = fw.sb("mc", [128, 8, 2], F32)
        xs = [fw.sb(f"xs{i}", [128, T], F32) for i in range(2)]
        hT = fw.sb("hT", [128, 8, T], BF16)
        wb = fw.sb("wb", [128, 8, 4096], BF16)
        wst = [fw.sb(f"wst{i}", [128, 2048], F32) for i in range(2)]
        lnt = fw.sb("lnt", [128, 2, 2048], F32)
        wsf = fw.sb("wsf", [128, 16, 128], F32)
        wsb = fw.sb("wsb", [128, 16, 128], BF16)
        bst = fw.sb("bst", [128, 16, 128], F32)
        uT = fw.sb("uT", [128, 16, 512], BF16)
        v = fw.sb("v", [128, 2048], F32)
        vln = fw.sb("vln", [128, 2048], BF16)
        tmp = fw.sb("tmp", [128, 4, 128], F32)
        go = [fw.sb(f"go{i}", [128, 16, 128], BF16) for i in range(2)]
        stats = fw.sb("stats", [128, 4, 6], F32)
        mv = fw.sb("mv", [128, 2], F32)
        rstd = fw.sb("rstd", [128, 1], F32)
        pu = [fw.ps(f"pu{i}", [128, 512]) for i in range(2)]
        pv = [fw.ps(f"pv{i}", [128, 512]) for i in range(2)]
        psp = [fw.ps(f"psp{i}", [128, 4, 128]) for i in range(2)]
        B = {n: Buf(n) for n in ["mc", "xs0", "xs1", "hT", "wb", "wst0", "wst1", "lnt", "wsf", "wsb", "bst", "uT", "v", "vln", "tmp",
                                 "go0", "go1", "stats", "pu0", "pu1", "pv0", "pv1", "psp0", "psp1", "o"]}
        fw.dma("sp", mc[:], mcol, writes=[B["mc"]])
        fw.dma("sp", lnt[:].rearrange("p a b -> p (a b)"), lnr.partition_broadcast(128), writes=[B["lnt"]])
        fw.dma("sp", wsf[:], wsT, writes=[B["wsf"]])
        fw.dma("sp", bst[:].rearrange("p a b -> p (a b)"), bsr.partition_broadcast(128), writes=[B["bst"]])
        fw.op("dve", lambda: V.tensor_copy(out=wsb[:], in_=wsf[:]), reads=[B["wsf"]], writes=[B["wsb"]])
        fw.op("dve", lambda: V.tensor_scalar(out=mc[:, :, 0], in0=mc[:, :, 0], scalar1=1.0, scalar2=None, op0=ALU.add), reads=[B["mc"]], writes=[B["mc"]])
        for k in range(8):
            s = k % 2
            fw.dma("sp", xs[s][:], xT[:, k, :], writes=[B[f"xs{s}"]])
            fw.op("act", lambda k=k: nc.scalar.activation(out=hT[:, k, :], in_=xs[s][:], func=AF.Identity, scale=mc[:, k, 0:1], bias=mc[:, k, 1:2]),
                  reads=[B[f"xs{s}"], B["mc"]], writes=[B["hT"]])
        ctr = [0]
        wbv = wb[:].rearrange("p k (h w) -> p (k h) w", h=2)
        winv = win.rearrange("p k (h w) -> p (k h) w", h=2)
        load_cast(fw, nc, wbv, B["wb"], lambda k: winv[:, k, :], 16, 2048, wst, [B["wst0"], B["wst1"]], ctr)
        ev = 0
        for grp in range(NGRP if stage > 0 else 0):
            tsl = slice(grp * 512, (grp + 1) * 512)
            for uf in range(16):
                s = uf % 2
                for k in range(8):
                    fw.op("pe", lambda k=k: nc.tensor.matmul(pu[s][:], lhsT=wb[:, k, uf * 128:(uf + 1) * 128], rhs=hT[:, k, tsl], start=(k == 0), stop=(k == 7)),
                          reads=[B["wb"], B["hT"]], writes=[B[f"pu{s}"]])
                fw.op("act", lambda: nc.scalar.activation(out=uT[:, uf, :], in_=pu[s][:], func=AF.Gelu), reads=[B[f"pu{s}"]], writes=[B["uT"]])
            for cc in range(4 if stage > 1 else 0):
                ch = grp * 4 + cc
                csl = slice(ch * 128, (ch + 1) * 128)
                for n in range(4):
                    s = n % 2
                    for k in range(8):
                        fw.op("pe", lambda k=k: nc.tensor.matmul(pv[s][:], lhsT=hT[:, k, csl], rhs=wb[:, k, 2048 + n * 512:2048 + (n + 1) * 512],
                                                                  start=(k == 0), stop=(k == 7)), reads=[B["wb"], B["hT"]], writes=[B[f"pv{s}"]])
                    fw.op("act", lambda: nc.scalar.activation(out=v[:, n * 512:(n + 1) * 512], in_=pv[s][:], func=AF.Gelu), reads=[B[f"pv{s}"]], writes=[B["v"]])
                for j in range(4):
                    fw.op("dve", lambda j=j: V.bn_stats(out=stats[:, j, :], in_=v[:, j * 512:(j + 1) * 512]), reads=[B["v"]], writes=[B["stats"]])
                fw.op("dve", lambda: V.bn_aggr(out=mv[:], in_=stats[:].rearrange("p a b -> p (a b)")), reads=[B["stats"]], writes=[B["stats"]])
                fw.op("act", lambda: nc.scalar.activation(out=rstd[:], in_=mv[:, 1:2], func=AF.Sqrt, bias=fw.eps_ln[:, 0:1], scale=1.0),
                      reads=[B["stats"], fw.Bconst], writes=[B["stats"]])
                fw.op("dve", lambda: V.reciprocal(out=rstd[:], in_=rstd[:]), reads=[B["stats"]], writes=[B["stats"]])
                fw.op("dve", lambda: V.tensor_scalar(out=v[:], in0=v[:], scalar1=mv[:, 0:1], scalar2=rstd[:, 0:1], op0=ALU.subtract, op1=ALU.mult),
                      reads=[B["stats"], B["v"]], writes=[B["v"]])
                fw.op("pool", lambda: nc.gpsimd.tensor_tensor(out=v[:], in0=v[:], in1=lnt[:, 0, :], op=ALU.mult), reads=[B["v"], B["lnt"]], writes=[B["v"]])
                fw.op("pool", lambda: nc.gpsimd.tensor_tensor(out=vln[:], in0=v[:], in1=lnt[:, 1, :], op=ALU.add), reads=[B["v"], B["lnt"]], writes=[B["vln"]])
                os_ = ch % 2
                for g4 in range(4 if stage > 2 else 0):
                    s = g4 % 2
                    for gg in range(4):
                        g = g4 * 4 + gg
                        fw.op("pe", lambda g=g, gg=gg: nc.tensor.matmul(psp[s][:, gg, :], lhsT=vln[:, g * 128:(g + 1) * 128], rhs=wsb[:, g, :], start=True, stop=True),
                              reads=[B["vln"], B["wsb"]], writes=[B[f"psp{s}"]])
                    fw.op("dve", lambda: V.tensor_tensor(out=tmp[:], in0=psp[s][:], in1=bst[:, g4 * 4:(g4 + 1) * 4, :], op=ALU.add),
                          reads=[B[f"psp{s}"], B["bst"]], writes=[B["tmp"]])
                    fw.op("dve", lambda: V.tensor_tensor(out=go[os_][:, g4 * 4:(g4 + 1) * 4, :], in0=tmp[:], in1=uT[:, g4 * 4:(g4 + 1) * 4, cc * 128:(cc + 1) * 128], op=ALU.mult),
                          reads=[B["tmp"], B["uT"]], writes=[B[f"go{os_}"]])
                if stage != 3:
                    for q4 in range(4):
                        fw.dma("sp", gTo[:, q4 * 4:(q4 + 1) * 4, csl], go[os_][:, q4 * 4:(q4 + 1) * 4, :], reads=[B[f"go{os_}"]], writes=[B["o"]])
        fw.finish([B["o"]])
    return nc


def build_MA():
    NQT = SEQ // 128
    NCT = CTX // 128
    NKT = NQT + NCT
    nc = new_nc()
    xT = dram_in(nc, "xT", [128, 8, SEQ])
    cxT = dram_in(nc, "cxT", [128, 8, CTX])
    mcol = dram_in(nc, "mcol", [128, 8, 4])
    w = dram_in(nc, "w", [128, 8, 768])
    gains = dram_in(nc, "gains", [1, 640])
    cs = dram_in(nc, "cs", [128, 2, NQT, 32])
    identb = dram_in(nc, "identb", [128, 128], BF16)
    oTo = dram_out(nc, "oT", [128, 4, SEQ], BF16)
    with ExitStack() as st:
        fw = FW(nc, st)
        V = nc.vector
        G = nc.gpsimd
        mc = fw.sb("mc", [128, 8, 4], F32)
        xs = [fw.sb(f"xs{i}", [128, 2048], F32) for i in range(2)]
        hT = fw.sb("hT", [128, 8, CTX + 2048], BF16)
        wst = [fw.sb(f"wst{i}", [128, 768], F32) for i in range(2)]
        wb = fw.sb("wb", [128, 8, 768], BF16)
        g10 = fw.sb("g10", [128, 10, 64], F32)
        cst = fw.sb("cst", [128, 2, NQT, 32], F32)
        idb = fw.sb("idb", [128, 128], BF16)
        qk = fw.sb("qk", [128, 10, 64], F32)
        sq = fw.sb("sq", [128, 10, 64], F32)
        ss = fw.sb("ss", [128, 10], F32)
        t1 = fw.sb("t1", [128, 10, 32], F32)
        t2 = fw.sb("t2", [128, 10, 32], F32)
        qr = fw.sb("qr", [128, 10, 64], BF16)
        qT = fw.sb("qT", [64, 8, SEQ], BF16)
        kT = fw.sb("kT", [64, 2, NKT * 128], BF16)
        vall = fw.sb("vall", [128, NKT, 2, 65], BF16)
        E = [fw.sb(f"E{i}", [128, 512], BF16) for i in range(2)]
        rden = fw.sb("rden", [128, 4], F32)
        otok = fw.sb("otok", [128, 8, 64], BF16)
        oTt = [fw.sb(f"oTt{i}", [128, 4, 128], BF16) for i in range(2)]
        epsr = fw.sb("epsr", [128, 1], F32)
        pA = [fw.ps(f"pA{i}", [128, 512]) for i in range(2)]
        pT = fw.ps("pT", [128, 4, 128], BF16)
        pO = [fw.ps(f"pO{i}", [128, 512]) for i in range(4)]
        B = {n: Buf(n) for n in ["mc", "xs0", "xs1", "hT", "wst0", "wst1", "wb", "g10", "cst", "idb", "qk", "sq", "ss", "t1", "t2", "qr", "qT", "kT",
                                 "vall", "E0", "E1", "rden", "otok", "oTt0", "oTt1", "epsr", "pA0", "pA1", "pT", "pO0", "pO1", "pO2", "pO3", "o"]}
        fw.dma("sp", mc[:], mcol, writes=[B["mc"]])
        fw.dma("sp", g10[:].rearrange("p a b -> p (a b)"), gains.partition_broadcast(128), writes=[B["g10"]])
        fw.dma("sp", cst[:], cs, writes=[B["cst"]])
        fw.dma("sp", idb[:], identb, writes=[B["idb"]])
        fw.op("pool", lambda: G.memset(epsr[:], RMS_EPS), writes=[B["epsr"]])
        fw.op("pool", lambda: G.memset(vall[:, :, :, 64:65], 1.0), writes=[B["vall"]])
        fw.op("dve", lambda: V.tensor_scalar(out=g10[:, 0:8, :], in0=g10[:, 0:8, :], scalar1=0.125, scalar2=None, op0=ALU.mult), reads=[B["g10"]], writes=[B["g10"]])
        for c in (0, 2):
            fw.op("dve", lambda c=c: V.tensor_scalar(out=mc[:, :, c], in0=mc[:, :, c], scalar1=1.0, scalar2=None, op0=ALU.add), reads=[B["mc"]], writes=[B["mc"]])
        li = [0]

        def load_half(hf):
            for k in range(8):
                if hf == 0:
                    s = li[0] % 2
                    li[0] += 1
                    fw.dma("sp", xs[s][:, :CTX], cxT[:, k, :], writes=[B[f"xs{s}"]])
                    fw.op("act", lambda k=k: nc.scalar.activation(out=hT[:, k, 0:CTX], in_=xs[s][:, :CTX], func=AF.Identity, scale=mc[:, k, 2:3], bias=mc[:, k, 3:4]),
                          reads=[B[f"xs{s}"], B["mc"]], writes=[B["hT"]])
                s = li[0] % 2
                li[0] += 1
                fw.dma("sp", xs[s][:], xT[:, k, hf * 2048:(hf + 1) * 2048], writes=[B[f"xs{s}"]])
                fw.op("act", lambda k=k: nc.scalar.activation(out=hT[:, k, CTX:CTX + 2048], in_=xs[s][:], func=AF.Identity,
                                                              scale=mc[:, k, 0:1], bias=mc[:, k, 1:2]),
                      reads=[B[f"xs{s}"], B["mc"]], writes=[B["hT"]])

        ctr = [0]
        load_cast(fw, nc, wb, B["wb"], lambda k: w[:, k, :], 8, 768, wst, [B["wst0"], B["wst1"]], ctr)

        def bc10(ap2, n):
            return ap2.unsqueeze(2).to_broadcast([128, 10, n])

        for tt in range(NKT):
            is_ctx = tt < NCT
            tsl = slice(tt * 128, (tt + 1) * 128)
            if tt == 0:
                load_half(0)
            if tt == NCT + 16:
                load_half(1)
            hc = tt * 128 if tt < NCT + 16 else (tt - 16) * 128
            hsl = slice(hc, hc + 128)
            for k in range(8):
                fw.op("pe", lambda k=k: nc.tensor.matmul(pA[0][:], lhsT=hT[:, k, hsl], rhs=wb[:, k, 0:512], start=(k == 0), stop=(k == 7)),
                      reads=[B["hT"], B["wb"]], writes=[B["pA0"]])
            for k in range(8):
                fw.op("pe", lambda k=k: nc.tensor.matmul(pA[1][:, 0:256], lhsT=hT[:, k, hsl], rhs=wb[:, k, 512:768], start=(k == 0), stop=(k == 7)),
                      reads=[B["hT"], B["wb"]], writes=[B["pA1"]])
            fw.op("act", lambda: nc.scalar.copy(out=qk[:, 0:8, :], in_=pA[0][:].rearrange("p (a b) -> p a b", b=64)), reads=[B["pA0"]], writes=[B["qk"]])
            fw.op("act", lambda: nc.scalar.copy(out=qk[:, 8:10, :], in_=pA[1][:, 0:128].rearrange("p (a b) -> p a b", b=64)), reads=[B["pA1"]], writes=[B["qk"]])
            fw.op("act", lambda: nc.scalar.copy(out=vall[:, tt, :, 0:64], in_=pA[1][:, 128:256].rearrange("p (a b) -> p a b", b=64)), reads=[B["pA1"]], writes=[B["vall"]])
            fw.op("pool", lambda: G.tensor_tensor(out=sq[:], in0=qk[:], in1=qk[:], op=ALU.mult), reads=[B["qk"]], writes=[B["sq"]])
            fw.op("dve", lambda: V.tensor_reduce(out=ss[:], in_=sq[:], axis=AX.X, op=ALU.add), reads=[B["sq"]], writes=[B["ss"]])
            fw.op("act", lambda: nc.scalar.activation(out=ss[:], in_=ss[:], func=AF.Sqrt, scale=1.0 / 64.0, bias=epsr[:, 0:1]), reads=[B["ss"], B["epsr"]], writes=[B["ss"]])
            fw.op("dve", lambda: V.reciprocal(out=ss[:], in_=ss[:]), reads=[B["ss"]], writes=[B["ss"]])
            fw.op("dve", lambda: V.tensor_tensor(out=qk[:], in0=qk[:], in1=bc10(ss[:, :], 64), op=ALU.mult), reads=[B["qk"], B["ss"]], writes=[B["qk"]])
            if is_ctx:
                fw.op("dve", lambda: V.tensor_tensor(out=qr[:], in0=qk[:], in1=g10[:], op=ALU.mult), reads=[B["qk"], B["g10"]], writes=[B["qr"]])
            else:
                lt = tt - NCT
                fw.op("pool", lambda: G.tensor_tensor(out=qk[:], in0=qk[:], in1=g10[:], op=ALU.mult), reads=[B["qk"], B["g10"]], writes=[B["qk"]])
                cosb = cst[:, 0, lt, :].unsqueeze(1).to_broadcast([128, 10, 32])
                sinb = cst[:, 1, lt, :].unsqueeze(1).to_broadcast([128, 10, 32])
                fw.op("dve", lambda: V.tensor_tensor(out=t1[:], in0=qk[:, :, 0:32], in1=cosb, op=ALU.mult), reads=[B["qk"], B["cst"]], writes=[B["t1"]])
                fw.op("pool", lambda: G.tensor_tensor(out=t2[:], in0=qk[:, :, 32:64], in1=sinb, op=ALU.mult), reads=[B["qk"], B["cst"]], writes=[B["t2"]])
                fw.op("dve", lambda: V.tensor_tensor(out=qr[:, :, 0:32], in0=t1[:], in1=t2[:], op=ALU.subtract), reads=[B["t1"], B["t2"]], writes=[B["qr"]])
                fw.op("dve", lambda: V.tensor_tensor(out=t1[:], in0=qk[:, :, 0:32], in1=sinb, op=ALU.mult), reads=[B["qk"], B["cst"], B["qr"]], writes=[B["t1"]])
                fw.op("pool", lambda: G.tensor_tensor(out=t2[:], in0=qk[:, :, 32:64], in1=cosb, op=ALU.mult), reads=[B["qk"], B["cst"], B["qr"]], writes=[B["t2"]])
                fw.op("dve", lambda: V.tensor_tensor(out=qr[:, :, 32:64], in0=t1[:], in1=t2[:], op=ALU.add), reads=[B["t1"], B["t2"]], writes=[B["qr"]])
            heads = [8, 9] if is_ctx else list(range(10))
            for i0 in range(0, len(heads), 4):
                hs = heads[i0:i0 + 4]
                for j, hh in enumerate(hs):
                    fw.op("pe", lambda j=j, hh=hh: nc.tensor.transpose(out=pT[0:64, j, :], in_=qr[:, hh, :], identity=idb[:]),
                          reads=[B["qr"], B["idb"]], writes=[B["pT"]])
                if hs[0] < 8:
                    lt = tt - NCT
                    fw.op("act", lambda: nc.scalar.copy(out=qT[:, hs[0]:hs[0] + 4, lt * 128:(lt + 1) * 128], in_=pT[0:64, 0:4, :]), reads=[B["pT"]], writes=[B["qT"]])
                else:
                    fw.op("act", lambda: nc.scalar.copy(out=kT[:, :, tsl], in_=pT[0:64, 0:2, :]), reads=[B["pT"]], writes=[B["kT"]])
        steps = [(qt, kh, kt) for qt in range(NQT) for kh in range(2) for kt in range(NKT)]

        def emit_S(i):
            qt, kh, kt = steps[i]
            s = i % 2
            qsl = slice(qt * 128, (qt + 1) * 128)
            fw.op("pe", lambda: nc.tensor.matmul(pA[s][:].rearrange("p (a b) -> p a b", b=128), lhsT=kT[:, kh, kt * 128:(kt + 1) * 128],
                                                 rhs=qT[:, kh * 4:(kh + 1) * 4, qsl], start=True, stop=True),
                  reads=[B["kT"], B["qT"]], writes=[B[f"pA{s}"]])
            fw.op("act", lambda: nc.scalar.activation(out=E[s][:], in_=pA[s][:], func=AF.Exp), reads=[B[f"pA{s}"]], writes=[B[f"E{s}"]])

        emit_S(0)
        for i, (qt, kh, kt) in enumerate(steps):
            s = i % 2
            if i + 1 < len(steps):
                emit_S(i + 1)
            for g in range(4):
                fw.op("pe", lambda g=g: nc.tensor.matmul(pO[g][:, 0:65], lhsT=E[s][:, g * 128:(g + 1) * 128], rhs=vall[:, kt, kh, :],
                                                          start=(kt == 0), stop=(kt == NKT - 1)),
                      reads=[B[f"E{s}"], B["vall"]], writes=[B[f"pO{g}"]])
            if kt == NKT - 1:
                for g in range(4):
                    fw.op("dve", lambda g=g: V.reciprocal(out=rden[:, g:g + 1], in_=pO[g][:, 64:65]), reads=[B[f"pO{g}"]], writes=[B["rden"]])
                    fw.op("dve", lambda g=g: V.tensor_scalar(out=otok[:, kh * 4 + g, :], in0=pO[g][:, 0:64], scalar1=rden[:, g:g + 1], scalar2=None, op0=ALU.mult),
                          reads=[B[f"pO{g}"], B["rden"]], writes=[B["otok"]])
                if kh == 1:
                    os_ = qt % 2
                    qsl = slice(qt * 128, (qt + 1) * 128)
                    for j in range(4):
                        fw.op("pe", lambda j=j: nc.tensor.transpose(out=pT[:, j, :], in_=otok[:, 2 * j:2 * j + 2, :].rearrange("p a b -> p (a b)"), identity=idb[:]),
                              reads=[B["otok"], B["idb"]], writes=[B["pT"]])
                    fw.op("act", lambda: nc.scalar.copy(out=oTt[os_][:], in_=pT[:]), reads=[B["pT"]], writes=[B[f"oTt{os_}"]])
                    fw.dma("sp", oTo[:, :, qsl], oTt[os_][:], reads=[B[f"oTt{os_}"]], writes=[B["o"]])
        fw.finish([B["o"]])
    return nc


def rope_tables():
    L = SEQ
    rows = L // 64
    row = np.broadcast_to(np.arange(rows, dtype=np.float32)[:, None], (rows, 64)).reshape(L)
    col = np.broadcast_to(np.arange(64, dtype=np.float32)[None, :], (rows, 64)).reshape(L)
    inv = (10000.0 ** (-np.arange(16, dtype=np.float32) / 16)).astype(np.float32)
    ang = np.concatenate([row[:, None] * inv, col[:, None] * inv], axis=-1).astype(np.float32)
    cs = np.stack([np.cos(ang), np.sin(ang)], axis=0).astype(np.float32)
    return np.ascontiguousarray(cs.reshape(2, L // 128, 128, 32).transpose(2, 0, 1, 3))


_TAB = {}


def dft_tables(L):
    if L in _TAB:
        return _TAB[L]
    N = 2 * L
    TW = min(512, L)
    t = np.arange(L, dtype=np.int64)
    ph = (np.outer(t, t) % N).astype(np.float64) * (2.0 * np.pi / N)
    C = np.cos(ph)
    S = -np.sin(ph)
    S[:, 0] = np.where(t % 2 == 0, 1.0, -1.0)

    def slab(M):
        return np.ascontiguousarray(M.reshape(L // 128, 128, L // TW, TW).transpose(2, 1, 0, 3).astype(np.float32).astype(NPBF))
    _TAB[L] = (slab(C), slab(S), slab(np.ascontiguousarray(S.T)))
    return _TAB[L]


def hyena_consts(L):
    t = np.linspace(0.0, 1.0, L, dtype=np.float32)[:, None]
    w = ((2.0 * math.pi / L) * np.arange(L, dtype=np.float32))[:, None].astype(np.float32)
    f = np.linspace(1e-4, 15, 16, dtype=np.float32)[None, :]
    z = np.concatenate([t, np.cos(f * w), -np.sin(f * w)], axis=-1).astype(np.float32)
    min_decay = math.log(1e-2) / 1.5
    max_decay = math.log(1e-2) / 0.3
    deltas = np.abs(np.linspace(min_decay, max_decay, D, dtype=np.float32))
    decay = np.exp(-t * deltas).astype(np.float32)
    return np.ascontiguousarray(z.T), np.ascontiguousarray(decay.T)


def emit_fwd_dft(fw, nc, L, x_tm, Bx, tabC, tabS, slabs, Bslab, pacc, Bpacc, consume):
    TW = min(512, L)
    NK = L // 128
    sc = [0]
    for nt in range(L // TW):
        for which, tab in ((0, tabC), (1, tabS)):
            s = sc[0] % 2
            sc[0] += 1
            fw.dma("sp" if s == 0 else "act", slabs[s][:, :NK, :TW], tab[nt], writes=[Bslab[s]])
            for st_ in range(4):
                for k in range(NK):
                    fw.op("pe", lambda k=k: nc.tensor.matmul(pacc[st_][:, :TW], lhsT=x_tm[:, st_, k, :], rhs=slabs[s][:, k, :TW], start=(k == 0), stop=(k == NK - 1)),
                          reads=[Bx, Bslab[s]], writes=[Bpacc[st_]])
                consume(nt, which, st_, pacc[st_][:, :TW])


def wrap_pi(fw, nc, a, Ba, m, Bm, P):
    V = nc.vector
    PI = math.pi
    for _ in range(2):
        fw.op("dve", lambda: V.tensor_scalar(out=m[:P], in0=a[:P], scalar1=-PI, scalar2=2 * PI, op0=ALU.is_lt, op1=ALU.mult), reads=[Ba], writes=[Bm])
        fw.op("dve", lambda: V.tensor_tensor(out=a[:P], in0=a[:P], in1=m[:P], op=ALU.add), reads=[Ba, Bm], writes=[Ba])
        fw.op("dve", lambda: V.tensor_scalar(out=m[:P], in0=a[:P], scalar1=PI, scalar2=2 * PI, op0=ALU.is_gt, op1=ALU.mult), reads=[Ba], writes=[Bm])
        fw.op("dve", lambda: V.tensor_tensor(out=a[:P], in0=a[:P], in1=m[:P], op=ALU.subtract), reads=[Ba, Bm], writes=[Ba])
    fw.op("dve", lambda: V.tensor_scalar(out=a[:P], in0=a[:P], scalar1=-PI, scalar2=PI, op0=ALU.max, op1=ALU.min), reads=[Ba], writes=[Ba])


def build_F(L):
    TW = min(512, L)
    NK = L // 128
    NTL = L // TW
    N = 2 * L
    nc = new_nc()
    zT = dram_in(nc, "zT", [33, L])
    decay = dram_in(nc, "decay", [128, L])
    w1 = dram_in(nc, "w1", [33, 64])
    w2 = dram_in(nc, "w2", [64, 64])
    w3 = dram_in(nc, "w3", [64, 4, 128])
    vecs = dram_in(nc, "vecs", [64, 4])
    fbias = dram_in(nc, "fbias", [128, 2])
    identf = dram_in(nc, "identf", [128, 128])
    tabC = dram_in(nc, "tabC", [NTL, 128, NK, TW], BF16)
    tabS = dram_in(nc, "tabS", [NTL, 128, NK, TW], BF16)
    Kfo = dram_out(nc, "Kf", [128, 2, 2, L])
    with ExitStack() as st:
        fw = FW(nc, st)
        V = nc.vector
        G = nc.gpsimd
        w1t = fw.sb("w1t", [33, 64], F32)
        w2t = fw.sb("w2t", [64, 64], F32)
        w3t = fw.sb("w3t", [64, 4, 128], F32)
        vt = fw.sb("vt", [64, 6], F32)
        fbt = fw.sb("fbt", [128, 2], F32)
        idf = fw.sb("idf", [128, 128], F32)
        nrm = fw.sb("nrm", [128, 8], F32)
        x_tm = fw.sb("x_tm", [128, 4, NK, 128], BF16)
        pacc = [fw.ps(f"pacc{i}", [128, 512]) for i in range(4)]
        ptr = [fw.ps(f"ptr{i}", [128, 4, 128]) for i in range(2)]
        scope1 = fw.scoped()
        scope1.__enter__()
        zt = fw.sb("zt", [33, L], F32)
        dct = fw.sb("dct", [128, L], F32)
        h1 = fw.sb("h1", [64, L], F32)
        h2 = fw.sb("h2", [64, L], F32)
        mk = fw.sb("mk", [64, L], F32)
        kk = [fw.sb(f"kk{i}", [128, L], F32) for i in range(4)]
        B = {n: Buf(n) for n in ["zt", "dct", "w1t", "w2t", "w3t", "vt", "fbt", "idf", "h1", "h2", "mk", "kk0", "kk1", "kk2", "kk3", "nrm", "x_tm",
                                 "slab0", "slab1", "Kf", "pacc0", "pacc1", "pacc2", "pacc3", "ptr0", "ptr1", "o"]}
        for t_, src, nm in ((zt, zT, "zt"), (dct, decay, "dct"), (w1t, w1, "w1t"), (w2t, w2, "w2t"), (w3t, w3, "w3t"), (fbt, fbias, "fbt"), (idf, identf, "idf")):
            fw.dma("sp", t_[:], src, writes=[B[nm]])
        fw.dma("sp", vt[:, 0:4], vecs, writes=[B["vt"]])
        fw.op("dve", lambda: V.tensor_tensor(out=vt[:, 4:6], in0=vt[:, 0:2], in1=vt[:, 2:4], op=ALU.mult), reads=[B["vt"]], writes=[B["vt"]])
        for layer, (wt, Bw, src, Bsrc, dst, Bdst, KP) in enumerate(((w1t, B["w1t"], zt, B["zt"], h1, B["h1"], 33), (w2t, B["w2t"], h1, B["h1"], h2, B["h2"], 64))):
            for j in range(L // TW):
                s = j % 2
                sl = slice(j * TW, (j + 1) * TW)
                fw.op("pe", lambda: nc.tensor.matmul(pacc[s][0:64, :TW], lhsT=wt[:KP, :], rhs=src[:KP, sl], start=True, stop=True), reads=[Bw, Bsrc], writes=[B[f"pacc{s}"]])
                fw.op("act", lambda: nc.scalar.activation(out=dst[:, sl], in_=pacc[s][0:64, :TW], func=AF.Identity, scale=vt[:, 2 + layer:3 + layer], bias=vt[:, 4 + layer:5 + layer]),
                      reads=[B[f"pacc{s}"], B["vt"]], writes=[Bdst])
            wrap_pi(fw, nc, dst, Bdst, mk, B["mk"], 64)
            fw.op("act", lambda: nc.scalar.activation(out=dst[:, :], in_=dst[:, :], func=AF.Sin), reads=[Bdst], writes=[Bdst])
        for st_ in range(4):
            for j in range(L // TW):
                s = j % 2
                sl = slice(j * TW, (j + 1) * TW)
                fw.op("pe", lambda: nc.tensor.matmul(pacc[s][:, :TW], lhsT=w3t[:, st_, :], rhs=h2[:, sl], start=True, stop=True), reads=[B["w3t"], B["h2"]], writes=[B[f"pacc{s}"]])
                fw.op("dve", lambda: V.tensor_tensor(out=kk[st_][:, sl], in0=pacc[s][:, :TW], in1=dct[:, sl], op=ALU.mult), reads=[B[f"pacc{s}"], B["dct"]], writes=[B[f"kk{st_}"]])
            if st_ % 2 == 1:
                fw.op("pool", lambda: G.memset(kk[st_][:, 0:1], 0.0), reads=[], writes=[B[f"kk{st_}"]])
            fw.op("dve", lambda: V.tensor_reduce(out=nrm[:, st_:st_ + 1], in_=kk[st_][:], axis=AX.X, op=ALU.add, apply_absolute_value=True),
                  reads=[B[f"kk{st_}"]], writes=[B["nrm"]])
        for o in range(2):
            fw.op("dve", lambda: V.tensor_tensor(out=nrm[:, 4 + o:5 + o], in0=nrm[:, 2 * o:2 * o + 1], in1=nrm[:, 2 * o + 1:2 * o + 2], op=ALU.add), reads=[B["nrm"]], writes=[B["nrm"]])
            fw.op("dve", lambda: V.tensor_scalar(out=nrm[:, 4 + o:5 + o], in0=nrm[:, 4 + o:5 + o], scalar1=RMS_EPS, scalar2=None, op0=ALU.add), reads=[B["nrm"]], writes=[B["nrm"]])
            fw.op("dve", lambda: V.reciprocal(out=nrm[:, 6 + o:7 + o], in_=nrm[:, 4 + o:5 + o]), reads=[B["nrm"]], writes=[B["nrm"]])
        for st_ in range(4):
            o = st_ // 2
            fw.op("pool", lambda: G.tensor_scalar(out=kk[st_][:], in0=kk[st_][:], scalar1=nrm[:, 6 + o:7 + o], scalar2=None, op0=ALU.mult), reads=[B[f"kk{st_}"], B["nrm"]], writes=[B[f"kk{st_}"]])
            for k4 in range(NK // 4 if NK >= 4 else 1):
                s = k4 % 2
                nn = min(4, NK)
                for kq in range(nn):
                    k = k4 * 4 + kq
                    fw.op("pe", lambda k=k, kq=kq: nc.tensor.transpose(out=ptr[s][:, kq, :], in_=kk[st_][:, k * 128:(k + 1) * 128], identity=idf[:]),
                          reads=[B[f"kk{st_}"], B["idf"]], writes=[B[f"ptr{s}"]])
                fw.op("act", lambda: nc.scalar.copy(out=x_tm[:, st_, k4 * 4:k4 * 4 + nn, :], in_=ptr[s][:, 0:nn, :]), reads=[B[f"ptr{s}"]], writes=[B["x_tm"]])

        scope1.__exit__(None, None, None)
        slabs = [fw.sb(f"slab{i}", [128, NK, TW], BF16) for i in range(2)]
        Kf = fw.sb("Kfs", [128, 2, 2, L], F32)

        def consume(nt, which, st_, ps):
            o, d = st_ // 2, st_ % 2
            dst = Kf[:, o, which, nt * TW:(nt + 1) * TW]
            if d == 0:
                fw.op("act", lambda: nc.scalar.copy(out=dst, in_=ps), reads=[B[f"pacc{st_}"]], writes=[B["Kf"]])
            else:
                op = ALU.add if which == 0 else ALU.subtract
                fw.op("dve", lambda: V.tensor_tensor(out=dst, in0=dst, in1=ps, op=op), reads=[B[f"pacc{st_}"], B["Kf"]], writes=[B["Kf"]])
                if which == 1 and nt == 0:
                    fw.op("dve", lambda: V.scalar_tensor_tensor(out=Kf[:, o, 1, 0:1], in0=ps[:, 0:1], scalar=2.0, in1=Kf[:, o, 1, 0:1], op0=ALU.mult, op1=ALU.add),
                          reads=[B[f"pacc{st_}"], B["Kf"]], writes=[B["Kf"]])
        emit_fwd_dft(fw, nc, L, x_tm, B["x_tm"], tabC, tabS, slabs, [B["slab0"], B["slab1"]], pacc, [B[f"pacc{i}"] for i in range(4)], consume)
        for o in range(2):
            fw.op("dve", lambda: V.tensor_scalar(out=Kf[:, o, 0, :], in0=Kf[:, o, 0, :], scalar1=fbt[:, o:o + 1], scalar2=2.0 / N, op0=ALU.add, op1=ALU.mult),
                  reads=[B["Kf"], B["fbt"]], writes=[B["Kf"]])
            fw.op("dve", lambda: V.tensor_scalar(out=Kf[:, o, 1, 0:1], in0=Kf[:, o, 1, 0:1], scalar1=fbt[:, o:o + 1], scalar2=None, op0=ALU.add), reads=[B["Kf"], B["fbt"]], writes=[B["Kf"]])
            fw.op("dve", lambda: V.tensor_scalar(out=Kf[:, o, 1, :], in0=Kf[:, o, 1, :], scalar1=2.0 / N, scalar2=None, op0=ALU.mult), reads=[B["Kf"]], writes=[B["Kf"]])
            fw.op("dve", lambda: V.tensor_scalar(out=Kf[:, o, :, 0:1], in0=Kf[:, o, :, 0:1], scalar1=0.5, scalar2=None, op0=ALU.mult), reads=[B["Kf"]], writes=[B["Kf"]])
            for ri in range(2):
                fw.dma("sp", Kfo[:, o, ri, :], Kf[:, o, ri, :], reads=[B["Kf"]], writes=[B["o"]])
        fw.finish([B["o"]])
    return nc


def build_MH(L):
    TW = min(512, L)
    NK = L // 128
    NTL = L // TW
    TJ = TW // 128
    nc = new_nc()
    xT = dram_in(nc, "xT", [128, 8, L])
    mcol = dram_in(nc, "mcol", [128, 8, 2])
    win = dram_in(nc, "win", [128, 8, 1536])
    cw = dram_in(nc, "cw", [128, 12, 4])
    Kfi = dram_in(nc, "Kf", [4, 128, 2, 2, L])
    tabC = dram_in(nc, "tabC", [NTL, 128, NK, TW], BF16)
    tabS = dram_in(nc, "tabS", [NTL, 128, NK, TW], BF16)
    tabST = dram_in(nc, "tabST", [NTL, 128, NK, TW], BF16)
    identf = dram_in(nc, "identf", [128, 128])
    zTo = dram_out(nc, "zT", [128, 4, L], BF16)
    x12 = nc.dram_tensor("x12", [4, 2, 128, L], F32).ap()
    with ExitStack() as st:
        fw = FW(nc, st)
        V = nc.vector
        G = nc.gpsimd
        mc = fw.sb("mc", [128, 8, 2], F32)
        cwt = fw.sb("cwt", [128, 12, 4], F32)
        idf = fw.sb("idf", [128, 128], F32)
        x_tm = fw.sb("x_tm", [128, 4, NK, 128], BF16)
        pacc = [fw.ps(f"pacc{i}", [128, 512]) for i in range(4)]
        ptr = [fw.ps(f"ptr{i}", [128, 4, 128]) for i in range(2)]
        B = {n: Buf(n) for n in ["mc", "cwt", "idf", "x_tm", "pacc0", "pacc1", "pacc2", "pacc3", "ptr0", "ptr1", "x12", "o",
                                 "xs0", "xs1", "hT", "wb", "wst0", "wst1", "pb0", "pb1", "ob0", "ob1",
                                 "slab0", "slab1", "y_fm", "Xr", "kt0", "kt1", "Y", "ta", "tb", "xq0", "xq1", "zb0", "zb1", "zo0", "zo1"]}
        Bpacc = [B[f"pacc{i}"] for i in range(4)]
        fw.dma("sp", mc[:], mcol, writes=[B["mc"]])
        fw.dma("sp", cwt[:], cw, writes=[B["cwt"]])
        fw.dma("sp", idf[:], identf, writes=[B["idf"]])
        fw.op("dve", lambda: V.tensor_scalar(out=mc[:, :, 0], in0=mc[:, :, 0], scalar1=1.0, scalar2=None, op0=ALU.add), reads=[B["mc"]], writes=[B["mc"]])
        scA = fw.scoped()
        scA.__enter__()
        XW = min(1024, L)
        xs = [fw.sb(f"xs{i}", [128, XW], F32) for i in range(2)]
        hT = fw.sb("hT", [128, 8, L], BF16)
        wb = fw.sb("wb", [128, 8, 1536], BF16)
        wst = [fw.sb(f"wst{i}", [128, 768], F32) for i in range(2)]
        pb = [fw.sb(f"pb{i}", [128, L + 2], F32) for i in range(2)]
        ob = [fw.sb(f"ob{i}", [128, L], F32) for i in range(2)]
        li = 0
        for k in range(8):
            for hf in range(L // XW):
                s = li % 2
                li += 1
                fw.dma("sp", xs[s][:], xT[:, k, hf * XW:(hf + 1) * XW], writes=[B[f"xs{s}"]])
                fw.op("act", lambda k=k, hf=hf: nc.scalar.activation(out=hT[:, k, hf * XW:(hf + 1) * XW], in_=xs[s][:], func=AF.Identity, scale=mc[:, k, 0:1], bias=mc[:, k, 1:2]),
                      reads=[B[f"xs{s}"], B["mc"]], writes=[B["hT"]])
        ctr = [0]
        wbv = wb[:].rearrange("p k (h w) -> p (k h) w", h=2)
        winv = win.rearrange("p k (h w) -> p (k h) w", h=2)
        load_cast(fw, nc, wbv, B["wb"], lambda k: winv[:, k, :], 16, 768, wst, [B["wst0"], B["wst1"]], ctr)
        for s in range(2):
            fw.op("pool", lambda s=s: G.memset(pb[s][:, 0:1], 0.0), writes=[B[f"pb{s}"]])
            fw.op("pool", lambda s=s: G.memset(pb[s][:, L + 1:L + 2], 0.0), writes=[B[f"pb{s}"]])
        it = 0
        for st_ in range(4):
            for q in range(3):
                s = it % 2
                it += 1
                col0 = (st_ * 3 + q) * 128
                for tg in range(NTL):
                    pa = tg % 4
                    for k in range(8):
                        fw.op("pe", lambda k=k: nc.tensor.matmul(pacc[pa][:, :TW], lhsT=wb[:, k, col0:col0 + 128], rhs=hT[:, k, tg * TW:(tg + 1) * TW], start=(k == 0), stop=(k == 7)),
                              reads=[B["wb"], B["hT"]], writes=[Bpacc[pa]])
                    fw.op("act", lambda: nc.scalar.copy(out=pb[s][:, 1 + tg * TW:1 + (tg + 1) * TW], in_=pacc[pa][:, :TW]), reads=[Bpacc[pa]], writes=[B[f"pb{s}"]])
                ci = st_ * 3 + q
                fw.op("act", lambda: nc.scalar.activation(out=ob[s][:], in_=pb[s][:, 1:L + 1], func=AF.Identity, scale=cwt[:, ci, 1:2], bias=cwt[:, ci, 3:4]),
                      reads=[B[f"pb{s}"], B["cwt"]], writes=[B[f"ob{s}"]])
                fw.op("dve", lambda: V.scalar_tensor_tensor(out=ob[s][:], in0=pb[s][:, 0:L], scalar=cwt[:, ci, 0:1], in1=ob[s][:], op0=ALU.mult, op1=ALU.add),
                      reads=[B[f"pb{s}"], B["cwt"], B[f"ob{s}"]], writes=[B[f"ob{s}"]])
                fw.op("dve", lambda: V.scalar_tensor_tensor(out=ob[s][:], in0=pb[s][:, 2:L + 2], scalar=cwt[:, ci, 2:3], in1=ob[s][:], op0=ALU.mult, op1=ALU.add),
                      reads=[B[f"pb{s}"], B["cwt"], B[f"ob{s}"]], writes=[B[f"ob{s}"]])
                if q == 0:
                    for k4 in range(max(1, NK // 4)):
                        ps_ = k4 % 2
                        nn = min(4, NK)
                        for kq in range(nn):
                            k = k4 * 4 + kq
                            fw.op("pe", lambda k=k, kq=kq: nc.tensor.transpose(out=ptr[ps_][:, kq, :], in_=ob[s][:, k * 128:(k + 1) * 128], identity=idf[:]),
                                  reads=[B[f"ob{s}"], B["idf"]], writes=[B[f"ptr{ps_}"]])
                        fw.op("act", lambda: nc.scalar.copy(out=x_tm[:, st_, k4 * 4:k4 * 4 + nn, :], in_=ptr[ps_][:, 0:nn, :]), reads=[B[f"ptr{ps_}"]], writes=[B["x_tm"]])
                else:
                    fw.dma("sp", x12[st_, q - 1], ob[s][:], reads=[B[f"ob{s}"]], writes=[B["x12"]])
        scA.__exit__(None, None, None)
        slabs = [fw.sb(f"slab{i}", [128, NK, TW], BF16) for i in range(2)]
        Bslab = [B["slab0"], B["slab1"]]
        y_fm = fw.sb("y_fm", [128, 4, 2, NK, 128], BF16)
        Xr = fw.sb("Xr", [128, 4, TW], F32)
        kt = [fw.sb(f"kt{i}", [128, 2, TW], F32) for i in range(2)]
        Y = fw.sb("Y", [128, 2, TW], F32)
        ta = fw.sb("ta", [128, TW], F32)
        tb = fw.sb("tb", [128, TW], F32)
        xq = [fw.sb(f"xq{i}", [128, TW], F32) for i in range(2)]
        zb = [fw.sb(f"zb{i}", [128, TW], F32) for i in range(2)]
        zo = [fw.sb(f"zo{i}", [128, TW], BF16) for i in range(2)]
        cnt = {"kt": 0, "tr": 0, "xq": 0, "z": 0}
        for o in range(2):
            def consume(nt, which, st_, ps):
                fsl = slice(nt * TW, (nt + 1) * TW)
                if which == 0:
                    fw.op("act", lambda: nc.scalar.copy(out=Xr[:, st_, :], in_=ps), reads=[Bpacc[st_]], writes=[B["Xr"]])
                    return
                ks = cnt["kt"] % 2
                cnt["kt"] += 1
                fw.dma("pool", kt[ks][:], Kfi[st_, :, o, :, fsl], writes=[B[f"kt{ks}"]])
                Kr, Ki = kt[ks][:, 0, :], kt[ks][:, 1, :]
                rd = [B["Xr"], B[f"kt{ks}"]]
                fw.op("pool", lambda: G.tensor_tensor(out=ta[:], in0=Xr[:, st_, :], in1=Kr, op=ALU.mult), reads=rd, writes=[B["ta"]])
                fw.op("dve", lambda: V.tensor_tensor(out=tb[:], in0=ps, in1=Ki, op=ALU.mult), reads=[Bpacc[st_], B[f"kt{ks}"]], writes=[B["tb"]])
                fw.op("pool", lambda: G.tensor_tensor(out=Y[:, 0, :], in0=ta[:], in1=tb[:], op=ALU.subtract), reads=[B["ta"], B["tb"]], writes=[B["Y"]])
                fw.op("pool", lambda: G.tensor_tensor(out=ta[:], in0=Xr[:, st_, :], in1=Ki, op=ALU.mult), reads=rd, writes=[B["ta"]])
                fw.op("dve", lambda: V.tensor_tensor(out=tb[:], in0=ps, in1=Kr, op=ALU.mult), reads=[Bpacc[st_], B[f"kt{ks}"]], writes=[B["tb"]])
                fw.op("pool", lambda: G.tensor_tensor(out=Y[:, 1, :], in0=ta[:], in1=tb[:], op=ALU.add), reads=[B["ta"], B["tb"]], writes=[B["Y"]])
                if nt == 0:
                    fw.op("dve", lambda: V.tensor_tensor(out=Y[:, 0, 0:1], in0=Xr[:, st_, 0:1], in1=kt[ks][:, 0, 0:1], op=ALU.mult), reads=rd, writes=[B["Y"]])
                    fw.op("dve", lambda: V.tensor_tensor(out=Y[:, 1, 0:1], in0=ps[:, 0:1], in1=kt[ks][:, 1, 0:1], op=ALU.mult), reads=[Bpacc[st_], B[f"kt{ks}"]], writes=[B["Y"]])
                for ri in range(2):
                    ps_ = cnt["tr"] % 2
                    cnt["tr"] += 1
                    for j in range(TJ):
                        fw.op("pe", lambda j=j: nc.tensor.transpose(out=ptr[ps_][:, j, :], in_=Y[:, ri, j * 128:(j + 1) * 128], identity=idf[:]),
                              reads=[B["Y"], B["idf"]], writes=[B[f"ptr{ps_}"]])
                    fw.op("act", lambda: nc.scalar.copy(out=y_fm[:, st_, ri, nt * TJ:(nt + 1) * TJ, :], in_=ptr[ps_][:, 0:TJ, :]), reads=[B[f"ptr{ps_}"]], writes=[B["y_fm"]])
            emit_fwd_dft(fw, nc, L, x_tm, B["x_tm"], tabC, tabS, slabs, Bslab, pacc, Bpacc, consume)
            for tt in range(NTL):
                tsl = slice(tt * TW, (tt + 1) * TW)
                for ri, tab in ((0, tabC), (1, tabST)):
                    fw.dma("sp" if ri == 0 else "act", slabs[ri][:, :, :], tab[tt], writes=[Bslab[ri]])
                    for st_ in range(4):
                        for k in range(NK):
                            fw.op("pe", lambda k=k: nc.tensor.matmul(pacc[st_][:, :TW], lhsT=y_fm[:, st_, ri, k, :], rhs=slabs[ri][:, k, :],
                                                                      start=(ri == 0 and k == 0), stop=(ri == 1 and k == NK - 1)),
                                  reads=[B["y_fm"], Bslab[ri]], writes=[Bpacc[st_]])
                for st_ in range(4):
                    xs_ = cnt["xq"] % 2
                    cnt["xq"] += 1
                    fw.dma("pool", xq[xs_][:], x12[st_, o, :, tsl], reads=[B["x12"]], writes=[B[f"xq{xs_}"]])
                    zs = cnt["z"] % 2
                    cnt["z"] += 1
                    if o == 0:
                        fw.op("dve", lambda: V.tensor_tensor(out=zb[zs][:], in0=pacc[st_][:, :TW], in1=xq[xs_][:], op=ALU.mult), reads=[Bpacc[st_], B[f"xq{xs_}"]], writes=[B[f"zb{zs}"]])
                        ps_ = cnt["tr"] % 2
                        cnt["tr"] += 1
                        for j in range(TJ):
                            fw.op("pe", lambda j=j: nc.tensor.transpose(out=ptr[ps_][:, j, :], in_=zb[zs][:, j * 128:(j + 1) * 128], identity=idf[:]),
                                  reads=[B[f"zb{zs}"], B["idf"]], writes=[B[f"ptr{ps_}"]])
                        fw.op("act", lambda: nc.scalar.copy(out=x_tm[:, st_, tt * TJ:(tt + 1) * TJ, :], in_=ptr[ps_][:, 0:TJ, :]), reads=[B[f"ptr{ps_}"]], writes=[B["x_tm"]])
                    else:
                        fw.op("dve", lambda: V.tensor_tensor(out=zo[zs][:], in0=pacc[st_][:, :TW], in1=xq[xs_][:], op=ALU.mult), reads=[Bpacc[st_], B[f"xq{xs_}"]], writes=[B[f"zo{zs}"]])
                        fw.dma("sp", zTo[:, st_, tsl], zo[zs][:], reads=[B[f"zo{zs}"]], writes=[B["o"]])
        fw.finish([B["o"]])
    return nc


def _ident_f():
    return np.eye(128, dtype=np.float32)


def _ident_b():
    return np.eye(128, dtype=np.float32).astype(NPBF)


def stage_filters(L, slot, p):
    nc = cached(("F", L), lambda: build_F(L))
    zT, decay = hyena_consts(L)
    tC, tS, _ = dft_tables(L)
    vecs = np.ascontiguousarray(np.stack([p["hy_f_b1"][slot], p["hy_f_b2"][slot], p["hy_f_freq"][slot, 0], p["hy_f_freq"][slot, 1]], axis=-1))
    maps = []
    for core in range(NCORES):
        ch = slice(core * 128, (core + 1) * 128)
        w3 = p["hy_f_w3"][slot].reshape(64, 2, 2, D)[:, :, :, ch].reshape(64, 4, 128)
        maps.append({"zT": zT, "decay": np.ascontiguousarray(decay[ch]), "w1": np.ascontiguousarray(p["hy_f_w1"][slot]),
                     "w2": np.ascontiguousarray(p["hy_f_w2"][slot]), "w3": np.ascontiguousarray(w3), "vecs": vecs,
                     "fbias": np.ascontiguousarray(p["hy_f_bias"][slot][:, ch].T), "identf": _ident_f(), "tabC": tC, "tabS": tS})
    res = run(nc, maps)
    return np.stack([res[c]["Kf"] for c in range(NCORES)])


def stage_hyena(L, slot, xs, mrows, mv, KfAll, p):
    nc = cached(("MH", L), lambda: build_MH(L))
    tC, tS, tST = dft_tables(L)
    sh1, sc1 = mv[:, 0:D], mv[:, D:2 * D]
    maps = []
    for core in range(NCORES):
        b, h = core // 2, core % 2
        r = mrows[b]
        mc = np.stack([sc1[r], sh1[r]], axis=-1).reshape(8, 128, 2).transpose(1, 0, 2)
        cols = np.concatenate([np.arange(q * D + 512 * h + 128 * s, q * D + 512 * h + 128 * s + 128) for s in range(4) for q in range(3)])
        cwm = np.concatenate([p["hy_conv_w"][slot][:, cols], p["hy_conv_b"][slot][None, cols]], axis=0).reshape(4, 12, 128).transpose(2, 1, 0)
        maps.append({"xT": fm_layout(np.ascontiguousarray(xs[b].T)), "mcol": np.ascontiguousarray(mc), "win": fm_layout(p["hy_w_in"][slot][:, cols]),
                     "cw": np.ascontiguousarray(cwm), "Kf": np.ascontiguousarray(KfAll[4 * h:4 * h + 4]), "tabC": tC, "tabS": tS, "tabST": tST,
                     "identf": _ident_f()})
    res = run(nc, maps)
    return [np.concatenate([res[2 * b]["zT"], res[2 * b + 1]["zT"]], axis=1) for b in range(NB)]


def stage_attn(x_lat, x_ctx, mv, p):
    nc = cached(("MA",), build_MA)
    sh1, sc1 = mv[:, 0:D], mv[:, D:2 * D]
    wqkv = p["at_w_qkv"][0]
    gains = np.ascontiguousarray(np.concatenate([np.tile(p["at_q_gain"][0], 8), np.tile(p["at_k_gain"][0], 2)])[None, :])
    cs = rope_tables()
    maps = []
    for core in range(NCORES):
        b, h = core // 2, core % 2
        mc = np.stack([sc1[b], sh1[b], sc1[4], sh1[4]], axis=-1).reshape(8, 128, 4).transpose(1, 0, 2)
        wcat = np.concatenate([wqkv[:, 512 * h:512 * h + 512], wqkv[:, 1024 + 128 * h:1024 + 128 * h + 128],
                               wqkv[:, 1280 + 128 * h:1280 + 128 * h + 128]], axis=1)
        maps.append({"xT": fm_layout(np.ascontiguousarray(x_lat[b].T)), "cxT": fm_layout(np.ascontiguousarray(x_ctx[b].T)),
                     "mcol": np.ascontiguousarray(mc), "w": fm_layout(wcat), "gains": gains, "cs": cs, "identb": _ident_b()})
    res = run(nc, maps)
    return [np.concatenate([res[2 * b]["oT"], res[2 * b + 1]["oT"]], axis=1) for b in range(NB)]


def stage_gmlp(x_lat, mv, p):
    nc = cached(("MG",), lambda: build_MG(16))
    sh1, sc1 = mv[:, 0:D], mv[:, D:2 * D]
    lnr = np.ascontiguousarray(np.concatenate([p["cm_ln_g"][0], p["cm_ln_b"][0]])[None, :])
    wsT = np.ascontiguousarray(p["cm_w_s"][0].transpose(2, 0, 1))
    bsr = np.ascontiguousarray(p["cm_b_s"][0].reshape(1, -1))
    win = fm_layout(p["cm_w_in"][0])
    maps = []
    for core in range(NCORES):
        b, hf = core // 2, core % 2
        mc = np.stack([sc1[b], sh1[b]], axis=-1).reshape(8, 128, 2).transpose(1, 0, 2)
        maps.append({"xT": fm_layout(np.ascontiguousarray(x_lat[b, hf * 2048:(hf + 1) * 2048].T)), "mcol": np.ascontiguousarray(mc), "win": win,
                     "lnr": lnr, "wsT": wsT, "bsr": bsr})
    res = run(nc, maps)
    return [np.concatenate([res[2 * b]["gT"], res[2 * b + 1]["gT"]], axis=2) for b in range(NB)]


def stage_norm_router(aT, w_out, xs, mv, mrows, i, p, T):
    KC = aT[0].shape[1]
    NT = T // 128
    Lseq = xs.shape[1]
    per_b = Lseq // T
    nc = cached(("N", KC, NT), lambda: build_N(KC, NT))
    wo = fm_layout(w_out)
    wr = fm_layout(p["moe_router"][i])
    maps = []
    for core in range(NCORES):
        b, hf = core // per_b, core % per_b
        r = mrows[b]
        rows = np.concatenate([mv[r, 2 * D:3 * D], p["ln_g"][i, 0], p["ln_b"][i, 0], mv[r, 4 * D:5 * D], mv[r, 3 * D:4 * D]])[None, :]
        maps.append({"aT": np.ascontiguousarray(aT[b][:, :, hf * T:(hf + 1) * T]), "wo": wo, "x": np.ascontiguousarray(xs[b, hf * T:(hf + 1) * T]),
                     "rows": np.ascontiguousarray(rows), "wr": wr, "ident": _ident_f()})
    res = run(nc, maps)
    x1 = np.stack([np.concatenate([res[b * per_b + hf]["x1"] for hf in range(per_b)], axis=0) for b in range(NB)])
    h2 = np.concatenate([res[c]["h2"] for c in range(NCORES)], axis=0)
    aff = np.stack([np.concatenate([res[b * per_b + hf]["aff"] for hf in range(per_b)], axis=0) for b in range(NB)])
    return x1, h2, aff


def stage_experts(aff, h2, i, p, CAP, GB):
    Lseq = aff.shape[1]
    NTT = Lseq // 128
    NS = GB * CAP
    nc = cached(("E", NTT, CAP, GB), lambda: build_E(NTT, CAP, GB))
    tokid = (np.arange(NB)[None, :, None] * Lseq + np.arange(NTT)[None, None, :] * 128 + np.arange(128)[:, None, None])
    tok = np.ascontiguousarray(np.stack([tokid // 128, tokid % 128], axis=1).astype(np.float32))
    so = np.zeros((128, NB, 2), np.float32)
    so += ((np.arange(NB) % GB) * CAP).astype(np.float32)[None, :, None]
    so = np.ascontiguousarray(so.reshape(128, 8))
    tri = np.triu(np.ones((128, 128), np.float32), 1).astype(NPBF)
    iota = np.ascontiguousarray(np.broadcast_to(np.arange(NS, dtype=np.float32), (128, NS)))
    maps = []
    for core in range(NCORES):
        e0 = 2 * core
        a = aff[:, :, e0:e0 + 2].reshape(NB, NTT, 128, 2).transpose(2, 0, 3, 1).reshape(128, 8, NTT)
        maps.append({"aff": np.ascontiguousarray(a), "h2": h2, "tok": tok, "slotoff": so, "tri": tri, "iota": iota, "identb": _ident_b(),
                     "wg": np.ascontiguousarray(p["moe_w_gate"][i, e0:e0 + 2].reshape(2, 8, 128, FF).transpose(0, 2, 1, 3)),
                     "wu": np.ascontiguousarray(p["moe_w_up"][i, e0:e0 + 2].reshape(2, 8, 128, FF).transpose(0, 2, 1, 3)),
                     "wd": np.ascontiguousarray(p["moe_w_down"][i, e0:e0 + 2].reshape(2, 16, 128, D).transpose(0, 2, 1, 3))})
    res = run(nc, maps)
    yc = np.concatenate([res[c]["yc"] for c in range(NCORES)], axis=0)
    pt = np.stack([res[c]["postab"] for c in range(NCORES)], axis=0)
    return yc, pt


def stage_combine(x1, yc, pt, mv, mrows, i, p, T, GB):
    Lseq = x1.shape[1]
    NT = T // 128
    per_b = Lseq // T
    NS = yc.shape[2] - 128
    nc = cached(("P", NT, NS), lambda: build_P(NT, NS))
    maps = []
    for core in range(NCORES):
        b, hf = core // per_b, core % per_b
        r = mrows[b]
        rows = np.concatenate([mv[r, 5 * D:6 * D], p["ln_g"][i, 1], p["ln_b"][i, 1]])[None, :]
        ycb = yc[:, b // GB].reshape(16 * (NS + 128), D)
        ptb = pt[:, :, 2 * b:2 * b + 2, hf * NT:(hf + 1) * NT]
        ptb = ptb.transpose(1, 0, 2, 3).reshape(128, 16, NT)
        maps.append({"x1": np.ascontiguousarray(x1[b, hf * T:(hf + 1) * T]), "ycb": np.ascontiguousarray(ycb), "postab": np.ascontiguousarray(ptb),
                     "rows": np.ascontiguousarray(rows)})
    res = run(nc, maps)
    return np.stack([np.concatenate([res[b * per_b + hf]["x2"] for hf in range(per_b)], axis=0) for b in range(NB)])


def moe_block(aT, w_out, xs, mv, mrows, i, p, T, CAP, GB):
    x1, h2, aff = stage_norm_router(aT, w_out, xs, mv, mrows, i, p, T)
    yc, pt = stage_experts(aff, h2, i, p, CAP, GB)
    return stage_combine(x1, yc, pt, mv, mrows, i, p, T, GB)


def kernel(**inputs):
    p = {k: np.asarray(v) for k, v in inputs.items()}
    x_lat = np.ascontiguousarray(p["x"], dtype=np.float32)
    x_ctx = np.ascontiguousarray(p["ctx"], dtype=np.float32)
    modvec = run_A(p["c"], p["c_ctx"], p["mod_w"], p["mod_b"])
    lat_rows = [0, 1, 2, 3]
    ctx_rows = [4, 4, 4, 4]
    mv = modvec[0]
    Kf = stage_filters(SEQ, 0, p)
    aT = stage_hyena(SEQ, 0, x_lat, lat_rows, mv, Kf, p)
    Kfc = stage_filters(CTX, 0, p)
    aTc = stage_hyena(CTX, 0, x_ctx, ctx_rows, mv, Kfc, p)
    x_lat = moe_block(aT, p["hy_w_out"][0], x_lat, mv, lat_rows, 0, p, 2048, 512, 1)
    x_ctx = moe_block(aTc, p["hy_w_out"][0], x_ctx, mv, ctx_rows, 0, p, 128, 32, 4)
    mv = modvec[1]
    aT = stage_attn(x_lat, x_ctx, mv, p)
    x_lat = moe_block(aT, p["at_w_out"][0], x_lat, mv, lat_rows, 1, p, 2048, 512, 1)
    mv = modvec[2]
    aT = stage_gmlp(x_lat, mv, p)
    x_lat = moe_block(aT, p["cm_w_out"][0], x_lat, mv, lat_rows, 2, p, 2048, 512, 1)
    mv = modvec[3]
    Kf = stage_filters(SEQ, 1, p)
    aT = stage_hyena(SEQ, 1, x_lat, lat_rows, mv, Kf, p)
    x_lat = moe_block(aT, p["hy_w_out"][1], x_lat, mv, lat_rows, 3, p, 2048, 512, 1)
    return x_lat.astype(np.float32)
```
